# Optimizing a Trainium2 kernel written in Bass

```python
import jax, jax.numpy as jnp
from jax import lax
import numpy as np

D_MODEL = 1024
BATCH = 16
SEQ = 4096
DEPTH = 1

CONV_WIDTH = 512
CONV_K = 3
N_HEADS = 8
HEAD_DIM = 64
ATTN_WIDTH = N_HEADS * HEAD_DIM
IDX_HEADS = 8
IDX_DIM = 64
TOPK_ATTN = 256
Q_BLOCK = 128
PEER_HEADS = 8
PEER_N_KEYS = 128
PEER_N_EXPERTS = PEER_N_KEYS * PEER_N_KEYS
PEER_KEY_DIM = 128
PEER_HALF = PEER_KEY_DIM // 2
PEER_TOPK = 16
PEER_CHUNK = 128
N_MOD = 6
EPS = 1e-6

kernel_name = 'hybrid_conv_dsa_peer_block'


def _in_proj_sizes():
    return [CONV_WIDTH, CONV_WIDTH, CONV_WIDTH,
            ATTN_WIDTH, ATTN_WIDTH, ATTN_WIDTH,
            IDX_HEADS * IDX_DIM, IDX_DIM, IDX_HEADS,
            D_MODEL, D_MODEL]


def rms_norm(x, w):
    xf = x.astype(jnp.float32)
    y = xf * lax.rsqrt(jnp.mean(xf * xf, axis=-1, keepdims=True) + EPS)
    return (y * w.astype(jnp.float32)).astype(x.dtype)


def causal_dwconv(u, w):
    s = u.shape[1]
    pad = jnp.pad(u, ((0, 0), (CONV_K - 1, 0), (0, 0)))
    out = w[0] * pad[:, 0:s]
    for j in range(1, CONV_K):
        out = out + w[j] * pad[:, j:j + s]
    return out


def dsa_attention(q, k, v, q_idx, k_idx, w_idx):
    b, s = q.shape[0], q.shape[1]
    n_sel = min(TOPK_ATTN, s // 4)
    n_blk = s // Q_BLOCK
    key_pos = jnp.arange(s)
    k_idx_f = k_idx.astype(jnp.float32)
    gather = jax.vmap(lambda t, i: t[i])

    def to_blocks(t):
        return jnp.swapaxes(t.reshape(b, n_blk, Q_BLOCK, *t.shape[2:]), 0, 1)

    def block_fn(args):
        blk, qb, qib, wb = args
        q_pos = blk * Q_BLOCK + jnp.arange(Q_BLOCK)
        logits = jnp.einsum('bqhd,bsd->bqhs', qib.astype(jnp.float32), k_idx_f)
        score = jnp.einsum('bqhs,bqh->bqs', jax.nn.relu(logits), wb.astype(jnp.float32))
        causal = key_pos[None, :] <= q_pos[:, None]
        score = jnp.where(causal[None], score, -jnp.inf)
        _, sel = lax.top_k(score, n_sel)
        ks = gather(k, sel)
        vs = gather(v, sel)
        att = jnp.einsum('bqhd,bqkhd->bqhk', qb, ks).astype(jnp.float32) * (HEAD_DIM ** -0.5)
        valid = sel <= q_pos[None, :, None]
        att = jnp.where(valid[:, :, None, :], att, -jnp.inf)
        p = jax.nn.softmax(att, axis=-1).astype(vs.dtype)
        return jnp.einsum('bqhk,bqkhd->bqhd', p, vs)

    out = lax.map(block_fn, (jnp.arange(n_blk), to_blocks(q), to_blocks(q_idx), to_blocks(w_idx)))
    return jnp.swapaxes(out, 0, 1).reshape(b, s, N_HEADS * HEAD_DIM)


def peer_ffn(h, w_q, sub_keys, u, v):
    b, s, d = h.shape
    tok = h.reshape(-1, PEER_CHUNK, d)

    def chunk_fn(xc):
        q = (xc @ w_q).reshape(-1, PEER_HEADS, 2, PEER_HALF)
        s1 = jnp.einsum('chd,hnd->chn', q[:, :, 0], sub_keys[:, 0]).astype(jnp.float32)
        s2 = jnp.einsum('chd,hnd->chn', q[:, :, 1], sub_keys[:, 1]).astype(jnp.float32)
        v1, i1 = lax.top_k(s1, PEER_TOPK)
        v2, i2 = lax.top_k(s2, PEER_TOPK)
        cand = (v1[..., :, None] + v2[..., None, :]).reshape(v1.shape[0], PEER_HEADS, PEER_TOPK * PEER_TOPK)
        sc, ci = lax.top_k(cand, PEER_TOPK)
        e1 = jnp.take_along_axis(i1, ci // PEER_TOPK, axis=-1)
        e2 = jnp.take_along_axis(i2, ci % PEER_TOPK, axis=-1)
        expert = e1 * PEER_N_KEYS + e2
        g = jax.nn.softmax(sc, axis=-1)
        ue = u[expert]
        ve = v[expert]
        act = jax.nn.gelu(jnp.einsum('chkd,cd->chk', ue, xc).astype(jnp.float32), approximate=False)
        return jnp.einsum('chk,chkd->cd', (g * act).astype(ve.dtype), ve)

    return lax.map(chunk_fn, tok).reshape(b, s, d)


def setup_inputs(seed: int = 0) -> dict:
    key = jax.random.key(seed)
    ks = jax.random.split(key, 20)
    f32 = jnp.float32
    d = D_MODEL
    n_in = sum(_in_proj_sizes())
    nrm = lambda k, shape, scale: jax.random.normal(k, shape, f32) * scale
    return {
        'x': nrm(ks[0], (BATCH, SEQ, d), 1.0),
        'c': nrm(ks[1], (BATCH, d), 1.0),
        'w_ada': nrm(ks[2], (DEPTH, d, N_MOD * d), 0.5 * d ** -0.5),
        'b_ada': nrm(ks[3], (DEPTH, N_MOD * d), 0.01),
        'norm1_w': 1.0 + nrm(ks[4], (DEPTH, d), 0.02),
        'w_in': nrm(ks[5], (DEPTH, d, n_in), d ** -0.5),
        'conv_w': nrm(ks[6], (DEPTH, CONV_K, CONV_WIDTH), CONV_K ** -0.5),
        'w_conv_out': nrm(ks[7], (DEPTH, CONV_WIDTH, d), CONV_WIDTH ** -0.5),
        'q_norm_w': 1.0 + nrm(ks[8], (DEPTH, HEAD_DIM), 0.02),
        'k_norm_w': 1.0 + nrm(ks[9], (DEPTH, HEAD_DIM), 0.02),
        'w_attn_out': nrm(ks[10], (DEPTH, ATTN_WIDTH, d), ATTN_WIDTH ** -0.5),
        'w_o': nrm(ks[11], (DEPTH, d, d), d ** -0.5),
        'norm2_w': 1.0 + nrm(ks[12], (DEPTH, d), 0.02),
        'w_peer_q': nrm(ks[13], (DEPTH, d, PEER_HEADS * PEER_KEY_DIM), d ** -0.5),
        'peer_sub_keys': nrm(ks[14], (DEPTH, PEER_HEADS, 2, PEER_N_KEYS, PEER_HALF), PEER_HALF ** -0.5),
        'peer_u': nrm(ks[15], (DEPTH, PEER_N_EXPERTS, d), d ** -0.5),
        'peer_v': nrm(ks[16], (DEPTH, PEER_N_EXPERTS, d), 1.0),
    }


def reference(x, c, w_ada, b_ada, norm1_w, w_in, conv_w, w_conv_out, q_norm_w, k_norm_w,
              w_attn_out, w_o, norm2_w, w_peer_q, peer_sub_keys, peer_u, peer_v):
    b, s, _ = x.shape
    offsets = [int(o) for o in np.cumsum(_in_proj_sizes())[:-1]]
    for layer in range(DEPTH):
        mod = jax.nn.silu(c) @ w_ada[layer] + b_ada[layer]
        sh1, sc1, g1, sh2, sc2, g2 = jnp.split(mod[:, None, :], N_MOD, axis=-1)

        h = rms_norm(x, norm1_w[layer]) * (1.0 + sc1) + sh1
        proj = h @ w_in[layer]
        (cb, cc, cx, q, k, v, qi, ki, wi, gate_conv, gate_attn) = jnp.split(proj, offsets, axis=-1)

        y_conv = (cb * causal_dwconv(cc * cx, conv_w[layer])) @ w_conv_out[layer]

        q = rms_norm(q.reshape(b, s, N_HEADS, HEAD_DIM), q_norm_w[layer])
        k = rms_norm(k.reshape(b, s, N_HEADS, HEAD_DIM), k_norm_w[layer])
        v = v.reshape(b, s, N_HEADS, HEAD_DIM)
        qi = qi.reshape(b, s, IDX_HEADS, IDX_DIM)
        y_attn = dsa_attention(q, k, v, qi, ki, wi) @ w_attn_out[layer]

        mix = jax.nn.sigmoid(gate_conv) * y_conv + jax.nn.sigmoid(gate_attn) * y_attn
        x = x + g1 * (mix @ w_o[layer])

        h2 = rms_norm(x, norm2_w[layer]) * (1.0 + sc2) + sh2
        x = x + g2 * peer_ffn(h2, w_peer_q[layer], peer_sub_keys[layer], peer_u[layer], peer_v[layer])
    return x
```

```python
import math
from contextlib import ExitStack

import numpy as np
import concourse.bass as bass
import concourse.mybir as mybir
from concourse.bass_utils import run_bass_kernel_spmd

F32 = mybir.dt.float32
BF16 = mybir.dt.bfloat16
U32 = mybir.dt.uint32
AF = mybir.ActivationFunctionType
ALU = mybir.AluOpType
AX = mybir.AxisListType

D = 1024
KC = 8
NIN = 5704
O_CB, O_CC, O_CX, O_Q, O_K, O_V, O_QI, O_KI, O_WI, O_GC, O_GA = 0, 512, 1024, 1536, 2048, 2560, 3072, 3584, 3648, 3656, 4680
NEXP = 16384
EPS = 1e-6
NEG = -1.0e30
NIT = 16
DCUT = 99
SELF_SYNC = True


class Buf:
    __slots__ = ("w", "r")

    def __init__(self):
        self.w = {}
        self.r = {}


class Tl:
    __slots__ = ("t", "b")

    def __init__(self, t):
        self.t = t
        self.b = Buf()


class Ring:
    def __init__(self, items):
        self.items = items
        self.i = 0

    def next(self):
        it = self.items[self.i % len(self.items)]
        self.i += 1
        return it


class Ctx:
    COMPUTE = ("pe", "act", "dve", "pool")
    ALL = ("pe", "act", "dve", "pool", "sp")
    NL = 8

    def __init__(self, nc):
        self.nc = nc
        self.prog = {n: [] for n in self.ALL}
        self.sems = {}
        self.cnt = {}
        self.known = {n: {} for n in self.ALL}
        for n in self.COMPUTE:
            self.sems[n] = nc.alloc_semaphore(name="s_" + n)
            self.cnt[n] = 0
        self.lane_rr = {}
        for q in ("sp", "act", "pool"):
            self.lane_rr[q] = 0
            for l in range(self.NL):
                k = (q, l)
                self.sems[k] = nc.alloc_semaphore(name="d_%s%d" % (q, l))
                self.cnt[k] = 0
        self.ninstr = 0

    def _wait(self, eng, key, val):
        if val <= 0:
            return
        if key == eng and (eng == "pe" or not SELF_SYNC):
            return
        if self.known[eng].get(key, 0) < val:
            self.prog[eng].append(("w", key, val))
            self.known[eng][key] = val

    def _deps(self, eng, reads, writes, pwrites):
        deps = {}
        for b in reads:
            for k, v in b.w.items():
                if deps.get(k, 0) < v:
                    deps[k] = v
        for b in writes:
            for dct in (b.w, b.r):
                for k, v in dct.items():
                    if deps.get(k, 0) < v:
                        deps[k] = v
        for b in pwrites:
            for k, v in b.r.items():
                if deps.get(k, 0) < v:
                    deps[k] = v
            for k, v in b.w.items():
                if k != eng and deps.get(k, 0) < v:
                    deps[k] = v
        for k, v in deps.items():
            self._wait(eng, k, v)

    def _mark(self, key, n, reads, writes, pwrites):
        for b in reads:
            b.r[key] = n
        for b in writes:
            b.w = {key: n}
            b.r = {}
        for b in pwrites:
            b.w[key] = n

    def op(self, eng, fn, reads=(), writes=(), pwrites=()):
        self._deps(eng, reads, writes, pwrites)
        self.cnt[eng] += 1
        self.prog[eng].append(("o", fn))
        self._mark(eng, self.cnt[eng], reads, writes, pwrites)
        self.ninstr += 1

    def dma(self, q, out, in_, reads=(), writes=(), pwrites=(), slow=False):
        l = self.lane_rr[q] % self.NL
        self.lane_rr[q] += 1
        key = (q, l)
        self._wait(q, key, self.cnt[key])
        self._deps(q, reads, writes, pwrites)
        self.cnt[key] += 16
        self.prog[q].append(("d", out, in_, key, slow))
        self._mark(key, self.cnt[key], reads, writes, pwrites)
        self.ninstr += 1

    def barrier(self):
        for e in self.ALL:
            for k in self.sems:
                self._wait(e, k, self.cnt[k])

    def finish(self):
        for k in self.sems:
            self._wait("sp", k, self.cnt[k])
        nc = self.nc

        def mk(name):
            def body(e):
                for it in self.prog[name]:
                    if it[0] == "w":
                        e.wait_ge(self.sems[it[1]], it[2])
                    elif it[0] == "o":
                        it[1](e).then_inc(self.sems[name], 1)
                    else:
                        if it[4]:
                            e.dma_start(out=it[1], in_=it[2], allow_slow_non_contiguous=True).then_inc(self.sems[it[3]], 16)
                        else:
                            e.dma_start(out=it[1], in_=it[2]).then_inc(self.sems[it[3]], 16)
            return body

        with nc.Block() as block:
            block.tensor(mk("pe"))
            block.scalar(mk("act"))
            block.vector(mk("dve"))
            block.gpsimd(mk("pool"))
            block.sync(mk("sp"))


def ACTF(out, in_, func, **kw):
    return lambda e: e.activation(out=out, in_=in_, func=func, **kw)


def MM(out, lhsT, rhs, start=True, stop=True):
    return lambda e: e.matmul(out, lhsT, rhs, start=start, stop=stop)


def TR(out, in_, ident):
    return lambda e: e.transpose(out, in_, ident)


def TS(out, in0, s1, s2, op0, op1=None, accum_out=None):
    if op1 is None:
        return lambda e: e.tensor_scalar(out=out, in0=in0, scalar1=s1, scalar2=s2, op0=op0, accum_out=accum_out)
    return lambda e: e.tensor_scalar(out=out, in0=in0, scalar1=s1, scalar2=s2, op0=op0, op1=op1, accum_out=accum_out)


def TT(out, in0, in1, op):
    return lambda e: e.tensor_tensor(out=out, in0=in0, in1=in1, op=op)


def STT(out, in0, scalar, in1, op0, op1):
    return lambda e: e.scalar_tensor_tensor(out=out, in0=in0, scalar=scalar, in1=in1, op0=op0, op1=op1)


def CP(out, in_):
    return lambda e: e.tensor_copy(out=out, in_=in_)


def RED(out, in_, op):
    return lambda e: e.tensor_reduce(out=out, in_=in_, axis=AX.X, op=op)


def RCP(out, in_):
    return lambda e: e.reciprocal(out=out, in_=in_)


def MSET(ap, c):
    return lambda e: e.memset(ap, c)


def build(NSEQ, S, GB=256, dbg=False, phases="ABCDE"):
    T = NSEQ * S
    NT = T // 128
    KSEL = min(256, S // 4)
    JB = GB // 128
    nc = bass.Bass("TRN2", target_bir_lowering=False)
    ctx = Ctx(nc)
    op, dma = ctx.op, ctx.dma

    def din(name, shape, dt=F32):
        return nc.dram_tensor(name, list(shape), dt, kind="ExternalInput").ap()

    io = {
        "x": din("x", [T, D]), "cT": din("cT", [128, KC, NSEQ]), "w_ada": din("w_ada", [D, 6 * D]),
        "b_ada": din("b_ada", [6 * D]), "b_adaT": din("b_adaT", [128, 48]),
        "n1T": din("n1T", [128, KC]), "n2T": din("n2T", [128, KC]), "w_in": din("w_in", [D, NIN]),
        "convT": din("convT", [128, 4, 3]), "w_conv_out": din("w_conv_out", [512, D]),
        "qk_w": din("qk_w", [128, 2]), "w_attn_out": din("w_attn_out", [512, D]), "w_o": din("w_o", [D, D]),
        "w_peer_q": din("w_peer_q", [D, D]), "skT": din("skT", [128, 8, 128]),
        "uT": din("uT", [D, NEXP]), "vp": din("vp", [NEXP, D]), "consts": din("consts", [128, 5, 128]),
    }
    y = nc.dram_tensor("y", [T, D], F32, kind="ExternalOutput").ap()
    skind = "ExternalOutput" if dbg else "Internal"

    def dscr(name, shape, dt):
        t = nc.dram_tensor(name, list(shape), dt, kind=skind).ap()
        return Tl(t)

    scr = {
        "mixA": dscr("s_mixA", [D, T], BF16), "sga": dscr("s_sga", [D, T], BF16),
        "qT": dscr("s_qT", [512, T], BF16), "kT": dscr("s_kT", [512, T], BF16),
        "qiT": dscr("s_qiT", [512, T], BF16), "kiT": dscr("s_kiT", [64, T], BF16),
        "V": dscr("s_V", [T, 520], BF16), "attnT": dscr("s_attnT", [512, T], BF16),
        "gbc": dscr("s_gbc", [NSEQ, 2, 128, D], F32), "x1": dscr("s_x1", [T, D], F32),
        "h2T": dscr("s_h2T", [D, T], BF16), "sel": dscr("s_sel", [3, 128, T], BF16),
        "uTb": dscr("s_uTb", [D, NEXP], BF16), "vb": dscr("s_vb", [NEXP, D], BF16),
    }

    with ExitStack() as gs:
        def tile(es, name, shape, dt):
            return Tl(es.enter_context(nc.sbuf_tensor("sb_" + name, list(shape), dt)))

        psum = gs.enter_context(nc.psum_tensor("psum", [128, 8, 512], F32))
        pb = [Buf() for _ in range(8)]

        class PB:
            def __init__(self, i):
                self.i = i
                self.b = pb[i]
                self.f = psum[:, i, :]
                self.h = psum[:, i, :].bitcast(BF16)

        PBs = [PB(i) for i in range(8)]

        cst = tile(gs, "cst", [128, 5, 128], F32)
        ident_bf = tile(gs, "ident_bf", [128, 128], BF16)
        bones_bf = tile(gs, "bones_bf", [128, 128], BF16)
        iota_bf = tile(gs, "iota_bf", [128, 128], BF16)
        epst = tile(gs, "epst", [128, 1], F32)
        modTb = tile(gs, "modTb", [128, 48, NSEQ], F32)
        A1 = tile(gs, "A1", [128, KC, NSEQ], F32)
        A2 = tile(gs, "A2", [128, KC, NSEQ], F32)
        widx = tile(gs, "widx", [128, NT, 8], F32)
        qkw = tile(gs, "qkw", [128, 2], F32)
        qws = tile(gs, "qws", [128, 1], F32)
        ident_f = cst.t[:, 0, :]
        causal_f = cst.t[:, 1, :]
        ones_f = cst.t[:, 4, :]

        dma("sp", cst.t[:], io["consts"], writes=[cst.b])
        dma("sp", qkw.t[:], io["qk_w"], writes=[qkw.b])
        op("dve", CP(ident_bf.t[:], cst.t[:, 0, :]), reads=[cst.b], writes=[ident_bf.b])
        op("dve", CP(bones_bf.t[:], cst.t[:, 2, :]), reads=[cst.b], writes=[bones_bf.b])
        op("dve", CP(iota_bf.t[:], cst.t[:, 3, :]), reads=[cst.b], writes=[iota_bf.b])
        op("dve", MSET(epst.t[:], EPS), writes=[epst.b])
        op("dve", TS(qws.t[:], qkw.t[:, 0:1], 0.125, None, ALU.mult), reads=[qkw.b], writes=[qws.b])

        if "A" in phases:
            with ExitStack() as es:
                cT = tile(es, "cT", [128, KC, NSEQ], F32)
                sc = tile(es, "sc", [128, KC, NSEQ], F32)
                scb = tile(es, "scb", [128, KC * NSEQ, 128], F32)
                badaT = tile(es, "badaT", [128, 48], F32)
                bbc = tile(es, "bbc", [128, 2, D], F32)
                n1T = tile(es, "n1T", [128, KC], F32)
                n2T = tile(es, "n2T", [128, KC], F32)
                war = Ring([tile(es, "wa%d" % i, [128, KC, 512], F32) for i in range(2)])
                gst = Ring([tile(es, "gst%d" % i, [128, 512], F32) for i in range(2)])
                dma("sp", cT.t[:], io["cT"], writes=[cT.b])
                dma("sp", badaT.t[:], io["b_adaT"], writes=[badaT.b])
                dma("sp", n1T.t[:], io["n1T"], writes=[n1T.b])
                dma("sp", n2T.t[:], io["n2T"], writes=[n2T.b])
                dma("sp", bbc.t[:, 0, :], io["b_ada"][2 * D:3 * D].partition_broadcast(128), pwrites=[bbc.b])
                dma("sp", bbc.t[:, 1, :], io["b_ada"][5 * D:6 * D].partition_broadcast(128), pwrites=[bbc.b])
                op("act", ACTF(sc.t[:], cT.t[:], AF.Silu), reads=[cT.b], writes=[sc.b])
                op("dve", CP(scb.t[:], sc.t[:].rearrange("p k s -> p (k s)").unsqueeze(2).to_broadcast([128, KC * NSEQ, 128])),
                   reads=[sc.b], writes=[scb.b])
                pM = PBs[0]
                pG = Ring([PBs[1], PBs[2]])
                wav = io["w_ada"].rearrange("(kc p) f -> p kc f", p=128)
                for blk in range(12):
                    wa = war.next()
                    dma("sp", wa.t[:], wav[:, :, blk * 512:(blk + 1) * 512], writes=[wa.b])
                    for fl in range(4):
                        fc = blk * 4 + fl
                        for kc in range(KC):
                            op("pe", MM(pM.f[:, fc * NSEQ:(fc + 1) * NSEQ], wa.t[:, kc, fl * 128:(fl + 1) * 128], sc.t[:, kc, :],
                                        start=(kc == 0), stop=(kc == KC - 1)), reads=[wa.b, sc.b], pwrites=[pM.b])
                    if blk in (4, 5, 10, 11):
                        which = 0 if blk < 6 else 1
                        half = blk % 2
                        for s in range(NSEQ):
                            p = pG.next()
                            for kc in range(KC):
                                op("pe", MM(p.f[:, :], scb.t[:, kc * NSEQ + s, :], wa.t[:, kc, :], start=(kc == 0), stop=(kc == KC - 1)),
                                   reads=[wa.b, scb.b], pwrites=[p.b])
                            g = gst.next()
                            op("dve", TT(g.t[:], p.f[:, :], bbc.t[:, which, half * 512:(half + 1) * 512], ALU.add),
                               reads=[p.b, bbc.b], writes=[g.b])
                            dma("sp", scr["gbc"].t[s, which, :, half * 512:(half + 1) * 512], g.t[:], reads=[g.b], pwrites=[scr["gbc"].b])
                op("dve", TT(modTb.t[:], pM.f[:, 0:48 * NSEQ].rearrange("p (f s) -> p f s", s=NSEQ),
                             badaT.t[:].unsqueeze(2).to_broadcast([128, 48, NSEQ]), ALU.add),
                   reads=[pM.b, badaT.b], writes=[modTb.b])
                op("dve", STT(A1.t[:], modTb.t[:, 8:16, :], 1.0, n1T.t[:].unsqueeze(2).to_broadcast([128, KC, NSEQ]), ALU.add, ALU.mult),
                   reads=[modTb.b, n1T.b], writes=[A1.b])
                op("dve", STT(A2.t[:], modTb.t[:, 32:40, :], 1.0, n2T.t[:].unsqueeze(2).to_broadcast([128, KC, NSEQ]), ALU.add, ALU.mult),
                   reads=[modTb.b, n2T.b], writes=[A2.b])
                ctx.barrier()

        if "B" in phases:
            with ExitStack() as es:
                win = tile(es, "win", [128, KC, NIN], BF16)
                wco = tile(es, "wco", [128, 4, D], BF16)
                convT = tile(es, "convT", [128, 4, 3], F32)
                dma("sp", convT.t[:], io["convT"], writes=[convT.b])
                with ExitStack() as es2:
                    stg = Ring([tile(es2, "stg%d" % i, [128, KC, 512], F32) for i in range(2)])
                    wiv = io["w_in"].rearrange("(kc p) f -> p kc f", p=128)
                    col = 0
                    i = 0
                    while col < NIN:
                        w = min(512, NIN - col)
                        st = stg.next()
                        dma("sp", st.t[:, :, 0:w], wiv[:, :, col:col + w], writes=[st.b])
                        eng = ("dve", "act", "pool")[i % 3]
                        if eng == "act":
                            op("act", ACTF(win.t[:, :, col:col + w], st.t[:, :, 0:w], AF.Copy), reads=[st.b], pwrites=[win.b])
                        else:
                            op(eng, CP(win.t[:, :, col:col + w], st.t[:, :, 0:w]), reads=[st.b], pwrites=[win.b])
                        col += w
                        i += 1
                    st = stg.next()
                    stv = st.t[:].rearrange("p k f -> p (k f)").rearrange("p (c d) -> p c d", c=4)
                    dma("sp", stv, io["w_conv_out"].rearrange("(c p) d -> p c d", p=128), writes=[st.b])
                    op("dve", CP(wco.t[:], stv), reads=[st.b], writes=[wco.b])
                    ctx.barrier()
                xgr = Ring([tile(es, "xg%d" % i, [128, JB, D], F32) for i in range(2)])
                junk = tile(es, "junkB", [128, D], BF16)
                ssq = tile(es, "ssq", [128, JB], F32)
                rt = tile(es, "rt", [128, JB], F32)
                rstd = tile(es, "rstd", [128, JB], F32)
                xs = tile(es, "xs", [128, JB, D], BF16)
                hTr = Ring([tile(es, "hT%d" % i, [128, KC, GB], BF16) for i in range(2)])
                ubuf = tile(es, "ubuf", [128, 4, GB + 2], F32)
                tmpf = Ring([tile(es, "tmpf%d" % i, [128, GB], F32) for i in range(3)])
                c1 = tile(es, "c1", [128, GB], F32)
                c2 = tile(es, "c2", [128, GB], F32)
                c3 = tile(es, "c3", [128, GB], F32)
                yA = tile(es, "yA", [128, 4, GB], BF16)
                mixA_st = tile(es, "mixA_st", [128, 8, GB], BF16)
                sga_st = tile(es, "sga_st", [128, 8, GB], BF16)
                q_st = tile(es, "q_st", [128, 4, GB], BF16)
                k_st = tile(es, "k_st", [128, 4, GB], BF16)
                qi_st = tile(es, "qi_st", [128, 4, GB], BF16)
                ki_st = tile(es, "ki_st", [64, GB], BF16)
                v_st = tile(es, "v_st", [128, JB, 8, 65], BF16)
                sqr = Ring([tile(es, "sq%d" % i, [128, GB], BF16) for i in range(2)])
                rtr = Ring([tile(es, "rtq%d" % i, [128, GB], F32) for i in range(2)])
                rrr = Ring([tile(es, "rrq%d" % i, [128, GB], F32) for i in range(2)])
                op("pool", MSET(v_st.t[:], 1.0), writes=[v_st.b])
                ptr = Ring([PBs[0], PBs[1]])
                gen = Ring([PBs[i] for i in range(2, 8)])
                evi = [0]

                def evac(out, in_, rd, wr):
                    evi[0] += 1
                    if evi[0] % 2:
                        op("act", ACTF(out, in_, AF.Copy), reads=rd, pwrites=wr)
                    else:
                        op("dve", CP(out, in_), reads=rd, pwrites=wr)

                for g in range(T // GB):
                    t0 = g * GB
                    seq = t0 // S
                    first = (t0 % S == 0)
                    xg = xgr.next()
                    hT = hTr.next()
                    dma("sp", xg.t[:], io["x"][t0:t0 + GB, :].rearrange("(j p) d -> p j d", p=128), writes=[xg.b])
                    for j in range(JB):
                        op("act", ACTF(junk.t[:], xg.t[:, j, :], AF.Square, accum_out=ssq.t[:, j:j + 1]), reads=[xg.b], writes=[junk.b], pwrites=[ssq.b])
                    op("act", ACTF(rt.t[:], ssq.t[:], AF.Sqrt, scale=1.0 / D, bias=epst.t[:, 0:1]), reads=[ssq.b, epst.b], writes=[rt.b])
                    op("dve", RCP(rstd.t[:], rt.t[:]), reads=[rt.b], writes=[rstd.b])
                    for j in range(JB):
                        op("dve", TS(xs.t[:, j, :], xg.t[:, j, :], rstd.t[:, j:j + 1], None, ALU.mult), reads=[xg.b, rstd.b], pwrites=[xs.b])
                    for kc in range(KC):
                        pt = ptr.next()
                        for j in range(JB):
                            op("pe", TR(pt.h[:, j * 128:(j + 1) * 128], xs.t[:, j, kc * 128:(kc + 1) * 128], ident_bf.t[:]),
                               reads=[xs.b, ident_bf.b], pwrites=[pt.b])
                        op("act", ACTF(hT.t[:, kc, :], pt.h[:, 0:GB], AF.Identity, scale=A1.t[:, kc, seq:seq + 1], bias=modTb.t[:, kc, seq:seq + 1]),
                           reads=[pt.b, A1.b, modTb.b], pwrites=[hT.b])

                    def proj(c0, M=128):
                        p = gen.next()
                        for kc in range(KC):
                            op("pe", MM(p.f[0:M, 0:GB], win.t[:, kc, c0:c0 + M], hT.t[:, kc, :], start=(kc == 0), stop=(kc == KC - 1)),
                               reads=[win.b, hT.b], pwrites=[p.b])
                        return p

                    if first:
                        op("dve", MSET(ubuf.t[:, :, 0:2], 0.0), pwrites=[ubuf.b])
                    for cch in range(4):
                        pcc = proj(O_CC + cch * 128)
                        pcx = proj(O_CX + cch * 128)
                        pcb = proj(O_CB + cch * 128)
                        tf = tmpf.next()
                        op("act", ACTF(tf.t[:], pcc.f[:, 0:GB], AF.Copy), reads=[pcc.b], writes=[tf.b])
                        op("dve", TT(ubuf.t[:, cch, 2:2 + GB], pcx.f[:, 0:GB], tf.t[:], ALU.mult), reads=[pcx.b, tf.b], pwrites=[ubuf.b])
                        op("dve", TS(c1.t[:], ubuf.t[:, cch, 0:GB], convT.t[:, cch, 0:1], None, ALU.mult), reads=[ubuf.b, convT.b], writes=[c1.b])
                        op("dve", STT(c2.t[:], ubuf.t[:, cch, 1:1 + GB], convT.t[:, cch, 1:2], c1.t[:], ALU.mult, ALU.add),
                           reads=[ubuf.b, convT.b, c1.b], writes=[c2.b])
                        op("dve", STT(c3.t[:], ubuf.t[:, cch, 2:2 + GB], convT.t[:, cch, 2:3], c2.t[:], ALU.mult, ALU.add),
                           reads=[ubuf.b, convT.b, c2.b], writes=[c3.b])
                        op("dve", TT(yA.t[:, cch, :], pcb.f[:, 0:GB], c3.t[:], ALU.mult), reads=[pcb.b, c3.b], pwrites=[yA.b])
                        op("dve", CP(ubuf.t[:, cch, 0:2], ubuf.t[:, cch, GB:GB + 2]), reads=[ubuf.b], pwrites=[ubuf.b])
                    for dc in range(8):
                        pyc = gen.next()
                        for cch in range(4):
                            op("pe", MM(pyc.f[:, 0:GB], wco.t[:, cch, dc * 128:(dc + 1) * 128], yA.t[:, cch, :], start=(cch == 0), stop=(cch == 3)),
                               reads=[wco.b, yA.b], pwrites=[pyc.b])
                        pgc = proj(O_GC + dc * 128)
                        tf = tmpf.next()
                        op("act", ACTF(tf.t[:], pgc.f[:, 0:GB], AF.Sigmoid), reads=[pgc.b], writes=[tf.b])
                        op("dve", TT(mixA_st.t[:, dc, :], pyc.f[:, 0:GB], tf.t[:], ALU.mult), reads=[pyc.b, tf.b], pwrites=[mixA_st.b])
                    dma("sp", scr["mixA"].t.rearrange("(dc p) t -> p dc t", p=128)[:, :, t0:t0 + GB], mixA_st.t[:], reads=[mixA_st.b], pwrites=[scr["mixA"].b])
                    for dc in range(8):
                        pga = proj(O_GA + dc * 128)
                        op("act", ACTF(sga_st.t[:, dc, :], pga.f[:, 0:GB], AF.Sigmoid), reads=[pga.b], pwrites=[sga_st.b])
                    dma("sp", scr["sga"].t.rearrange("(dc p) t -> p dc t", p=128)[:, :, t0:t0 + GB], sga_st.t[:], reads=[sga_st.b], pwrites=[scr["sga"].b])
                    for (base, wap, wb, st, nm) in ((O_Q, qws.t[:, 0:1], qws.b, q_st, "qT"), (O_K, qkw.t[:, 1:2], qkw.b, k_st, "kT")):
                        for c in range(4):
                            pq = proj(base + c * 128)
                            sq = sqr.next()
                            op("act", ACTF(sq.t[:], pq.f[:, 0:GB], AF.Square), reads=[pq.b], writes=[sq.b])
                            ps2 = gen.next()
                            op("pe", MM(ps2.f[:, 0:GB], bones_bf.t[:], sq.t[:]), reads=[bones_bf.b, sq.b], pwrites=[ps2.b])
                            r1 = rtr.next()
                            op("act", ACTF(r1.t[:], ps2.f[:, 0:GB], AF.Sqrt, scale=1.0 / 64, bias=epst.t[:, 0:1]), reads=[ps2.b, epst.b], writes=[r1.b])
                            r2 = rrr.next()
                            op("dve", RCP(r2.t[:], r1.t[:]), reads=[r1.b], writes=[r2.b])
                            op("dve", STT(st.t[:, c, :], pq.f[:, 0:GB], wap, r2.t[:], ALU.mult, ALU.mult), reads=[pq.b, wb, r2.b], pwrites=[st.b])
                        dma("sp", scr[nm].t.rearrange("(c p) t -> p c t", p=128)[:, :, t0:t0 + GB], st.t[:], reads=[st.b], pwrites=[scr[nm].b])
                    for c in range(4):
                        pq = proj(O_QI + c * 128)
                        evac(qi_st.t[:, c, :], pq.f[:, 0:GB], [pq.b], [qi_st.b])
                    dma("sp", scr["qiT"].t.rearrange("(c p) t -> p c t", p=128)[:, :, t0:t0 + GB], qi_st.t[:], reads=[qi_st.b], pwrites=[scr["qiT"].b])
                    pq = proj(O_KI, M=64)
                    evac(ki_st.t[:, :], pq.f[0:64, 0:GB], [pq.b], [ki_st.b])
                    dma("sp", scr["kiT"].t[:, t0:t0 + GB], ki_st.t[:], reads=[ki_st.b], pwrites=[scr["kiT"].b])
                    for j in range(JB):
                        p = gen.next()
                        for kc in range(KC):
                            op("pe", MM(p.f[:, 0:512], hT.t[:, kc, j * 128:(j + 1) * 128], win.t[:, kc, O_V:O_V + 512], start=(kc == 0), stop=(kc == KC - 1)),
                               reads=[win.b, hT.b], pwrites=[p.b])
                        evac(v_st.t[:, j, :, 0:64], p.f[:, 0:512].rearrange("p (h d) -> p h d", h=8), [p.b], [v_st.b])
                        p = gen.next()
                        for kc in range(KC):
                            op("pe", MM(p.f[:, 0:8], hT.t[:, kc, j * 128:(j + 1) * 128], win.t[:, kc, O_WI:O_WI + 8], start=(kc == 0), stop=(kc == KC - 1)),
                               reads=[win.b, hT.b], pwrites=[p.b])
                        op("dve", CP(widx.t[:, t0 // 128 + j, :], p.f[:, 0:8]), reads=[p.b], pwrites=[widx.b])
                    dma("sp", scr["V"].t[t0:t0 + GB, :].rearrange("(j p) f -> p j f", p=128), v_st.t[:].rearrange("p j h d -> p j (h d)"),
                        reads=[v_st.b], pwrites=[scr["V"].b])
                ctx.barrier()

        if "C" in phases:
            with ExitStack() as es:
                NKB = S // 128
                QG = min(512, S)
                kTs = tile(es, "kTs", [128, 4, S], BF16)
                Vs = tile(es, "Vs", [128, NKB, 520], BF16)
                kiTs = tile(es, "kiTs", [128, S], BF16)
                qTg = Ring([tile(es, "qTg%d" % i, [128, 4, QG], BF16) for i in range(2)])
                qiTg = Ring([tile(es, "qiTg%d" % i, [128, 4, QG], BF16) for i in range(2)])
                Dh = tile(es, "Dh", [128, 8, 128], BF16)
                rr = Ring([tile(es, "relu%d" % i, [128, 512], BF16) for i in range(4)])
                score = tile(es, "score", [128, S], F32)
                junk = tile(es, "junkC", [128, S], BF16)
                bis = tile(es, "bis", [128, 8], F32)
                wf = tile(es, "wf", [128, NIT + 2], F32)
                pw2 = tile(es, "pw2", [128, NIT + 2], F32)
                for i_ in range(NIT + 2):
                    op("dve", MSET(pw2.t[:, i_:i_ + 1], 2.0 ** -i_), pwrites=[pw2.b])
                dthr = tile(es, "dthr", [128, 128], F32)
                thrbc = tile(es, "thrbc", [128, 128], F32)
                maskT = tile(es, "maskT", [128, NKB, 128], BF16)
                PTr = Ring([tile(es, "PT%d" % i, [128, 512], BF16) for i in range(3)])
                PMr = Ring([tile(es, "PM%d" % i, [128, 512], BF16) for i in range(3)])
                rz = tile(es, "rz", [128, 8], F32)
                attn_tm = tile(es, "attn_tm", [128, 8, 64], BF16)
                attnT_st = tile(es, "attnT_st", [128, 4, QG], BF16)
                psL = Ring([PBs[0], PBs[1]])
                psS = PBs[2]
                psT = PBs[3]
                psA = Ring([PBs[4], PBs[5]])
                psO = [PBs[6], PBs[7]]
                for s in range(NSEQ):
                    sl = slice(s * S, (s + 1) * S)
                    dma("sp", kTs.t[:], scr["kT"].t.rearrange("(c p) t -> p c t", p=128)[:, :, sl], reads=[scr["kT"].b], writes=[kTs.b])
                    dma("sp", Vs.t[:], scr["V"].t[sl, :].rearrange("(kb p) f -> p kb f", p=128), reads=[scr["V"].b], writes=[Vs.b])
                    dma("sp", kiTs.t[0:64, :], scr["kiT"].t[:, sl], reads=[scr["kiT"].b], writes=[kiTs.b])
                    dma("sp", kiTs.t[64:128, :], scr["kiT"].t[:, sl], reads=[scr["kiT"].b], pwrites=[kiTs.b])
                    for qg in range(S // QG):
                        qT = qTg.next()
                        qiT = qiTg.next()
                        tq0 = s * S + qg * QG
                        dma("sp", qT.t[:], scr["qT"].t.rearrange("(c p) t -> p c t", p=128)[:, :, tq0:tq0 + QG], reads=[scr["qT"].b], writes=[qT.b])
                        dma("sp", qiT.t[:], scr["qiT"].t.rearrange("(c p) t -> p c t", p=128)[:, :, tq0:tq0 + QG], reads=[scr["qiT"].b], writes=[qiT.b])
                        for qs in range(QG // 128):
                            qb = qg * (QG // 128) + qs
                            nkb = qb + 1
                            nk = nkb * 128
                            tix = (s * S) // 128 + qb
                            qsl = slice(qs * 128, (qs + 1) * 128)
                            op("dve", TT(Dh.t[:], ident_bf.t[:].unsqueeze(1).to_broadcast([128, 8, 128]),
                                         widx.t[:, tix, :].unsqueeze(2).to_broadcast([128, 8, 128]), ALU.mult),
                               reads=[ident_bf.b, widx.b], writes=[Dh.b])
                            nch = (nk + 511) // 512
                            for ch in range(nch):
                                k0 = ch * 512
                                w = min(512, nk - k0)
                                for h in range(8):
                                    hp = h % 2
                                    pl = psL.next()
                                    op("pe", MM(pl.f[:, 0:w], qiT.t[hp * 64:(hp + 1) * 64, h // 2, qsl], kiTs.t[hp * 64:(hp + 1) * 64, k0:k0 + w]),
                                       reads=[qiT.b, kiTs.b], pwrites=[pl.b])
                                    r = rr.next()
                                    if h % 2 == 0:
                                        op("act", ACTF(r.t[:, 0:w], pl.f[:, 0:w], AF.Relu), reads=[pl.b], writes=[r.b])
                                    else:
                                        op("dve", TS(r.t[:, 0:w], pl.f[:, 0:w], 0.0, None, ALU.max), reads=[pl.b], writes=[r.b])
                                    op("pe", MM(psS.f[:, 0:w], Dh.t[:, h, :], r.t[:, 0:w], start=(h == 0), stop=(h == 7)),
                                       reads=[Dh.b, r.b], pwrites=[psS.b])
                                if ch == nch - 1:
                                    wd = w - 128
                                    if wd > 0:
                                        op("act", ACTF(score.t[:, k0:k0 + wd], psS.f[:, 0:wd], AF.Copy), reads=[psS.b], pwrites=[score.b])
                                    op("dve", TT(score.t[:, nk - 128:nk], psS.f[:, wd:w], causal_f, ALU.add), reads=[psS.b, cst.b], pwrites=[score.b])
                                else:
                                    op("act", ACTF(score.t[:, k0:k0 + w], psS.f[:, 0:w], AF.Copy), reads=[psS.b], pwrites=[score.b])
                            lo, w0, mid, cnt, gg, mx = (bis.t[:, i:i + 1] for i in range(6))
                            if nk <= KSEL:
                                op("dve", MSET(lo, -1.0e29), writes=[bis.b])
                            else:
                                op("dve", RED(mx, score.t[:, 0:nk], ALU.max), reads=[score.b], writes=[bis.b])
                                op("dve", RED(lo, score.t[:, 0:nk - 128], ALU.min), reads=[score.b], writes=[bis.b])
                                op("dve", TT(w0, mx, lo, ALU.subtract), reads=[bis.b], writes=[bis.b])
                                op("dve", TS(wf.t[:], pw2.t[:], w0, None, ALU.mult), reads=[bis.b, pw2.b], writes=[wf.b])
                                op("dve", TT(mid, lo, wf.t[:, 1:2], ALU.add), reads=[bis.b, wf.b], writes=[bis.b])
                                for it in range(NIT):
                                    op("dve", TS(junk.t[:, 0:nk], score.t[:, 0:nk], mid, None, ALU.is_ge, ALU.add, accum_out=cnt),
                                       reads=[score.b, bis.b], writes=[bis.b, junk.b])
                                    op("dve", TS(gg, cnt, KSEL - 0.5, 0.5, ALU.is_ge, ALU.subtract), reads=[bis.b], writes=[bis.b])
                                    op("dve", STT(mid, gg, wf.t[:, it + 1:it + 2], mid, ALU.mult, ALU.add), reads=[bis.b, wf.b], writes=[bis.b])
                                op("dve", TT(lo, mid, wf.t[:, NIT + 1:NIT + 2], ALU.subtract), reads=[bis.b, wf.b], writes=[bis.b])
                            op("dve", TS(dthr.t[:], ident_f, lo, None, ALU.mult), reads=[cst.b, bis.b], writes=[dthr.b])
                            op("pe", MM(psT.f[:, 0:128], ones_f, dthr.t[:]), reads=[cst.b, dthr.b], pwrites=[psT.b])
                            op("act", ACTF(thrbc.t[:], psT.f[:, 0:128], AF.Copy), reads=[psT.b], writes=[thrbc.b])
                            for c4 in range((nkb + 3) // 4):
                                n4 = min(4, nkb - c4 * 4)
                                for i in range(n4):
                                    kb = c4 * 4 + i
                                    op("pe", TR(psT.f[:, i * 128:(i + 1) * 128], score.t[:, kb * 128:(kb + 1) * 128], ident_f),
                                       reads=[score.b, cst.b], pwrites=[psT.b])
                                op("dve", TT(maskT.t[:, c4 * 4:c4 * 4 + n4, :], psT.f[:, 0:n4 * 128].rearrange("p (a b) -> p a b", b=128),
                                             thrbc.t[:].unsqueeze(1).to_broadcast([128, n4, 128]), ALU.is_ge),
                                   reads=[psT.b, thrbc.b], pwrites=[maskT.b])
                            for h in range(8):
                                hp = h % 2
                                po = psO[h // 4]
                                for c4 in range((nkb + 3) // 4):
                                    n4 = min(4, nkb - c4 * 4)
                                    pa = psA.next()
                                    for i in range(n4):
                                        kb = c4 * 4 + i
                                        op("pe", MM(pa.f[:, i * 128:(i + 1) * 128], kTs.t[hp * 64:(hp + 1) * 64, h // 2, kb * 128:(kb + 1) * 128],
                                                    qT.t[hp * 64:(hp + 1) * 64, h // 2, qsl]), reads=[kTs.b, qT.b], pwrites=[pa.b])
                                    pt = PTr.next()
                                    op("act", ACTF(pt.t[:, 0:n4 * 128], pa.f[:, 0:n4 * 128], AF.Exp), reads=[pa.b], writes=[pt.b])
                                    pm = PMr.next()
                                    op("dve", TT(pm.t[:, 0:n4 * 128], pt.t[:, 0:n4 * 128], maskT.t[:, c4 * 4:c4 * 4 + n4, :].rearrange("p a b -> p (a b)"), ALU.mult),
                                       reads=[pt.b, maskT.b], writes=[pm.b])
                                    for i in range(n4):
                                        kb = c4 * 4 + i
                                        op("pe", MM(po.f[:, (h % 4) * 65:(h % 4) * 65 + 65], pm.t[:, i * 128:(i + 1) * 128], Vs.t[:, kb, h * 65:(h + 1) * 65],
                                                    start=(kb == 0), stop=(kb == nkb - 1)), reads=[pm.b, Vs.b], pwrites=[po.b])
                            for hh in range(2):
                                pv = psO[hh].f[:, 0:260].rearrange("p (h d) -> p h d", d=65)
                                op("dve", RCP(rz.t[:, hh * 4:(hh + 1) * 4], pv[:, :, 64]), reads=[psO[hh].b], pwrites=[rz.b])
                                op("dve", TT(attn_tm.t[:, hh * 4:(hh + 1) * 4, :], pv[:, :, 0:64],
                                             rz.t[:, hh * 4:(hh + 1) * 4].unsqueeze(2).to_broadcast([128, 4, 64]), ALU.mult),
                                   reads=[psO[hh].b, rz.b], pwrites=[attn_tm.b])
                            for c in range(4):
                                op("pe", TR(psT.h[:, c * 128:(c + 1) * 128], attn_tm.t[:, 2 * c:2 * c + 2, :].rearrange("p a b -> p (a b)"), ident_bf.t[:]),
                                   reads=[attn_tm.b, ident_bf.b], pwrites=[psT.b])
                            op("act", ACTF(attnT_st.t[:, :, qsl], psT.h[:, 0:512].rearrange("p (c t) -> p c t", c=4), AF.Copy),
                               reads=[psT.b], pwrites=[attnT_st.b])
                        dma("sp", scr["attnT"].t.rearrange("(c p) t -> p c t", p=128)[:, :, tq0:tq0 + QG], attnT_st.t[:], reads=[attnT_st.b], pwrites=[scr["attnT"].b])
                ctx.barrier()

        if "D" in phases:
            with ExitStack() as es:
                wao = tile(es, "wao", [128, 4, D], BF16)
                wo = tile(es, "wo", [128, KC, D], BF16)
                wpq = tile(es, "wpq", [128, KC, D], BF16)
                skTb = tile(es, "skTb", [128, 8, 128], BF16)
                with ExitStack() as es2:
                    stg = Ring([tile(es2, "stgD%d" % i, [128, KC, 512], F32) for i in range(2)])
                    ci_ = 0
                    for (src, dst) in ((io["w_o"], wo), (io["w_peer_q"], wpq)):
                        sv = src.rearrange("(kc p) f -> p kc f", p=128)
                        for hh in range(2):
                            st = stg.next()
                            dma("sp", st.t[:], sv[:, :, hh * 512:(hh + 1) * 512], writes=[st.b])
                            if ci_ % 2:
                                op("act", ACTF(dst.t[:, :, hh * 512:(hh + 1) * 512], st.t[:], AF.Copy), reads=[st.b], pwrites=[dst.b])
                            else:
                                op("dve", CP(dst.t[:, :, hh * 512:(hh + 1) * 512], st.t[:]), reads=[st.b], pwrites=[dst.b])
                            ci_ += 1
                    st = stg.next()
                    stv = st.t[:].rearrange("p k f -> p (k f)").rearrange("p (c d) -> p c d", c=4)
                    dma("sp", stv, io["w_attn_out"].rearrange("(c p) d -> p c d", p=128), writes=[st.b])
                    op("dve", CP(wao.t[:], stv), reads=[st.b], writes=[wao.b])
                    st = stg.next()
                    stv2 = st.t[:].rearrange("p k f -> p (k f)")[:, 0:1024].rearrange("p (h n) -> p h n", h=8)
                    dma("sp", stv2, io["skT"], writes=[st.b])
                    op("dve", CP(skTb.t[:], stv2), reads=[st.b], writes=[skTb.b])
                    ctx.barrier()
                gb1 = tile(es, "gb1", [128, D], F32)
                attg = Ring([tile(es, "attg%d" % i, [128, 4, GB], BF16) for i in range(2)])
                sgag = Ring([tile(es, "sgag%d" % i, [128, 8, GB], BF16) for i in range(2)])
                mxag = Ring([tile(es, "mxag%d" % i, [128, 8, GB], BF16) for i in range(2)])
                xgr = Ring([tile(es, "xgD%d" % i, [128, JB, D], F32) for i in range(2)])
                tmq = Ring([tile(es, "tmq%d" % i, [128, GB], F32) for i in range(2)])
                mixT = tile(es, "mixT", [128, 8, GB], BF16)
                tz = Ring([tile(es, "tz%d" % i, [128, 512], F32) for i in range(2)])
                x1 = tile(es, "x1", [128, JB, D], F32)
                junk = tile(es, "junkD", [128, D], BF16)
                ssq = tile(es, "ssqD", [128, JB], F32)
                rt = tile(es, "rtD", [128, JB], F32)
                rstd = tile(es, "rstdD", [128, JB], F32)
                xs = tile(es, "xsD", [128, JB, D], BF16)
                h2T = tile(es, "h2T", [128, KC, GB], BF16)
                qpT = tile(es, "qpT", [128, 8, GB], BF16)
                s_sb = tile(es, "s_sb", [128, 16, 128], F32)
                s_tmp = tile(es, "s_tmp", [128, 16, 128], F32)
                v_all = tile(es, "v_all", [128, 16, 16], F32)
                idx_all = tile(es, "idx_all", [128, 16, 16], U32)
                idx_bf = tile(es, "idx_bf", [128, 16, 16], BF16)
                cand = tile(es, "cand", [128, 8, 256], F32)
                cand2 = tile(es, "cand2", [128, 8, 256], F32)
                scv = tile(es, "scv", [128, 8, 16], F32)
                civ = tile(es, "civ", [128, 8, 16], U32)
                ca_u = tile(es, "ca_u", [128, 8, 16], U32)
                cb_u = tile(es, "cb_u", [128, 8, 16], U32)
                ca_bf = tile(es, "ca_bf", [128, 8, 16], BF16)
                cb_bf = tile(es, "cb_bf", [128, 8, 16], BF16)
                eqa = tile(es, "eqa", [128, 8, 16, 16], BF16)
                prd = tile(es, "prd", [128, 8, 16, 16], BF16)
                sm = tile(es, "sm", [128, 8, 16], F32)
                zz = tile(es, "zz", [128, 8], F32)
                sel_tm = tile(es, "sel_tm", [128, 3, 128], F32)
                selT = tile(es, "selT", [128, 3, 128], BF16)
                ptr = Ring([PBs[0], PBs[1]])
                gen = Ring([PBs[i] for i in range(2, 8)])
                evi = [0]

                def evacD(out, in_, rd, wr):
                    evi[0] += 1
                    if evi[0] % 2:
                        op("act", ACTF(out, in_, AF.Copy), reads=rd, pwrites=wr)
                    else:
                        op("dve", CP(out, in_), reads=rd, pwrites=wr)

                for g in range(T // GB):
                    t0 = g * GB
                    seq = t0 // S
                    if t0 % S == 0:
                        dma("sp", gb1.t[:], scr["gbc"].t[seq, 0, :, :], reads=[scr["gbc"].b], writes=[gb1.b])
                    at = attg.next()
                    sg = sgag.next()
                    ma = mxag.next()
                    xg = xgr.next()
                    dma("sp", at.t[:], scr["attnT"].t.rearrange("(c p) t -> p c t", p=128)[:, :, t0:t0 + GB], reads=[scr["attnT"].b], writes=[at.b])
                    dma("sp", sg.t[:], scr["sga"].t.rearrange("(c p) t -> p c t", p=128)[:, :, t0:t0 + GB], reads=[scr["sga"].b], writes=[sg.b])
                    dma("sp", ma.t[:], scr["mixA"].t.rearrange("(c p) t -> p c t", p=128)[:, :, t0:t0 + GB], reads=[scr["mixA"].b], writes=[ma.b])
                    dma("sp", xg.t[:], io["x"][t0:t0 + GB, :].rearrange("(j p) d -> p j d", p=128), writes=[xg.b])
                    for dc in range(8):
                        p = gen.next()
                        for c in range(4):
                            op("pe", MM(p.f[:, 0:GB], wao.t[:, c, dc * 128:(dc + 1) * 128], at.t[:, c, :], start=(c == 0), stop=(c == 3)),
                               reads=[wao.b, at.b], pwrites=[p.b])
                        tq = tmq.next()
                        op("dve", TT(tq.t[:], p.f[:, 0:GB], sg.t[:, dc, :], ALU.mult), reads=[p.b, sg.b], writes=[tq.b])
                        op("dve", TT(mixT.t[:, dc, :], tq.t[:], ma.t[:, dc, :], ALU.add), reads=[tq.b, ma.b], pwrites=[mixT.b])
                    for j in range(JB):
                        for dh in range(2):
                            p = gen.next()
                            for kc in range(KC):
                                op("pe", MM(p.f[:, 0:512], mixT.t[:, kc, j * 128:(j + 1) * 128], wo.t[:, kc, dh * 512:(dh + 1) * 512],
                                            start=(kc == 0), stop=(kc == KC - 1)), reads=[mixT.b, wo.b], pwrites=[p.b])
                            t_ = tz.next()
                            op("dve", TT(t_.t[:], p.f[:, 0:512], gb1.t[:, dh * 512:(dh + 1) * 512], ALU.mult), reads=[p.b, gb1.b], writes=[t_.b])
                            op("dve", TT(x1.t[:, j, dh * 512:(dh + 1) * 512], t_.t[:], xg.t[:, j, dh * 512:(dh + 1) * 512], ALU.add),
                               reads=[t_.b, xg.b], pwrites=[x1.b])
                    dma("sp", scr["x1"].t[t0:t0 + GB, :].rearrange("(j p) d -> p j d", p=128), x1.t[:], reads=[x1.b], pwrites=[scr["x1"].b])
                    for j in range(JB):
                        op("act", ACTF(junk.t[:], x1.t[:, j, :], AF.Square, accum_out=ssq.t[:, j:j + 1]), reads=[x1.b], writes=[junk.b], pwrites=[ssq.b])
                    op("act", ACTF(rt.t[:], ssq.t[:], AF.Sqrt, scale=1.0 / D, bias=epst.t[:, 0:1]), reads=[ssq.b, epst.b], writes=[rt.b])
                    op("dve", RCP(rstd.t[:], rt.t[:]), reads=[rt.b], writes=[rstd.b])
                    for j in range(JB):
                        op("dve", TS(xs.t[:, j, :], x1.t[:, j, :], rstd.t[:, j:j + 1], None, ALU.mult), reads=[x1.b, rstd.b], pwrites=[xs.b])
                    for kc in range(KC):
                        pt = ptr.next()
                        for j in range(JB):
                            op("pe", TR(pt.h[:, j * 128:(j + 1) * 128], xs.t[:, j, kc * 128:(kc + 1) * 128], ident_bf.t[:]),
                               reads=[xs.b, ident_bf.b], pwrites=[pt.b])
                        op("act", ACTF(h2T.t[:, kc, :], pt.h[:, 0:GB], AF.Identity, scale=A2.t[:, kc, seq:seq + 1], bias=modTb.t[:, 24 + kc, seq:seq + 1]),
                           reads=[pt.b, A2.b, modTb.b], pwrites=[h2T.b])
                    dma("sp", scr["h2T"].t.rearrange("(c p) t -> p c t", p=128)[:, :, t0:t0 + GB], h2T.t[:], reads=[h2T.b], pwrites=[scr["h2T"].b])
                    for h in range(8):
                        p = gen.next()
                        for kc in range(KC):
                            op("pe", MM(p.f[:, 0:GB], wpq.t[:, kc, h * 128:(h + 1) * 128], h2T.t[:, kc, :], start=(kc == 0), stop=(kc == KC - 1)),
                               reads=[wpq.b, h2T.b], pwrites=[p.b])
                        evacD(qpT.t[:, h, :], p.f[:, 0:GB], [p.b], [qpT.b])
                    for j in range(JB):
                        tt0 = t0 + j * 128
                        s4 = s_sb.t[:].rearrange("p (h s) n -> p h s n", s=2)
                        for b4 in range(4):
                            p = gen.next()
                            side, hb = b4 % 2, (b4 // 2) * 4
                            for i in range(4):
                                h = hb + i
                                op("pe", MM(p.f[:, i * 128:(i + 1) * 128], qpT.t[side * 64:(side + 1) * 64, h, j * 128:(j + 1) * 128],
                                            skTb.t[side * 64:(side + 1) * 64, h, :]), reads=[qpT.b, skTb.b], pwrites=[p.b])
                            op("act", ACTF(s4[:, hb:hb + 4, side, :], p.f[:, :].rearrange("p (a b) -> p a b", b=128), AF.Copy),
                               reads=[p.b], pwrites=[s_sb.b])
                        if DCUT < 2:
                            continue
                        for r in range(16):
                            op("dve", lambda e, r=r: e.max(out=v_all.t[:, r, 0:8], in_=s_sb.t[:, r, :]), reads=[s_sb.b], pwrites=[v_all.b])
                            op("dve", lambda e, r=r: e.max_index(out=idx_all.t[:, r, 0:8], in_max=v_all.t[:, r, 0:8], in_values=s_sb.t[:, r, :]),
                               reads=[s_sb.b, v_all.b], pwrites=[idx_all.b])
                            op("dve", lambda e, r=r: e.match_replace(out=s_tmp.t[:, r, :], in_to_replace=v_all.t[:, r, 0:8], in_values=s_sb.t[:, r, :], imm_value=NEG),
                               reads=[s_sb.b, v_all.b], pwrites=[s_tmp.b])
                            op("dve", lambda e, r=r: e.max(out=v_all.t[:, r, 8:16], in_=s_tmp.t[:, r, :]), reads=[s_tmp.b], pwrites=[v_all.b])
                            op("dve", lambda e, r=r: e.max_index(out=idx_all.t[:, r, 8:16], in_max=v_all.t[:, r, 8:16], in_values=s_tmp.t[:, r, :]),
                               reads=[s_tmp.b, v_all.b], pwrites=[idx_all.b])
                        if DCUT < 3:
                            continue
                        op("dve", CP(idx_bf.t[:], idx_all.t[:]), reads=[idx_all.b], writes=[idx_bf.b])
                        v4 = v_all.t[:].rearrange("p (h s) k -> p h s k", s=2)
                        op("dve", TT(cand.t[:].rearrange("p h (a b) -> p h a b", b=16), v4[:, :, 0, :].unsqueeze(3).to_broadcast([128, 8, 16, 16]),
                                     v4[:, :, 1, :].unsqueeze(2).to_broadcast([128, 8, 16, 16]), ALU.add), reads=[v_all.b], writes=[cand.b])
                        for h in range(8):
                            op("dve", lambda e, h=h: e.max(out=scv.t[:, h, 0:8], in_=cand.t[:, h, :]), reads=[cand.b], pwrites=[scv.b])
                            op("dve", lambda e, h=h: e.max_index(out=civ.t[:, h, 0:8], in_max=scv.t[:, h, 0:8], in_values=cand.t[:, h, :]),
                               reads=[cand.b, scv.b], pwrites=[civ.b])
                            op("dve", lambda e, h=h: e.match_replace(out=cand2.t[:, h, :], in_to_replace=scv.t[:, h, 0:8], in_values=cand.t[:, h, :], imm_value=NEG),
                               reads=[cand.b, scv.b], pwrites=[cand2.b])
                            op("dve", lambda e, h=h: e.max(out=scv.t[:, h, 8:16], in_=cand2.t[:, h, :]), reads=[cand2.b], pwrites=[scv.b])
                            op("dve", lambda e, h=h: e.max_index(out=civ.t[:, h, 8:16], in_max=scv.t[:, h, 8:16], in_values=cand2.t[:, h, :]),
                               reads=[cand2.b, scv.b], pwrites=[civ.b])
                        if DCUT < 4:
                            continue
                        op("dve", TT(sm.t[:], scv.t[:], scv.t[:, :, 0:1].to_broadcast([128, 8, 16]), ALU.subtract), reads=[scv.b], writes=[sm.b])
                        op("act", ACTF(sm.t[:], sm.t[:], AF.Exp), reads=[sm.b], writes=[sm.b])
                        op("dve", RED(zz.t[:], sm.t[:], ALU.add), reads=[sm.b], writes=[zz.b])
                        op("dve", RCP(zz.t[:], zz.t[:]), reads=[zz.b], writes=[zz.b])
                        op("dve", TT(sel_tm.t[:, 2, :].rearrange("p (h k) -> p h k", k=16), sm.t[:], zz.t[:].unsqueeze(2).to_broadcast([128, 8, 16]), ALU.mult),
                           reads=[sm.b, zz.b], pwrites=[sel_tm.b])
                        if DCUT < 5:
                            continue
                        op("dve", TS(ca_u.t[:], civ.t[:], 4, None, ALU.logical_shift_right), reads=[civ.b], writes=[ca_u.b])
                        op("dve", TS(cb_u.t[:], civ.t[:], 15, None, ALU.bitwise_and), reads=[civ.b], writes=[cb_u.b])
                        op("dve", CP(ca_bf.t[:], ca_u.t[:]), reads=[ca_u.b], writes=[ca_bf.b])
                        op("dve", CP(cb_bf.t[:], cb_u.t[:]), reads=[cb_u.b], writes=[cb_bf.b])
                        if DCUT < 6:
                            continue
                        i4 = idx_bf.t[:].rearrange("p (h s) k -> p h s k", s=2)
                        for side, cbf in ((0, ca_bf), (1, cb_bf)):
                            op("dve", TT(eqa.t[:], cbf.t[:].unsqueeze(3).to_broadcast([128, 8, 16, 16]),
                                         iota_bf.t[:, 0:16].unsqueeze(1).unsqueeze(1).to_broadcast([128, 8, 16, 16]), ALU.is_equal),
                               reads=[cbf.b, iota_bf.b], writes=[eqa.b])
                            op("dve", TT(prd.t[:], eqa.t[:], i4[:, :, side, :].unsqueeze(2).to_broadcast([128, 8, 16, 16]), ALU.mult),
                               reads=[eqa.b, idx_bf.b], writes=[prd.b])
                            op("dve", RED(sel_tm.t[:, side, :], prd.t[:].rearrange("p h k a -> p (h k) a"), ALU.add), reads=[prd.b], pwrites=[sel_tm.b])
                        if DCUT < 7:
                            continue
                        pt = ptr.next()
                        for c in range(3):
                            op("pe", TR(pt.f[:, c * 128:(c + 1) * 128], sel_tm.t[:, c, :], ident_f), reads=[sel_tm.b, cst.b], pwrites=[pt.b])
                        op("act", ACTF(selT.t[:], pt.f[:, 0:384].rearrange("p (c t) -> p c t", c=3), AF.Copy), reads=[pt.b], writes=[selT.b])
                        dma("sp", scr["sel"].t.rearrange("c p t -> p c t")[:, :, tt0:tt0 + 128], selT.t[:], reads=[selT.b], pwrites=[scr["sel"].b])
                ctx.barrier()

        if "E" in phases:
            with ExitStack() as es:
                sf = Ring([tile(es, "pf%d" % i, [128, 4096], F32) for i in range(3)])
                sbf = Ring([tile(es, "pb%d" % i, [128, 4096], BF16) for i in range(3)])
                uv = io["uT"].rearrange("(kc p) e -> p kc e", p=128)
                uo = scr["uTb"].t.rearrange("(kc p) e -> p kc e", p=128)
                vv = io["vp"].rearrange("(jj p) d -> p jj d", p=128)
                vo = scr["vb"].t.rearrange("(jj p) d -> p jj d", p=128)
                n = 0
                for i in range(NEXP // 512):
                    for (src, dst, key, v3) in ((uv[:, :, i * 512:(i + 1) * 512], uo[:, :, i * 512:(i + 1) * 512], "uTb", "p (a b) -> p a b"),
                                                (vv[:, i * 4:(i + 1) * 4, :], vo[:, i * 4:(i + 1) * 4, :], "vb", "p (a b) -> p a b")):
                        a_ = 8 if key == "uTb" else 4
                        f_ = sf.next()
                        b_ = sbf.next()
                        dma("sp", f_.t[:].rearrange(v3, a=a_), src, writes=[f_.b])
                        eng = ("dve", "act", "pool")[n % 3]
                        n += 1
                        if eng == "act":
                            op("act", ACTF(b_.t[:], f_.t[:], AF.Copy), reads=[f_.b], writes=[b_.b])
                        else:
                            op(eng, CP(b_.t[:], f_.t[:]), reads=[f_.b], writes=[b_.b])
                        dma("sp", dst, b_.t[:].rearrange(v3, a=a_), reads=[b_.b], pwrites=[scr[key].b])
                ctx.barrier()

        if "E" in phases:
            with ExitStack() as es:
                TE = 256
                SC = 2
                gb2 = tile(es, "gb2", [128, D], F32)
                x1r = Ring([tile(es, "x1E%d" % i, [128, 2, D], F32) for i in range(2)])
                h2r = Ring([tile(es, "h2E%d" % i, [128, KC, TE], BF16) for i in range(2)])
                selr = Ring([tile(es, "selE%d" % i, [128, 3, TE], BF16) for i in range(2)])
                Lr = Ring([tile(es, "L%d" % i, [128, 32, 128], BF16) for i in range(2)])
                L0r = Ring([tile(es, "L0%d" % i, [128, 32, 128], BF16) for i in range(1)])
                Rr = Ring([tile(es, "R%d" % i, [128, 32, 128], BF16) for i in range(2)])
                G = tile(es, "G", [128, TE, 128], BF16)
                uTr = Ring([tile(es, "uTs%d" % i, [128, KC, SC * 128], BF16) for i in range(3)])
                vr = Ring([tile(es, "vs%d" % i, [128, SC, D], BF16) for i in range(3)])
                Wr = Ring([tile(es, "W%d" % i, [128, TE], BF16) for i in range(3)])
                W2r = Ring([tile(es, "W2%d" % i, [128, TE], BF16) for i in range(3)])
                tz = Ring([tile(es, "tzE%d" % i, [128, 512], F32) for i in range(2)])
                yst = tile(es, "yst", [128, 2, D], F32)
                psOut = [[PBs[0], PBs[1]], [PBs[2], PBs[3]]]
                psAT = Ring([PBs[4], PBs[5]])
                psG = Ring([PBs[6], PBs[7]])
                uo = scr["uTb"].t.rearrange("(kc p) e -> p kc e", p=128)
                vo = scr["vb"].t.rearrange("(jj p) d -> p jj d", p=128)
                for tl in range(T // TE):
                    t0 = tl * TE
                    seq = t0 // S
                    if t0 % S == 0:
                        dma("sp", gb2.t[:], scr["gbc"].t[seq, 1, :, :], reads=[scr["gbc"].b], writes=[gb2.b])
                    x1t = x1r.next()
                    h2 = h2r.next()
                    sl_ = selr.next()
                    dma("sp", x1t.t[:], scr["x1"].t[t0:t0 + TE, :].rearrange("(j p) d -> p j d", p=128), reads=[scr["x1"].b], writes=[x1t.b])
                    dma("sp", h2.t[:], scr["h2T"].t.rearrange("(c p) t -> p c t", p=128)[:, :, t0:t0 + TE], reads=[scr["h2T"].b], writes=[h2.b])
                    dma("sp", sl_.t[:], scr["sel"].t.rearrange("c p t -> p c t")[:, :, t0:t0 + TE], reads=[scr["sel"].b], writes=[sl_.b])
                    for sb in range(TE // 32):
                        ts_ = slice(sb * 32, (sb + 1) * 32)
                        L0 = L0r.next()
                        L = Lr.next()
                        Rt = Rr.next()
                        iob = iota_bf.t[:].unsqueeze(1).to_broadcast([128, 32, 128])
                        op("dve", TT(L0.t[:], iob, sl_.t[:, 0, ts_].unsqueeze(2).to_broadcast([128, 32, 128]), ALU.is_equal),
                           reads=[iota_bf.b, sl_.b], writes=[L0.b])
                        op("dve", TT(L.t[:], L0.t[:], sl_.t[:, 2, ts_].unsqueeze(2).to_broadcast([128, 32, 128]), ALU.mult),
                           reads=[L0.b, sl_.b], writes=[L.b])
                        op("dve", TT(Rt.t[:], iob, sl_.t[:, 1, ts_].unsqueeze(2).to_broadcast([128, 32, 128]), ALU.is_equal),
                           reads=[iota_bf.b, sl_.b], writes=[Rt.b])
                        for q4 in range(8):
                            pg = psG.next()
                            for i in range(4):
                                t = q4 * 4 + i
                                op("pe", MM(pg.f[:, i * 128:(i + 1) * 128], L.t[:, t, :], Rt.t[:, t, :]), reads=[L.b, Rt.b], pwrites=[pg.b])
                            tt = sb * 32 + q4 * 4
                            op("act", ACTF(G.t[:, tt:tt + 4, :], pg.f[:, :].rearrange("p (a b) -> p a b", b=128), AF.Copy), reads=[pg.b], pwrites=[G.b])
                    chunks = {}

                    def load_sc(sc_):
                        uT = uTr.next()
                        vs = vr.next()
                        dma("sp", uT.t[:], uo[:, :, sc_ * SC * 128:(sc_ + 1) * SC * 128], reads=[scr["uTb"].b], writes=[uT.b])
                        dma("sp", vs.t[:], vo[:, sc_ * SC:(sc_ + 1) * SC, :], reads=[scr["vb"].b], writes=[vs.b])
                        for jj in range(SC):
                            chunks[sc_ * SC + jj] = (uT, vs, jj)

                    def stage1(j):
                        if j % SC == 0:
                            load_sc(j // SC)
                        uT, vs, jj = chunks[j]
                        pa = psAT.next()
                        for kc in range(KC):
                            op("pe", MM(pa.f[:, 0:TE], uT.t[:, kc, jj * 128:(jj + 1) * 128], h2.t[:, kc, :], start=(kc == 0), stop=(kc == KC - 1)),
                               reads=[uT.b, h2.b], pwrites=[pa.b])
                        W = Wr.next()
                        op("act", ACTF(W.t[:], pa.f[:, 0:TE], AF.Gelu), reads=[pa.b], writes=[W.b])
                        W2 = W2r.next()
                        op("dve", TT(W2.t[:], W.t[:], G.t[:, :, j], ALU.mult), reads=[W.b, G.b], writes=[W2.b])
                        return W2

                    def stage2(j, W2):
                        uT, vs, jj = chunks.pop(j)
                        for tb in range(2):
                            for dh in range(2):
                                po = psOut[tb][dh]
                                op("pe", MM(po.f[:, :], W2.t[:, tb * 128:(tb + 1) * 128], vs.t[:, jj, dh * 512:(dh + 1) * 512],
                                            start=(j == 0), stop=(j == 127)), reads=[W2.b, vs.b], pwrites=[po.b])

                    pend = stage1(0)
                    for j in range(128):
                        nxt = stage1(j + 1) if j + 1 < 128 else None
                        stage2(j, pend)
                        pend = nxt
                    for tb in range(2):
                        for dh in range(2):
                            po = psOut[tb][dh]
                            t_ = tz.next()
                            op("dve", TT(t_.t[:], po.f[:, :], gb2.t[:, dh * 512:(dh + 1) * 512], ALU.mult), reads=[po.b, gb2.b], writes=[t_.b])
                            op("dve", TT(yst.t[:, tb, dh * 512:(dh + 1) * 512], t_.t[:], x1t.t[:, tb, dh * 512:(dh + 1) * 512], ALU.add),
                               reads=[t_.b, x1t.b], pwrites=[yst.b])
                    dma("sp", y[t0:t0 + TE, :].rearrange("(j p) d -> p j d", p=128), yst.t[:], reads=[yst.b])
                ctx.barrier()

        ctx.finish()
    return nc, ctx


def host_consts():
    c = np.zeros((128, 5, 128), np.float32)
    c[:, 0, :] = np.eye(128, dtype=np.float32)
    qi = np.arange(128)[:, None]
    ki = np.arange(128)[None, :]
    c[:, 1, :] = np.where(ki <= qi, 0.0, NEG).astype(np.float32)
    c[:, 2, :] = ((qi // 64) == (ki // 64)).astype(np.float32)
    c[:, 3, :] = np.broadcast_to(np.arange(128, dtype=np.float32)[None, :], (128, 128))
    c[:, 4, :] = 1.0
    return c


def host_shared(inp):
    f = lambda a: np.ascontiguousarray(np.asarray(a, dtype=np.float32))
    sh = {}
    sh["w_ada"] = f(inp["w_ada"][0])
    sh["b_ada"] = f(inp["b_ada"][0])
    sh["b_adaT"] = f(np.asarray(inp["b_ada"][0]).reshape(48, 128).T)
    sh["n1T"] = f(np.asarray(inp["norm1_w"][0]).reshape(KC, 128).T)
    sh["n2T"] = f(np.asarray(inp["norm2_w"][0]).reshape(KC, 128).T)
    sh["w_in"] = f(inp["w_in"][0])
    sh["convT"] = f(np.asarray(inp["conv_w"][0]).reshape(3, 4, 128).transpose(2, 1, 0))
    sh["w_conv_out"] = f(inp["w_conv_out"][0])
    sh["qk_w"] = f(np.stack([np.tile(np.asarray(inp["q_norm_w"][0]), 2), np.tile(np.asarray(inp["k_norm_w"][0]), 2)], axis=1))
    sh["w_attn_out"] = f(inp["w_attn_out"][0])
    sh["w_o"] = f(inp["w_o"][0])
    sh["w_peer_q"] = f(inp["w_peer_q"][0])
    sk = np.asarray(inp["peer_sub_keys"][0])
    sh["skT"] = f(sk.transpose(1, 3, 0, 2).reshape(128, 8, 128))
    u = np.asarray(inp["peer_u"][0]).reshape(128, 128, D).transpose(1, 0, 2).reshape(NEXP, D)
    sh["uT"] = f(u.T)
    sh["vp"] = f(np.asarray(inp["peer_v"][0]).reshape(128, 128, D).transpose(1, 0, 2).reshape(NEXP, D))
    sh["consts"] = host_consts()
    return sh


def kernel(**inp):
    x = np.asarray(inp["x"], dtype=np.float32)
    c = np.asarray(inp["c"], dtype=np.float32)
    B, S, _ = x.shape
    ncores = 8
    NSEQ = B // ncores
    nc, _ = build(NSEQ, S)
    sh = host_shared(inp)
    in_maps = []
    for i in range(ncores):
        m = dict(sh)
        m["x"] = np.ascontiguousarray(x[i * NSEQ:(i + 1) * NSEQ].reshape(NSEQ * S, D))
        m["cT"] = np.ascontiguousarray(c[i * NSEQ:(i + 1) * NSEQ].reshape(NSEQ, KC, 128).transpose(2, 1, 0))
        in_maps.append(m)
    res = run_bass_kernel_spmd(nc, in_maps, core_ids=list(range(ncores)))
    out = np.concatenate([np.asarray(r["y"]).reshape(NSEQ, S, D) for r in res.results], axis=0)
    return out.astype(np.float32)
```

```python
import math
from contextlib import ExitStack

import numpy as np
import concourse.bass as bass
import concourse.mybir as mybir
from concourse.bass_utils import run_bass_kernel_spmd

F32 = mybir.dt.float32
BF16 = mybir.dt.bfloat16
U32 = mybir.dt.uint32
AF = mybir.ActivationFunctionType
ALU = mybir.AluOpType
AX = mybir.AxisListType

D = 1024
KC = 8
NIN = 5704
O_CB, O_CC, O_CX, O_Q, O_K, O_V, O_QI, O_KI, O_WI, O_GC, O_GA = 0, 512, 1024, 1536, 2048, 2560, 3072, 3584, 3648, 3656, 4680
NEXP = 16384
EPS = 1e-6
NEG = -1.0e30
NIT = 16
DCUT = 99
SELF_SYNC = True


class Buf:
    __slots__ = ("w", "r")

    def __init__(self):
        self.w = {}
        self.r = {}


class Tl:
    __slots__ = ("t", "b")

    def __init__(self, t):
        self.t = t
        self.b = Buf()


class Ring:
    def __init__(self, items):
        self.items = items
        self.i = 0

    def next(self):
        it = self.items[self.i % len(self.items)]
        self.i += 1
        return it


class Ctx:
    COMPUTE = ("pe", "act", "dve", "pool")
    ALL = ("pe", "act", "dve", "pool", "sp")
    NL = 8

    def __init__(self, nc):
        self.nc = nc
        self.prog = {n: [] for n in self.ALL}
        self.sems = {}
        self.cnt = {}
        self.known = {n: {} for n in self.ALL}
        for n in self.COMPUTE:
            self.sems[n] = nc.alloc_semaphore(name="s_" + n)
            self.cnt[n] = 0
        self.lane_rr = {}
        for q in ("sp", "act", "pool"):
            self.lane_rr[q] = 0
            for l in range(self.NL):
                k = (q, l)
                self.sems[k] = nc.alloc_semaphore(name="d_%s%d" % (q, l))
                self.cnt[k] = 0
        self.ninstr = 0

    def _wait(self, eng, key, val):
        if val <= 0:
            return
        if key == eng and (eng == "pe" or not SELF_SYNC):
            return
        if self.known[eng].get(key, 0) < val:
            self.prog[eng].append(("w", key, val))
            self.known[eng][key] = val

    def _deps(self, eng, reads, writes, pwrites):
        deps = {}
        for b in reads:
            for k, v in b.w.items():
                if deps.get(k, 0) < v:
                    deps[k] = v
        for b in writes:
            for dct in (b.w, b.r):
                for k, v in dct.items():
                    if deps.get(k, 0) < v:
                        deps[k] = v
        for b in pwrites:
            for k, v in b.r.items():
                if deps.get(k, 0) < v:
                    deps[k] = v
            for k, v in b.w.items():
                if k != eng and deps.get(k, 0) < v:
                    deps[k] = v
        for k, v in deps.items():
            self._wait(eng, k, v)

    def _mark(self, key, n, reads, writes, pwrites):
        for b in reads:
            b.r[key] = n
        for b in writes:
            b.w = {key: n}
            b.r = {}
        for b in pwrites:
            b.w[key] = n

    def op(self, eng, fn, reads=(), writes=(), pwrites=()):
        self._deps(eng, reads, writes, pwrites)
        self.cnt[eng] += 1
        self.prog[eng].append(("o", fn))
        self._mark(eng, self.cnt[eng], reads, writes, pwrites)
        self.ninstr += 1

    def dma(self, q, out, in_, reads=(), writes=(), pwrites=(), slow=False):
        l = self.lane_rr[q] % self.NL
        self.lane_rr[q] += 1
        key = (q, l)
        self._wait(q, key, self.cnt[key])
        self._deps(q, reads, writes, pwrites)
        self.cnt[key] += 16
        self.prog[q].append(("d", out, in_, key, slow))
        self._mark(key, self.cnt[key], reads, writes, pwrites)
        self.ninstr += 1

    def barrier(self):
        for e in self.ALL:
            for k in self.sems:
                self._wait(e, k, self.cnt[k])

    def finish(self):
        for k in self.sems:
            self._wait("sp", k, self.cnt[k])
        nc = self.nc

        def mk(name):
            def body(e):
                for it in self.prog[name]:
                    if it[0] == "w":
                        e.wait_ge(self.sems[it[1]], it[2])
                    elif it[0] == "o":
                        it[1](e).then_inc(self.sems[name], 1)
                    else:
                        if it[4]:
                            e.dma_start(out=it[1], in_=it[2], allow_slow_non_contiguous=True).then_inc(self.sems[it[3]], 16)
                        else:
                            e.dma_start(out=it[1], in_=it[2]).then_inc(self.sems[it[3]], 16)
            return body

        with nc.Block() as block:
            block.tensor(mk("pe"))
            block.scalar(mk("act"))
            block.vector(mk("dve"))
            block.gpsimd(mk("pool"))
            block.sync(mk("sp"))


def ACTF(out, in_, func, **kw):
    return lambda e: e.activation(out=out, in_=in_, func=func, **kw)


def MM(out, lhsT, rhs, start=True, stop=True):
    return lambda e: e.matmul(out, lhsT, rhs, start=start, stop=stop)


def TR(out, in_, ident):
    return lambda e: e.transpose(out, in_, ident)


def TS(out, in0, s1, s2, op0, op1=None, accum_out=None):
    if op1 is None:
        return lambda e: e.tensor_scalar(out=out, in0=in0, scalar1=s1, scalar2=s2, op0=op0, accum_out=accum_out)
    return lambda e: e.tensor_scalar(out=out, in0=in0, scalar1=s1, scalar2=s2, op0=op0, op1=op1, accum_out=accum_out)


def TT(out, in0, in1, op):
    return lambda e: e.tensor_tensor(out=out, in0=in0, in1=in1, op=op)


def STT(out, in0, scalar, in1, op0, op1):
    return lambda e: e.scalar_tensor_tensor(out=out, in0=in0, scalar=scalar, in1=in1, op0=op0, op1=op1)


def CP(out, in_):
    return lambda e: e.tensor_copy(out=out, in_=in_)


def RED(out, in_, op):
    return lambda e: e.tensor_reduce(out=out, in_=in_, axis=AX.X, op=op)


def RCP(out, in_):
    return lambda e: e.reciprocal(out=out, in_=in_)


def MSET(ap, c):
    return lambda e: e.memset(ap, c)


def build(NSEQ, S, GB=256, dbg=False, phases="ABCDE"):
    T = NSEQ * S
    NT = T // 128
    KSEL = min(256, S // 4)
    JB = GB // 128
    nc = bass.Bass("TRN2", target_bir_lowering=False)
    ctx = Ctx(nc)
    op, dma = ctx.op, ctx.dma

    def din(name, shape, dt=F32):
        return nc.dram_tensor(name, list(shape), dt, kind="ExternalInput").ap()

    io = {
        "x": din("x", [T, D]), "cT": din("cT", [128, KC, NSEQ]), "w_ada": din("w_ada", [D, 6 * D]),
        "b_ada": din("b_ada", [6 * D]), "b_adaT": din("b_adaT", [128, 48]),
        "n1T": din("n1T", [128, KC]), "n2T": din("n2T", [128, KC]), "w_in": din("w_in", [D, NIN]),
        "convT": din("convT", [128, 4, 3]), "w_conv_out": din("w_conv_out", [512, D]),
        "qk_w": din("qk_w", [128, 2]), "w_attn_out": din("w_attn_out", [512, D]), "w_o": din("w_o", [D, D]),
        "w_peer_q": din("w_peer_q", [D, D]), "skT": din("skT", [128, 8, 128]),
        "uT": din("uT", [D, NEXP]), "vp": din("vp", [NEXP, D]), "consts": din("consts", [128, 5, 128]),
    }
    y = nc.dram_tensor("y", [T, D], F32, kind="ExternalOutput").ap()
    skind = "ExternalOutput" if dbg else "Internal"

    def dscr(name, shape, dt):
        t = nc.dram_tensor(name, list(shape), dt, kind=skind).ap()
        return Tl(t)

    scr = {
        "mixA": dscr("s_mixA", [D, T], BF16), "sga": dscr("s_sga", [D, T], BF16),
        "qT": dscr("s_qT", [512, T], BF16), "kT": dscr("s_kT", [512, T], BF16),
        "qiT": dscr("s_qiT", [512, T], BF16), "kiT": dscr("s_kiT", [64, T], BF16),
        "V": dscr("s_V", [T, 520], BF16), "attnT": dscr("s_attnT", [512, T], BF16),
        "gbc": dscr("s_gbc", [NSEQ, 2, 128, D], F32), "x1": dscr("s_x1", [T, D], F32),
        "h2T": dscr("s_h2T", [D, T], BF16), "sel": dscr("s_sel", [3, 128, T], BF16),
        "uTb": dscr("s_uTb", [D, NEXP], BF16), "vb": dscr("s_vb", [NEXP, D], BF16),
    }

    with ExitStack() as gs:
        def tile(es, name, shape, dt):
            return Tl(es.enter_context(nc.sbuf_tensor("sb_" + name, list(shape), dt)))

        psum = gs.enter_context(nc.psum_tensor("psum", [128, 8, 512], F32))
        pb = [Buf() for _ in range(8)]

        class PB:
            def __init__(self, i):
                self.i = i
                self.b = pb[i]
                self.f = psum[:, i, :]
                self.h = psum[:, i, :].bitcast(BF16)

        PBs = [PB(i) for i in range(8)]

        cst = tile(gs, "cst", [128, 5, 128], F32)
        ident_bf = tile(gs, "ident_bf", [128, 128], BF16)
        bones_bf = tile(gs, "bones_bf", [128, 128], BF16)
        iota_bf = tile(gs, "iota_bf", [128, 128], BF16)
        epst = tile(gs, "epst", [128, 1], F32)
        modTb = tile(gs, "modTb", [128, 48, NSEQ], F32)
        A1 = tile(gs, "A1", [128, KC, NSEQ], F32)
        A2 = tile(gs, "A2", [128, KC, NSEQ], F32)
        widx = tile(gs, "widx", [128, NT, 8], F32)
        qkw = tile(gs, "qkw", [128, 2], F32)
        qws = tile(gs, "qws", [128, 1], F32)
        ident_f = cst.t[:, 0, :]
        causal_f = cst.t[:, 1, :]
        ones_f = cst.t[:, 4, :]

        dma("sp", cst.t[:], io["consts"], writes=[cst.b])
        dma("sp", qkw.t[:], io["qk_w"], writes=[qkw.b])
        op("dve", CP(ident_bf.t[:], cst.t[:, 0, :]), reads=[cst.b], writes=[ident_bf.b])
        op("dve", CP(bones_bf.t[:], cst.t[:, 2, :]), reads=[cst.b], writes=[bones_bf.b])
        op("dve", CP(iota_bf.t[:], cst.t[:, 3, :]), reads=[cst.b], writes=[iota_bf.b])
        op("dve", MSET(epst.t[:], EPS), writes=[epst.b])
        op("dve", TS(qws.t[:], qkw.t[:, 0:1], 0.125, None, ALU.mult), reads=[qkw.b], writes=[qws.b])

        if "A" in phases:
            with ExitStack() as es:
                cT = tile(es, "cT", [128, KC, NSEQ], F32)
                sc = tile(es, "sc", [128, KC, NSEQ], F32)
                scb = tile(es, "scb", [128, KC * NSEQ, 128], F32)
                badaT = tile(es, "badaT", [128, 48], F32)
                bbc = tile(es, "bbc", [128, 2, D], F32)
                n1T = tile(es, "n1T", [128, KC], F32)
                n2T = tile(es, "n2T", [128, KC], F32)
                war = Ring([tile(es, "wa%d" % i, [128, KC, 512], F32) for i in range(2)])
                gst = Ring([tile(es, "gst%d" % i, [128, 512], F32) for i in range(2)])
                dma("sp", cT.t[:], io["cT"], writes=[cT.b])
                dma("sp", badaT.t[:], io["b_adaT"], writes=[badaT.b])
                dma("sp", n1T.t[:], io["n1T"], writes=[n1T.b])
                dma("sp", n2T.t[:], io["n2T"], writes=[n2T.b])
                dma("sp", bbc.t[:, 0, :], io["b_ada"][2 * D:3 * D].partition_broadcast(128), pwrites=[bbc.b])
                dma("sp", bbc.t[:, 1, :], io["b_ada"][5 * D:6 * D].partition_broadcast(128), pwrites=[bbc.b])
                op("act", ACTF(sc.t[:], cT.t[:], AF.Silu), reads=[cT.b], writes=[sc.b])
                op("dve", CP(scb.t[:], sc.t[:].rearrange("p k s -> p (k s)").unsqueeze(2).to_broadcast([128, KC * NSEQ, 128])),
                   reads=[sc.b], writes=[scb.b])
                pM = PBs[0]
                pG = Ring([PBs[1], PBs[2]])
                wav = io["w_ada"].rearrange("(kc p) f -> p kc f", p=128)
                for blk in range(12):
                    wa = war.next()
                    dma("sp", wa.t[:], wav[:, :, blk * 512:(blk + 1) * 512], writes=[wa.b])
                    for fl in range(4):
                        fc = blk * 4 + fl
                        for kc in range(KC):
                            op("pe", MM(pM.f[:, fc * NSEQ:(fc + 1) * NSEQ], wa.t[:, kc, fl * 128:(fl + 1) * 128], sc.t[:, kc, :],
                                        start=(kc == 0), stop=(kc == KC - 1)), reads=[wa.b, sc.b], pwrites=[pM.b])
                    if blk in (4, 5, 10, 11):
                        which = 0 if blk < 6 else 1
                        half = blk % 2
                        for s in range(NSEQ):
                            p = pG.next()
                            for kc in range(KC):
                                op("pe", MM(p.f[:, :], scb.t[:, kc * NSEQ + s, :], wa.t[:, kc, :], start=(kc == 0), stop=(kc == KC - 1)),
                                   reads=[wa.b, scb.b], pwrites=[p.b])
                            g = gst.next()
                            op("dve", TT(g.t[:], p.f[:, :], bbc.t[:, which, half * 512:(half + 1) * 512], ALU.add),
                               reads=[p.b, bbc.b], writes=[g.b])
                            dma("sp", scr["gbc"].t[s, which, :, half * 512:(half + 1) * 512], g.t[:], reads=[g.b], pwrites=[scr["gbc"].b])
                op("dve", TT(modTb.t[:], pM.f[:, 0:48 * NSEQ].rearrange("p (f s) -> p f s", s=NSEQ),
                             badaT.t[:].unsqueeze(2).to_broadcast([128, 48, NSEQ]), ALU.add),
                   reads=[pM.b, badaT.b], writes=[modTb.b])
                op("dve", STT(A1.t[:], modTb.t[:, 8:16, :], 1.0, n1T.t[:].unsqueeze(2).to_broadcast([128, KC, NSEQ]), ALU.add, ALU.mult),
                   reads=[modTb.b, n1T.b], writes=[A1.b])
                op("dve", STT(A2.t[:], modTb.t[:, 32:40, :], 1.0, n2T.t[:].unsqueeze(2).to_broadcast([128, KC, NSEQ]), ALU.add, ALU.mult),
                   reads=[modTb.b, n2T.b], writes=[A2.b])
                ctx.barrier()

        if "B" in phases:
            with ExitStack() as es:
                win = tile(es, "win", [128, KC, NIN], BF16)
                wco = tile(es, "wco", [128, 4, D], BF16)
                convT = tile(es, "convT", [128, 4, 3], F32)
                dma("sp", convT.t[:], io["convT"], writes=[convT.b])
                with ExitStack() as es2:
                    stg = Ring([tile(es2, "stg%d" % i, [128, KC, 512], F32) for i in range(2)])
                    wiv = io["w_in"].rearrange("(kc p) f -> p kc f", p=128)
                    col = 0
                    i = 0
                    while col < NIN:
                        w = min(512, NIN - col)
                        st = stg.next()
                        dma("sp", st.t[:, :, 0:w], wiv[:, :, col:col + w], writes=[st.b])
                        eng = ("dve", "act", "pool")[i % 3]
                        if eng == "act":
                            op("act", ACTF(win.t[:, :, col:col + w], st.t[:, :, 0:w], AF.Copy), reads=[st.b], pwrites=[win.b])
                        else:
                            op(eng, CP(win.t[:, :, col:col + w], st.t[:, :, 0:w]), reads=[st.b], pwrites=[win.b])
                        col += w
                        i += 1
                    st = stg.next()
                    stv = st.t[:].rearrange("p k f -> p (k f)").rearrange("p (c d) -> p c d", c=4)
                    dma("sp", stv, io["w_conv_out"].rearrange("(c p) d -> p c d", p=128), writes=[st.b])
                    op("dve", CP(wco.t[:], stv), reads=[st.b], writes=[wco.b])
                    ctx.barrier()
                xgr = Ring([tile(es, "xg%d" % i, [128, JB, D], F32) for i in range(2)])
                junk = tile(es, "junkB", [128, D], BF16)
                ssq = tile(es, "ssq", [128, JB], F32)
                rt = tile(es, "rt", [128, JB], F32)
                rstd = tile(es, "rstd", [128, JB], F32)
                xs = tile(es, "xs", [128, JB, D], BF16)
                hTr = Ring([tile(es, "hT%d" % i, [128, KC, GB], BF16) for i in range(2)])
                ubuf = tile(es, "ubuf", [128, 4, GB + 2], F32)
                tmpf = Ring([tile(es, "tmpf%d" % i, [128, GB], F32) for i in range(3)])
                c1 = tile(es, "c1", [128, GB], F32)
                c2 = tile(es, "c2", [128, GB], F32)
                c3 = tile(es, "c3", [128, GB], F32)
                yA = tile(es, "yA", [128, 4, GB], BF16)
                mixA_st = tile(es, "mixA_st", [128, 8, GB], BF16)
                sga_st = tile(es, "sga_st", [128, 8, GB], BF16)
                q_st = tile(es, "q_st", [128, 4, GB], BF16)
                k_st = tile(es, "k_st", [128, 4, GB], BF16)
                qi_st = tile(es, "qi_st", [128, 4, GB], BF16)
                ki_st = tile(es, "ki_st", [64, GB], BF16)
                v_st = tile(es, "v_st", [128, JB, 8, 65], BF16)
                sqr = Ring([tile(es, "sq%d" % i, [128, GB], BF16) for i in range(2)])
                rtr = Ring([tile(es, "rtq%d" % i, [128, GB], F32) for i in range(2)])
                rrr = Ring([tile(es, "rrq%d" % i, [128, GB], F32) for i in range(2)])
                op("pool", MSET(v_st.t[:], 1.0), writes=[v_st.b])
                ptr = Ring([PBs[0], PBs[1]])
                gen = Ring([PBs[i] for i in range(2, 8)])
                evi = [0]

                def evac(out, in_, rd, wr):
                    evi[0] += 1
                    if evi[0] % 2:
                        op("act", ACTF(out, in_, AF.Copy), reads=rd, pwrites=wr)
                    else:
                        op("dve", CP(out, in_), reads=rd, pwrites=wr)

                for g in range(T // GB):
                    t0 = g * GB
                    seq = t0 // S
                    first = (t0 % S == 0)
                    xg = xgr.next()
                    hT = hTr.next()
                    dma("sp", xg.t[:], io["x"][t0:t0 + GB, :].rearrange("(j p) d -> p j d", p=128), writes=[xg.b])
                    for j in range(JB):
                        op("act", ACTF(junk.t[:], xg.t[:, j, :], AF.Square, accum_out=ssq.t[:, j:j + 1]), reads=[xg.b], writes=[junk.b], pwrites=[ssq.b])
                    op("act", ACTF(rt.t[:], ssq.t[:], AF.Sqrt, scale=1.0 / D, bias=epst.t[:, 0:1]), reads=[ssq.b, epst.b], writes=[rt.b])
                    op("dve", RCP(rstd.t[:], rt.t[:]), reads=[rt.b], writes=[rstd.b])
                    for j in range(JB):
                        op("dve", TS(xs.t[:, j, :], xg.t[:, j, :], rstd.t[:, j:j + 1], None, ALU.mult), reads=[xg.b, rstd.b], pwrites=[xs.b])
                    for kc in range(KC):
                        pt = ptr.next()
                        for j in range(JB):
                            op("pe", TR(pt.h[:, j * 128:(j + 1) * 128], xs.t[:, j, kc * 128:(kc + 1) * 128], ident_bf.t[:]),
                               reads=[xs.b, ident_bf.b], pwrites=[pt.b])
                        op("act", ACTF(hT.t[:, kc, :], pt.h[:, 0:GB], AF.Identity, scale=A1.t[:, kc, seq:seq + 1], bias=modTb.t[:, kc, seq:seq + 1]),
                           reads=[pt.b, A1.b, modTb.b], pwrites=[hT.b])

                    def proj(c0, M=128):
                        p = gen.next()
                        for kc in range(KC):
                            op("pe", MM(p.f[0:M, 0:GB], win.t[:, kc, c0:c0 + M], hT.t[:, kc, :], start=(kc == 0), stop=(kc == KC - 1)),
                               reads=[win.b, hT.b], pwrites=[p.b])
                        return p

                    if first:
                        op("dve", MSET(ubuf.t[:, :, 0:2], 0.0), pwrites=[ubuf.b])
                    for cch in range(4):
                        pcc = proj(O_CC + cch * 128)
                        pcx = proj(O_CX + cch * 128)
                        pcb = proj(O_CB + cch * 128)
                        tf = tmpf.next()
                        op("act", ACTF(tf.t[:], pcc.f[:, 0:GB], AF.Copy), reads=[pcc.b], writes=[tf.b])
                        op("dve", TT(ubuf.t[:, cch, 2:2 + GB], pcx.f[:, 0:GB], tf.t[:], ALU.mult), reads=[pcx.b, tf.b], pwrites=[ubuf.b])
                        op("dve", TS(c1.t[:], ubuf.t[:, cch, 0:GB], convT.t[:, cch, 0:1], None, ALU.mult), reads=[ubuf.b, convT.b], writes=[c1.b])
                        op("dve", STT(c2.t[:], ubuf.t[:, cch, 1:1 + GB], convT.t[:, cch, 1:2], c1.t[:], ALU.mult, ALU.add),
                           reads=[ubuf.b, convT.b, c1.b], writes=[c2.b])
                        op("dve", STT(c3.t[:], ubuf.t[:, cch, 2:2 + GB], convT.t[:, cch, 2:3], c2.t[:], ALU.mult, ALU.add),
                           reads=[ubuf.b, convT.b, c2.b], writes=[c3.b])
                        op("dve", TT(yA.t[:, cch, :], pcb.f[:, 0:GB], c3.t[:], ALU.mult), reads=[pcb.b, c3.b], pwrites=[yA.b])
                        op("dve", CP(ubuf.t[:, cch, 0:2], ubuf.t[:, cch, GB:GB + 2]), reads=[ubuf.b], pwrites=[ubuf.b])
                    for dc in range(8):
                        pyc = gen.next()
                        for cch in range(4):
                            op("pe", MM(pyc.f[:, 0:GB], wco.t[:, cch, dc * 128:(dc + 1) * 128], yA.t[:, cch, :], start=(cch == 0), stop=(cch == 3)),
                               reads=[wco.b, yA.b], pwrites=[pyc.b])
                        pgc = proj(O_GC + dc * 128)
                        tf = tmpf.next()
                        op("act", ACTF(tf.t[:], pgc.f[:, 0:GB], AF.Sigmoid), reads=[pgc.b], writes=[tf.b])
                        op("dve", TT(mixA_st.t[:, dc, :], pyc.f[:, 0:GB], tf.t[:], ALU.mult), reads=[pyc.b, tf.b], pwrites=[mixA_st.b])
                    dma("sp", scr["mixA"].t.rearrange("(dc p) t -> p dc t", p=128)[:, :, t0:t0 + GB], mixA_st.t[:], reads=[mixA_st.b], pwrites=[scr["mixA"].b])
                    for dc in range(8):
                        pga = proj(O_GA + dc * 128)
                        op("act", ACTF(sga_st.t[:, dc, :], pga.f[:, 0:GB], AF.Sigmoid), reads=[pga.b], pwrites=[sga_st.b])
                    dma("sp", scr["sga"].t.rearrange("(dc p) t -> p dc t", p=128)[:, :, t0:t0 + GB], sga_st.t[:], reads=[sga_st.b], pwrites=[scr["sga"].b])
                    for (base, wap, wb, st, nm) in ((O_Q, qws.t[:, 0:1], qws.b, q_st, "qT"), (O_K, qkw.t[:, 1:2], qkw.b, k_st, "kT")):
                        for c in range(4):
                            pq = proj(base + c * 128)
                            sq = sqr.next()
                            op("act", ACTF(sq.t[:], pq.f[:, 0:GB], AF.Square), reads=[pq.b], writes=[sq.b])
                            ps2 = gen.next()
                            op("pe", MM(ps2.f[:, 0:GB], bones_bf.t[:], sq.t[:]), reads=[bones_bf.b, sq.b], pwrites=[ps2.b])
                            r1 = rtr.next()
                            op("act", ACTF(r1.t[:], ps2.f[:, 0:GB], AF.Sqrt, scale=1.0 / 64, bias=epst.t[:, 0:1]), reads=[ps2.b, epst.b], writes=[r1.b])
                            r2 = rrr.next()
                            op("dve", RCP(r2.t[:], r1.t[:]), reads=[r1.b], writes=[r2.b])
                            op("dve", STT(st.t[:, c, :], pq.f[:, 0:GB], wap, r2.t[:], ALU.mult, ALU.mult), reads=[pq.b, wb, r2.b], pwrites=[st.b])
                        dma("sp", scr[nm].t.rearrange("(c p) t -> p c t", p=128)[:, :, t0:t0 + GB], st.t[:], reads=[st.b], pwrites=[scr[nm].b])
                    for c in range(4):
                        pq = proj(O_QI + c * 128)
                        evac(qi_st.t[:, c, :], pq.f[:, 0:GB], [pq.b], [qi_st.b])
                    dma("sp", scr["qiT"].t.rearrange("(c p) t -> p c t", p=128)[:, :, t0:t0 + GB], qi_st.t[:], reads=[qi_st.b], pwrites=[scr["qiT"].b])
                    pq = proj(O_KI, M=64)
                    evac(ki_st.t[:, :], pq.f[0:64, 0:GB], [pq.b], [ki_st.b])
                    dma("sp", scr["kiT"].t[:, t0:t0 + GB], ki_st.t[:], reads=[ki_st.b], pwrites=[scr["kiT"].b])
                    for j in range(JB):
                        p = gen.next()
                        for kc in range(KC):
                            op("pe", MM(p.f[:, 0:512], hT.t[:, kc, j * 128:(j + 1) * 128], win.t[:, kc, O_V:O_V + 512], start=(kc == 0), stop=(kc == KC - 1)),
                               reads=[win.b, hT.b], pwrites=[p.b])
                        evac(v_st.t[:, j, :, 0:64], p.f[:, 0:512].rearrange("p (h d) -> p h d", h=8), [p.b], [v_st.b])
                        p = gen.next()
                        for kc in range(KC):
                            op("pe", MM(p.f[:, 0:8], hT.t[:, kc, j * 128:(j + 1) * 128], win.t[:, kc, O_WI:O_WI + 8], start=(kc == 0), stop=(kc == KC - 1)),
                               reads=[win.b, hT.b], pwrites=[p.b])
                        op("dve", CP(widx.t[:, t0 // 128 + j, :], p.f[:, 0:8]), reads=[p.b], pwrites=[widx.b])
                    dma("sp", scr["V"].t[t0:t0 + GB, :].rearrange("(j p) f -> p j f", p=128), v_st.t[:].rearrange("p j h d -> p j (h d)"),
                        reads=[v_st.b], pwrites=[scr["V"].b])
                ctx.barrier()

        if "C" in phases:
            with ExitStack() as es:
                NKB = S // 128
                QG = min(512, S)
                kTs = tile(es, "kTs", [128, 4, S], BF16)
                Vs = tile(es, "Vs", [128, NKB, 520], BF16)
                kiTs = tile(es, "kiTs", [128, S], BF16)
                qTg = Ring([tile(es, "qTg%d" % i, [128, 4, QG], BF16) for i in range(2)])
                qiTg = Ring([tile(es, "qiTg%d" % i, [128, 4, QG], BF16) for i in range(2)])
                Dh = tile(es, "Dh", [128, 8, 128], BF16)
                rr = Ring([tile(es, "relu%d" % i, [128, 512], BF16) for i in range(4)])
                score = tile(es, "score", [128, S], F32)
                junk = tile(es, "junkC", [128, S], BF16)
                bis = tile(es, "bis", [128, 8], F32)
                wf = tile(es, "wf", [128, NIT + 2], F32)
                pw2 = tile(es, "pw2", [128, NIT + 2], F32)
                for i_ in range(NIT + 2):
                    op("dve", MSET(pw2.t[:, i_:i_ + 1], 2.0 ** -i_), pwrites=[pw2.b])
                dthr = tile(es, "dthr", [128, 128], F32)
                thrbc = tile(es, "thrbc", [128, 128], F32)
                maskT = tile(es, "maskT", [128, NKB, 128], BF16)
                maskT2 = tile(es, "maskT2", [128, NKB, 128], BF16)
                PTr = Ring([tile(es, "PT%d" % i, [128, 512], BF16) for i in range(3)])
                PMr = Ring([tile(es, "PM%d" % i, [128, 512], BF16) for i in range(3)])
                rz = tile(es, "rz", [128, 8], F32)
                attn_tm = tile(es, "attn_tm", [128, 8, 64], BF16)
                attnT_r = Ring([tile(es, "attnT_st%d" % i, [128, 4, QG], BF16) for i in range(2)])
                psL = Ring([PBs[0], PBs[1]])
                psS = PBs[2]
                psT = PBs[3]
                psA = Ring([PBs[4], PBs[5]])
                psO = [PBs[6], PBs[7]]
                maskTs = [maskT, maskT2]
                blocks = [(s_, qb_) for s_ in range(NSEQ) for qb_ in range(NKB)]
                gtiles = {}
                NQ = QG // 128

                def group_tiles(s_, qg):
                    key = (s_, qg)
                    if key not in gtiles:
                        qT = qTg.next()
                        qiT = qiTg.next()
                        tq0 = s_ * S + qg * QG
                        dma("sp", qiT.t[:], scr["qiT"].t.rearrange("(c p) t -> p c t", p=128)[:, :, tq0:tq0 + QG], reads=[scr["qiT"].b], writes=[qiT.b])
                        dma("sp", qT.t[:], scr["qT"].t.rearrange("(c p) t -> p c t", p=128)[:, :, tq0:tq0 + QG], reads=[scr["qT"].b], writes=[qT.b])
                        gtiles[key] = (qT, qiT, attnT_r.next())
                    return gtiles[key]

                def stage_X(bi):
                    s_, qb = blocks[bi]
                    sl = slice(s_ * S, (s_ + 1) * S)
                    if qb == 0:
                        dma("sp", kiTs.t[0:64, :], scr["kiT"].t[:, sl], reads=[scr["kiT"].b], writes=[kiTs.b])
                        dma("sp", kiTs.t[64:128, :], scr["kiT"].t[:, sl], reads=[scr["kiT"].b], pwrites=[kiTs.b])
                    qT, qiT, _ = group_tiles(s_, qb // NQ)
                    qs = qb % NQ
                    nkb = qb + 1
                    nk = nkb * 128
                    tix = (s_ * S) // 128 + qb
                    qsl = slice(qs * 128, (qs + 1) * 128)
                    mk = maskTs[bi % 2]
                    op("dve", TT(Dh.t[:], ident_bf.t[:].unsqueeze(1).to_broadcast([128, 8, 128]),
                                 widx.t[:, tix, :].unsqueeze(2).to_broadcast([128, 8, 128]), ALU.mult),
                       reads=[ident_bf.b, widx.b], writes=[Dh.b])
                    nch = (nk + 511) // 512
                    for ch in range(nch):
                        k0 = ch * 512
                        w = min(512, nk - k0)
                        for h in range(8):
                            hp = h % 2
                            pl = psL.next()
                            op("pe", MM(pl.f[:, 0:w], qiT.t[hp * 64:(hp + 1) * 64, h // 2, qsl], kiTs.t[hp * 64:(hp + 1) * 64, k0:k0 + w]),
                               reads=[qiT.b, kiTs.b], pwrites=[pl.b])
                            r = rr.next()
                            if h % 2 == 0:
                                op("act", ACTF(r.t[:, 0:w], pl.f[:, 0:w], AF.Relu), reads=[pl.b], writes=[r.b])
                            else:
                                op("dve", TS(r.t[:, 0:w], pl.f[:, 0:w], 0.0, None, ALU.max), reads=[pl.b], writes=[r.b])
                            op("pe", MM(psS.f[:, 0:w], Dh.t[:, h, :], r.t[:, 0:w], start=(h == 0), stop=(h == 7)),
                               reads=[Dh.b, r.b], pwrites=[psS.b])
                        if ch == nch - 1:
                            wd = w - 128
                            if wd > 0:
                                op("act", ACTF(score.t[:, k0:k0 + wd], psS.f[:, 0:wd], AF.Copy), reads=[psS.b], pwrites=[score.b])
                            op("dve", TT(score.t[:, nk - 128:nk], psS.f[:, wd:w], causal_f, ALU.add), reads=[psS.b, cst.b], pwrites=[score.b])
                        else:
                            op("act", ACTF(score.t[:, k0:k0 + w], psS.f[:, 0:w], AF.Copy), reads=[psS.b], pwrites=[score.b])
                    lo, w0, mid, cnt, gg, mx = (bis.t[:, i:i + 1] for i in range(6))
                    if nk <= KSEL:
                        op("dve", MSET(lo, -1.0e29), writes=[bis.b])
                    else:
                        op("dve", RED(mx, score.t[:, 0:nk], ALU.max), reads=[score.b], writes=[bis.b])
                        op("dve", RED(lo, score.t[:, 0:nk - 128], ALU.min), reads=[score.b], writes=[bis.b])
                        op("dve", TT(w0, mx, lo, ALU.subtract), reads=[bis.b], writes=[bis.b])
                        op("dve", TS(wf.t[:], pw2.t[:], w0, None, ALU.mult), reads=[bis.b, pw2.b], writes=[wf.b])
                        op("dve", TT(mid, lo, wf.t[:, 1:2], ALU.add), reads=[bis.b, wf.b], writes=[bis.b])
                        for it in range(NIT):
                            op("dve", TS(junk.t[:, 0:nk], score.t[:, 0:nk], mid, None, ALU.is_ge, ALU.add, accum_out=cnt),
                               reads=[score.b, bis.b], writes=[bis.b, junk.b])
                            op("dve", TS(gg, cnt, KSEL - 0.5, 0.5, ALU.is_ge, ALU.subtract), reads=[bis.b], writes=[bis.b])
                            op("dve", STT(mid, gg, wf.t[:, it + 1:it + 2], mid, ALU.mult, ALU.add), reads=[bis.b, wf.b], writes=[bis.b])
                        op("dve", TT(lo, mid, wf.t[:, NIT + 1:NIT + 2], ALU.subtract), reads=[bis.b, wf.b], writes=[bis.b])
                    op("dve", TS(dthr.t[:], ident_f, lo, None, ALU.mult), reads=[cst.b, bis.b], writes=[dthr.b])
                    op("pe", MM(psT.f[:, 0:128], ones_f, dthr.t[:]), reads=[cst.b, dthr.b], pwrites=[psT.b])
                    op("act", ACTF(thrbc.t[:], psT.f[:, 0:128], AF.Copy), reads=[psT.b], writes=[thrbc.b])
                    for c4 in range((nkb + 3) // 4):
                        n4 = min(4, nkb - c4 * 4)
                        for i in range(n4):
                            kb = c4 * 4 + i
                            op("pe", TR(psT.f[:, i * 128:(i + 1) * 128], score.t[:, kb * 128:(kb + 1) * 128], ident_f),
                               reads=[score.b, cst.b], pwrites=[psT.b])
                        op("dve", TT(mk.t[:, c4 * 4:c4 * 4 + n4, :], psT.f[:, 0:n4 * 128].rearrange("p (a b) -> p a b", b=128),
                                     thrbc.t[:].unsqueeze(1).to_broadcast([128, n4, 128]), ALU.is_ge),
                           reads=[psT.b, thrbc.b], pwrites=[mk.b])

                def stage_Y(bi):
                    s_, qb = blocks[bi]
                    sl = slice(s_ * S, (s_ + 1) * S)
                    if qb == 0:
                        dma("sp", kTs.t[:], scr["kT"].t.rearrange("(c p) t -> p c t", p=128)[:, :, sl], reads=[scr["kT"].b], writes=[kTs.b])
                        dma("sp", Vs.t[:], scr["V"].t[sl, :].rearrange("(kb p) f -> p kb f", p=128), reads=[scr["V"].b], writes=[Vs.b])
                    qT, qiT, ast = group_tiles(s_, qb // NQ)
                    qs = qb % NQ
                    nkb = qb + 1
                    qsl = slice(qs * 128, (qs + 1) * 128)
                    mk = maskTs[bi % 2]
                    for h in range(8):
                        hp = h % 2
                        po = psO[h // 4]
                        for c4 in range((nkb + 3) // 4):
                            n4 = min(4, nkb - c4 * 4)
                            pa = psA.next()
                            for i in range(n4):
                                kb = c4 * 4 + i
                                op("pe", MM(pa.f[:, i * 128:(i + 1) * 128], kTs.t[hp * 64:(hp + 1) * 64, h // 2, kb * 128:(kb + 1) * 128],
                                            qT.t[hp * 64:(hp + 1) * 64, h // 2, qsl]), reads=[kTs.b, qT.b], pwrites=[pa.b])
                            pt = PTr.next()
                            op("act", ACTF(pt.t[:, 0:n4 * 128], pa.f[:, 0:n4 * 128], AF.Exp), reads=[pa.b], writes=[pt.b])
                            pm = PMr.next()
                            op("dve", TT(pm.t[:, 0:n4 * 128], pt.t[:, 0:n4 * 128], mk.t[:, c4 * 4:c4 * 4 + n4, :].rearrange("p a b -> p (a b)"), ALU.mult),
                               reads=[pt.b, mk.b], writes=[pm.b])
                            for i in range(n4):
                                kb = c4 * 4 + i
                                op("pe", MM(po.f[:, (h % 4) * 65:(h % 4) * 65 + 65], pm.t[:, i * 128:(i + 1) * 128], Vs.t[:, kb, h * 65:(h + 1) * 65],
                                            start=(kb == 0), stop=(kb == nkb - 1)), reads=[pm.b, Vs.b], pwrites=[po.b])
                    for hh in range(2):
                        pv = psO[hh].f[:, 0:260].rearrange("p (h d) -> p h d", d=65)
                        op("dve", RCP(rz.t[:, hh * 4:(hh + 1) * 4], pv[:, :, 64]), reads=[psO[hh].b], pwrites=[rz.b])
                        op("dve", TT(attn_tm.t[:, hh * 4:(hh + 1) * 4, :], pv[:, :, 0:64],
                                     rz.t[:, hh * 4:(hh + 1) * 4].unsqueeze(2).to_broadcast([128, 4, 64]), ALU.mult),
                           reads=[psO[hh].b, rz.b], pwrites=[attn_tm.b])
                    px = psA.next()
                    for c in range(4):
                        op("pe", TR(px.h[:, c * 128:(c + 1) * 128], attn_tm.t[:, 2 * c:2 * c + 2, :].rearrange("p a b -> p (a b)"), ident_bf.t[:]),
                           reads=[attn_tm.b, ident_bf.b], pwrites=[px.b])
                    op("act", ACTF(ast.t[:, :, qsl], px.h[:, 0:512].rearrange("p (c t) -> p c t", c=4), AF.Copy),
                       reads=[px.b], pwrites=[ast.b])
                    if qs == NQ - 1:
                        tq0 = s_ * S + (qb // NQ) * QG
                        dma("sp", scr["attnT"].t.rearrange("(c p) t -> p c t", p=128)[:, :, tq0:tq0 + QG], ast.t[:], reads=[ast.b], pwrites=[scr["attnT"].b])

                stage_X(0)
                for bi in range(len(blocks)):
                    if bi + 1 < len(blocks):
                        stage_X(bi + 1)
                    stage_Y(bi)
                ctx.barrier()

        if "D" in phases:
            with ExitStack() as es:
                wao = tile(es, "wao", [128, 4, D], BF16)
                wo = tile(es, "wo", [128, KC, D], BF16)
                wpq = tile(es, "wpq", [128, KC, D], BF16)
                skTb = tile(es, "skTb", [128, 8, 128], BF16)
                with ExitStack() as es2:
                    stg = Ring([tile(es2, "stgD%d" % i, [128, KC, 512], F32) for i in range(2)])
                    ci_ = 0
                    for (src, dst) in ((io["w_o"], wo), (io["w_peer_q"], wpq)):
                        sv = src.rearrange("(kc p) f -> p kc f", p=128)
                        for hh in range(2):
                            st = stg.next()
                            dma("sp", st.t[:], sv[:, :, hh * 512:(hh + 1) * 512], writes=[st.b])
                            if ci_ % 2:
                                op("act", ACTF(dst.t[:, :, hh * 512:(hh + 1) * 512], st.t[:], AF.Copy), reads=[st.b], pwrites=[dst.b])
                            else:
                                op("dve", CP(dst.t[:, :, hh * 512:(hh + 1) * 512], st.t[:]), reads=[st.b], pwrites=[dst.b])
                            ci_ += 1
                    st = stg.next()
                    stv = st.t[:].rearrange("p k f -> p (k f)").rearrange("p (c d) -> p c d", c=4)
                    dma("sp", stv, io["w_attn_out"].rearrange("(c p) d -> p c d", p=128), writes=[st.b])
                    op("dve", CP(wao.t[:], stv), reads=[st.b], writes=[wao.b])
                    st = stg.next()
                    stv2 = st.t[:].rearrange("p k f -> p (k f)")[:, 0:1024].rearrange("p (h n) -> p h n", h=8)
                    dma("sp", stv2, io["skT"], writes=[st.b])
                    op("dve", CP(skTb.t[:], stv2), reads=[st.b], writes=[skTb.b])
                    ctx.barrier()
                gb1 = tile(es, "gb1", [128, D], F32)
                attg = Ring([tile(es, "attg%d" % i, [128, 4, GB], BF16) for i in range(2)])
                sgag = Ring([tile(es, "sgag%d" % i, [128, 8, GB], BF16) for i in range(2)])
                mxag = Ring([tile(es, "mxag%d" % i, [128, 8, GB], BF16) for i in range(2)])
                xgr = Ring([tile(es, "xgD%d" % i, [128, JB, D], F32) for i in range(2)])
                tmq = Ring([tile(es, "tmq%d" % i, [128, GB], F32) for i in range(2)])
                mixT = tile(es, "mixT", [128, 8, GB], BF16)
                tz = Ring([tile(es, "tz%d" % i, [128, 512], F32) for i in range(2)])
                x1 = tile(es, "x1", [128, JB, D], F32)
                junk = tile(es, "junkD", [128, D], BF16)
                ssq = tile(es, "ssqD", [128, JB], F32)
                rt = tile(es, "rtD", [128, JB], F32)
                rstd = tile(es, "rstdD", [128, JB], F32)
                xs = tile(es, "xsD", [128, JB, D], BF16)
                h2T = tile(es, "h2T", [128, KC, GB], BF16)
                qpT = tile(es, "qpT", [128, 8, GB], BF16)
                s_sb = tile(es, "s_sb", [128, 16, 128], F32)
                s_tmp = tile(es, "s_tmp", [128, 16, 128], F32)
                v_all = tile(es, "v_all", [128, 16, 16], F32)
                idx_all = tile(es, "idx_all", [128, 16, 16], U32)
                idx_bf = tile(es, "idx_bf", [128, 16, 16], BF16)
                cand = tile(es, "cand", [128, 8, 256], F32)
                cand2 = tile(es, "cand2", [128, 8, 256], F32)
                scv = tile(es, "scv", [128, 8, 16], F32)
                civ = tile(es, "civ", [128, 8, 16], U32)
                ca_u = tile(es, "ca_u", [128, 8, 16], U32)
                cb_u = tile(es, "cb_u", [128, 8, 16], U32)
                ca_bf = tile(es, "ca_bf", [128, 8, 16], BF16)
                cb_bf = tile(es, "cb_bf", [128, 8, 16], BF16)
                eqa = tile(es, "eqa", [128, 8, 16, 16], BF16)
                prd = tile(es, "prd", [128, 8, 16, 16], BF16)
                sm = tile(es, "sm", [128, 8, 16], F32)
                zz = tile(es, "zz", [128, 8], F32)
                sel_tm = tile(es, "sel_tm", [128, 3, 128], F32)
                selT = tile(es, "selT", [128, 3, 128], BF16)
                ptr = Ring([PBs[0], PBs[1]])
                gen = Ring([PBs[i] for i in range(2, 8)])
                evi = [0]

                def evacD(out, in_, rd, wr):
                    evi[0] += 1
                    if evi[0] % 2:
                        op("act", ACTF(out, in_, AF.Copy), reads=rd, pwrites=wr)
                    else:
                        op("dve", CP(out, in_), reads=rd, pwrites=wr)

                for g in range(T // GB):
                    t0 = g * GB
                    seq = t0 // S
                    if t0 % S == 0:
                        dma("sp", gb1.t[:], scr["gbc"].t[seq, 0, :, :], reads=[scr["gbc"].b], writes=[gb1.b])
                    at = attg.next()
                    sg = sgag.next()
                    ma = mxag.next()
                    xg = xgr.next()
                    dma("sp", at.t[:], scr["attnT"].t.rearrange("(c p) t -> p c t", p=128)[:, :, t0:t0 + GB], reads=[scr["attnT"].b], writes=[at.b])
                    dma("sp", sg.t[:], scr["sga"].t.rearrange("(c p) t -> p c t", p=128)[:, :, t0:t0 + GB], reads=[scr["sga"].b], writes=[sg.b])
                    dma("sp", ma.t[:], scr["mixA"].t.rearrange("(c p) t -> p c t", p=128)[:, :, t0:t0 + GB], reads=[scr["mixA"].b], writes=[ma.b])
                    dma("sp", xg.t[:], io["x"][t0:t0 + GB, :].rearrange("(j p) d -> p j d", p=128), writes=[xg.b])
                    for dc in range(8):
                        p = gen.next()
                        for c in range(4):
                            op("pe", MM(p.f[:, 0:GB], wao.t[:, c, dc * 128:(dc + 1) * 128], at.t[:, c, :], start=(c == 0), stop=(c == 3)),
                               reads=[wao.b, at.b], pwrites=[p.b])
                        tq = tmq.next()
                        op("dve", TT(tq.t[:], p.f[:, 0:GB], sg.t[:, dc, :], ALU.mult), reads=[p.b, sg.b], writes=[tq.b])
                        op("dve", TT(mixT.t[:, dc, :], tq.t[:], ma.t[:, dc, :], ALU.add), reads=[tq.b, ma.b], pwrites=[mixT.b])
                    for j in range(JB):
                        for dh in range(2):
                            p = gen.next()
                            for kc in range(KC):
                                op("pe", MM(p.f[:, 0:512], mixT.t[:, kc, j * 128:(j + 1) * 128], wo.t[:, kc, dh * 512:(dh + 1) * 512],
                                            start=(kc == 0), stop=(kc == KC - 1)), reads=[mixT.b, wo.b], pwrites=[p.b])
                            t_ = tz.next()
                            op("dve", TT(t_.t[:], p.f[:, 0:512], gb1.t[:, dh * 512:(dh + 1) * 512], ALU.mult), reads=[p.b, gb1.b], writes=[t_.b])
                            op("dve", TT(x1.t[:, j, dh * 512:(dh + 1) * 512], t_.t[:], xg.t[:, j, dh * 512:(dh + 1) * 512], ALU.add),
                               reads=[t_.b, xg.b], pwrites=[x1.b])
                    dma("sp", scr["x1"].t[t0:t0 + GB, :].rearrange("(j p) d -> p j d", p=128), x1.t[:], reads=[x1.b], pwrites=[scr["x1"].b])
                    for j in range(JB):
                        op("act", ACTF(junk.t[:], x1.t[:, j, :], AF.Square, accum_out=ssq.t[:, j:j + 1]), reads=[x1.b], writes=[junk.b], pwrites=[ssq.b])
                    op("act", ACTF(rt.t[:], ssq.t[:], AF.Sqrt, scale=1.0 / D, bias=epst.t[:, 0:1]), reads=[ssq.b, epst.b], writes=[rt.b])
                    op("dve", RCP(rstd.t[:], rt.t[:]), reads=[rt.b], writes=[rstd.b])
                    for j in range(JB):
                        op("dve", TS(xs.t[:, j, :], x1.t[:, j, :], rstd.t[:, j:j + 1], None, ALU.mult), reads=[x1.b, rstd.b], pwrites=[xs.b])
                    for kc in range(KC):
                        pt = ptr.next()
                        for j in range(JB):
                            op("pe", TR(pt.h[:, j * 128:(j + 1) * 128], xs.t[:, j, kc * 128:(kc + 1) * 128], ident_bf.t[:]),
                               reads=[xs.b, ident_bf.b], pwrites=[pt.b])
                        op("act", ACTF(h2T.t[:, kc, :], pt.h[:, 0:GB], AF.Identity, scale=A2.t[:, kc, seq:seq + 1], bias=modTb.t[:, 24 + kc, seq:seq + 1]),
                           reads=[pt.b, A2.b, modTb.b], pwrites=[h2T.b])
                    dma("sp", scr["h2T"].t.rearrange("(c p) t -> p c t", p=128)[:, :, t0:t0 + GB], h2T.t[:], reads=[h2T.b], pwrites=[scr["h2T"].b])
                    for h in range(8):
                        p = gen.next()
                        for kc in range(KC):
                            op("pe", MM(p.f[:, 0:GB], wpq.t[:, kc, h * 128:(h + 1) * 128], h2T.t[:, kc, :], start=(kc == 0), stop=(kc == KC - 1)),
                               reads=[wpq.b, h2T.b], pwrites=[p.b])
                        evacD(qpT.t[:, h, :], p.f[:, 0:GB], [p.b], [qpT.b])
                    for j in range(JB):
                        tt0 = t0 + j * 128
                        s4 = s_sb.t[:].rearrange("p (h s) n -> p h s n", s=2)
                        for b4 in range(4):
                            p = gen.next()
                            side, hb = b4 % 2, (b4 // 2) * 4
                            for i in range(4):
                                h = hb + i
                                op("pe", MM(p.f[:, i * 128:(i + 1) * 128], qpT.t[side * 64:(side + 1) * 64, h, j * 128:(j + 1) * 128],
                                            skTb.t[side * 64:(side + 1) * 64, h, :]), reads=[qpT.b, skTb.b], pwrites=[p.b])
                            op("act", ACTF(s4[:, hb:hb + 4, side, :], p.f[:, :].rearrange("p (a b) -> p a b", b=128), AF.Copy),
                               reads=[p.b], pwrites=[s_sb.b])
                        if DCUT < 2:
                            continue
                        for r in range(16):
                            op("dve", lambda e, r=r: e.max(out=v_all.t[:, r, 0:8], in_=s_sb.t[:, r, :]), reads=[s_sb.b], pwrites=[v_all.b])
                            op("dve", lambda e, r=r: e.max_index(out=idx_all.t[:, r, 0:8], in_max=v_all.t[:, r, 0:8], in_values=s_sb.t[:, r, :]),
                               reads=[s_sb.b, v_all.b], pwrites=[idx_all.b])
                            op("dve", lambda e, r=r: e.match_replace(out=s_tmp.t[:, r, :], in_to_replace=v_all.t[:, r, 0:8], in_values=s_sb.t[:, r, :], imm_value=NEG),
                               reads=[s_sb.b, v_all.b], pwrites=[s_tmp.b])
                            op("dve", lambda e, r=r: e.max(out=v_all.t[:, r, 8:16], in_=s_tmp.t[:, r, :]), reads=[s_tmp.b], pwrites=[v_all.b])
                            op("dve", lambda e, r=r: e.max_index(out=idx_all.t[:, r, 8:16], in_max=v_all.t[:, r, 8:16], in_values=s_tmp.t[:, r, :]),
                               reads=[s_tmp.b, v_all.b], pwrites=[idx_all.b])
                        if DCUT < 3:
                            continue
                        op("dve", CP(idx_bf.t[:], idx_all.t[:]), reads=[idx_all.b], writes=[idx_bf.b])
                        v4 = v_all.t[:].rearrange("p (h s) k -> p h s k", s=2)
                        op("dve", TT(cand.t[:].rearrange("p h (a b) -> p h a b", b=16), v4[:, :, 0, :].unsqueeze(3).to_broadcast([128, 8, 16, 16]),
                                     v4[:, :, 1, :].unsqueeze(2).to_broadcast([128, 8, 16, 16]), ALU.add), reads=[v_all.b], writes=[cand.b])
                        for h in range(8):
                            op("dve", lambda e, h=h: e.max(out=scv.t[:, h, 0:8], in_=cand.t[:, h, :]), reads=[cand.b], pwrites=[scv.b])
                            op("dve", lambda e, h=h: e.max_index(out=civ.t[:, h, 0:8], in_max=scv.t[:, h, 0:8], in_values=cand.t[:, h, :]),
                               reads=[cand.b, scv.b], pwrites=[civ.b])
                            op("dve", lambda e, h=h: e.match_replace(out=cand2.t[:, h, :], in_to_replace=scv.t[:, h, 0:8], in_values=cand.t[:, h, :], imm_value=NEG),
                               reads=[cand.b, scv.b], pwrites=[cand2.b])
                            op("dve", lambda e, h=h: e.max(out=scv.t[:, h, 8:16], in_=cand2.t[:, h, :]), reads=[cand2.b], pwrites=[scv.b])
                            op("dve", lambda e, h=h: e.max_index(out=civ.t[:, h, 8:16], in_max=scv.t[:, h, 8:16], in_values=cand2.t[:, h, :]),
                               reads=[cand2.b, scv.b], pwrites=[civ.b])
                        if DCUT < 4:
                            continue
                        op("dve", TT(sm.t[:], scv.t[:], scv.t[:, :, 0:1].to_broadcast([128, 8, 16]), ALU.subtract), reads=[scv.b], writes=[sm.b])
                        op("act", ACTF(sm.t[:], sm.t[:], AF.Exp), reads=[sm.b], writes=[sm.b])
                        op("dve", RED(zz.t[:], sm.t[:], ALU.add), reads=[sm.b], writes=[zz.b])
                        op("dve", RCP(zz.t[:], zz.t[:]), reads=[zz.b], writes=[zz.b])
                        op("dve", TT(sel_tm.t[:, 2, :].rearrange("p (h k) -> p h k", k=16), sm.t[:], zz.t[:].unsqueeze(2).to_broadcast([128, 8, 16]), ALU.mult),
                           reads=[sm.b, zz.b], pwrites=[sel_tm.b])
                        if DCUT < 5:
                            continue
                        op("dve", TS(ca_u.t[:], civ.t[:], 4, None, ALU.logical_shift_right), reads=[civ.b], writes=[ca_u.b])
                        op("dve", TS(cb_u.t[:], civ.t[:], 15, None, ALU.bitwise_and), reads=[civ.b], writes=[cb_u.b])
                        op("dve", CP(ca_bf.t[:], ca_u.t[:]), reads=[ca_u.b], writes=[ca_bf.b])
                        op("dve", CP(cb_bf.t[:], cb_u.t[:]), reads=[cb_u.b], writes=[cb_bf.b])
                        if DCUT < 6:
                            continue
                        i4 = idx_bf.t[:].rearrange("p (h s) k -> p h s k", s=2)
                        for side, cbf in ((0, ca_bf), (1, cb_bf)):
                            op("dve", TT(eqa.t[:], cbf.t[:].unsqueeze(3).to_broadcast([128, 8, 16, 16]),
                                         iota_bf.t[:, 0:16].unsqueeze(1).unsqueeze(1).to_broadcast([128, 8, 16, 16]), ALU.is_equal),
                               reads=[cbf.b, iota_bf.b], writes=[eqa.b])
                            op("dve", TT(prd.t[:], eqa.t[:], i4[:, :, side, :].unsqueeze(2).to_broadcast([128, 8, 16, 16]), ALU.mult),
                               reads=[eqa.b, idx_bf.b], writes=[prd.b])
                            op("dve", RED(sel_tm.t[:, side, :], prd.t[:].rearrange("p h k a -> p (h k) a"), ALU.add), reads=[prd.b], pwrites=[sel_tm.b])
                        if DCUT < 7:
                            continue
                        pt = ptr.next()
                        for c in range(3):
                            op("pe", TR(pt.f[:, c * 128:(c + 1) * 128], sel_tm.t[:, c, :], ident_f), reads=[sel_tm.b, cst.b], pwrites=[pt.b])
                        op("act", ACTF(selT.t[:], pt.f[:, 0:384].rearrange("p (c t) -> p c t", c=3), AF.Copy), reads=[pt.b], writes=[selT.b])
                        dma("sp", scr["sel"].t.rearrange("c p t -> p c t")[:, :, tt0:tt0 + 128], selT.t[:], reads=[selT.b], pwrites=[scr["sel"].b])
                ctx.barrier()

        if "E" in phases:
            with ExitStack() as es:
                sf = Ring([tile(es, "pf%d" % i, [128, 4096], F32) for i in range(3)])
                sbf = Ring([tile(es, "pb%d" % i, [128, 4096], BF16) for i in range(3)])
                uv = io["uT"].rearrange("(kc p) e -> p kc e", p=128)
                uo = scr["uTb"].t.rearrange("(kc p) e -> p kc e", p=128)
                vv = io["vp"].rearrange("(jj p) d -> p jj d", p=128)
                vo = scr["vb"].t.rearrange("(jj p) d -> p jj d", p=128)
                n = 0
                for i in range(NEXP // 512):
                    for (src, dst, key, v3) in ((uv[:, :, i * 512:(i + 1) * 512], uo[:, :, i * 512:(i + 1) * 512], "uTb", "p (a b) -> p a b"),
                                                (vv[:, i * 4:(i + 1) * 4, :], vo[:, i * 4:(i + 1) * 4, :], "vb", "p (a b) -> p a b")):
                        a_ = 8 if key == "uTb" else 4
                        f_ = sf.next()
                        b_ = sbf.next()
                        dma("sp", f_.t[:].rearrange(v3, a=a_), src, writes=[f_.b])
                        eng = ("dve", "act", "pool")[n % 3]
                        n += 1
                        if eng == "act":
                            op("act", ACTF(b_.t[:], f_.t[:], AF.Copy), reads=[f_.b], writes=[b_.b])
                        else:
                            op(eng, CP(b_.t[:], f_.t[:]), reads=[f_.b], writes=[b_.b])
                        dma("sp", dst, b_.t[:].rearrange(v3, a=a_), reads=[b_.b], pwrites=[scr[key].b])
                ctx.barrier()

        if "E" in phases:
            with ExitStack() as es:
                TE = 256
                SC = 2
                gb2 = tile(es, "gb2", [128, D], F32)
                x1r = Ring([tile(es, "x1E%d" % i, [128, 2, D], F32) for i in range(2)])
                h2r = Ring([tile(es, "h2E%d" % i, [128, KC, TE], BF16) for i in range(2)])
                selr = Ring([tile(es, "selE%d" % i, [128, 3, TE], BF16) for i in range(2)])
                Lr = Ring([tile(es, "L%d" % i, [128, 32, 128], BF16) for i in range(2)])
                L0r = Ring([tile(es, "L0%d" % i, [128, 32, 128], BF16) for i in range(1)])
                Rr = Ring([tile(es, "R%d" % i, [128, 32, 128], BF16) for i in range(2)])
                G = tile(es, "G", [128, TE, 128], BF16)
                uTr = Ring([tile(es, "uTs%d" % i, [128, KC, SC * 128], BF16) for i in range(3)])
                vr = Ring([tile(es, "vs%d" % i, [128, SC, D], BF16) for i in range(3)])
                Wr = Ring([tile(es, "W%d" % i, [128, TE], BF16) for i in range(3)])
                W2r = Ring([tile(es, "W2%d" % i, [128, TE], BF16) for i in range(3)])
                tz = Ring([tile(es, "tzE%d" % i, [128, 512], F32) for i in range(2)])
                yst = tile(es, "yst", [128, 2, D], F32)
                psOut = [[PBs[0], PBs[1]], [PBs[2], PBs[3]]]
                psAT = Ring([PBs[4], PBs[5]])
                psG = Ring([PBs[6], PBs[7]])
                uo = scr["uTb"].t.rearrange("(kc p) e -> p kc e", p=128)
                vo = scr["vb"].t.rearrange("(jj p) d -> p jj d", p=128)
                for tl in range(T // TE):
                    t0 = tl * TE
                    seq = t0 // S
                    if t0 % S == 0:
                        dma("sp", gb2.t[:], scr["gbc"].t[seq, 1, :, :], reads=[scr["gbc"].b], writes=[gb2.b])
                    x1t = x1r.next()
                    h2 = h2r.next()
                    sl_ = selr.next()
                    dma("sp", x1t.t[:], scr["x1"].t[t0:t0 + TE, :].rearrange("(j p) d -> p j d", p=128), reads=[scr["x1"].b], writes=[x1t.b])
                    dma("sp", h2.t[:], scr["h2T"].t.rearrange("(c p) t -> p c t", p=128)[:, :, t0:t0 + TE], reads=[scr["h2T"].b], writes=[h2.b])
                    dma("sp", sl_.t[:], scr["sel"].t.rearrange("c p t -> p c t")[:, :, t0:t0 + TE], reads=[scr["sel"].b], writes=[sl_.b])
                    for sb in range(TE // 32):
                        ts_ = slice(sb * 32, (sb + 1) * 32)
                        L0 = L0r.next()
                        L = Lr.next()
                        Rt = Rr.next()
                        iob = iota_bf.t[:].unsqueeze(1).to_broadcast([128, 32, 128])
                        op("dve", TT(L0.t[:], iob, sl_.t[:, 0, ts_].unsqueeze(2).to_broadcast([128, 32, 128]), ALU.is_equal),
                           reads=[iota_bf.b, sl_.b], writes=[L0.b])
                        op("dve", TT(L.t[:], L0.t[:], sl_.t[:, 2, ts_].unsqueeze(2).to_broadcast([128, 32, 128]), ALU.mult),
                           reads=[L0.b, sl_.b], writes=[L.b])
                        op("dve", TT(Rt.t[:], iob, sl_.t[:, 1, ts_].unsqueeze(2).to_broadcast([128, 32, 128]), ALU.is_equal),
                           reads=[iota_bf.b, sl_.b], writes=[Rt.b])
                        for q4 in range(8):
                            pg = psG.next()
                            for i in range(4):
                                t = q4 * 4 + i
                                op("pe", MM(pg.f[:, i * 128:(i + 1) * 128], L.t[:, t, :], Rt.t[:, t, :]), reads=[L.b, Rt.b], pwrites=[pg.b])
                            tt = sb * 32 + q4 * 4
                            op("act", ACTF(G.t[:, tt:tt + 4, :], pg.f[:, :].rearrange("p (a b) -> p a b", b=128), AF.Copy), reads=[pg.b], pwrites=[G.b])
                    chunks = {}

                    def load_sc(sc_):
                        uT = uTr.next()
                        vs = vr.next()
                        dma("sp", uT.t[:], uo[:, :, sc_ * SC * 128:(sc_ + 1) * SC * 128], reads=[scr["uTb"].b], writes=[uT.b])
                        dma("sp", vs.t[:], vo[:, sc_ * SC:(sc_ + 1) * SC, :], reads=[scr["vb"].b], writes=[vs.b])
                        for jj in range(SC):
                            chunks[sc_ * SC + jj] = (uT, vs, jj)

                    def stage1(j):
                        if j % SC == 0:
                            load_sc(j // SC)
                        uT, vs, jj = chunks[j]
                        pa = psAT.next()
                        for kc in range(KC):
                            op("pe", MM(pa.f[:, 0:TE], uT.t[:, kc, jj * 128:(jj + 1) * 128], h2.t[:, kc, :], start=(kc == 0), stop=(kc == KC - 1)),
                               reads=[uT.b, h2.b], pwrites=[pa.b])
                        W = Wr.next()
                        op("act", ACTF(W.t[:], pa.f[:, 0:TE], AF.Gelu), reads=[pa.b], writes=[W.b])
                        W2 = W2r.next()
                        op("dve", TT(W2.t[:], W.t[:], G.t[:, :, j], ALU.mult), reads=[W.b, G.b], writes=[W2.b])
                        return W2

                    def stage2(j, W2):
                        uT, vs, jj = chunks.pop(j)
                        for tb in range(2):
                            for dh in range(2):
                                po = psOut[tb][dh]
                                op("pe", MM(po.f[:, :], W2.t[:, tb * 128:(tb + 1) * 128], vs.t[:, jj, dh * 512:(dh + 1) * 512],
                                            start=(j == 0), stop=(j == 127)), reads=[W2.b, vs.b], pwrites=[po.b])

                    pend = stage1(0)
                    for j in range(128):
                        nxt = stage1(j + 1) if j + 1 < 128 else None
                        stage2(j, pend)
                        pend = nxt
                    for tb in range(2):
                        for dh in range(2):
                            po = psOut[tb][dh]
                            t_ = tz.next()
                            op("dve", TT(t_.t[:], po.f[:, :], gb2.t[:, dh * 512:(dh + 1) * 512], ALU.mult), reads=[po.b, gb2.b], writes=[t_.b])
                            op("dve", TT(yst.t[:, tb, dh * 512:(dh + 1) * 512], t_.t[:], x1t.t[:, tb, dh * 512:(dh + 1) * 512], ALU.add),
                               reads=[t_.b, x1t.b], pwrites=[yst.b])
                    dma("sp", y[t0:t0 + TE, :].rearrange("(j p) d -> p j d", p=128), yst.t[:], reads=[yst.b])
                ctx.barrier()

        ctx.finish()
    return nc, ctx


def host_consts():
    c = np.zeros((128, 5, 128), np.float32)
    c[:, 0, :] = np.eye(128, dtype=np.float32)
    qi = np.arange(128)[:, None]
    ki = np.arange(128)[None, :]
    c[:, 1, :] = np.where(ki <= qi, 0.0, NEG).astype(np.float32)
    c[:, 2, :] = ((qi // 64) == (ki // 64)).astype(np.float32)
    c[:, 3, :] = np.broadcast_to(np.arange(128, dtype=np.float32)[None, :], (128, 128))
    c[:, 4, :] = 1.0
    return c


def host_shared(inp):
    f = lambda a: np.ascontiguousarray(np.asarray(a, dtype=np.float32))
    sh = {}
    sh["w_ada"] = f(inp["w_ada"][0])
    sh["b_ada"] = f(inp["b_ada"][0])
    sh["b_adaT"] = f(np.asarray(inp["b_ada"][0]).reshape(48, 128).T)
    sh["n1T"] = f(np.asarray(inp["norm1_w"][0]).reshape(KC, 128).T)
    sh["n2T"] = f(np.asarray(inp["norm2_w"][0]).reshape(KC, 128).T)
    sh["w_in"] = f(inp["w_in"][0])
    sh["convT"] = f(np.asarray(inp["conv_w"][0]).reshape(3, 4, 128).transpose(2, 1, 0))
    sh["w_conv_out"] = f(inp["w_conv_out"][0])
    sh["qk_w"] = f(np.stack([np.tile(np.asarray(inp["q_norm_w"][0]), 2), np.tile(np.asarray(inp["k_norm_w"][0]), 2)], axis=1))
    sh["w_attn_out"] = f(inp["w_attn_out"][0])
    sh["w_o"] = f(inp["w_o"][0])
    sh["w_peer_q"] = f(inp["w_peer_q"][0])
    sk = np.asarray(inp["peer_sub_keys"][0])
    sh["skT"] = f(sk.transpose(1, 3, 0, 2).reshape(128, 8, 128))
    u = np.asarray(inp["peer_u"][0]).reshape(128, 128, D).transpose(1, 0, 2).reshape(NEXP, D)
    sh["uT"] = f(u.T)
    sh["vp"] = f(np.asarray(inp["peer_v"][0]).reshape(128, 128, D).transpose(1, 0, 2).reshape(NEXP, D))
    sh["consts"] = host_consts()
    return sh


def kernel(**inp):
    x = np.asarray(inp["x"], dtype=np.float32)
    c = np.asarray(inp["c"], dtype=np.float32)
    B, S, _ = x.shape
    ncores = 8
    NSEQ = B // ncores
    nc, _ = build(NSEQ, S)
    sh = host_shared(inp)
    in_maps = []
    for i in range(ncores):
        m = dict(sh)
        m["x"] = np.ascontiguousarray(x[i * NSEQ:(i + 1) * NSEQ].reshape(NSEQ * S, D))
        m["cT"] = np.ascontiguousarray(c[i * NSEQ:(i + 1) * NSEQ].reshape(NSEQ, KC, 128).transpose(2, 1, 0))
        in_maps.append(m)
    res = run_bass_kernel_spmd(nc, in_maps, core_ids=list(range(ncores)))
    out = np.concatenate([np.asarray(r["y"]).reshape(NSEQ, S, D) for r in res.results], axis=0)
    return out.astype(np.float32)
```

```python
import math
from contextlib import ExitStack

import numpy as np
import concourse.bass as bass
import concourse.mybir as mybir
from concourse.bass_utils import run_bass_kernel_spmd

F32 = mybir.dt.float32
BF16 = mybir.dt.bfloat16
U32 = mybir.dt.uint32
AF = mybir.ActivationFunctionType
ALU = mybir.AluOpType
AX = mybir.AxisListType

D = 1024
KC = 8
NIN = 5704
O_CB, O_CC, O_CX, O_Q, O_K, O_V, O_QI, O_KI, O_WI, O_GC, O_GA = 0, 512, 1024, 1536, 2048, 2560, 3072, 3584, 3648, 3656, 4680
NEXP = 16384
EPS = 1e-6
NEG = -1.0e30
NIT = 16
DCUT = 99
SELF_SYNC = True


class Buf:
    __slots__ = ("w", "r")

    def __init__(self):
        self.w = {}
        self.r = {}


class Tl:
    __slots__ = ("t", "b")

    def __init__(self, t):
        self.t = t
        self.b = Buf()


class Ring:
    def __init__(self, items):
        self.items = items
        self.i = 0

    def next(self):
        it = self.items[self.i % len(self.items)]
        self.i += 1
        return it


class Ctx:
    COMPUTE = ("pe", "act", "dve", "pool")
    ALL = ("pe", "act", "dve", "pool", "sp")
    NL = 8

    def __init__(self, nc):
        self.nc = nc
        self.prog = {n: [] for n in self.ALL}
        self.sems = {}
        self.cnt = {}
        self.known = {n: {} for n in self.ALL}
        for n in self.COMPUTE:
            self.sems[n] = nc.alloc_semaphore(name="s_" + n)
            self.cnt[n] = 0
        self.lane_rr = {}
        for q in ("sp", "act", "pool"):
            self.lane_rr[q] = 0
            for l in range(self.NL):
                k = (q, l)
                self.sems[k] = nc.alloc_semaphore(name="d_%s%d" % (q, l))
                self.cnt[k] = 0
        self.ninstr = 0

    def _wait(self, eng, key, val):
        if val <= 0:
            return
        if key == eng and (eng == "pe" or not SELF_SYNC):
            return
        if self.known[eng].get(key, 0) < val:
            self.prog[eng].append(("w", key, val))
            self.known[eng][key] = val

    def _deps(self, eng, reads, writes, pwrites):
        deps = {}
        for b in reads:
            for k, v in b.w.items():
                if deps.get(k, 0) < v:
                    deps[k] = v
        for b in writes:
            for dct in (b.w, b.r):
                for k, v in dct.items():
                    if deps.get(k, 0) < v:
                        deps[k] = v
        for b in pwrites:
            for k, v in b.r.items():
                if deps.get(k, 0) < v:
                    deps[k] = v
            for k, v in b.w.items():
                if k != eng and deps.get(k, 0) < v:
                    deps[k] = v
        for k, v in deps.items():
            self._wait(eng, k, v)

    def _mark(self, key, n, reads, writes, pwrites):
        for b in reads:
            b.r[key] = n
        for b in writes:
            b.w = {key: n}
            b.r = {}
        for b in pwrites:
            b.w[key] = n

    def op(self, eng, fn, reads=(), writes=(), pwrites=()):
        self._deps(eng, reads, writes, pwrites)
        self.cnt[eng] += 1
        self.prog[eng].append(("o", fn))
        self._mark(eng, self.cnt[eng], reads, writes, pwrites)
        self.ninstr += 1

    def dma(self, q, out, in_, reads=(), writes=(), pwrites=(), slow=False):
        l = self.lane_rr[q] % self.NL
        self.lane_rr[q] += 1
        key = (q, l)
        self._wait(q, key, self.cnt[key])
        self._deps(q, reads, writes, pwrites)
        self.cnt[key] += 16
        self.prog[q].append(("d", out, in_, key, slow))
        self._mark(key, self.cnt[key], reads, writes, pwrites)
        self.ninstr += 1

    def barrier(self):
        for e in self.ALL:
            for k in self.sems:
                self._wait(e, k, self.cnt[k])

    def finish(self):
        for k in self.sems:
            self._wait("sp", k, self.cnt[k])
        nc = self.nc

        def mk(name):
            def body(e):
                for it in self.prog[name]:
                    if it[0] == "w":
                        e.wait_ge(self.sems[it[1]], it[2])
                    elif it[0] == "o":
                        it[1](e).then_inc(self.sems[name], 1)
                    else:
                        if it[4]:
                            e.dma_start(out=it[1], in_=it[2], allow_slow_non_contiguous=True).then_inc(self.sems[it[3]], 16)
                        else:
                            e.dma_start(out=it[1], in_=it[2]).then_inc(self.sems[it[3]], 16)
            return body

        with nc.Block() as block:
            block.tensor(mk("pe"))
            block.scalar(mk("act"))
            block.vector(mk("dve"))
            block.gpsimd(mk("pool"))
            block.sync(mk("sp"))


def ACTF(out, in_, func, **kw):
    return lambda e: e.activation(out=out, in_=in_, func=func, **kw)


def MM(out, lhsT, rhs, start=True, stop=True):
    return lambda e: e.matmul(out, lhsT, rhs, start=start, stop=stop)


def TR(out, in_, ident):
    return lambda e: e.transpose(out, in_, ident)


def TS(out, in0, s1, s2, op0, op1=None, accum_out=None):
    if op1 is None:
        return lambda e: e.tensor_scalar(out=out, in0=in0, scalar1=s1, scalar2=s2, op0=op0, accum_out=accum_out)
    return lambda e: e.tensor_scalar(out=out, in0=in0, scalar1=s1, scalar2=s2, op0=op0, op1=op1, accum_out=accum_out)


def TT(out, in0, in1, op):
    return lambda e: e.tensor_tensor(out=out, in0=in0, in1=in1, op=op)


def STT(out, in0, scalar, in1, op0, op1):
    return lambda e: e.scalar_tensor_tensor(out=out, in0=in0, scalar=scalar, in1=in1, op0=op0, op1=op1)


def CP(out, in_):
    return lambda e: e.tensor_copy(out=out, in_=in_)


def RED(out, in_, op):
    return lambda e: e.tensor_reduce(out=out, in_=in_, axis=AX.X, op=op)


def RCP(out, in_):
    return lambda e: e.reciprocal(out=out, in_=in_)


def MSET(ap, c):
    return lambda e: e.memset(ap, c)


def build(NSEQ, S, GB=256, dbg=False, phases="ABCDE"):
    T = NSEQ * S
    NT = T // 128
    KSEL = min(256, S // 4)
    JB = GB // 128
    nc = bass.Bass("TRN2", target_bir_lowering=False)
    ctx = Ctx(nc)
    op, dma = ctx.op, ctx.dma

    def din(name, shape, dt=F32):
        return nc.dram_tensor(name, list(shape), dt, kind="ExternalInput").ap()

    io = {
        "x": din("x", [T, D]), "cT": din("cT", [128, KC, NSEQ]), "w_ada": din("w_ada", [D, 6 * D]),
        "b_ada": din("b_ada", [6 * D]), "b_adaT": din("b_adaT", [128, 48]),
        "n1T": din("n1T", [128, KC]), "n2T": din("n2T", [128, KC]), "w_in": din("w_in", [D, NIN]),
        "convT": din("convT", [128, 4, 3]), "w_conv_out": din("w_conv_out", [512, D]),
        "qk_w": din("qk_w", [128, 2]), "w_attn_out": din("w_attn_out", [512, D]), "w_o": din("w_o", [D, D]),
        "w_peer_q": din("w_peer_q", [D, D]), "skT": din("skT", [128, 8, 128]),
        "uT": din("uT", [D, NEXP]), "vp": din("vp", [NEXP, D]), "consts": din("consts", [128, 5, 128]),
    }
    y = nc.dram_tensor("y", [T, D], F32, kind="ExternalOutput").ap()
    skind = "ExternalOutput" if dbg else "Internal"

    def dscr(name, shape, dt):
        t = nc.dram_tensor(name, list(shape), dt, kind=skind).ap()
        return Tl(t)

    scr = {
        "mixA": dscr("s_mixA", [D, T], BF16), "sga": dscr("s_sga", [D, T], BF16),
        "qT": dscr("s_qT", [512, T], BF16), "kT": dscr("s_kT", [512, T], BF16),
        "qiT": dscr("s_qiT", [512, T], BF16), "kiT": dscr("s_kiT", [64, T], BF16),
        "V": dscr("s_V", [T, 520], BF16), "attnT": dscr("s_attnT", [512, T], BF16),
        "gbc": dscr("s_gbc", [NSEQ, 2, 128, D], F32), "x1": dscr("s_x1", [T, D], F32),
        "h2T": dscr("s_h2T", [D, T], BF16), "sel": dscr("s_sel", [3, 128, T], BF16),
        "uTb": dscr("s_uTb", [D, NEXP], BF16), "vb": dscr("s_vb", [NEXP, D], BF16),
    }

    with ExitStack() as gs:
        def tile(es, name, shape, dt):
            return Tl(es.enter_context(nc.sbuf_tensor("sb_" + name, list(shape), dt)))

        psum = gs.enter_context(nc.psum_tensor("psum", [128, 8, 512], F32))
        pb = [Buf() for _ in range(8)]

        class PB:
            def __init__(self, i):
                self.i = i
                self.b = pb[i]
                self.f = psum[:, i, :]
                self.h = psum[:, i, :].bitcast(BF16)

        PBs = [PB(i) for i in range(8)]

        cst = tile(gs, "cst", [128, 5, 128], F32)
        ident_bf = tile(gs, "ident_bf", [128, 128], BF16)
        bones_bf = tile(gs, "bones_bf", [128, 128], BF16)
        iota_bf = tile(gs, "iota_bf", [128, 128], BF16)
        epst = tile(gs, "epst", [128, 1], F32)
        modTb = tile(gs, "modTb", [128, 48, NSEQ], F32)
        A1 = tile(gs, "A1", [128, KC, NSEQ], F32)
        A2 = tile(gs, "A2", [128, KC, NSEQ], F32)
        widx = tile(gs, "widx", [128, NT, 8], F32)
        qkw = tile(gs, "qkw", [128, 2], F32)
        qws = tile(gs, "qws", [128, 1], F32)
        ident_f = cst.t[:, 0, :]
        causal_f = cst.t[:, 1, :]
        ones_f = cst.t[:, 4, :]

        dma("sp", cst.t[:], io["consts"], writes=[cst.b])
        dma("sp", qkw.t[:], io["qk_w"], writes=[qkw.b])
        op("dve", CP(ident_bf.t[:], cst.t[:, 0, :]), reads=[cst.b], writes=[ident_bf.b])
        op("dve", CP(bones_bf.t[:], cst.t[:, 2, :]), reads=[cst.b], writes=[bones_bf.b])
        op("dve", CP(iota_bf.t[:], cst.t[:, 3, :]), reads=[cst.b], writes=[iota_bf.b])
        op("dve", MSET(epst.t[:], EPS), writes=[epst.b])
        op("dve", TS(qws.t[:], qkw.t[:, 0:1], 0.125, None, ALU.mult), reads=[qkw.b], writes=[qws.b])

        if "A" in phases:
            with ExitStack() as es:
                cT = tile(es, "cT", [128, KC, NSEQ], F32)
                sc = tile(es, "sc", [128, KC, NSEQ], F32)
                scb = tile(es, "scb", [128, KC * NSEQ, 128], F32)
                badaT = tile(es, "badaT", [128, 48], F32)
                bbc = tile(es, "bbc", [128, 2, D], F32)
                n1T = tile(es, "n1T", [128, KC], F32)
                n2T = tile(es, "n2T", [128, KC], F32)
                war = Ring([tile(es, "wa%d" % i, [128, KC, 512], F32) for i in range(2)])
                gst = Ring([tile(es, "gst%d" % i, [128, 512], F32) for i in range(2)])
                dma("sp", cT.t[:], io["cT"], writes=[cT.b])
                dma("sp", badaT.t[:], io["b_adaT"], writes=[badaT.b])
                dma("sp", n1T.t[:], io["n1T"], writes=[n1T.b])
                dma("sp", n2T.t[:], io["n2T"], writes=[n2T.b])
                dma("sp", bbc.t[:, 0, :], io["b_ada"][2 * D:3 * D].partition_broadcast(128), pwrites=[bbc.b])
                dma("sp", bbc.t[:, 1, :], io["b_ada"][5 * D:6 * D].partition_broadcast(128), pwrites=[bbc.b])
                op("act", ACTF(sc.t[:], cT.t[:], AF.Silu), reads=[cT.b], writes=[sc.b])
                op("dve", CP(scb.t[:], sc.t[:].rearrange("p k s -> p (k s)").unsqueeze(2).to_broadcast([128, KC * NSEQ, 128])),
                   reads=[sc.b], writes=[scb.b])
                pM = PBs[0]
                pG = Ring([PBs[1], PBs[2]])
                wav = io["w_ada"].rearrange("(kc p) f -> p kc f", p=128)
                for blk in range(12):
                    wa = war.next()
                    dma("sp", wa.t[:], wav[:, :, blk * 512:(blk + 1) * 512], writes=[wa.b])
                    for fl in range(4):
                        fc = blk * 4 + fl
                        for kc in range(KC):
                            op("pe", MM(pM.f[:, fc * NSEQ:(fc + 1) * NSEQ], wa.t[:, kc, fl * 128:(fl + 1) * 128], sc.t[:, kc, :],
                                        start=(kc == 0), stop=(kc == KC - 1)), reads=[wa.b, sc.b], pwrites=[pM.b])
                    if blk in (4, 5, 10, 11):
                        which = 0 if blk < 6 else 1
                        half = blk % 2
                        for s in range(NSEQ):
                            p = pG.next()
                            for kc in range(KC):
                                op("pe", MM(p.f[:, :], scb.t[:, kc * NSEQ + s, :], wa.t[:, kc, :], start=(kc == 0), stop=(kc == KC - 1)),
                                   reads=[wa.b, scb.b], pwrites=[p.b])
                            g = gst.next()
                            op("dve", TT(g.t[:], p.f[:, :], bbc.t[:, which, half * 512:(half + 1) * 512], ALU.add),
                               reads=[p.b, bbc.b], writes=[g.b])
                            dma("sp", scr["gbc"].t[s, which, :, half * 512:(half + 1) * 512], g.t[:], reads=[g.b], pwrites=[scr["gbc"].b])
                op("dve", TT(modTb.t[:], pM.f[:, 0:48 * NSEQ].rearrange("p (f s) -> p f s", s=NSEQ),
                             badaT.t[:].unsqueeze(2).to_broadcast([128, 48, NSEQ]), ALU.add),
                   reads=[pM.b, badaT.b], writes=[modTb.b])
                op("dve", STT(A1.t[:], modTb.t[:, 8:16, :], 1.0, n1T.t[:].unsqueeze(2).to_broadcast([128, KC, NSEQ]), ALU.add, ALU.mult),
                   reads=[modTb.b, n1T.b], writes=[A1.b])
                op("dve", STT(A2.t[:], modTb.t[:, 32:40, :], 1.0, n2T.t[:].unsqueeze(2).to_broadcast([128, KC, NSEQ]), ALU.add, ALU.mult),
                   reads=[modTb.b, n2T.b], writes=[A2.b])
                ctx.barrier()

        if "B" in phases:
            with ExitStack() as es:
                win = tile(es, "win", [128, KC, NIN], BF16)
                wco = tile(es, "wco", [128, 4, D], BF16)
                convT = tile(es, "convT", [128, 4, 3], F32)
                dma("sp", convT.t[:], io["convT"], writes=[convT.b])
                with ExitStack() as es2:
                    stg = Ring([tile(es2, "stg%d" % i, [128, KC, 512], F32) for i in range(2)])
                    wiv = io["w_in"].rearrange("(kc p) f -> p kc f", p=128)
                    col = 0
                    i = 0
                    while col < NIN:
                        w = min(512, NIN - col)
                        st = stg.next()
                        dma("sp", st.t[:, :, 0:w], wiv[:, :, col:col + w], writes=[st.b])
                        eng = ("dve", "act", "pool")[i % 3]
                        if eng == "act":
                            op("act", ACTF(win.t[:, :, col:col + w], st.t[:, :, 0:w], AF.Copy), reads=[st.b], pwrites=[win.b])
                        else:
                            op(eng, CP(win.t[:, :, col:col + w], st.t[:, :, 0:w]), reads=[st.b], pwrites=[win.b])
                        col += w
                        i += 1
                    st = stg.next()
                    stv = st.t[:].rearrange("p k f -> p (k f)").rearrange("p (c d) -> p c d", c=4)
                    dma("sp", stv, io["w_conv_out"].rearrange("(c p) d -> p c d", p=128), writes=[st.b])
                    op("dve", CP(wco.t[:], stv), reads=[st.b], writes=[wco.b])
                    ctx.barrier()
                xgr = Ring([tile(es, "xg%d" % i, [128, JB, D], F32) for i in range(2)])
                junk = tile(es, "junkB", [128, D], BF16)
                ssq = tile(es, "ssq", [128, JB], F32)
                rt = tile(es, "rt", [128, JB], F32)
                rstd = tile(es, "rstd", [128, JB], F32)
                xs = tile(es, "xs", [128, JB, D], BF16)
                hTr = Ring([tile(es, "hT%d" % i, [128, KC, GB], BF16) for i in range(2)])
                ubuf = tile(es, "ubuf", [128, 4, GB + 2], F32)
                tmpf = Ring([tile(es, "tmpf%d" % i, [128, GB], F32) for i in range(3)])
                c1 = tile(es, "c1", [128, GB], F32)
                c2 = tile(es, "c2", [128, GB], F32)
                c3 = tile(es, "c3", [128, GB], F32)
                yA = tile(es, "yA", [128, 4, GB], BF16)
                mixA_st = tile(es, "mixA_st", [128, 8, GB], BF16)
                sga_st = tile(es, "sga_st", [128, 8, GB], BF16)
                q_st = tile(es, "q_st", [128, 4, GB], BF16)
                k_st = tile(es, "k_st", [128, 4, GB], BF16)
                qi_st = tile(es, "qi_st", [128, 4, GB], BF16)
                ki_st = tile(es, "ki_st", [64, GB], BF16)
                v_st = tile(es, "v_st", [128, JB, 8, 65], BF16)
                sqr = Ring([tile(es, "sq%d" % i, [128, GB], BF16) for i in range(2)])
                rtr = Ring([tile(es, "rtq%d" % i, [128, GB], F32) for i in range(2)])
                rrr = Ring([tile(es, "rrq%d" % i, [128, GB], F32) for i in range(2)])
                op("pool", MSET(v_st.t[:], 1.0), writes=[v_st.b])
                ptr = Ring([PBs[0], PBs[1]])
                gen = Ring([PBs[i] for i in range(2, 8)])
                evi = [0]

                def evac(out, in_, rd, wr):
                    evi[0] += 1
                    if evi[0] % 2:
                        op("act", ACTF(out, in_, AF.Copy), reads=rd, pwrites=wr)
                    else:
                        op("dve", CP(out, in_), reads=rd, pwrites=wr)

                for g in range(T // GB):
                    t0 = g * GB
                    seq = t0 // S
                    first = (t0 % S == 0)
                    xg = xgr.next()
                    hT = hTr.next()
                    dma("sp", xg.t[:], io["x"][t0:t0 + GB, :].rearrange("(j p) d -> p j d", p=128), writes=[xg.b])
                    for j in range(JB):
                        op("act", ACTF(junk.t[:], xg.t[:, j, :], AF.Square, accum_out=ssq.t[:, j:j + 1]), reads=[xg.b], writes=[junk.b], pwrites=[ssq.b])
                    op("act", ACTF(rt.t[:], ssq.t[:], AF.Sqrt, scale=1.0 / D, bias=epst.t[:, 0:1]), reads=[ssq.b, epst.b], writes=[rt.b])
                    op("dve", RCP(rstd.t[:], rt.t[:]), reads=[rt.b], writes=[rstd.b])
                    for j in range(JB):
                        op("dve", TS(xs.t[:, j, :], xg.t[:, j, :], rstd.t[:, j:j + 1], None, ALU.mult), reads=[xg.b, rstd.b], pwrites=[xs.b])
                    for kc in range(KC):
                        pt = ptr.next()
                        for j in range(JB):
                            op("pe", TR(pt.h[:, j * 128:(j + 1) * 128], xs.t[:, j, kc * 128:(kc + 1) * 128], ident_bf.t[:]),
                               reads=[xs.b, ident_bf.b], pwrites=[pt.b])
                        op("act", ACTF(hT.t[:, kc, :], pt.h[:, 0:GB], AF.Identity, scale=A1.t[:, kc, seq:seq + 1], bias=modTb.t[:, kc, seq:seq + 1]),
                           reads=[pt.b, A1.b, modTb.b], pwrites=[hT.b])

                    def proj(c0, M=128):
                        p = gen.next()
                        for kc in range(KC):
                            op("pe", MM(p.f[0:M, 0:GB], win.t[:, kc, c0:c0 + M], hT.t[:, kc, :], start=(kc == 0), stop=(kc == KC - 1)),
                               reads=[win.b, hT.b], pwrites=[p.b])
                        return p

                    if first:
                        op("dve", MSET(ubuf.t[:, :, 0:2], 0.0), pwrites=[ubuf.b])
                    for cch in range(4):
                        pcc = proj(O_CC + cch * 128)
                        pcx = proj(O_CX + cch * 128)
                        pcb = proj(O_CB + cch * 128)
                        tf = tmpf.next()
                        op("act", ACTF(tf.t[:], pcc.f[:, 0:GB], AF.Copy), reads=[pcc.b], writes=[tf.b])
                        op("dve", TT(ubuf.t[:, cch, 2:2 + GB], pcx.f[:, 0:GB], tf.t[:], ALU.mult), reads=[pcx.b, tf.b], pwrites=[ubuf.b])
                        op("dve", TS(c1.t[:], ubuf.t[:, cch, 0:GB], convT.t[:, cch, 0:1], None, ALU.mult), reads=[ubuf.b, convT.b], writes=[c1.b])
                        op("dve", STT(c2.t[:], ubuf.t[:, cch, 1:1 + GB], convT.t[:, cch, 1:2], c1.t[:], ALU.mult, ALU.add),
                           reads=[ubuf.b, convT.b, c1.b], writes=[c2.b])
                        op("dve", STT(c3.t[:], ubuf.t[:, cch, 2:2 + GB], convT.t[:, cch, 2:3], c2.t[:], ALU.mult, ALU.add),
                           reads=[ubuf.b, convT.b, c2.b], writes=[c3.b])
                        op("dve", TT(yA.t[:, cch, :], pcb.f[:, 0:GB], c3.t[:], ALU.mult), reads=[pcb.b, c3.b], pwrites=[yA.b])
                        op("dve", CP(ubuf.t[:, cch, 0:2], ubuf.t[:, cch, GB:GB + 2]), reads=[ubuf.b], pwrites=[ubuf.b])
                    for dc in range(8):
                        pyc = gen.next()
                        for cch in range(4):
                            op("pe", MM(pyc.f[:, 0:GB], wco.t[:, cch, dc * 128:(dc + 1) * 128], yA.t[:, cch, :], start=(cch == 0), stop=(cch == 3)),
                               reads=[wco.b, yA.b], pwrites=[pyc.b])
                        pgc = proj(O_GC + dc * 128)
                        tf = tmpf.next()
                        op("act", ACTF(tf.t[:], pgc.f[:, 0:GB], AF.Sigmoid), reads=[pgc.b], writes=[tf.b])
                        op("dve", TT(mixA_st.t[:, dc, :], pyc.f[:, 0:GB], tf.t[:], ALU.mult), reads=[pyc.b, tf.b], pwrites=[mixA_st.b])
                    dma("sp", scr["mixA"].t.rearrange("(dc p) t -> p dc t", p=128)[:, :, t0:t0 + GB], mixA_st.t[:], reads=[mixA_st.b], pwrites=[scr["mixA"].b])
                    for dc in range(8):
                        pga = proj(O_GA + dc * 128)
                        op("act", ACTF(sga_st.t[:, dc, :], pga.f[:, 0:GB], AF.Sigmoid), reads=[pga.b], pwrites=[sga_st.b])
                    dma("sp", scr["sga"].t.rearrange("(dc p) t -> p dc t", p=128)[:, :, t0:t0 + GB], sga_st.t[:], reads=[sga_st.b], pwrites=[scr["sga"].b])
                    for (base, wap, wb, st, nm) in ((O_Q, qws.t[:, 0:1], qws.b, q_st, "qT"), (O_K, qkw.t[:, 1:2], qkw.b, k_st, "kT")):
                        for c in range(4):
                            pq = proj(base + c * 128)
                            sq = sqr.next()
                            op("act", ACTF(sq.t[:], pq.f[:, 0:GB], AF.Square), reads=[pq.b], writes=[sq.b])
                            ps2 = gen.next()
                            op("pe", MM(ps2.f[:, 0:GB], bones_bf.t[:], sq.t[:]), reads=[bones_bf.b, sq.b], pwrites=[ps2.b])
                            r1 = rtr.next()
                            op("act", ACTF(r1.t[:], ps2.f[:, 0:GB], AF.Sqrt, scale=1.0 / 64, bias=epst.t[:, 0:1]), reads=[ps2.b, epst.b], writes=[r1.b])
                            r2 = rrr.next()
                            op("dve", RCP(r2.t[:], r1.t[:]), reads=[r1.b], writes=[r2.b])
                            op("dve", STT(st.t[:, c, :], pq.f[:, 0:GB], wap, r2.t[:], ALU.mult, ALU.mult), reads=[pq.b, wb, r2.b], pwrites=[st.b])
                        dma("sp", scr[nm].t.rearrange("(c p) t -> p c t", p=128)[:, :, t0:t0 + GB], st.t[:], reads=[st.b], pwrites=[scr[nm].b])
                    for c in range(4):
                        pq = proj(O_QI + c * 128)
                        evac(qi_st.t[:, c, :], pq.f[:, 0:GB], [pq.b], [qi_st.b])
                    dma("sp", scr["qiT"].t.rearrange("(c p) t -> p c t", p=128)[:, :, t0:t0 + GB], qi_st.t[:], reads=[qi_st.b], pwrites=[scr["qiT"].b])
                    pq = proj(O_KI, M=64)
                    evac(ki_st.t[:, :], pq.f[0:64, 0:GB], [pq.b], [ki_st.b])
                    dma("sp", scr["kiT"].t[:, t0:t0 + GB], ki_st.t[:], reads=[ki_st.b], pwrites=[scr["kiT"].b])
                    for j in range(JB):
                        p = gen.next()
                        for kc in range(KC):
                            op("pe", MM(p.f[:, 0:512], hT.t[:, kc, j * 128:(j + 1) * 128], win.t[:, kc, O_V:O_V + 512], start=(kc == 0), stop=(kc == KC - 1)),
                               reads=[win.b, hT.b], pwrites=[p.b])
                        evac(v_st.t[:, j, :, 0:64], p.f[:, 0:512].rearrange("p (h d) -> p h d", h=8), [p.b], [v_st.b])
                        p = gen.next()
                        for kc in range(KC):
                            op("pe", MM(p.f[:, 0:8], hT.t[:, kc, j * 128:(j + 1) * 128], win.t[:, kc, O_WI:O_WI + 8], start=(kc == 0), stop=(kc == KC - 1)),
                               reads=[win.b, hT.b], pwrites=[p.b])
                        op("dve", CP(widx.t[:, t0 // 128 + j, :], p.f[:, 0:8]), reads=[p.b], pwrites=[widx.b])
                    dma("sp", scr["V"].t[t0:t0 + GB, :].rearrange("(j p) f -> p j f", p=128), v_st.t[:].rearrange("p j h d -> p j (h d)"),
                        reads=[v_st.b], pwrites=[scr["V"].b])
                ctx.barrier()

        if "C" in phases:
            with ExitStack() as es:
                NKB = S // 128
                QG = min(512, S)
                kTs = tile(es, "kTs", [128, 4, S], BF16)
                Vs = tile(es, "Vs", [128, NKB, 520], BF16)
                kiTs = tile(es, "kiTs", [128, S], BF16)
                qTg = Ring([tile(es, "qTg%d" % i, [128, 4, QG], BF16) for i in range(2)])
                qiTg = Ring([tile(es, "qiTg%d" % i, [128, 4, QG], BF16) for i in range(2)])
                Dh = tile(es, "Dh", [128, 8, 128], BF16)
                rr = Ring([tile(es, "relu%d" % i, [128, 512], BF16) for i in range(4)])
                score = tile(es, "score", [128, S], F32)
                junk = tile(es, "junkC", [128, S], BF16)
                bis = tile(es, "bis", [128, 8], F32)
                wf = tile(es, "wf", [128, NIT + 2], F32)
                pw2 = tile(es, "pw2", [128, NIT + 2], F32)
                for i_ in range(NIT + 2):
                    op("dve", MSET(pw2.t[:, i_:i_ + 1], 2.0 ** -i_), pwrites=[pw2.b])
                dthr = tile(es, "dthr", [128, 128], F32)
                thrbc = tile(es, "thrbc", [128, 128], F32)
                maskT = tile(es, "maskT", [128, NKB, 128], BF16)
                maskT2 = tile(es, "maskT2", [128, NKB, 128], BF16)
                PTr = Ring([tile(es, "PT%d" % i, [128, 512], BF16) for i in range(3)])
                PMr = Ring([tile(es, "PM%d" % i, [128, 512], BF16) for i in range(3)])
                rz = tile(es, "rz", [128, 8], F32)
                attn_tm = tile(es, "attn_tm", [128, 8, 64], BF16)
                attnT_r = Ring([tile(es, "attnT_st%d" % i, [128, 4, QG], BF16) for i in range(2)])
                psL = Ring([PBs[0], PBs[1]])
                psS = PBs[2]
                psT = PBs[3]
                psA = Ring([PBs[4], PBs[5]])
                psO = [PBs[6], PBs[7]]
                maskTs = [maskT, maskT2]
                blocks = [(s_, qb_) for s_ in range(NSEQ) for qb_ in range(NKB)]
                gtiles = {}
                NQ = QG // 128

                def group_tiles(s_, qg):
                    key = (s_, qg)
                    if key not in gtiles:
                        qT = qTg.next()
                        qiT = qiTg.next()
                        tq0 = s_ * S + qg * QG
                        dma("sp", qiT.t[:], scr["qiT"].t.rearrange("(c p) t -> p c t", p=128)[:, :, tq0:tq0 + QG], reads=[scr["qiT"].b], writes=[qiT.b])
                        dma("sp", qT.t[:], scr["qT"].t.rearrange("(c p) t -> p c t", p=128)[:, :, tq0:tq0 + QG], reads=[scr["qT"].b], writes=[qT.b])
                        gtiles[key] = (qT, qiT, attnT_r.next())
                    return gtiles[key]

                def stage_X(bi):
                    s_, qb = blocks[bi]
                    sl = slice(s_ * S, (s_ + 1) * S)
                    if qb == 0:
                        dma("sp", kiTs.t[0:64, :], scr["kiT"].t[:, sl], reads=[scr["kiT"].b], writes=[kiTs.b])
                        dma("sp", kiTs.t[64:128, :], scr["kiT"].t[:, sl], reads=[scr["kiT"].b], pwrites=[kiTs.b])
                    qT, qiT, _ = group_tiles(s_, qb // NQ)
                    qs = qb % NQ
                    nkb = qb + 1
                    nk = nkb * 128
                    tix = (s_ * S) // 128 + qb
                    qsl = slice(qs * 128, (qs + 1) * 128)
                    mk = maskTs[bi % 2]
                    op("dve", TT(Dh.t[:], ident_bf.t[:].unsqueeze(1).to_broadcast([128, 8, 128]),
                                 widx.t[:, tix, :].unsqueeze(2).to_broadcast([128, 8, 128]), ALU.mult),
                       reads=[ident_bf.b, widx.b], writes=[Dh.b])
                    nch = (nk + 511) // 512
                    for ch in range(nch):
                        k0 = ch * 512
                        w = min(512, nk - k0)
                        for h in range(8):
                            hp = h % 2
                            pl = psL.next()
                            op("pe", MM(pl.f[:, 0:w], qiT.t[hp * 64:(hp + 1) * 64, h // 2, qsl], kiTs.t[hp * 64:(hp + 1) * 64, k0:k0 + w]),
                               reads=[qiT.b, kiTs.b], pwrites=[pl.b])
                            r = rr.next()
                            if h % 2 == 0:
                                op("act", ACTF(r.t[:, 0:w], pl.f[:, 0:w], AF.Relu), reads=[pl.b], writes=[r.b])
                            else:
                                op("dve", TS(r.t[:, 0:w], pl.f[:, 0:w], 0.0, None, ALU.max), reads=[pl.b], writes=[r.b])
                            op("pe", MM(psS.f[:, 0:w], Dh.t[:, h, :], r.t[:, 0:w], start=(h == 0), stop=(h == 7)),
                               reads=[Dh.b, r.b], pwrites=[psS.b])
                        if ch == nch - 1:
                            wd = w - 128
                            if wd > 0:
                                op("act", ACTF(score.t[:, k0:k0 + wd], psS.f[:, 0:wd], AF.Copy), reads=[psS.b], pwrites=[score.b])
                            op("dve", TT(score.t[:, nk - 128:nk], psS.f[:, wd:w], causal_f, ALU.add), reads=[psS.b, cst.b], pwrites=[score.b])
                        else:
                            op("act", ACTF(score.t[:, k0:k0 + w], psS.f[:, 0:w], AF.Copy), reads=[psS.b], pwrites=[score.b])
                    lo, w0, mid, cnt, gg, mx = (bis.t[:, i:i + 1] for i in range(6))
                    if nk <= KSEL:
                        op("dve", MSET(lo, -1.0e29), writes=[bis.b])
                    else:
                        op("dve", RED(mx, score.t[:, 0:nk], ALU.max), reads=[score.b], writes=[bis.b])
                        op("dve", RED(lo, score.t[:, 0:nk - 128], ALU.min), reads=[score.b], writes=[bis.b])
                        op("dve", TT(w0, mx, lo, ALU.subtract), reads=[bis.b], writes=[bis.b])
                        op("dve", TS(wf.t[:], pw2.t[:], w0, None, ALU.mult), reads=[bis.b, pw2.b], writes=[wf.b])
                        op("dve", TT(mid, lo, wf.t[:, 1:2], ALU.add), reads=[bis.b, wf.b], writes=[bis.b])

                def stage_Xbis(bi, it0, it1):
                    s_, qb = blocks[bi]
                    nk = (qb + 1) * 128
                    if nk <= KSEL:
                        return
                    lo, w0, mid, cnt, gg, mx = (bis.t[:, i:i + 1] for i in range(6))
                    for it in range(it0, it1):
                        op("dve", TS(junk.t[:, 0:nk], score.t[:, 0:nk], mid, None, ALU.is_ge, ALU.add, accum_out=cnt),
                           reads=[score.b, bis.b], writes=[bis.b, junk.b])
                        op("dve", TS(gg, cnt, KSEL - 0.5, 0.5, ALU.is_ge, ALU.subtract), reads=[bis.b], writes=[bis.b])
                        op("dve", STT(mid, gg, wf.t[:, it + 1:it + 2], mid, ALU.mult, ALU.add), reads=[bis.b, wf.b], writes=[bis.b])

                def stage_Xpost(bi):
                    s_, qb = blocks[bi]
                    nkb = qb + 1
                    nk = nkb * 128
                    mk = maskTs[bi % 2]
                    lo, w0, mid, cnt, gg, mx = (bis.t[:, i:i + 1] for i in range(6))
                    if nk > KSEL:
                        op("dve", TT(lo, mid, wf.t[:, NIT + 1:NIT + 2], ALU.subtract), reads=[bis.b, wf.b], writes=[bis.b])
                    op("dve", TS(dthr.t[:], ident_f, lo, None, ALU.mult), reads=[cst.b, bis.b], writes=[dthr.b])
                    op("pe", MM(psT.f[:, 0:128], ones_f, dthr.t[:]), reads=[cst.b, dthr.b], pwrites=[psT.b])
                    op("act", ACTF(thrbc.t[:], psT.f[:, 0:128], AF.Copy), reads=[psT.b], writes=[thrbc.b])
                    for c4 in range((nkb + 3) // 4):
                        n4 = min(4, nkb - c4 * 4)
                        for i in range(n4):
                            kb = c4 * 4 + i
                            op("pe", TR(psT.f[:, i * 128:(i + 1) * 128], score.t[:, kb * 128:(kb + 1) * 128], ident_f),
                               reads=[score.b, cst.b], pwrites=[psT.b])
                        op("dve", TT(mk.t[:, c4 * 4:c4 * 4 + n4, :], psT.f[:, 0:n4 * 128].rearrange("p (a b) -> p a b", b=128),
                                     thrbc.t[:].unsqueeze(1).to_broadcast([128, n4, 128]), ALU.is_ge),
                           reads=[psT.b, thrbc.b], pwrites=[mk.b])

                def stage_Y(bi, hs, fin):
                    s_, qb = blocks[bi]
                    sl = slice(s_ * S, (s_ + 1) * S)
                    if qb == 0 and 0 in hs:
                        dma("sp", kTs.t[:], scr["kT"].t.rearrange("(c p) t -> p c t", p=128)[:, :, sl], reads=[scr["kT"].b], writes=[kTs.b])
                        dma("sp", Vs.t[:], scr["V"].t[sl, :].rearrange("(kb p) f -> p kb f", p=128), reads=[scr["V"].b], writes=[Vs.b])
                    qT, qiT, ast = group_tiles(s_, qb // NQ)
                    qs = qb % NQ
                    nkb = qb + 1
                    qsl = slice(qs * 128, (qs + 1) * 128)
                    mk = maskTs[bi % 2]
                    for h in hs:
                        hp = h % 2
                        po = psO[h // 4]
                        for c4 in range((nkb + 3) // 4):
                            n4 = min(4, nkb - c4 * 4)
                            pa = psA.next()
                            for i in range(n4):
                                kb = c4 * 4 + i
                                op("pe", MM(pa.f[:, i * 128:(i + 1) * 128], kTs.t[hp * 64:(hp + 1) * 64, h // 2, kb * 128:(kb + 1) * 128],
                                            qT.t[hp * 64:(hp + 1) * 64, h // 2, qsl]), reads=[kTs.b, qT.b], pwrites=[pa.b])
                            pt = PTr.next()
                            op("act", ACTF(pt.t[:, 0:n4 * 128], pa.f[:, 0:n4 * 128], AF.Exp), reads=[pa.b], writes=[pt.b])
                            pm = PMr.next()
                            op("dve", TT(pm.t[:, 0:n4 * 128], pt.t[:, 0:n4 * 128], mk.t[:, c4 * 4:c4 * 4 + n4, :].rearrange("p a b -> p (a b)"), ALU.mult),
                               reads=[pt.b, mk.b], writes=[pm.b])
                            for i in range(n4):
                                kb = c4 * 4 + i
                                op("pe", MM(po.f[:, (h % 4) * 65:(h % 4) * 65 + 65], pm.t[:, i * 128:(i + 1) * 128], Vs.t[:, kb, h * 65:(h + 1) * 65],
                                            start=(kb == 0), stop=(kb == nkb - 1)), reads=[pm.b, Vs.b], pwrites=[po.b])
                    if not fin:
                        return
                    for hh in range(2):
                        pv = psO[hh].f[:, 0:260].rearrange("p (h d) -> p h d", d=65)
                        op("dve", RCP(rz.t[:, hh * 4:(hh + 1) * 4], pv[:, :, 64]), reads=[psO[hh].b], pwrites=[rz.b])
                        op("dve", TT(attn_tm.t[:, hh * 4:(hh + 1) * 4, :], pv[:, :, 0:64],
                                     rz.t[:, hh * 4:(hh + 1) * 4].unsqueeze(2).to_broadcast([128, 4, 64]), ALU.mult),
                           reads=[psO[hh].b, rz.b], pwrites=[attn_tm.b])
                    px = psA.next()
                    for c in range(4):
                        op("pe", TR(px.h[:, c * 128:(c + 1) * 128], attn_tm.t[:, 2 * c:2 * c + 2, :].rearrange("p a b -> p (a b)"), ident_bf.t[:]),
                           reads=[attn_tm.b, ident_bf.b], pwrites=[px.b])
                    op("act", ACTF(ast.t[:, :, qsl], px.h[:, 0:512].rearrange("p (c t) -> p c t", c=4), AF.Copy),
                       reads=[px.b], pwrites=[ast.b])
                    if qs == NQ - 1:
                        tq0 = s_ * S + (qb // NQ) * QG
                        dma("sp", scr["attnT"].t.rearrange("(c p) t -> p c t", p=128)[:, :, tq0:tq0 + QG], ast.t[:], reads=[ast.b], pwrites=[scr["attnT"].b])

                NB = len(blocks)
                IPH = NIT // 8
                stage_X(0)
                stage_Xbis(0, 0, NIT)
                stage_Xpost(0)
                for bi in range(NB):
                    if bi + 1 < NB:
                        stage_X(bi + 1)
                    for h in range(8):
                        if bi + 1 < NB:
                            stage_Xbis(bi + 1, h * IPH, (h + 1) * IPH if h < 7 else NIT)
                        stage_Y(bi, [h], h == 7)
                    if bi + 1 < NB:
                        stage_Xpost(bi + 1)
                ctx.barrier()

        if "D" in phases:
            with ExitStack() as es:
                wao = tile(es, "wao", [128, 4, D], BF16)
                wo = tile(es, "wo", [128, KC, D], BF16)
                wpq = tile(es, "wpq", [128, KC, D], BF16)
                skTb = tile(es, "skTb", [128, 8, 128], BF16)
                with ExitStack() as es2:
                    stg = Ring([tile(es2, "stgD%d" % i, [128, KC, 512], F32) for i in range(2)])
                    ci_ = 0
                    for (src, dst) in ((io["w_o"], wo), (io["w_peer_q"], wpq)):
                        sv = src.rearrange("(kc p) f -> p kc f", p=128)
                        for hh in range(2):
                            st = stg.next()
                            dma("sp", st.t[:], sv[:, :, hh * 512:(hh + 1) * 512], writes=[st.b])
                            if ci_ % 2:
                                op("act", ACTF(dst.t[:, :, hh * 512:(hh + 1) * 512], st.t[:], AF.Copy), reads=[st.b], pwrites=[dst.b])
                            else:
                                op("dve", CP(dst.t[:, :, hh * 512:(hh + 1) * 512], st.t[:]), reads=[st.b], pwrites=[dst.b])
                            ci_ += 1
                    st = stg.next()
                    stv = st.t[:].rearrange("p k f -> p (k f)").rearrange("p (c d) -> p c d", c=4)
                    dma("sp", stv, io["w_attn_out"].rearrange("(c p) d -> p c d", p=128), writes=[st.b])
                    op("dve", CP(wao.t[:], stv), reads=[st.b], writes=[wao.b])
                    st = stg.next()
                    stv2 = st.t[:].rearrange("p k f -> p (k f)")[:, 0:1024].rearrange("p (h n) -> p h n", h=8)
                    dma("sp", stv2, io["skT"], writes=[st.b])
                    op("dve", CP(skTb.t[:], stv2), reads=[st.b], writes=[skTb.b])
                    ctx.barrier()
                gb1 = tile(es, "gb1", [128, D], F32)
                attg = Ring([tile(es, "attg%d" % i, [128, 4, GB], BF16) for i in range(2)])
                sgag = Ring([tile(es, "sgag%d" % i, [128, 8, GB], BF16) for i in range(2)])
                mxag = Ring([tile(es, "mxag%d" % i, [128, 8, GB], BF16) for i in range(2)])
                xgr = Ring([tile(es, "xgD%d" % i, [128, JB, D], F32) for i in range(2)])
                tmq = Ring([tile(es, "tmq%d" % i, [128, GB], F32) for i in range(2)])
                mixT = tile(es, "mixT", [128, 8, GB], BF16)
                tz = Ring([tile(es, "tz%d" % i, [128, 512], F32) for i in range(2)])
                x1 = tile(es, "x1", [128, JB, D], F32)
                junk = tile(es, "junkD", [128, D], BF16)
                ssq = tile(es, "ssqD", [128, JB], F32)
                rt = tile(es, "rtD", [128, JB], F32)
                rstd = tile(es, "rstdD", [128, JB], F32)
                xs = tile(es, "xsD", [128, JB, D], BF16)
                h2T = tile(es, "h2T", [128, KC, GB], BF16)
                qpT = tile(es, "qpT", [128, 8, GB], BF16)
                s_sb = tile(es, "s_sb", [128, 16, 128], F32)
                s_tmp = tile(es, "s_tmp", [128, 16, 128], F32)
                v_all = tile(es, "v_all", [128, 16, 16], F32)
                idx_all = tile(es, "idx_all", [128, 16, 16], U32)
                idx_bf = tile(es, "idx_bf", [128, 16, 16], BF16)
                cand = tile(es, "cand", [128, 8, 256], F32)
                cand2 = tile(es, "cand2", [128, 8, 256], F32)
                scv = tile(es, "scv", [128, 8, 16], F32)
                civ = tile(es, "civ", [128, 8, 16], U32)
                ca_u = tile(es, "ca_u", [128, 8, 16], U32)
                cb_u = tile(es, "cb_u", [128, 8, 16], U32)
                ca_bf = tile(es, "ca_bf", [128, 8, 16], BF16)
                cb_bf = tile(es, "cb_bf", [128, 8, 16], BF16)
                eqa = tile(es, "eqa", [128, 8, 16, 16], BF16)
                prd = tile(es, "prd", [128, 8, 16, 16], BF16)
                sm = tile(es, "sm", [128, 8, 16], F32)
                zz = tile(es, "zz", [128, 8], F32)
                sel_tm = tile(es, "sel_tm", [128, 3, 128], F32)
                selT = tile(es, "selT", [128, 3, 128], BF16)
                ptr = Ring([PBs[0], PBs[1]])
                gen = Ring([PBs[i] for i in range(2, 8)])
                evi = [0]

                def evacD(out, in_, rd, wr):
                    evi[0] += 1
                    if evi[0] % 2:
                        op("act", ACTF(out, in_, AF.Copy), reads=rd, pwrites=wr)
                    else:
                        op("dve", CP(out, in_), reads=rd, pwrites=wr)

                for g in range(T // GB):
                    t0 = g * GB
                    seq = t0 // S
                    if t0 % S == 0:
                        dma("sp", gb1.t[:], scr["gbc"].t[seq, 0, :, :], reads=[scr["gbc"].b], writes=[gb1.b])
                    at = attg.next()
                    sg = sgag.next()
                    ma = mxag.next()
                    xg = xgr.next()
                    dma("sp", at.t[:], scr["attnT"].t.rearrange("(c p) t -> p c t", p=128)[:, :, t0:t0 + GB], reads=[scr["attnT"].b], writes=[at.b])
                    dma("sp", sg.t[:], scr["sga"].t.rearrange("(c p) t -> p c t", p=128)[:, :, t0:t0 + GB], reads=[scr["sga"].b], writes=[sg.b])
                    dma("sp", ma.t[:], scr["mixA"].t.rearrange("(c p) t -> p c t", p=128)[:, :, t0:t0 + GB], reads=[scr["mixA"].b], writes=[ma.b])
                    dma("sp", xg.t[:], io["x"][t0:t0 + GB, :].rearrange("(j p) d -> p j d", p=128), writes=[xg.b])
                    for dc in range(8):
                        p = gen.next()
                        for c in range(4):
                            op("pe", MM(p.f[:, 0:GB], wao.t[:, c, dc * 128:(dc + 1) * 128], at.t[:, c, :], start=(c == 0), stop=(c == 3)),
                               reads=[wao.b, at.b], pwrites=[p.b])
                        tq = tmq.next()
                        op("dve", TT(tq.t[:], p.f[:, 0:GB], sg.t[:, dc, :], ALU.mult), reads=[p.b, sg.b], writes=[tq.b])
                        op("dve", TT(mixT.t[:, dc, :], tq.t[:], ma.t[:, dc, :], ALU.add), reads=[tq.b, ma.b], pwrites=[mixT.b])
                    for j in range(JB):
                        for dh in range(2):
                            p = gen.next()
                            for kc in range(KC):
                                op("pe", MM(p.f[:, 0:512], mixT.t[:, kc, j * 128:(j + 1) * 128], wo.t[:, kc, dh * 512:(dh + 1) * 512],
                                            start=(kc == 0), stop=(kc == KC - 1)), reads=[mixT.b, wo.b], pwrites=[p.b])
                            t_ = tz.next()
                            op("dve", TT(t_.t[:], p.f[:, 0:512], gb1.t[:, dh * 512:(dh + 1) * 512], ALU.mult), reads=[p.b, gb1.b], writes=[t_.b])
                            op("dve", TT(x1.t[:, j, dh * 512:(dh + 1) * 512], t_.t[:], xg.t[:, j, dh * 512:(dh + 1) * 512], ALU.add),
                               reads=[t_.b, xg.b], pwrites=[x1.b])
                    dma("sp", scr["x1"].t[t0:t0 + GB, :].rearrange("(j p) d -> p j d", p=128), x1.t[:], reads=[x1.b], pwrites=[scr["x1"].b])
                    for j in range(JB):
                        op("act", ACTF(junk.t[:], x1.t[:, j, :], AF.Square, accum_out=ssq.t[:, j:j + 1]), reads=[x1.b], writes=[junk.b], pwrites=[ssq.b])
                    op("act", ACTF(rt.t[:], ssq.t[:], AF.Sqrt, scale=1.0 / D, bias=epst.t[:, 0:1]), reads=[ssq.b, epst.b], writes=[rt.b])
                    op("dve", RCP(rstd.t[:], rt.t[:]), reads=[rt.b], writes=[rstd.b])
                    for j in range(JB):
                        op("dve", TS(xs.t[:, j, :], x1.t[:, j, :], rstd.t[:, j:j + 1], None, ALU.mult), reads=[x1.b, rstd.b], pwrites=[xs.b])
                    for kc in range(KC):
                        pt = ptr.next()
                        for j in range(JB):
                            op("pe", TR(pt.h[:, j * 128:(j + 1) * 128], xs.t[:, j, kc * 128:(kc + 1) * 128], ident_bf.t[:]),
                               reads=[xs.b, ident_bf.b], pwrites=[pt.b])
                        op("act", ACTF(h2T.t[:, kc, :], pt.h[:, 0:GB], AF.Identity, scale=A2.t[:, kc, seq:seq + 1], bias=modTb.t[:, 24 + kc, seq:seq + 1]),
                           reads=[pt.b, A2.b, modTb.b], pwrites=[h2T.b])
                    dma("sp", scr["h2T"].t.rearrange("(c p) t -> p c t", p=128)[:, :, t0:t0 + GB], h2T.t[:], reads=[h2T.b], pwrites=[scr["h2T"].b])
                    for h in range(8):
                        p = gen.next()
                        for kc in range(KC):
                            op("pe", MM(p.f[:, 0:GB], wpq.t[:, kc, h * 128:(h + 1) * 128], h2T.t[:, kc, :], start=(kc == 0), stop=(kc == KC - 1)),
                               reads=[wpq.b, h2T.b], pwrites=[p.b])
                        evacD(qpT.t[:, h, :], p.f[:, 0:GB], [p.b], [qpT.b])
                    for j in range(JB):
                        tt0 = t0 + j * 128
                        s4 = s_sb.t[:].rearrange("p (h s) n -> p h s n", s=2)
                        for b4 in range(4):
                            p = gen.next()
                            side, hb = b4 % 2, (b4 // 2) * 4
                            for i in range(4):
                                h = hb + i
                                op("pe", MM(p.f[:, i * 128:(i + 1) * 128], qpT.t[side * 64:(side + 1) * 64, h, j * 128:(j + 1) * 128],
                                            skTb.t[side * 64:(side + 1) * 64, h, :]), reads=[qpT.b, skTb.b], pwrites=[p.b])
                            op("act", ACTF(s4[:, hb:hb + 4, side, :], p.f[:, :].rearrange("p (a b) -> p a b", b=128), AF.Copy),
                               reads=[p.b], pwrites=[s_sb.b])
                        if DCUT < 2:
                            continue
                        for r in range(16):
                            op("dve", lambda e, r=r: e.max(out=v_all.t[:, r, 0:8], in_=s_sb.t[:, r, :]), reads=[s_sb.b], pwrites=[v_all.b])
                            op("dve", lambda e, r=r: e.max_index(out=idx_all.t[:, r, 0:8], in_max=v_all.t[:, r, 0:8], in_values=s_sb.t[:, r, :]),
                               reads=[s_sb.b, v_all.b], pwrites=[idx_all.b])
                            op("dve", lambda e, r=r: e.match_replace(out=s_tmp.t[:, r, :], in_to_replace=v_all.t[:, r, 0:8], in_values=s_sb.t[:, r, :], imm_value=NEG),
                               reads=[s_sb.b, v_all.b], pwrites=[s_tmp.b])
                            op("dve", lambda e, r=r: e.max(out=v_all.t[:, r, 8:16], in_=s_tmp.t[:, r, :]), reads=[s_tmp.b], pwrites=[v_all.b])
                            op("dve", lambda e, r=r: e.max_index(out=idx_all.t[:, r, 8:16], in_max=v_all.t[:, r, 8:16], in_values=s_tmp.t[:, r, :]),
                               reads=[s_tmp.b, v_all.b], pwrites=[idx_all.b])
                        if DCUT < 3:
                            continue
                        op("dve", CP(idx_bf.t[:], idx_all.t[:]), reads=[idx_all.b], writes=[idx_bf.b])
                        v4 = v_all.t[:].rearrange("p (h s) k -> p h s k", s=2)
                        op("dve", TT(cand.t[:].rearrange("p h (a b) -> p h a b", b=16), v4[:, :, 0, :].unsqueeze(3).to_broadcast([128, 8, 16, 16]),
                                     v4[:, :, 1, :].unsqueeze(2).to_broadcast([128, 8, 16, 16]), ALU.add), reads=[v_all.b], writes=[cand.b])
                        for h in range(8):
                            op("dve", lambda e, h=h: e.max(out=scv.t[:, h, 0:8], in_=cand.t[:, h, :]), reads=[cand.b], pwrites=[scv.b])
                            op("dve", lambda e, h=h: e.max_index(out=civ.t[:, h, 0:8], in_max=scv.t[:, h, 0:8], in_values=cand.t[:, h, :]),
                               reads=[cand.b, scv.b], pwrites=[civ.b])
                            op("dve", lambda e, h=h: e.match_replace(out=cand2.t[:, h, :], in_to_replace=scv.t[:, h, 0:8], in_values=cand.t[:, h, :], imm_value=NEG),
                               reads=[cand.b, scv.b], pwrites=[cand2.b])
                            op("dve", lambda e, h=h: e.max(out=scv.t[:, h, 8:16], in_=cand2.t[:, h, :]), reads=[cand2.b], pwrites=[scv.b])
                            op("dve", lambda e, h=h: e.max_index(out=civ.t[:, h, 8:16], in_max=scv.t[:, h, 8:16], in_values=cand2.t[:, h, :]),
                               reads=[cand2.b, scv.b], pwrites=[civ.b])
                        if DCUT < 4:
                            continue
                        op("dve", TT(sm.t[:], scv.t[:], scv.t[:, :, 0:1].to_broadcast([128, 8, 16]), ALU.subtract), reads=[scv.b], writes=[sm.b])
                        op("act", ACTF(sm.t[:], sm.t[:], AF.Exp), reads=[sm.b], writes=[sm.b])
                        op("dve", RED(zz.t[:], sm.t[:], ALU.add), reads=[sm.b], writes=[zz.b])
                        op("dve", RCP(zz.t[:], zz.t[:]), reads=[zz.b], writes=[zz.b])
                        op("dve", TT(sel_tm.t[:, 2, :].rearrange("p (h k) -> p h k", k=16), sm.t[:], zz.t[:].unsqueeze(2).to_broadcast([128, 8, 16]), ALU.mult),
                           reads=[sm.b, zz.b], pwrites=[sel_tm.b])
                        if DCUT < 5:
                            continue
                        op("dve", TS(ca_u.t[:], civ.t[:], 4, None, ALU.logical_shift_right), reads=[civ.b], writes=[ca_u.b])
                        op("dve", TS(cb_u.t[:], civ.t[:], 15, None, ALU.bitwise_and), reads=[civ.b], writes=[cb_u.b])
                        op("dve", CP(ca_bf.t[:], ca_u.t[:]), reads=[ca_u.b], writes=[ca_bf.b])
                        op("dve", CP(cb_bf.t[:], cb_u.t[:]), reads=[cb_u.b], writes=[cb_bf.b])
                        if DCUT < 6:
                            continue
                        i4 = idx_bf.t[:].rearrange("p (h s) k -> p h s k", s=2)
                        for side, cbf in ((0, ca_bf), (1, cb_bf)):
                            op("dve", TT(eqa.t[:], cbf.t[:].unsqueeze(3).to_broadcast([128, 8, 16, 16]),
                                         iota_bf.t[:, 0:16].unsqueeze(1).unsqueeze(1).to_broadcast([128, 8, 16, 16]), ALU.is_equal),
                               reads=[cbf.b, iota_bf.b], writes=[eqa.b])
                            op("dve", TT(prd.t[:], eqa.t[:], i4[:, :, side, :].unsqueeze(2).to_broadcast([128, 8, 16, 16]), ALU.mult),
                               reads=[eqa.b, idx_bf.b], writes=[prd.b])
                            op("dve", RED(sel_tm.t[:, side, :], prd.t[:].rearrange("p h k a -> p (h k) a"), ALU.add), reads=[prd.b], pwrites=[sel_tm.b])
                        if DCUT < 7:
                            continue
                        pt = ptr.next()
                        for c in range(3):
                            op("pe", TR(pt.f[:, c * 128:(c + 1) * 128], sel_tm.t[:, c, :], ident_f), reads=[sel_tm.b, cst.b], pwrites=[pt.b])
                        op("act", ACTF(selT.t[:], pt.f[:, 0:384].rearrange("p (c t) -> p c t", c=3), AF.Copy), reads=[pt.b], writes=[selT.b])
                        dma("sp", scr["sel"].t.rearrange("c p t -> p c t")[:, :, tt0:tt0 + 128], selT.t[:], reads=[selT.b], pwrites=[scr["sel"].b])
                ctx.barrier()

        if "E" in phases:
            with ExitStack() as es:
                sf = Ring([tile(es, "pf%d" % i, [128, 4096], F32) for i in range(3)])
                sbf = Ring([tile(es, "pb%d" % i, [128, 4096], BF16) for i in range(3)])
                uv = io["uT"].rearrange("(kc p) e -> p kc e", p=128)
                uo = scr["uTb"].t.rearrange("(kc p) e -> p kc e", p=128)
                vv = io["vp"].rearrange("(jj p) d -> p jj d", p=128)
                vo = scr["vb"].t.rearrange("(jj p) d -> p jj d", p=128)
                n = 0
                for i in range(NEXP // 512):
                    for (src, dst, key, v3) in ((uv[:, :, i * 512:(i + 1) * 512], uo[:, :, i * 512:(i + 1) * 512], "uTb", "p (a b) -> p a b"),
                                                (vv[:, i * 4:(i + 1) * 4, :], vo[:, i * 4:(i + 1) * 4, :], "vb", "p (a b) -> p a b")):
                        a_ = 8 if key == "uTb" else 4
                        f_ = sf.next()
                        b_ = sbf.next()
                        dma("sp", f_.t[:].rearrange(v3, a=a_), src, writes=[f_.b])
                        eng = ("dve", "act", "pool")[n % 3]
                        n += 1
                        if eng == "act":
                            op("act", ACTF(b_.t[:], f_.t[:], AF.Copy), reads=[f_.b], writes=[b_.b])
                        else:
                            op(eng, CP(b_.t[:], f_.t[:]), reads=[f_.b], writes=[b_.b])
                        dma("sp", dst, b_.t[:].rearrange(v3, a=a_), reads=[b_.b], pwrites=[scr[key].b])
                ctx.barrier()

        if "E" in phases:
            with ExitStack() as es:
                TE = 256
                SC = 2
                gb2 = tile(es, "gb2", [128, D], F32)
                x1r = Ring([tile(es, "x1E%d" % i, [128, 2, D], F32) for i in range(2)])
                h2r = Ring([tile(es, "h2E%d" % i, [128, KC, TE], BF16) for i in range(2)])
                selr = Ring([tile(es, "selE%d" % i, [128, 3, TE], BF16) for i in range(2)])
                Lr = Ring([tile(es, "L%d" % i, [128, 32, 128], BF16) for i in range(2)])
                L0r = Ring([tile(es, "L0%d" % i, [128, 32, 128], BF16) for i in range(1)])
                Rr = Ring([tile(es, "R%d" % i, [128, 32, 128], BF16) for i in range(2)])
                G = tile(es, "G", [128, TE, 128], BF16)
                uTr = Ring([tile(es, "uTs%d" % i, [128, KC, SC * 128], BF16) for i in range(3)])
                vr = Ring([tile(es, "vs%d" % i, [128, SC, D], BF16) for i in range(3)])
                Wr = Ring([tile(es, "W%d" % i, [128, TE], BF16) for i in range(3)])
                W2r = Ring([tile(es, "W2%d" % i, [128, TE], BF16) for i in range(3)])
                tz = Ring([tile(es, "tzE%d" % i, [128, 512], F32) for i in range(2)])
                yst = tile(es, "yst", [128, 2, D], F32)
                psOut = [[PBs[0], PBs[1]], [PBs[2], PBs[3]]]
                psAT = Ring([PBs[4], PBs[5]])
                psG = Ring([PBs[6], PBs[7]])
                uo = scr["uTb"].t.rearrange("(kc p) e -> p kc e", p=128)
                vo = scr["vb"].t.rearrange("(jj p) d -> p jj d", p=128)
                for tl in range(T // TE):
                    t0 = tl * TE
                    seq = t0 // S
                    if t0 % S == 0:
                        dma("sp", gb2.t[:], scr["gbc"].t[seq, 1, :, :], reads=[scr["gbc"].b], writes=[gb2.b])
                    x1t = x1r.next()
                    h2 = h2r.next()
                    sl_ = selr.next()
                    dma("sp", x1t.t[:], scr["x1"].t[t0:t0 + TE, :].rearrange("(j p) d -> p j d", p=128), reads=[scr["x1"].b], writes=[x1t.b])
                    dma("sp", h2.t[:], scr["h2T"].t.rearrange("(c p) t -> p c t", p=128)[:, :, t0:t0 + TE], reads=[scr["h2T"].b], writes=[h2.b])
                    dma("sp", sl_.t[:], scr["sel"].t.rearrange("c p t -> p c t")[:, :, t0:t0 + TE], reads=[scr["sel"].b], writes=[sl_.b])
                    for sb in range(TE // 32):
                        ts_ = slice(sb * 32, (sb + 1) * 32)
                        L0 = L0r.next()
                        L = Lr.next()
                        Rt = Rr.next()
                        iob = iota_bf.t[:].unsqueeze(1).to_broadcast([128, 32, 128])
                        op("dve", TT(L0.t[:], iob, sl_.t[:, 0, ts_].unsqueeze(2).to_broadcast([128, 32, 128]), ALU.is_equal),
                           reads=[iota_bf.b, sl_.b], writes=[L0.b])
                        op("dve", TT(L.t[:], L0.t[:], sl_.t[:, 2, ts_].unsqueeze(2).to_broadcast([128, 32, 128]), ALU.mult),
                           reads=[L0.b, sl_.b], writes=[L.b])
                        op("dve", TT(Rt.t[:], iob, sl_.t[:, 1, ts_].unsqueeze(2).to_broadcast([128, 32, 128]), ALU.is_equal),
                           reads=[iota_bf.b, sl_.b], writes=[Rt.b])
                        for q4 in range(8):
                            pg = psG.next()
                            for i in range(4):
                                t = q4 * 4 + i
                                op("pe", MM(pg.f[:, i * 128:(i + 1) * 128], L.t[:, t, :], Rt.t[:, t, :]), reads=[L.b, Rt.b], pwrites=[pg.b])
                            tt = sb * 32 + q4 * 4
                            op("act", ACTF(G.t[:, tt:tt + 4, :], pg.f[:, :].rearrange("p (a b) -> p a b", b=128), AF.Copy), reads=[pg.b], pwrites=[G.b])
                    chunks = {}

                    def load_sc(sc_):
                        uT = uTr.next()
                        vs = vr.next()
                        dma("sp", uT.t[:], uo[:, :, sc_ * SC * 128:(sc_ + 1) * SC * 128], reads=[scr["uTb"].b], writes=[uT.b])
                        dma("sp", vs.t[:], vo[:, sc_ * SC:(sc_ + 1) * SC, :], reads=[scr["vb"].b], writes=[vs.b])
                        for jj in range(SC):
                            chunks[sc_ * SC + jj] = (uT, vs, jj)

                    def stage1(j):
                        if j % SC == 0:
                            load_sc(j // SC)
                        uT, vs, jj = chunks[j]
                        pa = psAT.next()
                        for kc in range(KC):
                            op("pe", MM(pa.f[:, 0:TE], uT.t[:, kc, jj * 128:(jj + 1) * 128], h2.t[:, kc, :], start=(kc == 0), stop=(kc == KC - 1)),
                               reads=[uT.b, h2.b], pwrites=[pa.b])
                        W = Wr.next()
                        op("act", ACTF(W.t[:], pa.f[:, 0:TE], AF.Gelu), reads=[pa.b], writes=[W.b])
                        W2 = W2r.next()
                        op("dve", TT(W2.t[:], W.t[:], G.t[:, :, j], ALU.mult), reads=[W.b, G.b], writes=[W2.b])
                        return W2

                    def stage2(j, W2):
                        uT, vs, jj = chunks.pop(j)
                        for tb in range(2):
                            for dh in range(2):
                                po = psOut[tb][dh]
                                op("pe", MM(po.f[:, :], W2.t[:, tb * 128:(tb + 1) * 128], vs.t[:, jj, dh * 512:(dh + 1) * 512],
                                            start=(j == 0), stop=(j == 127)), reads=[W2.b, vs.b], pwrites=[po.b])

                    pend = stage1(0)
                    for j in range(128):
                        nxt = stage1(j + 1) if j + 1 < 128 else None
                        stage2(j, pend)
                        pend = nxt
                    for tb in range(2):
                        for dh in range(2):
                            po = psOut[tb][dh]
                            t_ = tz.next()
                            op("dve", TT(t_.t[:], po.f[:, :], gb2.t[:, dh * 512:(dh + 1) * 512], ALU.mult), reads=[po.b, gb2.b], writes=[t_.b])
                            op("dve", TT(yst.t[:, tb, dh * 512:(dh + 1) * 512], t_.t[:], x1t.t[:, tb, dh * 512:(dh + 1) * 512], ALU.add),
                               reads=[t_.b, x1t.b], pwrites=[yst.b])
                    dma("sp", y[t0:t0 + TE, :].rearrange("(j p) d -> p j d", p=128), yst.t[:], reads=[yst.b])
                ctx.barrier()

        ctx.finish()
    return nc, ctx


def host_consts():
    c = np.zeros((128, 5, 128), np.float32)
    c[:, 0, :] = np.eye(128, dtype=np.float32)
    qi = np.arange(128)[:, None]
    ki = np.arange(128)[None, :]
    c[:, 1, :] = np.where(ki <= qi, 0.0, NEG).astype(np.float32)
    c[:, 2, :] = ((qi // 64) == (ki // 64)).astype(np.float32)
    c[:, 3, :] = np.broadcast_to(np.arange(128, dtype=np.float32)[None, :], (128, 128))
    c[:, 4, :] = 1.0
    return c


def host_shared(inp):
    f = lambda a: np.ascontiguousarray(np.asarray(a, dtype=np.float32))
    sh = {}
    sh["w_ada"] = f(inp["w_ada"][0])
    sh["b_ada"] = f(inp["b_ada"][0])
    sh["b_adaT"] = f(np.asarray(inp["b_ada"][0]).reshape(48, 128).T)
    sh["n1T"] = f(np.asarray(inp["norm1_w"][0]).reshape(KC, 128).T)
    sh["n2T"] = f(np.asarray(inp["norm2_w"][0]).reshape(KC, 128).T)
    sh["w_in"] = f(inp["w_in"][0])
    sh["convT"] = f(np.asarray(inp["conv_w"][0]).reshape(3, 4, 128).transpose(2, 1, 0))
    sh["w_conv_out"] = f(inp["w_conv_out"][0])
    sh["qk_w"] = f(np.stack([np.tile(np.asarray(inp["q_norm_w"][0]), 2), np.tile(np.asarray(inp["k_norm_w"][0]), 2)], axis=1))
    sh["w_attn_out"] = f(inp["w_attn_out"][0])
    sh["w_o"] = f(inp["w_o"][0])
    sh["w_peer_q"] = f(inp["w_peer_q"][0])
    sk = np.asarray(inp["peer_sub_keys"][0])
    sh["skT"] = f(sk.transpose(1, 3, 0, 2).reshape(128, 8, 128))
    u = np.asarray(inp["peer_u"][0]).reshape(128, 128, D).transpose(1, 0, 2).reshape(NEXP, D)
    sh["uT"] = f(u.T)
    sh["vp"] = f(np.asarray(inp["peer_v"][0]).reshape(128, 128, D).transpose(1, 0, 2).reshape(NEXP, D))
    sh["consts"] = host_consts()
    return sh


def kernel(**inp):
    x = np.asarray(inp["x"], dtype=np.float32)
    c = np.asarray(inp["c"], dtype=np.float32)
    B, S, _ = x.shape
    ncores = 8
    NSEQ = B // ncores
    nc, _ = build(NSEQ, S)
    sh = host_shared(inp)
    in_maps = []
    for i in range(ncores):
        m = dict(sh)
        m["x"] = np.ascontiguousarray(x[i * NSEQ:(i + 1) * NSEQ].reshape(NSEQ * S, D))
        m["cT"] = np.ascontiguousarray(c[i * NSEQ:(i + 1) * NSEQ].reshape(NSEQ, KC, 128).transpose(2, 1, 0))
        in_maps.append(m)
    res = run_bass_kernel_spmd(nc, in_maps, core_ids=list(range(ncores)))
    out = np.concatenate([np.asarray(r["y"]).reshape(NSEQ, S, D) for r in res.results], axis=0)
    return out.astype(np.float32)
```

```python
import math
from contextlib import ExitStack

import numpy as np
import concourse.bass as bass
import concourse.mybir as mybir
from concourse.bass_utils import run_bass_kernel_spmd

F32 = mybir.dt.float32
BF16 = mybir.dt.bfloat16
U32 = mybir.dt.uint32
AF = mybir.ActivationFunctionType
ALU = mybir.AluOpType
AX = mybir.AxisListType

D = 1024
KC = 8
NIN = 5704
O_CB, O_CC, O_CX, O_Q, O_K, O_V, O_QI, O_KI, O_WI, O_GC, O_GA = 0, 512, 1024, 1536, 2048, 2560, 3072, 3584, 3648, 3656, 4680
NEXP = 16384
EPS = 1e-6
NEG = -1.0e30
NIT = 16
DCUT = 99
SELF_SYNC = True


class Buf:
    __slots__ = ("w", "r")

    def __init__(self):
        self.w = {}
        self.r = {}


class Tl:
    __slots__ = ("t", "b")

    def __init__(self, t):
        self.t = t
        self.b = Buf()


class Ring:
    def __init__(self, items):
        self.items = items
        self.i = 0

    def next(self):
        it = self.items[self.i % len(self.items)]
        self.i += 1
        return it


class Ctx:
    COMPUTE = ("pe", "act", "dve", "pool")
    ALL = ("pe", "act", "dve", "pool", "sp")
    NL = 8

    def __init__(self, nc):
        self.nc = nc
        self.prog = {n: [] for n in self.ALL}
        self.sems = {}
        self.cnt = {}
        self.known = {n: {} for n in self.ALL}
        for n in self.COMPUTE:
            self.sems[n] = nc.alloc_semaphore(name="s_" + n)
            self.cnt[n] = 0
        self.lane_rr = {}
        for q in ("sp", "act", "pool"):
            self.lane_rr[q] = 0
            for l in range(self.NL):
                k = (q, l)
                self.sems[k] = nc.alloc_semaphore(name="d_%s%d" % (q, l))
                self.cnt[k] = 0
        self.ninstr = 0

    def _wait(self, eng, key, val):
        if val <= 0:
            return
        if key == eng and (eng == "pe" or not SELF_SYNC):
            return
        if self.known[eng].get(key, 0) < val:
            self.prog[eng].append(("w", key, val))
            self.known[eng][key] = val

    def _deps(self, eng, reads, writes, pwrites):
        deps = {}
        for b in reads:
            for k, v in b.w.items():
                if deps.get(k, 0) < v:
                    deps[k] = v
        for b in writes:
            for dct in (b.w, b.r):
                for k, v in dct.items():
                    if deps.get(k, 0) < v:
                        deps[k] = v
        for b in pwrites:
            for k, v in b.r.items():
                if deps.get(k, 0) < v:
                    deps[k] = v
            for k, v in b.w.items():
                if k != eng and deps.get(k, 0) < v:
                    deps[k] = v
        for k, v in deps.items():
            self._wait(eng, k, v)

    def _mark(self, key, n, reads, writes, pwrites):
        for b in reads:
            b.r[key] = n
        for b in writes:
            b.w = {key: n}
            b.r = {}
        for b in pwrites:
            b.w[key] = n

    def op(self, eng, fn, reads=(), writes=(), pwrites=()):
        self._deps(eng, reads, writes, pwrites)
        self.cnt[eng] += 1
        self.prog[eng].append(("o", fn))
        self._mark(eng, self.cnt[eng], reads, writes, pwrites)
        self.ninstr += 1

    def dma(self, q, out, in_, reads=(), writes=(), pwrites=(), slow=False):
        l = self.lane_rr[q] % self.NL
        self.lane_rr[q] += 1
        key = (q, l)
        self._wait(q, key, self.cnt[key])
        self._deps(q, reads, writes, pwrites)
        self.cnt[key] += 16
        self.prog[q].append(("d", out, in_, key, slow))
        self._mark(key, self.cnt[key], reads, writes, pwrites)
        self.ninstr += 1

    def barrier(self):
        for e in self.ALL:
            for k in self.sems:
                self._wait(e, k, self.cnt[k])

    def finish(self):
        for k in self.sems:
            self._wait("sp", k, self.cnt[k])
        nc = self.nc

        def mk(name):
            def body(e):
                for it in self.prog[name]:
                    if it[0] == "w":
                        e.wait_ge(self.sems[it[1]], it[2])
                    elif it[0] == "o":
                        it[1](e).then_inc(self.sems[name], 1)
                    else:
                        if it[4]:
                            e.dma_start(out=it[1], in_=it[2], allow_slow_non_contiguous=True).then_inc(self.sems[it[3]], 16)
                        else:
                            e.dma_start(out=it[1], in_=it[2]).then_inc(self.sems[it[3]], 16)
            return body

        with nc.Block() as block:
            block.tensor(mk("pe"))
            block.scalar(mk("act"))
            block.vector(mk("dve"))
            block.gpsimd(mk("pool"))
            block.sync(mk("sp"))


def ACTF(out, in_, func, **kw):
    return lambda e: e.activation(out=out, in_=in_, func=func, **kw)


def MM(out, lhsT, rhs, start=True, stop=True):
    return lambda e: e.matmul(out, lhsT, rhs, start=start, stop=stop)


def TR(out, in_, ident):
    return lambda e: e.transpose(out, in_, ident)


def TS(out, in0, s1, s2, op0, op1=None, accum_out=None):
    if op1 is None:
        return lambda e: e.tensor_scalar(out=out, in0=in0, scalar1=s1, scalar2=s2, op0=op0, accum_out=accum_out)
    return lambda e: e.tensor_scalar(out=out, in0=in0, scalar1=s1, scalar2=s2, op0=op0, op1=op1, accum_out=accum_out)


def TT(out, in0, in1, op):
    return lambda e: e.tensor_tensor(out=out, in0=in0, in1=in1, op=op)


def STT(out, in0, scalar, in1, op0, op1):
    return lambda e: e.scalar_tensor_tensor(out=out, in0=in0, scalar=scalar, in1=in1, op0=op0, op1=op1)


def CP(out, in_):
    return lambda e: e.tensor_copy(out=out, in_=in_)


def RED(out, in_, op):
    return lambda e: e.tensor_reduce(out=out, in_=in_, axis=AX.X, op=op)


def RCP(out, in_):
    return lambda e: e.reciprocal(out=out, in_=in_)


def MSET(ap, c):
    return lambda e: e.memset(ap, c)


def build(NSEQ, S, GB=256, dbg=False, phases="ABCDE"):
    T = NSEQ * S
    NT = T // 128
    KSEL = min(256, S // 4)
    JB = GB // 128
    nc = bass.Bass("TRN2", target_bir_lowering=False)
    ctx = Ctx(nc)
    op, dma = ctx.op, ctx.dma

    def din(name, shape, dt=F32):
        return nc.dram_tensor(name, list(shape), dt, kind="ExternalInput").ap()

    io = {
        "x": din("x", [T, D]), "cT": din("cT", [128, KC, NSEQ]), "w_ada": din("w_ada", [D, 6 * D]),
        "b_ada": din("b_ada", [6 * D]), "b_adaT": din("b_adaT", [128, 48]),
        "n1T": din("n1T", [128, KC]), "n2T": din("n2T", [128, KC]), "w_in": din("w_in", [D, NIN]),
        "convT": din("convT", [128, 4, 3]), "w_conv_out": din("w_conv_out", [512, D]),
        "qk_w": din("qk_w", [128, 2]), "w_attn_out": din("w_attn_out", [512, D]), "w_o": din("w_o", [D, D]),
        "w_peer_q": din("w_peer_q", [D, D]), "skT": din("skT", [128, 8, 128]),
        "uT": din("uT", [D, NEXP]), "vp": din("vp", [NEXP, D]), "consts": din("consts", [128, 5, 128]),
    }
    y = nc.dram_tensor("y", [T, D], F32, kind="ExternalOutput").ap()
    skind = "ExternalOutput" if dbg else "Internal"

    def dscr(name, shape, dt):
        t = nc.dram_tensor(name, list(shape), dt, kind=skind).ap()
        return Tl(t)

    scr = {
        "mixA": dscr("s_mixA", [D, T], BF16), "sga": dscr("s_sga", [D, T], BF16),
        "qT": dscr("s_qT", [512, T], BF16), "kT": dscr("s_kT", [512, T], BF16),
        "qiT": dscr("s_qiT", [512, T], BF16), "kiT": dscr("s_kiT", [64, T], BF16),
        "V": dscr("s_V", [T, 520], BF16), "attnT": dscr("s_attnT", [512, T], BF16),
        "gbc": dscr("s_gbc", [NSEQ, 2, 128, D], F32), "x1": dscr("s_x1", [T, D], F32),
        "h2T": dscr("s_h2T", [D, T], BF16), "sel": dscr("s_sel", [3, 128, T], BF16),
        "uTb": dscr("s_uTb", [D, NEXP], BF16), "vb": dscr("s_vb", [NEXP, D], BF16),
    }

    with ExitStack() as gs:
        def tile(es, name, shape, dt):
            return Tl(es.enter_context(nc.sbuf_tensor("sb_" + name, list(shape), dt)))

        psum = gs.enter_context(nc.psum_tensor("psum", [128, 8, 512], F32))
        pb = [Buf() for _ in range(8)]

        class PB:
            def __init__(self, i):
                self.i = i
                self.b = pb[i]
                self.f = psum[:, i, :]
                self.h = psum[:, i, :].bitcast(BF16)

        PBs = [PB(i) for i in range(8)]

        cst = tile(gs, "cst", [128, 5, 128], F32)
        ident_bf = tile(gs, "ident_bf", [128, 128], BF16)
        bones_bf = tile(gs, "bones_bf", [128, 128], BF16)
        iota_bf = tile(gs, "iota_bf", [128, 128], BF16)
        epst = tile(gs, "epst", [128, 1], F32)
        modTb = tile(gs, "modTb", [128, 48, NSEQ], F32)
        A1 = tile(gs, "A1", [128, KC, NSEQ], F32)
        A2 = tile(gs, "A2", [128, KC, NSEQ], F32)
        widx = tile(gs, "widx", [128, NT, 8], F32)
        qkw = tile(gs, "qkw", [128, 2], F32)
        qws = tile(gs, "qws", [128, 1], F32)
        ident_f = cst.t[:, 0, :]
        causal_f = cst.t[:, 1, :]
        ones_f = cst.t[:, 4, :]

        dma("sp", cst.t[:], io["consts"], writes=[cst.b])
        dma("sp", qkw.t[:], io["qk_w"], writes=[qkw.b])
        op("dve", CP(ident_bf.t[:], cst.t[:, 0, :]), reads=[cst.b], writes=[ident_bf.b])
        op("dve", CP(bones_bf.t[:], cst.t[:, 2, :]), reads=[cst.b], writes=[bones_bf.b])
        op("dve", CP(iota_bf.t[:], cst.t[:, 3, :]), reads=[cst.b], writes=[iota_bf.b])
        op("dve", MSET(epst.t[:], EPS), writes=[epst.b])
        op("dve", TS(qws.t[:], qkw.t[:, 0:1], 0.125, None, ALU.mult), reads=[qkw.b], writes=[qws.b])

        if "A" in phases:
            with ExitStack() as es:
                cT = tile(es, "cT", [128, KC, NSEQ], F32)
                sc = tile(es, "sc", [128, KC, NSEQ], F32)
                scb = tile(es, "scb", [128, KC * NSEQ, 128], F32)
                badaT = tile(es, "badaT", [128, 48], F32)
                bbc = tile(es, "bbc", [128, 2, D], F32)
                n1T = tile(es, "n1T", [128, KC], F32)
                n2T = tile(es, "n2T", [128, KC], F32)
                war = Ring([tile(es, "wa%d" % i, [128, KC, 512], F32) for i in range(2)])
                gst = Ring([tile(es, "gst%d" % i, [128, 512], F32) for i in range(2)])
                dma("sp", cT.t[:], io["cT"], writes=[cT.b])
                dma("sp", badaT.t[:], io["b_adaT"], writes=[badaT.b])
                dma("sp", n1T.t[:], io["n1T"], writes=[n1T.b])
                dma("sp", n2T.t[:], io["n2T"], writes=[n2T.b])
                dma("sp", bbc.t[:, 0, :], io["b_ada"][2 * D:3 * D].partition_broadcast(128), pwrites=[bbc.b])
                dma("sp", bbc.t[:, 1, :], io["b_ada"][5 * D:6 * D].partition_broadcast(128), pwrites=[bbc.b])
                op("act", ACTF(sc.t[:], cT.t[:], AF.Silu), reads=[cT.b], writes=[sc.b])
                op("dve", CP(scb.t[:], sc.t[:].rearrange("p k s -> p (k s)").unsqueeze(2).to_broadcast([128, KC * NSEQ, 128])),
                   reads=[sc.b], writes=[scb.b])
                pM = PBs[0]
                pG = Ring([PBs[1], PBs[2]])
                wav = io["w_ada"].rearrange("(kc p) f -> p kc f", p=128)
                for blk in range(12):
                    wa = war.next()
                    dma("sp", wa.t[:], wav[:, :, blk * 512:(blk + 1) * 512], writes=[wa.b])
                    for fl in range(4):
                        fc = blk * 4 + fl
                        for kc in range(KC):
                            op("pe", MM(pM.f[:, fc * NSEQ:(fc + 1) * NSEQ], wa.t[:, kc, fl * 128:(fl + 1) * 128], sc.t[:, kc, :],
                                        start=(kc == 0), stop=(kc == KC - 1)), reads=[wa.b, sc.b], pwrites=[pM.b])
                    if blk in (4, 5, 10, 11):
                        which = 0 if blk < 6 else 1
                        half = blk % 2
                        for s in range(NSEQ):
                            p = pG.next()
                            for kc in range(KC):
                                op("pe", MM(p.f[:, :], scb.t[:, kc * NSEQ + s, :], wa.t[:, kc, :], start=(kc == 0), stop=(kc == KC - 1)),
                                   reads=[wa.b, scb.b], pwrites=[p.b])
                            g = gst.next()
                            op("dve", TT(g.t[:], p.f[:, :], bbc.t[:, which, half * 512:(half + 1) * 512], ALU.add),
                               reads=[p.b, bbc.b], writes=[g.b])
                            dma("sp", scr["gbc"].t[s, which, :, half * 512:(half + 1) * 512], g.t[:], reads=[g.b], pwrites=[scr["gbc"].b])
                op("dve", TT(modTb.t[:], pM.f[:, 0:48 * NSEQ].rearrange("p (f s) -> p f s", s=NSEQ),
                             badaT.t[:].unsqueeze(2).to_broadcast([128, 48, NSEQ]), ALU.add),
                   reads=[pM.b, badaT.b], writes=[modTb.b])
                op("dve", STT(A1.t[:], modTb.t[:, 8:16, :], 1.0, n1T.t[:].unsqueeze(2).to_broadcast([128, KC, NSEQ]), ALU.add, ALU.mult),
                   reads=[modTb.b, n1T.b], writes=[A1.b])
                op("dve", STT(A2.t[:], modTb.t[:, 32:40, :], 1.0, n2T.t[:].unsqueeze(2).to_broadcast([128, KC, NSEQ]), ALU.add, ALU.mult),
                   reads=[modTb.b, n2T.b], writes=[A2.b])
                ctx.barrier()

        if "B" in phases:
            with ExitStack() as es:
                win = tile(es, "win", [128, KC, NIN], BF16)
                wco = tile(es, "wco", [128, 4, D], BF16)
                convT = tile(es, "convT", [128, 4, 3], F32)
                dma("sp", convT.t[:], io["convT"], writes=[convT.b])
                with ExitStack() as es2:
                    stg = Ring([tile(es2, "stg%d" % i, [128, KC, 512], F32) for i in range(2)])
                    wiv = io["w_in"].rearrange("(kc p) f -> p kc f", p=128)
                    col = 0
                    i = 0
                    while col < NIN:
                        w = min(512, NIN - col)
                        st = stg.next()
                        dma("sp", st.t[:, :, 0:w], wiv[:, :, col:col + w], writes=[st.b])
                        eng = ("dve", "act", "pool")[i % 3]
                        if eng == "act":
                            op("act", ACTF(win.t[:, :, col:col + w], st.t[:, :, 0:w], AF.Copy), reads=[st.b], pwrites=[win.b])
                        else:
                            op(eng, CP(win.t[:, :, col:col + w], st.t[:, :, 0:w]), reads=[st.b], pwrites=[win.b])
                        col += w
                        i += 1
                    st = stg.next()
                    stv = st.t[:].rearrange("p k f -> p (k f)").rearrange("p (c d) -> p c d", c=4)
                    dma("sp", stv, io["w_conv_out"].rearrange("(c p) d -> p c d", p=128), writes=[st.b])
                    op("dve", CP(wco.t[:], stv), reads=[st.b], writes=[wco.b])
                    ctx.barrier()
                xgr = Ring([tile(es, "xg%d" % i, [128, JB, D], F32) for i in range(2)])
                junk = tile(es, "junkB", [128, D], BF16)
                ssq = tile(es, "ssq", [128, JB], F32)
                rt = tile(es, "rt", [128, JB], F32)
                rstd = tile(es, "rstd", [128, JB], F32)
                xs = tile(es, "xs", [128, JB, D], BF16)
                hTr = Ring([tile(es, "hT%d" % i, [128, KC, GB], BF16) for i in range(2)])
                ubuf = tile(es, "ubuf", [128, 4, GB + 2], F32)
                tmpf = Ring([tile(es, "tmpf%d" % i, [128, GB], F32) for i in range(3)])
                c1 = tile(es, "c1", [128, GB], F32)
                c2 = tile(es, "c2", [128, GB], F32)
                c3 = tile(es, "c3", [128, GB], F32)
                yA = tile(es, "yA", [128, 4, GB], BF16)
                mixA_st = tile(es, "mixA_st", [128, 8, GB], BF16)
                sga_st = tile(es, "sga_st", [128, 8, GB], BF16)
                q_st = tile(es, "q_st", [128, 4, GB], BF16)
                k_st = tile(es, "k_st", [128, 4, GB], BF16)
                qi_st = tile(es, "qi_st", [128, 4, GB], BF16)
                ki_st = tile(es, "ki_st", [64, GB], BF16)
                v_st = tile(es, "v_st", [128, JB, 8, 65], BF16)
                sqr = Ring([tile(es, "sq%d" % i, [128, GB], BF16) for i in range(2)])
                rtr = Ring([tile(es, "rtq%d" % i, [128, GB], F32) for i in range(2)])
                rrr = Ring([tile(es, "rrq%d" % i, [128, GB], F32) for i in range(2)])
                op("pool", MSET(v_st.t[:], 1.0), writes=[v_st.b])
                ptr = Ring([PBs[0], PBs[1]])
                gen = Ring([PBs[i] for i in range(2, 8)])
                evi = [0]

                def evac(out, in_, rd, wr):
                    evi[0] += 1
                    if evi[0] % 2:
                        op("act", ACTF(out, in_, AF.Copy), reads=rd, pwrites=wr)
                    else:
                        op("dve", CP(out, in_), reads=rd, pwrites=wr)

                for g in range(T // GB):
                    t0 = g * GB
                    seq = t0 // S
                    first = (t0 % S == 0)
                    xg = xgr.next()
                    hT = hTr.next()
                    dma("sp", xg.t[:], io["x"][t0:t0 + GB, :].rearrange("(j p) d -> p j d", p=128), writes=[xg.b])
                    for j in range(JB):
                        op("act", ACTF(junk.t[:], xg.t[:, j, :], AF.Square, accum_out=ssq.t[:, j:j + 1]), reads=[xg.b], writes=[junk.b], pwrites=[ssq.b])
                    op("act", ACTF(rt.t[:], ssq.t[:], AF.Sqrt, scale=1.0 / D, bias=epst.t[:, 0:1]), reads=[ssq.b, epst.b], writes=[rt.b])
                    op("dve", RCP(rstd.t[:], rt.t[:]), reads=[rt.b], writes=[rstd.b])
                    for j in range(JB):
                        op("dve", TS(xs.t[:, j, :], xg.t[:, j, :], rstd.t[:, j:j + 1], None, ALU.mult), reads=[xg.b, rstd.b], pwrites=[xs.b])
                    for kc in range(KC):
                        pt = ptr.next()
                        for j in range(JB):
                            op("pe", TR(pt.h[:, j * 128:(j + 1) * 128], xs.t[:, j, kc * 128:(kc + 1) * 128], ident_bf.t[:]),
                               reads=[xs.b, ident_bf.b], pwrites=[pt.b])
                        op("act", ACTF(hT.t[:, kc, :], pt.h[:, 0:GB], AF.Identity, scale=A1.t[:, kc, seq:seq + 1], bias=modTb.t[:, kc, seq:seq + 1]),
                           reads=[pt.b, A1.b, modTb.b], pwrites=[hT.b])

                    def proj(c0, M=128):
                        p = gen.next()
                        for kc in range(KC):
                            op("pe", MM(p.f[0:M, 0:GB], win.t[:, kc, c0:c0 + M], hT.t[:, kc, :], start=(kc == 0), stop=(kc == KC - 1)),
                               reads=[win.b, hT.b], pwrites=[p.b])
                        return p

                    if first:
                        op("dve", MSET(ubuf.t[:, :, 0:2], 0.0), pwrites=[ubuf.b])
                    for cch in range(4):
                        pcc = proj(O_CC + cch * 128)
                        pcx = proj(O_CX + cch * 128)
                        pcb = proj(O_CB + cch * 128)
                        tf = tmpf.next()
                        op("act", ACTF(tf.t[:], pcc.f[:, 0:GB], AF.Copy), reads=[pcc.b], writes=[tf.b])
                        op("dve", TT(ubuf.t[:, cch, 2:2 + GB], pcx.f[:, 0:GB], tf.t[:], ALU.mult), reads=[pcx.b, tf.b], pwrites=[ubuf.b])
                        op("dve", TS(c1.t[:], ubuf.t[:, cch, 0:GB], convT.t[:, cch, 0:1], None, ALU.mult), reads=[ubuf.b, convT.b], writes=[c1.b])
                        op("dve", STT(c2.t[:], ubuf.t[:, cch, 1:1 + GB], convT.t[:, cch, 1:2], c1.t[:], ALU.mult, ALU.add),
                           reads=[ubuf.b, convT.b, c1.b], writes=[c2.b])
                        op("dve", STT(c3.t[:], ubuf.t[:, cch, 2:2 + GB], convT.t[:, cch, 2:3], c2.t[:], ALU.mult, ALU.add),
                           reads=[ubuf.b, convT.b, c2.b], writes=[c3.b])
                        op("dve", TT(yA.t[:, cch, :], pcb.f[:, 0:GB], c3.t[:], ALU.mult), reads=[pcb.b, c3.b], pwrites=[yA.b])
                        op("dve", CP(ubuf.t[:, cch, 0:2], ubuf.t[:, cch, GB:GB + 2]), reads=[ubuf.b], pwrites=[ubuf.b])
                    for dc in range(8):
                        pyc = gen.next()
                        for cch in range(4):
                            op("pe", MM(pyc.f[:, 0:GB], wco.t[:, cch, dc * 128:(dc + 1) * 128], yA.t[:, cch, :], start=(cch == 0), stop=(cch == 3)),
                               reads=[wco.b, yA.b], pwrites=[pyc.b])
                        pgc = proj(O_GC + dc * 128)
                        tf = tmpf.next()
                        op("act", ACTF(tf.t[:], pgc.f[:, 0:GB], AF.Sigmoid), reads=[pgc.b], writes=[tf.b])
                        op("dve", TT(mixA_st.t[:, dc, :], pyc.f[:, 0:GB], tf.t[:], ALU.mult), reads=[pyc.b, tf.b], pwrites=[mixA_st.b])
                    dma("sp", scr["mixA"].t.rearrange("(dc p) t -> p dc t", p=128)[:, :, t0:t0 + GB], mixA_st.t[:], reads=[mixA_st.b], pwrites=[scr["mixA"].b])
                    for dc in range(8):
                        pga = proj(O_GA + dc * 128)
                        op("act", ACTF(sga_st.t[:, dc, :], pga.f[:, 0:GB], AF.Sigmoid), reads=[pga.b], pwrites=[sga_st.b])
                    dma("sp", scr["sga"].t.rearrange("(dc p) t -> p dc t", p=128)[:, :, t0:t0 + GB], sga_st.t[:], reads=[sga_st.b], pwrites=[scr["sga"].b])
                    for (base, wap, wb, st, nm) in ((O_Q, qws.t[:, 0:1], qws.b, q_st, "qT"), (O_K, qkw.t[:, 1:2], qkw.b, k_st, "kT")):
                        for c in range(4):
                            pq = proj(base + c * 128)
                            sq = sqr.next()
                            op("act", ACTF(sq.t[:], pq.f[:, 0:GB], AF.Square), reads=[pq.b], writes=[sq.b])
                            ps2 = gen.next()
                            op("pe", MM(ps2.f[:, 0:GB], bones_bf.t[:], sq.t[:]), reads=[bones_bf.b, sq.b], pwrites=[ps2.b])
                            r1 = rtr.next()
                            op("act", ACTF(r1.t[:], ps2.f[:, 0:GB], AF.Sqrt, scale=1.0 / 64, bias=epst.t[:, 0:1]), reads=[ps2.b, epst.b], writes=[r1.b])
                            r2 = rrr.next()
                            op("dve", RCP(r2.t[:], r1.t[:]), reads=[r1.b], writes=[r2.b])
                            op("dve", STT(st.t[:, c, :], pq.f[:, 0:GB], wap, r2.t[:], ALU.mult, ALU.mult), reads=[pq.b, wb, r2.b], pwrites=[st.b])
                        dma("sp", scr[nm].t.rearrange("(c p) t -> p c t", p=128)[:, :, t0:t0 + GB], st.t[:], reads=[st.b], pwrites=[scr[nm].b])
                    for c in range(4):
                        pq = proj(O_QI + c * 128)
                        evac(qi_st.t[:, c, :], pq.f[:, 0:GB], [pq.b], [qi_st.b])
                    dma("sp", scr["qiT"].t.rearrange("(c p) t -> p c t", p=128)[:, :, t0:t0 + GB], qi_st.t[:], reads=[qi_st.b], pwrites=[scr["qiT"].b])
                    pq = proj(O_KI, M=64)
                    evac(ki_st.t[:, :], pq.f[0:64, 0:GB], [pq.b], [ki_st.b])
                    dma("sp", scr["kiT"].t[:, t0:t0 + GB], ki_st.t[:], reads=[ki_st.b], pwrites=[scr["kiT"].b])
                    for j in range(JB):
                        p = gen.next()
                        for kc in range(KC):
                            op("pe", MM(p.f[:, 0:512], hT.t[:, kc, j * 128:(j + 1) * 128], win.t[:, kc, O_V:O_V + 512], start=(kc == 0), stop=(kc == KC - 1)),
                               reads=[win.b, hT.b], pwrites=[p.b])
                        evac(v_st.t[:, j, :, 0:64], p.f[:, 0:512].rearrange("p (h d) -> p h d", h=8), [p.b], [v_st.b])
                        p = gen.next()
                        for kc in range(KC):
                            op("pe", MM(p.f[:, 0:8], hT.t[:, kc, j * 128:(j + 1) * 128], win.t[:, kc, O_WI:O_WI + 8], start=(kc == 0), stop=(kc == KC - 1)),
                               reads=[win.b, hT.b], pwrites=[p.b])
                        op("dve", CP(widx.t[:, t0 // 128 + j, :], p.f[:, 0:8]), reads=[p.b], pwrites=[widx.b])
                    dma("sp", scr["V"].t[t0:t0 + GB, :].rearrange("(j p) f -> p j f", p=128), v_st.t[:].rearrange("p j h d -> p j (h d)"),
                        reads=[v_st.b], pwrites=[scr["V"].b])
                ctx.barrier()

        if "C" in phases:
            with ExitStack() as es:
                NKB = S // 128
                QG = min(512, S)
                kTs = tile(es, "kTs", [128, 4, S], BF16)
                Vs = tile(es, "Vs", [128, NKB, 520], BF16)
                kiTs = tile(es, "kiTs", [128, S], BF16)
                qTg = Ring([tile(es, "qTg%d" % i, [128, 4, QG], BF16) for i in range(2)])
                qiTg = Ring([tile(es, "qiTg%d" % i, [128, 4, QG], BF16) for i in range(2)])
                Dh = tile(es, "Dh", [128, 8, 128], BF16)
                rr = Ring([tile(es, "relu%d" % i, [128, 512], BF16) for i in range(4)])
                scores = [tile(es, "score%d" % i, [128, S], F32) for i in range(2)]
                junk = tile(es, "junkC", [128, S], BF16)
                biss = [tile(es, "bis%d" % i, [128, 8], F32) for i in range(2)]
                wfs = [tile(es, "wf%d" % i, [128, NIT + 2], F32) for i in range(2)]
                pw2 = tile(es, "pw2", [128, NIT + 2], F32)
                for i_ in range(NIT + 2):
                    op("dve", MSET(pw2.t[:, i_:i_ + 1], 2.0 ** -i_), pwrites=[pw2.b])
                dthr = tile(es, "dthr", [128, 128], F32)
                thrbc = tile(es, "thrbc", [128, 128], F32)
                maskT = tile(es, "maskT", [128, NKB, 128], BF16)
                maskT2 = tile(es, "maskT2", [128, NKB, 128], BF16)
                PTr = Ring([tile(es, "PT%d" % i, [128, 512], BF16) for i in range(3)])
                PMr = Ring([tile(es, "PM%d" % i, [128, 512], BF16) for i in range(3)])
                rz = tile(es, "rz", [128, 8], F32)
                attn_tm = tile(es, "attn_tm", [128, 8, 64], BF16)
                attnT_r = Ring([tile(es, "attnT_st%d" % i, [128, 4, QG], BF16) for i in range(2)])
                psL = Ring([PBs[0], PBs[1]])
                psS = PBs[2]
                psT = PBs[3]
                psA = Ring([PBs[4], PBs[5]])
                psO = [PBs[6], PBs[7]]
                maskTs = [maskT, maskT2]
                blocks = [(s_, qb_) for s_ in range(NSEQ) for qb_ in range(NKB)]
                gtiles = {}
                NQ = QG // 128

                def group_tiles(s_, qg):
                    key = (s_, qg)
                    if key not in gtiles:
                        qT = qTg.next()
                        qiT = qiTg.next()
                        tq0 = s_ * S + qg * QG
                        dma("sp", qiT.t[:], scr["qiT"].t.rearrange("(c p) t -> p c t", p=128)[:, :, tq0:tq0 + QG], reads=[scr["qiT"].b], writes=[qiT.b])
                        dma("sp", qT.t[:], scr["qT"].t.rearrange("(c p) t -> p c t", p=128)[:, :, tq0:tq0 + QG], reads=[scr["qT"].b], writes=[qT.b])
                        gtiles[key] = (qT, qiT, attnT_r.next())
                    return gtiles[key]

                xstate = {}

                def stage_X(bi, part, nparts):
                    s_, qb = blocks[bi]
                    sl = slice(s_ * S, (s_ + 1) * S)
                    qT, qiT, _ = group_tiles(s_, qb // NQ)
                    qs = qb % NQ
                    nkb = qb + 1
                    nk = nkb * 128
                    tix = (s_ * S) // 128 + qb
                    qsl = slice(qs * 128, (qs + 1) * 128)
                    score = scores[bi % 2]
                    bis = biss[bi % 2]
                    wf = wfs[bi % 2]
                    nch = (nk + 511) // 512
                    units = [(ch, h) for ch in range(nch) for h in range(8)]
                    nu = len(units)
                    ua, ub = (part * nu) // nparts, ((part + 1) * nu) // nparts

                    def logits(u):
                        ch, h = units[u]
                        hp = h % 2
                        k0 = ch * 512
                        w = min(512, nk - k0)
                        pl = psL.next()
                        op("pe", MM(pl.f[:, 0:w], qiT.t[hp * 64:(hp + 1) * 64, h // 2, qsl], kiTs.t[hp * 64:(hp + 1) * 64, k0:k0 + w]),
                           reads=[qiT.b, kiTs.b], pwrites=[pl.b])
                        xstate[(bi, u)] = pl

                    if part == 0:
                        if qb == 0:
                            dma("sp", kiTs.t[0:64, :], scr["kiT"].t[:, sl], reads=[scr["kiT"].b], writes=[kiTs.b])
                            dma("sp", kiTs.t[64:128, :], scr["kiT"].t[:, sl], reads=[scr["kiT"].b], pwrites=[kiTs.b])
                        op("dve", TT(Dh.t[:], ident_bf.t[:].unsqueeze(1).to_broadcast([128, 8, 128]),
                                     widx.t[:, tix, :].unsqueeze(2).to_broadcast([128, 8, 128]), ALU.mult),
                           reads=[ident_bf.b, widx.b], writes=[Dh.b])
                        logits(0)
                    for u in range(ua, ub):
                        if u + 1 < nu:
                            logits(u + 1)
                        ch, h = units[u]
                        k0 = ch * 512
                        w = min(512, nk - k0)
                        pl = xstate.pop((bi, u))
                        r = rr.next()
                        if h % 2 == 0:
                            op("act", ACTF(r.t[:, 0:w], pl.f[:, 0:w], AF.Relu), reads=[pl.b], writes=[r.b])
                        else:
                            op("dve", TS(r.t[:, 0:w], pl.f[:, 0:w], 0.0, None, ALU.max), reads=[pl.b], writes=[r.b])
                        op("pe", MM(psS.f[:, 0:w], Dh.t[:, h, :], r.t[:, 0:w], start=(h == 0), stop=(h == 7)),
                           reads=[Dh.b, r.b], pwrites=[psS.b])
                        if h == 7:
                            if ch == nch - 1:
                                wd = w - 128
                                if wd > 0:
                                    op("act", ACTF(score.t[:, k0:k0 + wd], psS.f[:, 0:wd], AF.Copy), reads=[psS.b], pwrites=[score.b])
                                op("dve", TT(score.t[:, nk - 128:nk], psS.f[:, wd:w], causal_f, ALU.add), reads=[psS.b, cst.b], pwrites=[score.b])
                            else:
                                op("act", ACTF(score.t[:, k0:k0 + w], psS.f[:, 0:w], AF.Copy), reads=[psS.b], pwrites=[score.b])
                    if part != nparts - 1:
                        return
                    lo, w0, mid, cnt, gg, mx = (bis.t[:, i:i + 1] for i in range(6))
                    if nk <= KSEL:
                        op("dve", MSET(lo, -1.0e29), writes=[bis.b])
                    else:
                        op("dve", RED(mx, score.t[:, 0:nk], ALU.max), reads=[score.b], writes=[bis.b])
                        op("dve", RED(lo, score.t[:, 0:nk - 128], ALU.min), reads=[score.b], writes=[bis.b])
                        op("dve", TT(w0, mx, lo, ALU.subtract), reads=[bis.b], writes=[bis.b])
                        op("dve", TS(wf.t[:], pw2.t[:], w0, None, ALU.mult), reads=[bis.b, pw2.b], writes=[wf.b])
                        op("dve", TT(mid, lo, wf.t[:, 1:2], ALU.add), reads=[bis.b, wf.b], writes=[bis.b])

                def stage_Xbis(bi, it0, it1):
                    s_, qb = blocks[bi]
                    nk = (qb + 1) * 128
                    if nk <= KSEL:
                        return
                    score = scores[bi % 2]
                    bis = biss[bi % 2]
                    wf = wfs[bi % 2]
                    lo, w0, mid, cnt, gg, mx = (bis.t[:, i:i + 1] for i in range(6))
                    for it in range(it0, it1):
                        op("dve", TS(junk.t[:, 0:nk], score.t[:, 0:nk], mid, None, ALU.is_ge, ALU.add, accum_out=cnt),
                           reads=[score.b, bis.b], writes=[bis.b, junk.b])
                        op("dve", TS(gg, cnt, KSEL - 0.5, 0.5, ALU.is_ge, ALU.subtract), reads=[bis.b], writes=[bis.b])
                        op("dve", STT(mid, gg, wf.t[:, it + 1:it + 2], mid, ALU.mult, ALU.add), reads=[bis.b, wf.b], writes=[bis.b])

                def stage_Xpost(bi):
                    s_, qb = blocks[bi]
                    nkb = qb + 1
                    nk = nkb * 128
                    mk = maskTs[bi % 2]
                    score = scores[bi % 2]
                    bis = biss[bi % 2]
                    wf = wfs[bi % 2]
                    lo, w0, mid, cnt, gg, mx = (bis.t[:, i:i + 1] for i in range(6))
                    if nk > KSEL:
                        op("dve", TT(lo, mid, wf.t[:, NIT + 1:NIT + 2], ALU.subtract), reads=[bis.b, wf.b], writes=[bis.b])
                    op("dve", TS(dthr.t[:], ident_f, lo, None, ALU.mult), reads=[cst.b, bis.b], writes=[dthr.b])
                    op("pe", MM(psT.f[:, 0:128], ones_f, dthr.t[:]), reads=[cst.b, dthr.b], pwrites=[psT.b])
                    op("act", ACTF(thrbc.t[:], psT.f[:, 0:128], AF.Copy), reads=[psT.b], writes=[thrbc.b])
                    for c4 in range((nkb + 3) // 4):
                        n4 = min(4, nkb - c4 * 4)
                        for i in range(n4):
                            kb = c4 * 4 + i
                            op("pe", TR(psT.f[:, i * 128:(i + 1) * 128], score.t[:, kb * 128:(kb + 1) * 128], ident_f),
                               reads=[score.b, cst.b], pwrites=[psT.b])
                        op("dve", TT(mk.t[:, c4 * 4:c4 * 4 + n4, :], psT.f[:, 0:n4 * 128].rearrange("p (a b) -> p a b", b=128),
                                     thrbc.t[:].unsqueeze(1).to_broadcast([128, n4, 128]), ALU.is_ge),
                           reads=[psT.b, thrbc.b], pwrites=[mk.b])

                def stage_Y(bi, hs, fin):
                    s_, qb = blocks[bi]
                    sl = slice(s_ * S, (s_ + 1) * S)
                    if qb == 0 and 0 in hs:
                        dma("sp", kTs.t[:], scr["kT"].t.rearrange("(c p) t -> p c t", p=128)[:, :, sl], reads=[scr["kT"].b], writes=[kTs.b])
                        dma("sp", Vs.t[:], scr["V"].t[sl, :].rearrange("(kb p) f -> p kb f", p=128), reads=[scr["V"].b], writes=[Vs.b])
                    qT, qiT, ast = group_tiles(s_, qb // NQ)
                    qs = qb % NQ
                    nkb = qb + 1
                    qsl = slice(qs * 128, (qs + 1) * 128)
                    mk = maskTs[bi % 2]
                    for h in hs:
                        hp = h % 2
                        po = psO[h // 4]
                        for c4 in range((nkb + 3) // 4):
                            n4 = min(4, nkb - c4 * 4)
                            pa = psA.next()
                            for i in range(n4):
                                kb = c4 * 4 + i
                                op("pe", MM(pa.f[:, i * 128:(i + 1) * 128], kTs.t[hp * 64:(hp + 1) * 64, h // 2, kb * 128:(kb + 1) * 128],
                                            qT.t[hp * 64:(hp + 1) * 64, h // 2, qsl]), reads=[kTs.b, qT.b], pwrites=[pa.b])
                            pt = PTr.next()
                            op("act", ACTF(pt.t[:, 0:n4 * 128], pa.f[:, 0:n4 * 128], AF.Exp), reads=[pa.b], writes=[pt.b])
                            pm = PMr.next()
                            op("dve", TT(pm.t[:, 0:n4 * 128], pt.t[:, 0:n4 * 128], mk.t[:, c4 * 4:c4 * 4 + n4, :].rearrange("p a b -> p (a b)"), ALU.mult),
                               reads=[pt.b, mk.b], writes=[pm.b])
                            for i in range(n4):
                                kb = c4 * 4 + i
                                op("pe", MM(po.f[:, (h % 4) * 65:(h % 4) * 65 + 65], pm.t[:, i * 128:(i + 1) * 128], Vs.t[:, kb, h * 65:(h + 1) * 65],
                                            start=(kb == 0), stop=(kb == nkb - 1)), reads=[pm.b, Vs.b], pwrites=[po.b])
                    if not fin:
                        return
                    for hh in range(2):
                        pv = psO[hh].f[:, 0:260].rearrange("p (h d) -> p h d", d=65)
                        op("dve", RCP(rz.t[:, hh * 4:(hh + 1) * 4], pv[:, :, 64]), reads=[psO[hh].b], pwrites=[rz.b])
                        op("dve", TT(attn_tm.t[:, hh * 4:(hh + 1) * 4, :], pv[:, :, 0:64],
                                     rz.t[:, hh * 4:(hh + 1) * 4].unsqueeze(2).to_broadcast([128, 4, 64]), ALU.mult),
                           reads=[psO[hh].b, rz.b], pwrites=[attn_tm.b])
                    px = psA.next()
                    for c in range(4):
                        op("pe", TR(px.h[:, c * 128:(c + 1) * 128], attn_tm.t[:, 2 * c:2 * c + 2, :].rearrange("p a b -> p (a b)"), ident_bf.t[:]),
                           reads=[attn_tm.b, ident_bf.b], pwrites=[px.b])
                    op("act", ACTF(ast.t[:, :, qsl], px.h[:, 0:512].rearrange("p (c t) -> p c t", c=4), AF.Copy),
                       reads=[px.b], pwrites=[ast.b])
                    if qs == NQ - 1:
                        tq0 = s_ * S + (qb // NQ) * QG
                        dma("sp", scr["attnT"].t.rearrange("(c p) t -> p c t", p=128)[:, :, tq0:tq0 + QG], ast.t[:], reads=[ast.b], pwrites=[scr["attnT"].b])

                NB = len(blocks)
                IPH = NIT // 8
                stage_X(0, 0, 1)
                stage_Xbis(0, 0, NIT)
                stage_Xpost(0)
                if NB > 1:
                    stage_X(1, 0, 1)
                for bi in range(NB):
                    for h in range(8):
                        if bi + 1 < NB:
                            stage_Xbis(bi + 1, h * IPH, (h + 1) * IPH if h < 7 else NIT)
                        if bi + 2 < NB:
                            stage_X(bi + 2, h, 8)
                        stage_Y(bi, [h], h == 7)
                    if bi + 1 < NB:
                        stage_Xpost(bi + 1)
                ctx.barrier()

        if "D" in phases:
            with ExitStack() as es:
                wao = tile(es, "wao", [128, 4, D], BF16)
                wo = tile(es, "wo", [128, KC, D], BF16)
                wpq = tile(es, "wpq", [128, KC, D], BF16)
                skTb = tile(es, "skTb", [128, 8, 128], BF16)
                with ExitStack() as es2:
                    stg = Ring([tile(es2, "stgD%d" % i, [128, KC, 512], F32) for i in range(2)])
                    ci_ = 0
                    for (src, dst) in ((io["w_o"], wo), (io["w_peer_q"], wpq)):
                        sv = src.rearrange("(kc p) f -> p kc f", p=128)
                        for hh in range(2):
                            st = stg.next()
                            dma("sp", st.t[:], sv[:, :, hh * 512:(hh + 1) * 512], writes=[st.b])
                            if ci_ % 2:
                                op("act", ACTF(dst.t[:, :, hh * 512:(hh + 1) * 512], st.t[:], AF.Copy), reads=[st.b], pwrites=[dst.b])
                            else:
                                op("dve", CP(dst.t[:, :, hh * 512:(hh + 1) * 512], st.t[:]), reads=[st.b], pwrites=[dst.b])
                            ci_ += 1
                    st = stg.next()
                    stv = st.t[:].rearrange("p k f -> p (k f)").rearrange("p (c d) -> p c d", c=4)
                    dma("sp", stv, io["w_attn_out"].rearrange("(c p) d -> p c d", p=128), writes=[st.b])
                    op("dve", CP(wao.t[:], stv), reads=[st.b], writes=[wao.b])
                    st = stg.next()
                    stv2 = st.t[:].rearrange("p k f -> p (k f)")[:, 0:1024].rearrange("p (h n) -> p h n", h=8)
                    dma("sp", stv2, io["skT"], writes=[st.b])
                    op("dve", CP(skTb.t[:], stv2), reads=[st.b], writes=[skTb.b])
                    ctx.barrier()
                gb1 = tile(es, "gb1", [128, D], F32)
                attg = Ring([tile(es, "attg%d" % i, [128, 4, GB], BF16) for i in range(2)])
                sgag = Ring([tile(es, "sgag%d" % i, [128, 8, GB], BF16) for i in range(2)])
                mxag = Ring([tile(es, "mxag%d" % i, [128, 8, GB], BF16) for i in range(2)])
                xgr = Ring([tile(es, "xgD%d" % i, [128, JB, D], F32) for i in range(2)])
                tmq = Ring([tile(es, "tmq%d" % i, [128, GB], F32) for i in range(2)])
                mixT = tile(es, "mixT", [128, 8, GB], BF16)
                tz = Ring([tile(es, "tz%d" % i, [128, 512], F32) for i in range(2)])
                x1 = tile(es, "x1", [128, JB, D], F32)
                junk = tile(es, "junkD", [128, D], BF16)
                ssq = tile(es, "ssqD", [128, JB], F32)
                rt = tile(es, "rtD", [128, JB], F32)
                rstd = tile(es, "rstdD", [128, JB], F32)
                xs = tile(es, "xsD", [128, JB, D], BF16)
                h2T = tile(es, "h2T", [128, KC, GB], BF16)
                qpT = tile(es, "qpT", [128, 8, GB], BF16)
                s_sb = tile(es, "s_sb", [128, 16, 128], F32)
                s_tmp = tile(es, "s_tmp", [128, 16, 128], F32)
                v_all = tile(es, "v_all", [128, 16, 16], F32)
                idx_all = tile(es, "idx_all", [128, 16, 16], U32)
                idx_bf = tile(es, "idx_bf", [128, 16, 16], BF16)
                cand = tile(es, "cand", [128, 8, 256], F32)
                cand2 = tile(es, "cand2", [128, 8, 256], F32)
                scv = tile(es, "scv", [128, 8, 16], F32)
                civ = tile(es, "civ", [128, 8, 16], U32)
                ca_u = tile(es, "ca_u", [128, 8, 16], U32)
                cb_u = tile(es, "cb_u", [128, 8, 16], U32)
                ca_bf = tile(es, "ca_bf", [128, 8, 16], BF16)
                cb_bf = tile(es, "cb_bf", [128, 8, 16], BF16)
                eqa = tile(es, "eqa", [128, 8, 16, 16], BF16)
                prd = tile(es, "prd", [128, 8, 16, 16], BF16)
                sm = tile(es, "sm", [128, 8, 16], F32)
                zz = tile(es, "zz", [128, 8], F32)
                sel_tm = tile(es, "sel_tm", [128, 3, 128], F32)
                selT = tile(es, "selT", [128, 3, 128], BF16)
                ptr = Ring([PBs[0], PBs[1]])
                gen = Ring([PBs[i] for i in range(2, 8)])
                evi = [0]

                def evacD(out, in_, rd, wr):
                    evi[0] += 1
                    if evi[0] % 2:
                        op("act", ACTF(out, in_, AF.Copy), reads=rd, pwrites=wr)
                    else:
                        op("dve", CP(out, in_), reads=rd, pwrites=wr)

                for g in range(T // GB):
                    t0 = g * GB
                    seq = t0 // S
                    if t0 % S == 0:
                        dma("sp", gb1.t[:], scr["gbc"].t[seq, 0, :, :], reads=[scr["gbc"].b], writes=[gb1.b])
                    at = attg.next()
                    sg = sgag.next()
                    ma = mxag.next()
                    xg = xgr.next()
                    dma("sp", at.t[:], scr["attnT"].t.rearrange("(c p) t -> p c t", p=128)[:, :, t0:t0 + GB], reads=[scr["attnT"].b], writes=[at.b])
                    dma("sp", sg.t[:], scr["sga"].t.rearrange("(c p) t -> p c t", p=128)[:, :, t0:t0 + GB], reads=[scr["sga"].b], writes=[sg.b])
                    dma("sp", ma.t[:], scr["mixA"].t.rearrange("(c p) t -> p c t", p=128)[:, :, t0:t0 + GB], reads=[scr["mixA"].b], writes=[ma.b])
                    dma("sp", xg.t[:], io["x"][t0:t0 + GB, :].rearrange("(j p) d -> p j d", p=128), writes=[xg.b])
                    for dc in range(8):
                        p = gen.next()
                        for c in range(4):
                            op("pe", MM(p.f[:, 0:GB], wao.t[:, c, dc * 128:(dc + 1) * 128], at.t[:, c, :], start=(c == 0), stop=(c == 3)),
                               reads=[wao.b, at.b], pwrites=[p.b])
                        tq = tmq.next()
                        op("dve", TT(tq.t[:], p.f[:, 0:GB], sg.t[:, dc, :], ALU.mult), reads=[p.b, sg.b], writes=[tq.b])
                        op("dve", TT(mixT.t[:, dc, :], tq.t[:], ma.t[:, dc, :], ALU.add), reads=[tq.b, ma.b], pwrites=[mixT.b])
                    for j in range(JB):
                        for dh in range(2):
                            p = gen.next()
                            for kc in range(KC):
                                op("pe", MM(p.f[:, 0:512], mixT.t[:, kc, j * 128:(j + 1) * 128], wo.t[:, kc, dh * 512:(dh + 1) * 512],
                                            start=(kc == 0), stop=(kc == KC - 1)), reads=[mixT.b, wo.b], pwrites=[p.b])
                            t_ = tz.next()
                            op("dve", TT(t_.t[:], p.f[:, 0:512], gb1.t[:, dh * 512:(dh + 1) * 512], ALU.mult), reads=[p.b, gb1.b], writes=[t_.b])
                            op("dve", TT(x1.t[:, j, dh * 512:(dh + 1) * 512], t_.t[:], xg.t[:, j, dh * 512:(dh + 1) * 512], ALU.add),
                               reads=[t_.b, xg.b], pwrites=[x1.b])
                    dma("sp", scr["x1"].t[t0:t0 + GB, :].rearrange("(j p) d -> p j d", p=128), x1.t[:], reads=[x1.b], pwrites=[scr["x1"].b])
                    for j in range(JB):
                        op("act", ACTF(junk.t[:], x1.t[:, j, :], AF.Square, accum_out=ssq.t[:, j:j + 1]), reads=[x1.b], writes=[junk.b], pwrites=[ssq.b])
                    op("act", ACTF(rt.t[:], ssq.t[:], AF.Sqrt, scale=1.0 / D, bias=epst.t[:, 0:1]), reads=[ssq.b, epst.b], writes=[rt.b])
                    op("dve", RCP(rstd.t[:], rt.t[:]), reads=[rt.b], writes=[rstd.b])
                    for j in range(JB):
                        op("dve", TS(xs.t[:, j, :], x1.t[:, j, :], rstd.t[:, j:j + 1], None, ALU.mult), reads=[x1.b, rstd.b], pwrites=[xs.b])
                    for kc in range(KC):
                        pt = ptr.next()
                        for j in range(JB):
                            op("pe", TR(pt.h[:, j * 128:(j + 1) * 128], xs.t[:, j, kc * 128:(kc + 1) * 128], ident_bf.t[:]),
                               reads=[xs.b, ident_bf.b], pwrites=[pt.b])
                        op("act", ACTF(h2T.t[:, kc, :], pt.h[:, 0:GB], AF.Identity, scale=A2.t[:, kc, seq:seq + 1], bias=modTb.t[:, 24 + kc, seq:seq + 1]),
                           reads=[pt.b, A2.b, modTb.b], pwrites=[h2T.b])
                    dma("sp", scr["h2T"].t.rearrange("(c p) t -> p c t", p=128)[:, :, t0:t0 + GB], h2T.t[:], reads=[h2T.b], pwrites=[scr["h2T"].b])
                    for h in range(8):
                        p = gen.next()
                        for kc in range(KC):
                            op("pe", MM(p.f[:, 0:GB], wpq.t[:, kc, h * 128:(h + 1) * 128], h2T.t[:, kc, :], start=(kc == 0), stop=(kc == KC - 1)),
                               reads=[wpq.b, h2T.b], pwrites=[p.b])
                        evacD(qpT.t[:, h, :], p.f[:, 0:GB], [p.b], [qpT.b])
                    for j in range(JB):
                        tt0 = t0 + j * 128
                        s4 = s_sb.t[:].rearrange("p (h s) n -> p h s n", s=2)
                        for b4 in range(4):
                            p = gen.next()
                            side, hb = b4 % 2, (b4 // 2) * 4
                            for i in range(4):
                                h = hb + i
                                op("pe", MM(p.f[:, i * 128:(i + 1) * 128], qpT.t[side * 64:(side + 1) * 64, h, j * 128:(j + 1) * 128],
                                            skTb.t[side * 64:(side + 1) * 64, h, :]), reads=[qpT.b, skTb.b], pwrites=[p.b])
                            op("act", ACTF(s4[:, hb:hb + 4, side, :], p.f[:, :].rearrange("p (a b) -> p a b", b=128), AF.Copy),
                               reads=[p.b], pwrites=[s_sb.b])
                        if DCUT < 2:
                            continue
                        for r in range(16):
                            op("dve", lambda e, r=r: e.max(out=v_all.t[:, r, 0:8], in_=s_sb.t[:, r, :]), reads=[s_sb.b], pwrites=[v_all.b])
                            op("dve", lambda e, r=r: e.max_index(out=idx_all.t[:, r, 0:8], in_max=v_all.t[:, r, 0:8], in_values=s_sb.t[:, r, :]),
                               reads=[s_sb.b, v_all.b], pwrites=[idx_all.b])
                            op("dve", lambda e, r=r: e.match_replace(out=s_tmp.t[:, r, :], in_to_replace=v_all.t[:, r, 0:8], in_values=s_sb.t[:, r, :], imm_value=NEG),
                               reads=[s_sb.b, v_all.b], pwrites=[s_tmp.b])
                            op("dve", lambda e, r=r: e.max(out=v_all.t[:, r, 8:16], in_=s_tmp.t[:, r, :]), reads=[s_tmp.b], pwrites=[v_all.b])
                            op("dve", lambda e, r=r: e.max_index(out=idx_all.t[:, r, 8:16], in_max=v_all.t[:, r, 8:16], in_values=s_tmp.t[:, r, :]),
                               reads=[s_tmp.b, v_all.b], pwrites=[idx_all.b])
                        if DCUT < 3:
                            continue
                        op("dve", CP(idx_bf.t[:], idx_all.t[:]), reads=[idx_all.b], writes=[idx_bf.b])
                        v4 = v_all.t[:].rearrange("p (h s) k -> p h s k", s=2)
                        op("dve", TT(cand.t[:].rearrange("p h (a b) -> p h a b", b=16), v4[:, :, 0, :].unsqueeze(3).to_broadcast([128, 8, 16, 16]),
                                     v4[:, :, 1, :].unsqueeze(2).to_broadcast([128, 8, 16, 16]), ALU.add), reads=[v_all.b], writes=[cand.b])
                        for h in range(8):
                            op("dve", lambda e, h=h: e.max(out=scv.t[:, h, 0:8], in_=cand.t[:, h, :]), reads=[cand.b], pwrites=[scv.b])
                            op("dve", lambda e, h=h: e.max_index(out=civ.t[:, h, 0:8], in_max=scv.t[:, h, 0:8], in_values=cand.t[:, h, :]),
                               reads=[cand.b, scv.b], pwrites=[civ.b])
                            op("dve", lambda e, h=h: e.match_replace(out=cand2.t[:, h, :], in_to_replace=scv.t[:, h, 0:8], in_values=cand.t[:, h, :], imm_value=NEG),
                               reads=[cand.b, scv.b], pwrites=[cand2.b])
                            op("dve", lambda e, h=h: e.max(out=scv.t[:, h, 8:16], in_=cand2.t[:, h, :]), reads=[cand2.b], pwrites=[scv.b])
                            op("dve", lambda e, h=h: e.max_index(out=civ.t[:, h, 8:16], in_max=scv.t[:, h, 8:16], in_values=cand2.t[:, h, :]),
                               reads=[cand2.b, scv.b], pwrites=[civ.b])
                        if DCUT < 4:
                            continue
                        op("dve", TT(sm.t[:], scv.t[:], scv.t[:, :, 0:1].to_broadcast([128, 8, 16]), ALU.subtract), reads=[scv.b], writes=[sm.b])
                        op("act", ACTF(sm.t[:], sm.t[:], AF.Exp), reads=[sm.b], writes=[sm.b])
                        op("dve", RED(zz.t[:], sm.t[:], ALU.add), reads=[sm.b], writes=[zz.b])
                        op("dve", RCP(zz.t[:], zz.t[:]), reads=[zz.b], writes=[zz.b])
                        op("dve", TT(sel_tm.t[:, 2, :].rearrange("p (h k) -> p h k", k=16), sm.t[:], zz.t[:].unsqueeze(2).to_broadcast([128, 8, 16]), ALU.mult),
                           reads=[sm.b, zz.b], pwrites=[sel_tm.b])
                        if DCUT < 5:
                            continue
                        op("dve", TS(ca_u.t[:], civ.t[:], 4, None, ALU.logical_shift_right), reads=[civ.b], writes=[ca_u.b])
                        op("dve", TS(cb_u.t[:], civ.t[:], 15, None, ALU.bitwise_and), reads=[civ.b], writes=[cb_u.b])
                        op("dve", CP(ca_bf.t[:], ca_u.t[:]), reads=[ca_u.b], writes=[ca_bf.b])
                        op("dve", CP(cb_bf.t[:], cb_u.t[:]), reads=[cb_u.b], writes=[cb_bf.b])
                        if DCUT < 6:
                            continue
                        i4 = idx_bf.t[:].rearrange("p (h s) k -> p h s k", s=2)
                        for side, cbf in ((0, ca_bf), (1, cb_bf)):
                            op("dve", TT(eqa.t[:], cbf.t[:].unsqueeze(3).to_broadcast([128, 8, 16, 16]),
                                         iota_bf.t[:, 0:16].unsqueeze(1).unsqueeze(1).to_broadcast([128, 8, 16, 16]), ALU.is_equal),
                               reads=[cbf.b, iota_bf.b], writes=[eqa.b])
                            op("dve", TT(prd.t[:], eqa.t[:], i4[:, :, side, :].unsqueeze(2).to_broadcast([128, 8, 16, 16]), ALU.mult),
                               reads=[eqa.b, idx_bf.b], writes=[prd.b])
                            op("dve", RED(sel_tm.t[:, side, :], prd.t[:].rearrange("p h k a -> p (h k) a"), ALU.add), reads=[prd.b], pwrites=[sel_tm.b])
                        if DCUT < 7:
                            continue
                        pt = ptr.next()
                        for c in range(3):
                            op("pe", TR(pt.f[:, c * 128:(c + 1) * 128], sel_tm.t[:, c, :], ident_f), reads=[sel_tm.b, cst.b], pwrites=[pt.b])
                        op("act", ACTF(selT.t[:], pt.f[:, 0:384].rearrange("p (c t) -> p c t", c=3), AF.Copy), reads=[pt.b], writes=[selT.b])
                        dma("sp", scr["sel"].t.rearrange("c p t -> p c t")[:, :, tt0:tt0 + 128], selT.t[:], reads=[selT.b], pwrites=[scr["sel"].b])
                ctx.barrier()

        if "E" in phases:
            with ExitStack() as es:
                sf = Ring([tile(es, "pf%d" % i, [128, 4096], F32) for i in range(3)])
                sbf = Ring([tile(es, "pb%d" % i, [128, 4096], BF16) for i in range(3)])
                uv = io["uT"].rearrange("(kc p) e -> p kc e", p=128)
                uo = scr["uTb"].t.rearrange("(kc p) e -> p kc e", p=128)
                vv = io["vp"].rearrange("(jj p) d -> p jj d", p=128)
                vo = scr["vb"].t.rearrange("(jj p) d -> p jj d", p=128)
                n = 0
                for i in range(NEXP // 512):
                    for (src, dst, key, v3) in ((uv[:, :, i * 512:(i + 1) * 512], uo[:, :, i * 512:(i + 1) * 512], "uTb", "p (a b) -> p a b"),
                                                (vv[:, i * 4:(i + 1) * 4, :], vo[:, i * 4:(i + 1) * 4, :], "vb", "p (a b) -> p a b")):
                        a_ = 8 if key == "uTb" else 4
                        f_ = sf.next()
                        b_ = sbf.next()
                        dma("sp", f_.t[:].rearrange(v3, a=a_), src, writes=[f_.b])
                        eng = ("dve", "act", "pool")[n % 3]
                        n += 1
                        if eng == "act":
                            op("act", ACTF(b_.t[:], f_.t[:], AF.Copy), reads=[f_.b], writes=[b_.b])
                        else:
                            op(eng, CP(b_.t[:], f_.t[:]), reads=[f_.b], writes=[b_.b])
                        dma("sp", dst, b_.t[:].rearrange(v3, a=a_), reads=[b_.b], pwrites=[scr[key].b])
                ctx.barrier()

        if "E" in phases:
            with ExitStack() as es:
                TE = 256
                SC = 2
                gb2 = tile(es, "gb2", [128, D], F32)
                x1r = Ring([tile(es, "x1E%d" % i, [128, 2, D], F32) for i in range(2)])
                h2r = Ring([tile(es, "h2E%d" % i, [128, KC, TE], BF16) for i in range(2)])
                selr = Ring([tile(es, "selE%d" % i, [128, 3, TE], BF16) for i in range(2)])
                Lr = Ring([tile(es, "L%d" % i, [128, 32, 128], BF16) for i in range(2)])
                L0r = Ring([tile(es, "L0%d" % i, [128, 32, 128], BF16) for i in range(1)])
                Rr = Ring([tile(es, "R%d" % i, [128, 32, 128], BF16) for i in range(2)])
                G = tile(es, "G", [128, TE, 128], BF16)
                uTr = Ring([tile(es, "uTs%d" % i, [128, KC, SC * 128], BF16) for i in range(3)])
                vr = Ring([tile(es, "vs%d" % i, [128, SC, D], BF16) for i in range(3)])
                Wr = Ring([tile(es, "W%d" % i, [128, TE], BF16) for i in range(3)])
                W2r = Ring([tile(es, "W2%d" % i, [128, TE], BF16) for i in range(3)])
                tz = Ring([tile(es, "tzE%d" % i, [128, 512], F32) for i in range(2)])
                yst = tile(es, "yst", [128, 2, D], F32)
                psOut = [[PBs[0], PBs[1]], [PBs[2], PBs[3]]]
                psAT = Ring([PBs[4], PBs[5]])
                psG = Ring([PBs[6], PBs[7]])
                uo = scr["uTb"].t.rearrange("(kc p) e -> p kc e", p=128)
                vo = scr["vb"].t.rearrange("(jj p) d -> p jj d", p=128)
                for tl in range(T // TE):
                    t0 = tl * TE
                    seq = t0 // S
                    if t0 % S == 0:
                        dma("sp", gb2.t[:], scr["gbc"].t[seq, 1, :, :], reads=[scr["gbc"].b], writes=[gb2.b])
                    x1t = x1r.next()
                    h2 = h2r.next()
                    sl_ = selr.next()
                    dma("sp", x1t.t[:], scr["x1"].t[t0:t0 + TE, :].rearrange("(j p) d -> p j d", p=128), reads=[scr["x1"].b], writes=[x1t.b])
                    dma("sp", h2.t[:], scr["h2T"].t.rearrange("(c p) t -> p c t", p=128)[:, :, t0:t0 + TE], reads=[scr["h2T"].b], writes=[h2.b])
                    dma("sp", sl_.t[:], scr["sel"].t.rearrange("c p t -> p c t")[:, :, t0:t0 + TE], reads=[scr["sel"].b], writes=[sl_.b])
                    for sb in range(TE // 32):
                        ts_ = slice(sb * 32, (sb + 1) * 32)
                        L0 = L0r.next()
                        L = Lr.next()
                        Rt = Rr.next()
                        iob = iota_bf.t[:].unsqueeze(1).to_broadcast([128, 32, 128])
                        op("dve", TT(L0.t[:], iob, sl_.t[:, 0, ts_].unsqueeze(2).to_broadcast([128, 32, 128]), ALU.is_equal),
                           reads=[iota_bf.b, sl_.b], writes=[L0.b])
                        op("dve", TT(L.t[:], L0.t[:], sl_.t[:, 2, ts_].unsqueeze(2).to_broadcast([128, 32, 128]), ALU.mult),
                           reads=[L0.b, sl_.b], writes=[L.b])
                        op("dve", TT(Rt.t[:], iob, sl_.t[:, 1, ts_].unsqueeze(2).to_broadcast([128, 32, 128]), ALU.is_equal),
                           reads=[iota_bf.b, sl_.b], writes=[Rt.b])
                        for q4 in range(8):
                            pg = psG.next()
                            for i in range(4):
                                t = q4 * 4 + i
                                op("pe", MM(pg.f[:, i * 128:(i + 1) * 128], L.t[:, t, :], Rt.t[:, t, :]), reads=[L.b, Rt.b], pwrites=[pg.b])
                            tt = sb * 32 + q4 * 4
                            op("act", ACTF(G.t[:, tt:tt + 4, :], pg.f[:, :].rearrange("p (a b) -> p a b", b=128), AF.Copy), reads=[pg.b], pwrites=[G.b])
                    chunks = {}

                    def load_sc(sc_):
                        uT = uTr.next()
                        vs = vr.next()
                        dma("sp", uT.t[:], uo[:, :, sc_ * SC * 128:(sc_ + 1) * SC * 128], reads=[scr["uTb"].b], writes=[uT.b])
                        dma("sp", vs.t[:], vo[:, sc_ * SC:(sc_ + 1) * SC, :], reads=[scr["vb"].b], writes=[vs.b])
                        for jj in range(SC):
                            chunks[sc_ * SC + jj] = (uT, vs, jj)

                    def stage1(j):
                        if j % SC == 0:
                            load_sc(j // SC)
                        uT, vs, jj = chunks[j]
                        pa = psAT.next()
                        for kc in range(KC):
                            op("pe", MM(pa.f[:, 0:TE], uT.t[:, kc, jj * 128:(jj + 1) * 128], h2.t[:, kc, :], start=(kc == 0), stop=(kc == KC - 1)),
                               reads=[uT.b, h2.b], pwrites=[pa.b])
                        W = Wr.next()
                        op("act", ACTF(W.t[:], pa.f[:, 0:TE], AF.Gelu), reads=[pa.b], writes=[W.b])
                        W2 = W2r.next()
                        op("dve", TT(W2.t[:], W.t[:], G.t[:, :, j], ALU.mult), reads=[W.b, G.b], writes=[W2.b])
                        return W2

                    def stage2(j, W2):
                        uT, vs, jj = chunks.pop(j)
                        for tb in range(2):
                            for dh in range(2):
                                po = psOut[tb][dh]
                                op("pe", MM(po.f[:, :], W2.t[:, tb * 128:(tb + 1) * 128], vs.t[:, jj, dh * 512:(dh + 1) * 512],
                                            start=(j == 0), stop=(j == 127)), reads=[W2.b, vs.b], pwrites=[po.b])

                    pend = stage1(0)
                    for j in range(128):
                        nxt = stage1(j + 1) if j + 1 < 128 else None
                        stage2(j, pend)
                        pend = nxt
                    for tb in range(2):
                        for dh in range(2):
                            po = psOut[tb][dh]
                            t_ = tz.next()
                            op("dve", TT(t_.t[:], po.f[:, :], gb2.t[:, dh * 512:(dh + 1) * 512], ALU.mult), reads=[po.b, gb2.b], writes=[t_.b])
                            op("dve", TT(yst.t[:, tb, dh * 512:(dh + 1) * 512], t_.t[:], x1t.t[:, tb, dh * 512:(dh + 1) * 512], ALU.add),
                               reads=[t_.b, x1t.b], pwrites=[yst.b])
                    dma("sp", y[t0:t0 + TE, :].rearrange("(j p) d -> p j d", p=128), yst.t[:], reads=[yst.b])
                ctx.barrier()

        ctx.finish()
    return nc, ctx


def host_consts():
    c = np.zeros((128, 5, 128), np.float32)
    c[:, 0, :] = np.eye(128, dtype=np.float32)
    qi = np.arange(128)[:, None]
    ki = np.arange(128)[None, :]
    c[:, 1, :] = np.where(ki <= qi, 0.0, NEG).astype(np.float32)
    c[:, 2, :] = ((qi // 64) == (ki // 64)).astype(np.float32)
    c[:, 3, :] = np.broadcast_to(np.arange(128, dtype=np.float32)[None, :], (128, 128))
    c[:, 4, :] = 1.0
    return c


def host_shared(inp):
    f = lambda a: np.ascontiguousarray(np.asarray(a, dtype=np.float32))
    sh = {}
    sh["w_ada"] = f(inp["w_ada"][0])
    sh["b_ada"] = f(inp["b_ada"][0])
    sh["b_adaT"] = f(np.asarray(inp["b_ada"][0]).reshape(48, 128).T)
    sh["n1T"] = f(np.asarray(inp["norm1_w"][0]).reshape(KC, 128).T)
    sh["n2T"] = f(np.asarray(inp["norm2_w"][0]).reshape(KC, 128).T)
    sh["w_in"] = f(inp["w_in"][0])
    sh["convT"] = f(np.asarray(inp["conv_w"][0]).reshape(3, 4, 128).transpose(2, 1, 0))
    sh["w_conv_out"] = f(inp["w_conv_out"][0])
    sh["qk_w"] = f(np.stack([np.tile(np.asarray(inp["q_norm_w"][0]), 2), np.tile(np.asarray(inp["k_norm_w"][0]), 2)], axis=1))
    sh["w_attn_out"] = f(inp["w_attn_out"][0])
    sh["w_o"] = f(inp["w_o"][0])
    sh["w_peer_q"] = f(inp["w_peer_q"][0])
    sk = np.asarray(inp["peer_sub_keys"][0])
    sh["skT"] = f(sk.transpose(1, 3, 0, 2).reshape(128, 8, 128))
    u = np.asarray(inp["peer_u"][0]).reshape(128, 128, D).transpose(1, 0, 2).reshape(NEXP, D)
    sh["uT"] = f(u.T)
    sh["vp"] = f(np.asarray(inp["peer_v"][0]).reshape(128, 128, D).transpose(1, 0, 2).reshape(NEXP, D))
    sh["consts"] = host_consts()
    return sh


def kernel(**inp):
    x = np.asarray(inp["x"], dtype=np.float32)
    c = np.asarray(inp["c"], dtype=np.float32)
    B, S, _ = x.shape
    ncores = 8
    NSEQ = B // ncores
    nc, _ = build(NSEQ, S)
    sh = host_shared(inp)
    in_maps = []
    for i in range(ncores):
        m = dict(sh)
        m["x"] = np.ascontiguousarray(x[i * NSEQ:(i + 1) * NSEQ].reshape(NSEQ * S, D))
        m["cT"] = np.ascontiguousarray(c[i * NSEQ:(i + 1) * NSEQ].reshape(NSEQ, KC, 128).transpose(2, 1, 0))
        in_maps.append(m)
    res = run_bass_kernel_spmd(nc, in_maps, core_ids=list(range(ncores)))
    out = np.concatenate([np.asarray(r["y"]).reshape(NSEQ, S, D) for r in res.results], axis=0)
    return out.astype(np.float32)
```

```python
import math
from contextlib import ExitStack

import numpy as np
import concourse.bass as bass
import concourse.mybir as mybir
from concourse.bass_utils import run_bass_kernel_spmd

F32 = mybir.dt.float32
BF16 = mybir.dt.bfloat16
U32 = mybir.dt.uint32
AF = mybir.ActivationFunctionType
ALU = mybir.AluOpType
AX = mybir.AxisListType

D = 1024
KC = 8
NIN = 5704
O_CB, O_CC, O_CX, O_Q, O_K, O_V, O_QI, O_KI, O_WI, O_GC, O_GA = 0, 512, 1024, 1536, 2048, 2560, 3072, 3584, 3648, 3656, 4680
NEXP = 16384
EPS = 1e-6
NEG = -1.0e30
NIT = 16
DCUT = 99
SELF_SYNC = True


class Buf:
    __slots__ = ("w", "r")

    def __init__(self):
        self.w = {}
        self.r = {}


class Tl:
    __slots__ = ("t", "b")

    def __init__(self, t):
        self.t = t
        self.b = Buf()


class Ring:
    def __init__(self, items):
        self.items = items
        self.i = 0

    def next(self):
        it = self.items[self.i % len(self.items)]
        self.i += 1
        return it


class Ctx:
    COMPUTE = ("pe", "act", "dve", "pool")
    ALL = ("pe", "act", "dve", "pool", "sp")
    NL = 8

    def __init__(self, nc):
        self.nc = nc
        self.prog = {n: [] for n in self.ALL}
        self.sems = {}
        self.cnt = {}
        self.known = {n: {} for n in self.ALL}
        for n in self.COMPUTE:
            self.sems[n] = nc.alloc_semaphore(name="s_" + n)
            self.cnt[n] = 0
        self.lane_rr = {}
        for q in ("sp", "act", "pool"):
            self.lane_rr[q] = 0
            for l in range(self.NL):
                k = (q, l)
                self.sems[k] = nc.alloc_semaphore(name="d_%s%d" % (q, l))
                self.cnt[k] = 0
        self.ninstr = 0

    def _wait(self, eng, key, val):
        if val <= 0:
            return
        if key == eng and (eng == "pe" or not SELF_SYNC):
            return
        if self.known[eng].get(key, 0) < val:
            self.prog[eng].append(("w", key, val))
            self.known[eng][key] = val

    def _deps(self, eng, reads, writes, pwrites):
        deps = {}
        for b in reads:
            for k, v in b.w.items():
                if deps.get(k, 0) < v:
                    deps[k] = v
        for b in writes:
            for dct in (b.w, b.r):
                for k, v in dct.items():
                    if deps.get(k, 0) < v:
                        deps[k] = v
        for b in pwrites:
            for k, v in b.r.items():
                if deps.get(k, 0) < v:
                    deps[k] = v
            for k, v in b.w.items():
                if k != eng and deps.get(k, 0) < v:
                    deps[k] = v
        for k, v in deps.items():
            self._wait(eng, k, v)

    def _mark(self, key, n, reads, writes, pwrites):
        for b in reads:
            b.r[key] = n
        for b in writes:
            b.w = {key: n}
            b.r = {}
        for b in pwrites:
            b.w[key] = n

    def op(self, eng, fn, reads=(), writes=(), pwrites=()):
        self._deps(eng, reads, writes, pwrites)
        self.cnt[eng] += 1
        self.prog[eng].append(("o", fn))
        self._mark(eng, self.cnt[eng], reads, writes, pwrites)
        self.ninstr += 1

    def dma(self, q, out, in_, reads=(), writes=(), pwrites=(), slow=False):
        l = self.lane_rr[q] % self.NL
        self.lane_rr[q] += 1
        key = (q, l)
        self._wait(q, key, self.cnt[key])
        self._deps(q, reads, writes, pwrites)
        self.cnt[key] += 16
        self.prog[q].append(("d", out, in_, key, slow))
        self._mark(key, self.cnt[key], reads, writes, pwrites)
        self.ninstr += 1

    def barrier(self):
        for e in self.ALL:
            for k in self.sems:
                self._wait(e, k, self.cnt[k])

    def finish(self):
        for k in self.sems:
            self._wait("sp", k, self.cnt[k])
        nc = self.nc

        def mk(name):
            def body(e):
                for it in self.prog[name]:
                    if it[0] == "w":
                        e.wait_ge(self.sems[it[1]], it[2])
                    elif it[0] == "o":
                        it[1](e).then_inc(self.sems[name], 1)
                    else:
                        if it[4]:
                            e.dma_start(out=it[1], in_=it[2], allow_slow_non_contiguous=True).then_inc(self.sems[it[3]], 16)
                        else:
                            e.dma_start(out=it[1], in_=it[2]).then_inc(self.sems[it[3]], 16)
            return body

        with nc.Block() as block:
            block.tensor(mk("pe"))
            block.scalar(mk("act"))
            block.vector(mk("dve"))
            block.gpsimd(mk("pool"))
            block.sync(mk("sp"))


def ACTF(out, in_, func, **kw):
    return lambda e: e.activation(out=out, in_=in_, func=func, **kw)


def MM(out, lhsT, rhs, start=True, stop=True):
    return lambda e: e.matmul(out, lhsT, rhs, start=start, stop=stop)


def TR(out, in_, ident):
    return lambda e: e.transpose(out, in_, ident)


def TS(out, in0, s1, s2, op0, op1=None, accum_out=None):
    if op1 is None:
        return lambda e: e.tensor_scalar(out=out, in0=in0, scalar1=s1, scalar2=s2, op0=op0, accum_out=accum_out)
    return lambda e: e.tensor_scalar(out=out, in0=in0, scalar1=s1, scalar2=s2, op0=op0, op1=op1, accum_out=accum_out)


def TT(out, in0, in1, op):
    return lambda e: e.tensor_tensor(out=out, in0=in0, in1=in1, op=op)


def STT(out, in0, scalar, in1, op0, op1):
    return lambda e: e.scalar_tensor_tensor(out=out, in0=in0, scalar=scalar, in1=in1, op0=op0, op1=op1)


def CP(out, in_):
    return lambda e: e.tensor_copy(out=out, in_=in_)


def RED(out, in_, op):
    return lambda e: e.tensor_reduce(out=out, in_=in_, axis=AX.X, op=op)


def RCP(out, in_):
    return lambda e: e.reciprocal(out=out, in_=in_)


def MSET(ap, c):
    return lambda e: e.memset(ap, c)


def build(NSEQ, S, GB=256, dbg=False, phases="ABCDE"):
    T = NSEQ * S
    NT = T // 128
    KSEL = min(256, S // 4)
    JB = GB // 128
    nc = bass.Bass("TRN2", target_bir_lowering=False)
    ctx = Ctx(nc)
    op, dma = ctx.op, ctx.dma

    def din(name, shape, dt=F32):
        return nc.dram_tensor(name, list(shape), dt, kind="ExternalInput").ap()

    io = {
        "x": din("x", [T, D]), "cT": din("cT", [128, KC, NSEQ]), "w_ada": din("w_ada", [D, 6 * D]),
        "b_ada": din("b_ada", [6 * D]), "b_adaT": din("b_adaT", [128, 48]),
        "n1T": din("n1T", [128, KC]), "n2T": din("n2T", [128, KC]), "w_in": din("w_in", [D, NIN]),
        "convT": din("convT", [128, 4, 3]), "w_conv_out": din("w_conv_out", [512, D]),
        "qk_w": din("qk_w", [128, 2]), "w_attn_out": din("w_attn_out", [512, D]), "w_o": din("w_o", [D, D]),
        "w_peer_q": din("w_peer_q", [D, D]), "skT": din("skT", [128, 8, 128]),
        "uT": din("uT", [D, NEXP]), "vp": din("vp", [NEXP, D]), "consts": din("consts", [128, 5, 128]),
    }
    y = nc.dram_tensor("y", [T, D], F32, kind="ExternalOutput").ap()
    skind = "ExternalOutput" if dbg else "Internal"

    def dscr(name, shape, dt):
        t = nc.dram_tensor(name, list(shape), dt, kind=skind).ap()
        return Tl(t)

    scr = {
        "mixA": dscr("s_mixA", [D, T], BF16), "sga": dscr("s_sga", [D, T], BF16),
        "qT": dscr("s_qT", [512, T], BF16), "kT": dscr("s_kT", [512, T], BF16),
        "qiT": dscr("s_qiT", [512, T], BF16), "kiT": dscr("s_kiT", [64, T], BF16),
        "V": dscr("s_V", [T, 520], BF16), "attnT": dscr("s_attnT", [512, T], BF16),
        "gbc": dscr("s_gbc", [NSEQ, 2, 128, D], F32), "x1": dscr("s_x1", [T, D], F32),
        "h2T": dscr("s_h2T", [D, T], BF16), "sel": dscr("s_sel", [3, 128, T], BF16),
        "uTb": dscr("s_uTb", [D, NEXP], BF16), "vb": dscr("s_vb", [NEXP, D], BF16),
    }

    with ExitStack() as gs:
        def tile(es, name, shape, dt):
            return Tl(es.enter_context(nc.sbuf_tensor("sb_" + name, list(shape), dt)))

        psum = gs.enter_context(nc.psum_tensor("psum", [128, 8, 512], F32))
        pb = [Buf() for _ in range(8)]

        class PB:
            def __init__(self, i):
                self.i = i
                self.b = pb[i]
                self.f = psum[:, i, :]
                self.h = psum[:, i, :].bitcast(BF16)

        PBs = [PB(i) for i in range(8)]

        cst = tile(gs, "cst", [128, 5, 128], F32)
        ident_bf = tile(gs, "ident_bf", [128, 128], BF16)
        bones_bf = tile(gs, "bones_bf", [128, 128], BF16)
        iota_bf = tile(gs, "iota_bf", [128, 128], BF16)
        epst = tile(gs, "epst", [128, 1], F32)
        modTb = tile(gs, "modTb", [128, 48, NSEQ], F32)
        A1 = tile(gs, "A1", [128, KC, NSEQ], F32)
        A2 = tile(gs, "A2", [128, KC, NSEQ], F32)
        widx = tile(gs, "widx", [128, NT, 8], F32)
        qkw = tile(gs, "qkw", [128, 2], F32)
        qws = tile(gs, "qws", [128, 1], F32)
        ident_f = cst.t[:, 0, :]
        causal_f = cst.t[:, 1, :]
        ones_f = cst.t[:, 4, :]

        dma("sp", cst.t[:], io["consts"], writes=[cst.b])
        dma("sp", qkw.t[:], io["qk_w"], writes=[qkw.b])
        op("dve", CP(ident_bf.t[:], cst.t[:, 0, :]), reads=[cst.b], writes=[ident_bf.b])
        op("dve", CP(bones_bf.t[:], cst.t[:, 2, :]), reads=[cst.b], writes=[bones_bf.b])
        op("dve", CP(iota_bf.t[:], cst.t[:, 3, :]), reads=[cst.b], writes=[iota_bf.b])
        op("dve", MSET(epst.t[:], EPS), writes=[epst.b])
        op("dve", TS(qws.t[:], qkw.t[:, 0:1], 0.125, None, ALU.mult), reads=[qkw.b], writes=[qws.b])

        if "A" in phases:
            with ExitStack() as es:
                cT = tile(es, "cT", [128, KC, NSEQ], F32)
                sc = tile(es, "sc", [128, KC, NSEQ], F32)
                scb = tile(es, "scb", [128, KC * NSEQ, 128], F32)
                badaT = tile(es, "badaT", [128, 48], F32)
                bbc = tile(es, "bbc", [128, 2, D], F32)
                n1T = tile(es, "n1T", [128, KC], F32)
                n2T = tile(es, "n2T", [128, KC], F32)
                war = Ring([tile(es, "wa%d" % i, [128, KC, 512], F32) for i in range(2)])
                gst = Ring([tile(es, "gst%d" % i, [128, 512], F32) for i in range(2)])
                dma("sp", cT.t[:], io["cT"], writes=[cT.b])
                dma("sp", badaT.t[:], io["b_adaT"], writes=[badaT.b])
                dma("sp", n1T.t[:], io["n1T"], writes=[n1T.b])
                dma("sp", n2T.t[:], io["n2T"], writes=[n2T.b])
                dma("sp", bbc.t[:, 0, :], io["b_ada"][2 * D:3 * D].partition_broadcast(128), pwrites=[bbc.b])
                dma("sp", bbc.t[:, 1, :], io["b_ada"][5 * D:6 * D].partition_broadcast(128), pwrites=[bbc.b])
                op("act", ACTF(sc.t[:], cT.t[:], AF.Silu), reads=[cT.b], writes=[sc.b])
                op("dve", CP(scb.t[:], sc.t[:].rearrange("p k s -> p (k s)").unsqueeze(2).to_broadcast([128, KC * NSEQ, 128])),
                   reads=[sc.b], writes=[scb.b])
                pM = PBs[0]
                pG = Ring([PBs[1], PBs[2]])
                wav = io["w_ada"].rearrange("(kc p) f -> p kc f", p=128)
                for blk in range(12):
                    wa = war.next()
                    dma("sp", wa.t[:], wav[:, :, blk * 512:(blk + 1) * 512], writes=[wa.b])
                    for fl in range(4):
                        fc = blk * 4 + fl
                        for kc in range(KC):
                            op("pe", MM(pM.f[:, fc * NSEQ:(fc + 1) * NSEQ], wa.t[:, kc, fl * 128:(fl + 1) * 128], sc.t[:, kc, :],
                                        start=(kc == 0), stop=(kc == KC - 1)), reads=[wa.b, sc.b], pwrites=[pM.b])
                    if blk in (4, 5, 10, 11):
                        which = 0 if blk < 6 else 1
                        half = blk % 2
                        for s in range(NSEQ):
                            p = pG.next()
                            for kc in range(KC):
                                op("pe", MM(p.f[:, :], scb.t[:, kc * NSEQ + s, :], wa.t[:, kc, :], start=(kc == 0), stop=(kc == KC - 1)),
                                   reads=[wa.b, scb.b], pwrites=[p.b])
                            g = gst.next()
                            op("dve", TT(g.t[:], p.f[:, :], bbc.t[:, which, half * 512:(half + 1) * 512], ALU.add),
                               reads=[p.b, bbc.b], writes=[g.b])
                            dma("sp", scr["gbc"].t[s, which, :, half * 512:(half + 1) * 512], g.t[:], reads=[g.b], pwrites=[scr["gbc"].b])
                op("dve", TT(modTb.t[:], pM.f[:, 0:48 * NSEQ].rearrange("p (f s) -> p f s", s=NSEQ),
                             badaT.t[:].unsqueeze(2).to_broadcast([128, 48, NSEQ]), ALU.add),
                   reads=[pM.b, badaT.b], writes=[modTb.b])
                op("dve", STT(A1.t[:], modTb.t[:, 8:16, :], 1.0, n1T.t[:].unsqueeze(2).to_broadcast([128, KC, NSEQ]), ALU.add, ALU.mult),
                   reads=[modTb.b, n1T.b], writes=[A1.b])
                op("dve", STT(A2.t[:], modTb.t[:, 32:40, :], 1.0, n2T.t[:].unsqueeze(2).to_broadcast([128, KC, NSEQ]), ALU.add, ALU.mult),
                   reads=[modTb.b, n2T.b], writes=[A2.b])
                ctx.barrier()

        if "B" in phases:
            with ExitStack() as es:
                win = tile(es, "win", [128, KC, NIN], BF16)
                wco = tile(es, "wco", [128, 4, D], BF16)
                convT = tile(es, "convT", [128, 4, 3], F32)
                dma("sp", convT.t[:], io["convT"], writes=[convT.b])
                with ExitStack() as es2:
                    stg = Ring([tile(es2, "stg%d" % i, [128, KC, 512], F32) for i in range(2)])
                    wiv = io["w_in"].rearrange("(kc p) f -> p kc f", p=128)
                    col = 0
                    i = 0
                    while col < NIN:
                        w = min(512, NIN - col)
                        st = stg.next()
                        dma("sp", st.t[:, :, 0:w], wiv[:, :, col:col + w], writes=[st.b])
                        eng = ("dve", "act", "pool")[i % 3]
                        if eng == "act":
                            op("act", ACTF(win.t[:, :, col:col + w], st.t[:, :, 0:w], AF.Copy), reads=[st.b], pwrites=[win.b])
                        else:
                            op(eng, CP(win.t[:, :, col:col + w], st.t[:, :, 0:w]), reads=[st.b], pwrites=[win.b])
                        col += w
                        i += 1
                    st = stg.next()
                    stv = st.t[:].rearrange("p k f -> p (k f)").rearrange("p (c d) -> p c d", c=4)
                    dma("sp", stv, io["w_conv_out"].rearrange("(c p) d -> p c d", p=128), writes=[st.b])
                    op("dve", CP(wco.t[:], stv), reads=[st.b], writes=[wco.b])
                    ctx.barrier()
                xgr = Ring([tile(es, "xg%d" % i, [128, JB, D], F32) for i in range(2)])
                junk = tile(es, "junkB", [128, D], BF16)
                ssq = tile(es, "ssq", [128, JB], F32)
                rt = tile(es, "rt", [128, JB], F32)
                rstd = tile(es, "rstd", [128, JB], F32)
                xs = tile(es, "xs", [128, JB, D], BF16)
                hTr = Ring([tile(es, "hT%d" % i, [128, KC, GB], BF16) for i in range(2)])
                ubuf = tile(es, "ubuf", [128, 4, GB + 2], F32)
                tmpf = Ring([tile(es, "tmpf%d" % i, [128, GB], F32) for i in range(3)])
                c1 = tile(es, "c1", [128, GB], F32)
                c2 = tile(es, "c2", [128, GB], F32)
                c3 = tile(es, "c3", [128, GB], F32)
                yA = tile(es, "yA", [128, 4, GB], BF16)
                mixA_st = tile(es, "mixA_st", [128, 8, GB], BF16)
                sga_st = tile(es, "sga_st", [128, 8, GB], BF16)
                q_st = tile(es, "q_st", [128, 4, GB], BF16)
                k_st = tile(es, "k_st", [128, 4, GB], BF16)
                qi_st = tile(es, "qi_st", [128, 4, GB], BF16)
                ki_st = tile(es, "ki_st", [64, GB], BF16)
                v_st = tile(es, "v_st", [128, JB, 8, 65], BF16)
                sqr = Ring([tile(es, "sq%d" % i, [128, GB], BF16) for i in range(2)])
                rtr = Ring([tile(es, "rtq%d" % i, [128, GB], F32) for i in range(2)])
                rrr = Ring([tile(es, "rrq%d" % i, [128, GB], F32) for i in range(2)])
                op("pool", MSET(v_st.t[:], 1.0), writes=[v_st.b])
                ptr = Ring([PBs[0], PBs[1]])
                gen = Ring([PBs[i] for i in range(2, 8)])
                evi = [0]

                def evac(out, in_, rd, wr):
                    evi[0] += 1
                    if evi[0] % 2:
                        op("act", ACTF(out, in_, AF.Copy), reads=rd, pwrites=wr)
                    else:
                        op("dve", CP(out, in_), reads=rd, pwrites=wr)

                for g in range(T // GB):
                    t0 = g * GB
                    seq = t0 // S
                    first = (t0 % S == 0)
                    xg = xgr.next()
                    hT = hTr.next()
                    dma("sp", xg.t[:], io["x"][t0:t0 + GB, :].rearrange("(j p) d -> p j d", p=128), writes=[xg.b])
                    for j in range(JB):
                        op("act", ACTF(junk.t[:], xg.t[:, j, :], AF.Square, accum_out=ssq.t[:, j:j + 1]), reads=[xg.b], writes=[junk.b], pwrites=[ssq.b])
                    op("act", ACTF(rt.t[:], ssq.t[:], AF.Sqrt, scale=1.0 / D, bias=epst.t[:, 0:1]), reads=[ssq.b, epst.b], writes=[rt.b])
                    op("dve", RCP(rstd.t[:], rt.t[:]), reads=[rt.b], writes=[rstd.b])
                    for j in range(JB):
                        op("dve", TS(xs.t[:, j, :], xg.t[:, j, :], rstd.t[:, j:j + 1], None, ALU.mult), reads=[xg.b, rstd.b], pwrites=[xs.b])
                    for kc in range(KC):
                        pt = ptr.next()
                        for j in range(JB):
                            op("pe", TR(pt.h[:, j * 128:(j + 1) * 128], xs.t[:, j, kc * 128:(kc + 1) * 128], ident_bf.t[:]),
                               reads=[xs.b, ident_bf.b], pwrites=[pt.b])
                        op("act", ACTF(hT.t[:, kc, :], pt.h[:, 0:GB], AF.Identity, scale=A1.t[:, kc, seq:seq + 1], bias=modTb.t[:, kc, seq:seq + 1]),
                           reads=[pt.b, A1.b, modTb.b], pwrites=[hT.b])

                    def proj(c0, M=128):
                        p = gen.next()
                        for kc in range(KC):
                            op("pe", MM(p.f[0:M, 0:GB], win.t[:, kc, c0:c0 + M], hT.t[:, kc, :], start=(kc == 0), stop=(kc == KC - 1)),
                               reads=[win.b, hT.b], pwrites=[p.b])
                        return p

                    if first:
                        op("dve", MSET(ubuf.t[:, :, 0:2], 0.0), pwrites=[ubuf.b])
                    for cch in range(4):
                        pcc = proj(O_CC + cch * 128)
                        pcx = proj(O_CX + cch * 128)
                        pcb = proj(O_CB + cch * 128)
                        tf = tmpf.next()
                        op("act", ACTF(tf.t[:], pcc.f[:, 0:GB], AF.Copy), reads=[pcc.b], writes=[tf.b])
                        op("dve", TT(ubuf.t[:, cch, 2:2 + GB], pcx.f[:, 0:GB], tf.t[:], ALU.mult), reads=[pcx.b, tf.b], pwrites=[ubuf.b])
                        op("dve", TS(c1.t[:], ubuf.t[:, cch, 0:GB], convT.t[:, cch, 0:1], None, ALU.mult), reads=[ubuf.b, convT.b], writes=[c1.b])
                        op("dve", STT(c2.t[:], ubuf.t[:, cch, 1:1 + GB], convT.t[:, cch, 1:2], c1.t[:], ALU.mult, ALU.add),
                           reads=[ubuf.b, convT.b, c1.b], writes=[c2.b])
                        op("dve", STT(c3.t[:], ubuf.t[:, cch, 2:2 + GB], convT.t[:, cch, 2:3], c2.t[:], ALU.mult, ALU.add),
                           reads=[ubuf.b, convT.b, c2.b], writes=[c3.b])
                        op("dve", TT(yA.t[:, cch, :], pcb.f[:, 0:GB], c3.t[:], ALU.mult), reads=[pcb.b, c3.b], pwrites=[yA.b])
                        op("dve", CP(ubuf.t[:, cch, 0:2], ubuf.t[:, cch, GB:GB + 2]), reads=[ubuf.b], pwrites=[ubuf.b])
                    for dc in range(8):
                        pyc = gen.next()
                        for cch in range(4):
                            op("pe", MM(pyc.f[:, 0:GB], wco.t[:, cch, dc * 128:(dc + 1) * 128], yA.t[:, cch, :], start=(cch == 0), stop=(cch == 3)),
                               reads=[wco.b, yA.b], pwrites=[pyc.b])
                        pgc = proj(O_GC + dc * 128)
                        tf = tmpf.next()
                        op("act", ACTF(tf.t[:], pgc.f[:, 0:GB], AF.Sigmoid), reads=[pgc.b], writes=[tf.b])
                        op("dve", TT(mixA_st.t[:, dc, :], pyc.f[:, 0:GB], tf.t[:], ALU.mult), reads=[pyc.b, tf.b], pwrites=[mixA_st.b])
                    dma("sp", scr["mixA"].t.rearrange("(dc p) t -> p dc t", p=128)[:, :, t0:t0 + GB], mixA_st.t[:], reads=[mixA_st.b], pwrites=[scr["mixA"].b])
                    for dc in range(8):
                        pga = proj(O_GA + dc * 128)
                        op("act", ACTF(sga_st.t[:, dc, :], pga.f[:, 0:GB], AF.Sigmoid), reads=[pga.b], pwrites=[sga_st.b])
                    dma("sp", scr["sga"].t.rearrange("(dc p) t -> p dc t", p=128)[:, :, t0:t0 + GB], sga_st.t[:], reads=[sga_st.b], pwrites=[scr["sga"].b])
                    for (base, wap, wb, st, nm) in ((O_Q, qws.t[:, 0:1], qws.b, q_st, "qT"), (O_K, qkw.t[:, 1:2], qkw.b, k_st, "kT")):
                        for c in range(4):
                            pq = proj(base + c * 128)
                            sq = sqr.next()
                            op("act", ACTF(sq.t[:], pq.f[:, 0:GB], AF.Square), reads=[pq.b], writes=[sq.b])
                            ps2 = gen.next()
                            op("pe", MM(ps2.f[:, 0:GB], bones_bf.t[:], sq.t[:]), reads=[bones_bf.b, sq.b], pwrites=[ps2.b])
                            r1 = rtr.next()
                            op("act", ACTF(r1.t[:], ps2.f[:, 0:GB], AF.Sqrt, scale=1.0 / 64, bias=epst.t[:, 0:1]), reads=[ps2.b, epst.b], writes=[r1.b])
                            r2 = rrr.next()
                            op("dve", RCP(r2.t[:], r1.t[:]), reads=[r1.b], writes=[r2.b])
                            op("dve", STT(st.t[:, c, :], pq.f[:, 0:GB], wap, r2.t[:], ALU.mult, ALU.mult), reads=[pq.b, wb, r2.b], pwrites=[st.b])
                        dma("sp", scr[nm].t.rearrange("(c p) t -> p c t", p=128)[:, :, t0:t0 + GB], st.t[:], reads=[st.b], pwrites=[scr[nm].b])
                    for c in range(4):
                        pq = proj(O_QI + c * 128)
                        evac(qi_st.t[:, c, :], pq.f[:, 0:GB], [pq.b], [qi_st.b])
                    dma("sp", scr["qiT"].t.rearrange("(c p) t -> p c t", p=128)[:, :, t0:t0 + GB], qi_st.t[:], reads=[qi_st.b], pwrites=[scr["qiT"].b])
                    pq = proj(O_KI, M=64)
                    evac(ki_st.t[:, :], pq.f[0:64, 0:GB], [pq.b], [ki_st.b])
                    dma("sp", scr["kiT"].t[:, t0:t0 + GB], ki_st.t[:], reads=[ki_st.b], pwrites=[scr["kiT"].b])
                    for j in range(JB):
                        p = gen.next()
                        for kc in range(KC):
                            op("pe", MM(p.f[:, 0:512], hT.t[:, kc, j * 128:(j + 1) * 128], win.t[:, kc, O_V:O_V + 512], start=(kc == 0), stop=(kc == KC - 1)),
                               reads=[win.b, hT.b], pwrites=[p.b])
                        evac(v_st.t[:, j, :, 0:64], p.f[:, 0:512].rearrange("p (h d) -> p h d", h=8), [p.b], [v_st.b])
                        p = gen.next()
                        for kc in range(KC):
                            op("pe", MM(p.f[:, 0:8], hT.t[:, kc, j * 128:(j + 1) * 128], win.t[:, kc, O_WI:O_WI + 8], start=(kc == 0), stop=(kc == KC - 1)),
                               reads=[win.b, hT.b], pwrites=[p.b])
                        op("dve", CP(widx.t[:, t0 // 128 + j, :], p.f[:, 0:8]), reads=[p.b], pwrites=[widx.b])
                    dma("sp", scr["V"].t[t0:t0 + GB, :].rearrange("(j p) f -> p j f", p=128), v_st.t[:].rearrange("p j h d -> p j (h d)"),
                        reads=[v_st.b], pwrites=[scr["V"].b])
                ctx.barrier()

        if "C" in phases:
            with ExitStack() as es:
                NKB = S // 128
                QG = min(512, S)
                kTs = tile(es, "kTs", [128, 4, S], BF16)
                Vs = tile(es, "Vs", [128, NKB, 520], BF16)
                kiTs = tile(es, "kiTs", [128, S], BF16)
                qTg = Ring([tile(es, "qTg%d" % i, [128, 4, QG], BF16) for i in range(2)])
                qiTg = Ring([tile(es, "qiTg%d" % i, [128, 4, QG], BF16) for i in range(2)])
                Dh = tile(es, "Dh", [128, 8, 128], BF16)
                rr = Ring([tile(es, "relu%d" % i, [128, 512], BF16) for i in range(4)])
                scores = [tile(es, "score%d" % i, [128, S], F32) for i in range(2)]
                junk = tile(es, "junkC", [128, S], BF16)
                biss = [tile(es, "bis%d" % i, [128, 8], F32) for i in range(2)]
                wfs = [tile(es, "wf%d" % i, [128, NIT + 2], F32) for i in range(2)]
                pw2 = tile(es, "pw2", [128, NIT + 2], F32)
                for i_ in range(NIT + 2):
                    op("dve", MSET(pw2.t[:, i_:i_ + 1], 2.0 ** -i_), pwrites=[pw2.b])
                dthr = tile(es, "dthr", [128, 128], F32)
                thrbc = tile(es, "thrbc", [128, 128], F32)
                maskT = tile(es, "maskT", [128, NKB, 128], BF16)
                maskT2 = tile(es, "maskT2", [128, NKB, 128], BF16)
                PTr = Ring([tile(es, "PT%d" % i, [128, 512], BF16) for i in range(3)])
                PMr = Ring([tile(es, "PM%d" % i, [128, 512], BF16) for i in range(3)])
                rz = tile(es, "rz", [128, 8], F32)
                attn_tm = tile(es, "attn_tm", [128, 8, 64], BF16)
                attnT_r = Ring([tile(es, "attnT_st%d" % i, [128, 4, QG], BF16) for i in range(2)])
                psL = Ring([PBs[0], PBs[1]])
                psS = PBs[2]
                psT = PBs[3]
                psA = Ring([PBs[4], PBs[5]])
                psO = [PBs[6], PBs[7]]
                maskTs = [maskT, maskT2]
                blocks = [(s_, qb_) for s_ in range(NSEQ) for qb_ in range(NKB)]
                gtiles = {}
                NQ = QG // 128

                def group_tiles(s_, qg):
                    key = (s_, qg)
                    if key not in gtiles:
                        qT = qTg.next()
                        qiT = qiTg.next()
                        tq0 = s_ * S + qg * QG
                        dma("sp", qiT.t[:], scr["qiT"].t.rearrange("(c p) t -> p c t", p=128)[:, :, tq0:tq0 + QG], reads=[scr["qiT"].b], writes=[qiT.b])
                        dma("sp", qT.t[:], scr["qT"].t.rearrange("(c p) t -> p c t", p=128)[:, :, tq0:tq0 + QG], reads=[scr["qT"].b], writes=[qT.b])
                        gtiles[key] = (qT, qiT, attnT_r.next())
                    return gtiles[key]

                xstate = {}

                def stage_X(bi, part, nparts):
                    s_, qb = blocks[bi]
                    sl = slice(s_ * S, (s_ + 1) * S)
                    qT, qiT, _ = group_tiles(s_, qb // NQ)
                    qs = qb % NQ
                    nkb = qb + 1
                    nk = nkb * 128
                    tix = (s_ * S) // 128 + qb
                    qsl = slice(qs * 128, (qs + 1) * 128)
                    score = scores[bi % 2]
                    bis = biss[bi % 2]
                    wf = wfs[bi % 2]
                    nch = (nk + 511) // 512
                    units = [(ch, h) for ch in range(nch) for h in range(8)]
                    nu = len(units)
                    ua, ub = (part * nu) // nparts, ((part + 1) * nu) // nparts

                    def logits(u):
                        ch, h = units[u]
                        hp = h % 2
                        k0 = ch * 512
                        w = min(512, nk - k0)
                        pl = psL.next()
                        op("pe", MM(pl.f[:, 0:w], qiT.t[hp * 64:(hp + 1) * 64, h // 2, qsl], kiTs.t[hp * 64:(hp + 1) * 64, k0:k0 + w]),
                           reads=[qiT.b, kiTs.b], pwrites=[pl.b])
                        xstate[(bi, u)] = pl

                    if part == 0:
                        if qb == 0:
                            dma("sp", kiTs.t[0:64, :], scr["kiT"].t[:, sl], reads=[scr["kiT"].b], writes=[kiTs.b])
                            dma("sp", kiTs.t[64:128, :], scr["kiT"].t[:, sl], reads=[scr["kiT"].b], pwrites=[kiTs.b])
                        op("dve", TT(Dh.t[:], ident_bf.t[:].unsqueeze(1).to_broadcast([128, 8, 128]),
                                     widx.t[:, tix, :].unsqueeze(2).to_broadcast([128, 8, 128]), ALU.mult),
                           reads=[ident_bf.b, widx.b], writes=[Dh.b])
                        logits(0)
                    for u in range(ua, ub):
                        if u + 1 < nu:
                            logits(u + 1)
                        ch, h = units[u]
                        k0 = ch * 512
                        w = min(512, nk - k0)
                        pl = xstate.pop((bi, u))
                        r = rr.next()
                        if h % 2 == 0:
                            op("act", ACTF(r.t[:, 0:w], pl.f[:, 0:w], AF.Relu), reads=[pl.b], writes=[r.b])
                        else:
                            op("dve", TS(r.t[:, 0:w], pl.f[:, 0:w], 0.0, None, ALU.max), reads=[pl.b], writes=[r.b])
                        op("pe", MM(psS.f[:, 0:w], Dh.t[:, h, :], r.t[:, 0:w], start=(h == 0), stop=(h == 7)),
                           reads=[Dh.b, r.b], pwrites=[psS.b])
                        if h == 7:
                            if ch == nch - 1:
                                wd = w - 128
                                if wd > 0:
                                    op("act", ACTF(score.t[:, k0:k0 + wd], psS.f[:, 0:wd], AF.Copy), reads=[psS.b], pwrites=[score.b])
                                op("dve", TT(score.t[:, nk - 128:nk], psS.f[:, wd:w], causal_f, ALU.add), reads=[psS.b, cst.b], pwrites=[score.b])
                            else:
                                op("act", ACTF(score.t[:, k0:k0 + w], psS.f[:, 0:w], AF.Copy), reads=[psS.b], pwrites=[score.b])
                    if part != nparts - 1:
                        return
                    lo, w0, mid, cnt, gg, mx = (bis.t[:, i:i + 1] for i in range(6))
                    if nk <= KSEL:
                        op("dve", MSET(lo, -1.0e29), writes=[bis.b])
                    else:
                        op("dve", RED(mx, score.t[:, 0:nk], ALU.max), reads=[score.b], writes=[bis.b])
                        op("dve", RED(lo, score.t[:, 0:nk - 128], ALU.min), reads=[score.b], writes=[bis.b])
                        op("dve", TT(w0, mx, lo, ALU.subtract), reads=[bis.b], writes=[bis.b])
                        op("dve", TS(wf.t[:], pw2.t[:], w0, None, ALU.mult), reads=[bis.b, pw2.b], writes=[wf.b])
                        op("dve", TT(mid, lo, wf.t[:, 1:2], ALU.add), reads=[bis.b, wf.b], writes=[bis.b])

                def stage_Xbis(bi, it0, it1):
                    s_, qb = blocks[bi]
                    nk = (qb + 1) * 128
                    if nk <= KSEL:
                        return
                    score = scores[bi % 2]
                    bis = biss[bi % 2]
                    wf = wfs[bi % 2]
                    lo, w0, mid, cnt, gg, mx = (bis.t[:, i:i + 1] for i in range(6))
                    for it in range(it0, it1):
                        op("dve", TS(junk.t[:, 0:nk], score.t[:, 0:nk], mid, None, ALU.is_ge, ALU.add, accum_out=cnt),
                           reads=[score.b, bis.b], writes=[bis.b, junk.b])
                        op("dve", TS(gg, cnt, KSEL - 0.5, 0.5, ALU.is_ge, ALU.subtract), reads=[bis.b], writes=[bis.b])
                        op("dve", STT(mid, gg, wf.t[:, it + 1:it + 2], mid, ALU.mult, ALU.add), reads=[bis.b, wf.b], writes=[bis.b])

                def stage_Xpost(bi):
                    s_, qb = blocks[bi]
                    nkb = qb + 1
                    nk = nkb * 128
                    mk = maskTs[bi % 2]
                    score = scores[bi % 2]
                    bis = biss[bi % 2]
                    wf = wfs[bi % 2]
                    lo, w0, mid, cnt, gg, mx = (bis.t[:, i:i + 1] for i in range(6))
                    if nk > KSEL:
                        op("dve", TT(lo, mid, wf.t[:, NIT + 1:NIT + 2], ALU.subtract), reads=[bis.b, wf.b], writes=[bis.b])
                    op("dve", TS(dthr.t[:], ident_f, lo, None, ALU.mult), reads=[cst.b, bis.b], writes=[dthr.b])
                    op("pe", MM(psT.f[:, 0:128], ones_f, dthr.t[:]), reads=[cst.b, dthr.b], pwrites=[psT.b])
                    op("act", ACTF(thrbc.t[:], psT.f[:, 0:128], AF.Copy), reads=[psT.b], writes=[thrbc.b])
                    for c4 in range((nkb + 3) // 4):
                        n4 = min(4, nkb - c4 * 4)
                        for i in range(n4):
                            kb = c4 * 4 + i
                            op("pe", TR(psT.f[:, i * 128:(i + 1) * 128], score.t[:, kb * 128:(kb + 1) * 128], ident_f),
                               reads=[score.b, cst.b], pwrites=[psT.b])
                        op("dve", TT(mk.t[:, c4 * 4:c4 * 4 + n4, :], psT.f[:, 0:n4 * 128].rearrange("p (a b) -> p a b", b=128),
                                     thrbc.t[:].unsqueeze(1).to_broadcast([128, n4, 128]), ALU.is_ge),
                           reads=[psT.b, thrbc.b], pwrites=[mk.b])

                def stage_Y(bi, hs, fin):
                    s_, qb = blocks[bi]
                    sl = slice(s_ * S, (s_ + 1) * S)
                    if qb == 0 and 0 in hs:
                        dma("sp", kTs.t[:], scr["kT"].t.rearrange("(c p) t -> p c t", p=128)[:, :, sl], reads=[scr["kT"].b], writes=[kTs.b])
                        dma("sp", Vs.t[:], scr["V"].t[sl, :].rearrange("(kb p) f -> p kb f", p=128), reads=[scr["V"].b], writes=[Vs.b])
                    qT, qiT, ast = group_tiles(s_, qb // NQ)
                    qs = qb % NQ
                    nkb = qb + 1
                    qsl = slice(qs * 128, (qs + 1) * 128)
                    mk = maskTs[bi % 2]
                    for h in hs:
                        hp = h % 2
                        po = psO[h // 4]
                        ng = (nkb + 3) // 4
                        pas = {}

                        def qk(c4, h=h, hp=hp):
                            n4 = min(4, nkb - c4 * 4)
                            pa = psA.next()
                            for i in range(n4):
                                kb = c4 * 4 + i
                                op("pe", MM(pa.f[:, i * 128:(i + 1) * 128], kTs.t[hp * 64:(hp + 1) * 64, h // 2, kb * 128:(kb + 1) * 128],
                                            qT.t[hp * 64:(hp + 1) * 64, h // 2, qsl]), reads=[kTs.b, qT.b], pwrites=[pa.b])
                            pas[c4] = pa

                        qk(0)
                        for c4 in range(ng):
                            n4 = min(4, nkb - c4 * 4)
                            if c4 + 1 < ng:
                                qk(c4 + 1)
                            pa = pas.pop(c4)
                            pt = PTr.next()
                            op("act", ACTF(pt.t[:, 0:n4 * 128], pa.f[:, 0:n4 * 128], AF.Exp), reads=[pa.b], writes=[pt.b])
                            pm = PMr.next()
                            op("dve", TT(pm.t[:, 0:n4 * 128], pt.t[:, 0:n4 * 128], mk.t[:, c4 * 4:c4 * 4 + n4, :].rearrange("p a b -> p (a b)"), ALU.mult),
                               reads=[pt.b, mk.b], writes=[pm.b])
                            for i in range(n4):
                                kb = c4 * 4 + i
                                op("pe", MM(po.f[:, (h % 4) * 65:(h % 4) * 65 + 65], pm.t[:, i * 128:(i + 1) * 128], Vs.t[:, kb, h * 65:(h + 1) * 65],
                                            start=(kb == 0), stop=(kb == nkb - 1)), reads=[pm.b, Vs.b], pwrites=[po.b])
                    if not fin:
                        return
                    for hh in range(2):
                        pv = psO[hh].f[:, 0:260].rearrange("p (h d) -> p h d", d=65)
                        op("dve", RCP(rz.t[:, hh * 4:(hh + 1) * 4], pv[:, :, 64]), reads=[psO[hh].b], pwrites=[rz.b])
                        op("dve", TT(attn_tm.t[:, hh * 4:(hh + 1) * 4, :], pv[:, :, 0:64],
                                     rz.t[:, hh * 4:(hh + 1) * 4].unsqueeze(2).to_broadcast([128, 4, 64]), ALU.mult),
                           reads=[psO[hh].b, rz.b], pwrites=[attn_tm.b])
                    px = psA.next()
                    for c in range(4):
                        op("pe", TR(px.h[:, c * 128:(c + 1) * 128], attn_tm.t[:, 2 * c:2 * c + 2, :].rearrange("p a b -> p (a b)"), ident_bf.t[:]),
                           reads=[attn_tm.b, ident_bf.b], pwrites=[px.b])
                    op("act", ACTF(ast.t[:, :, qsl], px.h[:, 0:512].rearrange("p (c t) -> p c t", c=4), AF.Copy),
                       reads=[px.b], pwrites=[ast.b])
                    if qs == NQ - 1:
                        tq0 = s_ * S + (qb // NQ) * QG
                        dma("sp", scr["attnT"].t.rearrange("(c p) t -> p c t", p=128)[:, :, tq0:tq0 + QG], ast.t[:], reads=[ast.b], pwrites=[scr["attnT"].b])

                NB = len(blocks)
                IPH = NIT // 8
                stage_X(0, 0, 1)
                stage_Xbis(0, 0, NIT)
                stage_Xpost(0)
                if NB > 1:
                    stage_X(1, 0, 1)
                for bi in range(NB):
                    for h in range(8):
                        if bi + 1 < NB:
                            stage_Xbis(bi + 1, h * IPH, (h + 1) * IPH if h < 7 else NIT)
                        if bi + 2 < NB:
                            stage_X(bi + 2, h, 8)
                        stage_Y(bi, [h], h == 7)
                    if bi + 1 < NB:
                        stage_Xpost(bi + 1)
                ctx.barrier()

        if "D" in phases:
            with ExitStack() as es:
                wao = tile(es, "wao", [128, 4, D], BF16)
                wo = tile(es, "wo", [128, KC, D], BF16)
                wpq = tile(es, "wpq", [128, KC, D], BF16)
                skTb = tile(es, "skTb", [128, 8, 128], BF16)
                with ExitStack() as es2:
                    stg = Ring([tile(es2, "stgD%d" % i, [128, KC, 512], F32) for i in range(2)])
                    ci_ = 0
                    for (src, dst) in ((io["w_o"], wo), (io["w_peer_q"], wpq)):
                        sv = src.rearrange("(kc p) f -> p kc f", p=128)
                        for hh in range(2):
                            st = stg.next()
                            dma("sp", st.t[:], sv[:, :, hh * 512:(hh + 1) * 512], writes=[st.b])
                            if ci_ % 2:
                                op("act", ACTF(dst.t[:, :, hh * 512:(hh + 1) * 512], st.t[:], AF.Copy), reads=[st.b], pwrites=[dst.b])
                            else:
                                op("dve", CP(dst.t[:, :, hh * 512:(hh + 1) * 512], st.t[:]), reads=[st.b], pwrites=[dst.b])
                            ci_ += 1
                    st = stg.next()
                    stv = st.t[:].rearrange("p k f -> p (k f)").rearrange("p (c d) -> p c d", c=4)
                    dma("sp", stv, io["w_attn_out"].rearrange("(c p) d -> p c d", p=128), writes=[st.b])
                    op("dve", CP(wao.t[:], stv), reads=[st.b], writes=[wao.b])
                    st = stg.next()
                    stv2 = st.t[:].rearrange("p k f -> p (k f)")[:, 0:1024].rearrange("p (h n) -> p h n", h=8)
                    dma("sp", stv2, io["skT"], writes=[st.b])
                    op("dve", CP(skTb.t[:], stv2), reads=[st.b], writes=[skTb.b])
                    ctx.barrier()
                gb1 = tile(es, "gb1", [128, D], F32)
                attg = Ring([tile(es, "attg%d" % i, [128, 4, GB], BF16) for i in range(2)])
                sgag = Ring([tile(es, "sgag%d" % i, [128, 8, GB], BF16) for i in range(2)])
                mxag = Ring([tile(es, "mxag%d" % i, [128, 8, GB], BF16) for i in range(2)])
                xgr = Ring([tile(es, "xgD%d" % i, [128, JB, D], F32) for i in range(2)])
                tmq = Ring([tile(es, "tmq%d" % i, [128, GB], F32) for i in range(2)])
                mixT = tile(es, "mixT", [128, 8, GB], BF16)
                tz = Ring([tile(es, "tz%d" % i, [128, 512], F32) for i in range(2)])
                x1 = tile(es, "x1", [128, JB, D], F32)
                junk = tile(es, "junkD", [128, D], BF16)
                ssq = tile(es, "ssqD", [128, JB], F32)
                rt = tile(es, "rtD", [128, JB], F32)
                rstd = tile(es, "rstdD", [128, JB], F32)
                xs = tile(es, "xsD", [128, JB, D], BF16)
                h2T = tile(es, "h2T", [128, KC, GB], BF16)
                qpT = tile(es, "qpT", [128, 8, GB], BF16)
                s_sb = tile(es, "s_sb", [128, 16, 128], F32)
                s_tmp = tile(es, "s_tmp", [128, 16, 128], F32)
                v_all = tile(es, "v_all", [128, 16, 16], F32)
                idx_all = tile(es, "idx_all", [128, 16, 16], U32)
                idx_bf = tile(es, "idx_bf", [128, 16, 16], BF16)
                cand = tile(es, "cand", [128, 8, 256], F32)
                cand2 = tile(es, "cand2", [128, 8, 256], F32)
                scv = tile(es, "scv", [128, 8, 16], F32)
                civ = tile(es, "civ", [128, 8, 16], U32)
                ca_u = tile(es, "ca_u", [128, 8, 16], U32)
                cb_u = tile(es, "cb_u", [128, 8, 16], U32)
                ca_bf = tile(es, "ca_bf", [128, 8, 16], BF16)
                cb_bf = tile(es, "cb_bf", [128, 8, 16], BF16)
                eqa = tile(es, "eqa", [128, 8, 16, 16], BF16)
                prd = tile(es, "prd", [128, 8, 16, 16], BF16)
                sm = tile(es, "sm", [128, 8, 16], F32)
                zz = tile(es, "zz", [128, 8], F32)
                sel_tm = tile(es, "sel_tm", [128, 3, 128], F32)
                selT = tile(es, "selT", [128, 3, 128], BF16)
                ptr = Ring([PBs[0], PBs[1]])
                gen = Ring([PBs[i] for i in range(2, 8)])
                evi = [0]

                def evacD(out, in_, rd, wr):
                    evi[0] += 1
                    if evi[0] % 2:
                        op("act", ACTF(out, in_, AF.Copy), reads=rd, pwrites=wr)
                    else:
                        op("dve", CP(out, in_), reads=rd, pwrites=wr)

                for g in range(T // GB):
                    t0 = g * GB
                    seq = t0 // S
                    if t0 % S == 0:
                        dma("sp", gb1.t[:], scr["gbc"].t[seq, 0, :, :], reads=[scr["gbc"].b], writes=[gb1.b])
                    at = attg.next()
                    sg = sgag.next()
                    ma = mxag.next()
                    xg = xgr.next()
                    dma("sp", at.t[:], scr["attnT"].t.rearrange("(c p) t -> p c t", p=128)[:, :, t0:t0 + GB], reads=[scr["attnT"].b], writes=[at.b])
                    dma("sp", sg.t[:], scr["sga"].t.rearrange("(c p) t -> p c t", p=128)[:, :, t0:t0 + GB], reads=[scr["sga"].b], writes=[sg.b])
                    dma("sp", ma.t[:], scr["mixA"].t.rearrange("(c p) t -> p c t", p=128)[:, :, t0:t0 + GB], reads=[scr["mixA"].b], writes=[ma.b])
                    dma("sp", xg.t[:], io["x"][t0:t0 + GB, :].rearrange("(j p) d -> p j d", p=128), writes=[xg.b])
                    for dc in range(8):
                        p = gen.next()
                        for c in range(4):
                            op("pe", MM(p.f[:, 0:GB], wao.t[:, c, dc * 128:(dc + 1) * 128], at.t[:, c, :], start=(c == 0), stop=(c == 3)),
                               reads=[wao.b, at.b], pwrites=[p.b])
                        tq = tmq.next()
                        op("dve", TT(tq.t[:], p.f[:, 0:GB], sg.t[:, dc, :], ALU.mult), reads=[p.b, sg.b], writes=[tq.b])
                        op("dve", TT(mixT.t[:, dc, :], tq.t[:], ma.t[:, dc, :], ALU.add), reads=[tq.b, ma.b], pwrites=[mixT.b])
                    for j in range(JB):
                        for dh in range(2):
                            p = gen.next()
                            for kc in range(KC):
                                op("pe", MM(p.f[:, 0:512], mixT.t[:, kc, j * 128:(j + 1) * 128], wo.t[:, kc, dh * 512:(dh + 1) * 512],
                                            start=(kc == 0), stop=(kc == KC - 1)), reads=[mixT.b, wo.b], pwrites=[p.b])
                            t_ = tz.next()
                            op("dve", TT(t_.t[:], p.f[:, 0:512], gb1.t[:, dh * 512:(dh + 1) * 512], ALU.mult), reads=[p.b, gb1.b], writes=[t_.b])
                            op("dve", TT(x1.t[:, j, dh * 512:(dh + 1) * 512], t_.t[:], xg.t[:, j, dh * 512:(dh + 1) * 512], ALU.add),
                               reads=[t_.b, xg.b], pwrites=[x1.b])
                    dma("sp", scr["x1"].t[t0:t0 + GB, :].rearrange("(j p) d -> p j d", p=128), x1.t[:], reads=[x1.b], pwrites=[scr["x1"].b])
                    for j in range(JB):
                        op("act", ACTF(junk.t[:], x1.t[:, j, :], AF.Square, accum_out=ssq.t[:, j:j + 1]), reads=[x1.b], writes=[junk.b], pwrites=[ssq.b])
                    op("act", ACTF(rt.t[:], ssq.t[:], AF.Sqrt, scale=1.0 / D, bias=epst.t[:, 0:1]), reads=[ssq.b, epst.b], writes=[rt.b])
                    op("dve", RCP(rstd.t[:], rt.t[:]), reads=[rt.b], writes=[rstd.b])
                    for j in range(JB):
                        op("dve", TS(xs.t[:, j, :], x1.t[:, j, :], rstd.t[:, j:j + 1], None, ALU.mult), reads=[x1.b, rstd.b], pwrites=[xs.b])
                    for kc in range(KC):
                        pt = ptr.next()
                        for j in range(JB):
                            op("pe", TR(pt.h[:, j * 128:(j + 1) * 128], xs.t[:, j, kc * 128:(kc + 1) * 128], ident_bf.t[:]),
                               reads=[xs.b, ident_bf.b], pwrites=[pt.b])
                        op("act", ACTF(h2T.t[:, kc, :], pt.h[:, 0:GB], AF.Identity, scale=A2.t[:, kc, seq:seq + 1], bias=modTb.t[:, 24 + kc, seq:seq + 1]),
                           reads=[pt.b, A2.b, modTb.b], pwrites=[h2T.b])
                    dma("sp", scr["h2T"].t.rearrange("(c p) t -> p c t", p=128)[:, :, t0:t0 + GB], h2T.t[:], reads=[h2T.b], pwrites=[scr["h2T"].b])
                    for h in range(8):
                        p = gen.next()
                        for kc in range(KC):
                            op("pe", MM(p.f[:, 0:GB], wpq.t[:, kc, h * 128:(h + 1) * 128], h2T.t[:, kc, :], start=(kc == 0), stop=(kc == KC - 1)),
                               reads=[wpq.b, h2T.b], pwrites=[p.b])
                        evacD(qpT.t[:, h, :], p.f[:, 0:GB], [p.b], [qpT.b])
                    for j in range(JB):
                        tt0 = t0 + j * 128
                        s4 = s_sb.t[:].rearrange("p (h s) n -> p h s n", s=2)
                        for b4 in range(4):
                            p = gen.next()
                            side, hb = b4 % 2, (b4 // 2) * 4
                            for i in range(4):
                                h = hb + i
                                op("pe", MM(p.f[:, i * 128:(i + 1) * 128], qpT.t[side * 64:(side + 1) * 64, h, j * 128:(j + 1) * 128],
                                            skTb.t[side * 64:(side + 1) * 64, h, :]), reads=[qpT.b, skTb.b], pwrites=[p.b])
                            op("act", ACTF(s4[:, hb:hb + 4, side, :], p.f[:, :].rearrange("p (a b) -> p a b", b=128), AF.Copy),
                               reads=[p.b], pwrites=[s_sb.b])
                        if DCUT < 2:
                            continue
                        for r in range(16):
                            op("dve", lambda e, r=r: e.max(out=v_all.t[:, r, 0:8], in_=s_sb.t[:, r, :]), reads=[s_sb.b], pwrites=[v_all.b])
                            op("dve", lambda e, r=r: e.max_index(out=idx_all.t[:, r, 0:8], in_max=v_all.t[:, r, 0:8], in_values=s_sb.t[:, r, :]),
                               reads=[s_sb.b, v_all.b], pwrites=[idx_all.b])
                            op("dve", lambda e, r=r: e.match_replace(out=s_tmp.t[:, r, :], in_to_replace=v_all.t[:, r, 0:8], in_values=s_sb.t[:, r, :], imm_value=NEG),
                               reads=[s_sb.b, v_all.b], pwrites=[s_tmp.b])
                            op("dve", lambda e, r=r: e.max(out=v_all.t[:, r, 8:16], in_=s_tmp.t[:, r, :]), reads=[s_tmp.b], pwrites=[v_all.b])
                            op("dve", lambda e, r=r: e.max_index(out=idx_all.t[:, r, 8:16], in_max=v_all.t[:, r, 8:16], in_values=s_tmp.t[:, r, :]),
                               reads=[s_tmp.b, v_all.b], pwrites=[idx_all.b])
                        if DCUT < 3:
                            continue
                        op("dve", CP(idx_bf.t[:], idx_all.t[:]), reads=[idx_all.b], writes=[idx_bf.b])
                        v4 = v_all.t[:].rearrange("p (h s) k -> p h s k", s=2)
                        op("dve", TT(cand.t[:].rearrange("p h (a b) -> p h a b", b=16), v4[:, :, 0, :].unsqueeze(3).to_broadcast([128, 8, 16, 16]),
                                     v4[:, :, 1, :].unsqueeze(2).to_broadcast([128, 8, 16, 16]), ALU.add), reads=[v_all.b], writes=[cand.b])
                        for h in range(8):
                            op("dve", lambda e, h=h: e.max(out=scv.t[:, h, 0:8], in_=cand.t[:, h, :]), reads=[cand.b], pwrites=[scv.b])
                            op("dve", lambda e, h=h: e.max_index(out=civ.t[:, h, 0:8], in_max=scv.t[:, h, 0:8], in_values=cand.t[:, h, :]),
                               reads=[cand.b, scv.b], pwrites=[civ.b])
                            op("dve", lambda e, h=h: e.match_replace(out=cand2.t[:, h, :], in_to_replace=scv.t[:, h, 0:8], in_values=cand.t[:, h, :], imm_value=NEG),
                               reads=[cand.b, scv.b], pwrites=[cand2.b])
                            op("dve", lambda e, h=h: e.max(out=scv.t[:, h, 8:16], in_=cand2.t[:, h, :]), reads=[cand2.b], pwrites=[scv.b])
                            op("dve", lambda e, h=h: e.max_index(out=civ.t[:, h, 8:16], in_max=scv.t[:, h, 8:16], in_values=cand2.t[:, h, :]),
                               reads=[cand2.b, scv.b], pwrites=[civ.b])
                        if DCUT < 4:
                            continue
                        op("dve", TT(sm.t[:], scv.t[:], scv.t[:, :, 0:1].to_broadcast([128, 8, 16]), ALU.subtract), reads=[scv.b], writes=[sm.b])
                        op("act", ACTF(sm.t[:], sm.t[:], AF.Exp), reads=[sm.b], writes=[sm.b])
                        op("dve", RED(zz.t[:], sm.t[:], ALU.add), reads=[sm.b], writes=[zz.b])
                        op("dve", RCP(zz.t[:], zz.t[:]), reads=[zz.b], writes=[zz.b])
                        op("dve", TT(sel_tm.t[:, 2, :].rearrange("p (h k) -> p h k", k=16), sm.t[:], zz.t[:].unsqueeze(2).to_broadcast([128, 8, 16]), ALU.mult),
                           reads=[sm.b, zz.b], pwrites=[sel_tm.b])
                        if DCUT < 5:
                            continue
                        op("dve", TS(ca_u.t[:], civ.t[:], 4, None, ALU.logical_shift_right), reads=[civ.b], writes=[ca_u.b])
                        op("dve", TS(cb_u.t[:], civ.t[:], 15, None, ALU.bitwise_and), reads=[civ.b], writes=[cb_u.b])
                        op("dve", CP(ca_bf.t[:], ca_u.t[:]), reads=[ca_u.b], writes=[ca_bf.b])
                        op("dve", CP(cb_bf.t[:], cb_u.t[:]), reads=[cb_u.b], writes=[cb_bf.b])
                        if DCUT < 6:
                            continue
                        i4 = idx_bf.t[:].rearrange("p (h s) k -> p h s k", s=2)
                        for side, cbf in ((0, ca_bf), (1, cb_bf)):
                            op("dve", TT(eqa.t[:], cbf.t[:].unsqueeze(3).to_broadcast([128, 8, 16, 16]),
                                         iota_bf.t[:, 0:16].unsqueeze(1).unsqueeze(1).to_broadcast([128, 8, 16, 16]), ALU.is_equal),
                               reads=[cbf.b, iota_bf.b], writes=[eqa.b])
                            op("dve", TT(prd.t[:], eqa.t[:], i4[:, :, side, :].unsqueeze(2).to_broadcast([128, 8, 16, 16]), ALU.mult),
                               reads=[eqa.b, idx_bf.b], writes=[prd.b])
                            op("dve", RED(sel_tm.t[:, side, :], prd.t[:].rearrange("p h k a -> p (h k) a"), ALU.add), reads=[prd.b], pwrites=[sel_tm.b])
                        if DCUT < 7:
                            continue
                        pt = ptr.next()
                        for c in range(3):
                            op("pe", TR(pt.f[:, c * 128:(c + 1) * 128], sel_tm.t[:, c, :], ident_f), reads=[sel_tm.b, cst.b], pwrites=[pt.b])
                        op("act", ACTF(selT.t[:], pt.f[:, 0:384].rearrange("p (c t) -> p c t", c=3), AF.Copy), reads=[pt.b], writes=[selT.b])
                        dma("sp", scr["sel"].t.rearrange("c p t -> p c t")[:, :, tt0:tt0 + 128], selT.t[:], reads=[selT.b], pwrites=[scr["sel"].b])
                ctx.barrier()

        if "E" in phases:
            with ExitStack() as es:
                sf = Ring([tile(es, "pf%d" % i, [128, 4096], F32) for i in range(3)])
                sbf = Ring([tile(es, "pb%d" % i, [128, 4096], BF16) for i in range(3)])
                uv = io["uT"].rearrange("(kc p) e -> p kc e", p=128)
                uo = scr["uTb"].t.rearrange("(kc p) e -> p kc e", p=128)
                vv = io["vp"].rearrange("(jj p) d -> p jj d", p=128)
                vo = scr["vb"].t.rearrange("(jj p) d -> p jj d", p=128)
                n = 0
                for i in range(NEXP // 512):
                    for (src, dst, key, v3) in ((uv[:, :, i * 512:(i + 1) * 512], uo[:, :, i * 512:(i + 1) * 512], "uTb", "p (a b) -> p a b"),
                                                (vv[:, i * 4:(i + 1) * 4, :], vo[:, i * 4:(i + 1) * 4, :], "vb", "p (a b) -> p a b")):
                        a_ = 8 if key == "uTb" else 4
                        f_ = sf.next()
                        b_ = sbf.next()
                        dma("sp", f_.t[:].rearrange(v3, a=a_), src, writes=[f_.b])
                        eng = ("dve", "act", "pool")[n % 3]
                        n += 1
                        if eng == "act":
                            op("act", ACTF(b_.t[:], f_.t[:], AF.Copy), reads=[f_.b], writes=[b_.b])
                        else:
                            op(eng, CP(b_.t[:], f_.t[:]), reads=[f_.b], writes=[b_.b])
                        dma("sp", dst, b_.t[:].rearrange(v3, a=a_), reads=[b_.b], pwrites=[scr[key].b])
                ctx.barrier()

        if "E" in phases:
            with ExitStack() as es:
                TE = 256
                SC = 2
                gb2 = tile(es, "gb2", [128, D], F32)
                x1r = Ring([tile(es, "x1E%d" % i, [128, 2, D], F32) for i in range(2)])
                h2r = Ring([tile(es, "h2E%d" % i, [128, KC, TE], BF16) for i in range(2)])
                selr = Ring([tile(es, "selE%d" % i, [128, 3, TE], BF16) for i in range(2)])
                Lr = Ring([tile(es, "L%d" % i, [128, 32, 128], BF16) for i in range(2)])
                L0r = Ring([tile(es, "L0%d" % i, [128, 32, 128], BF16) for i in range(1)])
                Rr = Ring([tile(es, "R%d" % i, [128, 32, 128], BF16) for i in range(2)])
                G = tile(es, "G", [128, TE, 128], BF16)
                uTr = Ring([tile(es, "uTs%d" % i, [128, KC, SC * 128], BF16) for i in range(3)])
                vr = Ring([tile(es, "vs%d" % i, [128, SC, D], BF16) for i in range(3)])
                Wr = Ring([tile(es, "W%d" % i, [128, TE], BF16) for i in range(3)])
                W2r = Ring([tile(es, "W2%d" % i, [128, TE], BF16) for i in range(3)])
                tz = Ring([tile(es, "tzE%d" % i, [128, 512], F32) for i in range(2)])
                yst = tile(es, "yst", [128, 2, D], F32)
                psOut = [[PBs[0], PBs[1]], [PBs[2], PBs[3]]]
                psAT = Ring([PBs[4], PBs[5]])
                psG = Ring([PBs[6], PBs[7]])
                uo = scr["uTb"].t.rearrange("(kc p) e -> p kc e", p=128)
                vo = scr["vb"].t.rearrange("(jj p) d -> p jj d", p=128)
                for tl in range(T // TE):
                    t0 = tl * TE
                    seq = t0 // S
                    if t0 % S == 0:
                        dma("sp", gb2.t[:], scr["gbc"].t[seq, 1, :, :], reads=[scr["gbc"].b], writes=[gb2.b])
                    x1t = x1r.next()
                    h2 = h2r.next()
                    sl_ = selr.next()
                    dma("sp", x1t.t[:], scr["x1"].t[t0:t0 + TE, :].rearrange("(j p) d -> p j d", p=128), reads=[scr["x1"].b], writes=[x1t.b])
                    dma("sp", h2.t[:], scr["h2T"].t.rearrange("(c p) t -> p c t", p=128)[:, :, t0:t0 + TE], reads=[scr["h2T"].b], writes=[h2.b])
                    dma("sp", sl_.t[:], scr["sel"].t.rearrange("c p t -> p c t")[:, :, t0:t0 + TE], reads=[scr["sel"].b], writes=[sl_.b])
                    for sb in range(TE // 32):
                        ts_ = slice(sb * 32, (sb + 1) * 32)
                        L0 = L0r.next()
                        L = Lr.next()
                        Rt = Rr.next()
                        iob = iota_bf.t[:].unsqueeze(1).to_broadcast([128, 32, 128])
                        op("dve", TT(L0.t[:], iob, sl_.t[:, 0, ts_].unsqueeze(2).to_broadcast([128, 32, 128]), ALU.is_equal),
                           reads=[iota_bf.b, sl_.b], writes=[L0.b])
                        op("dve", TT(L.t[:], L0.t[:], sl_.t[:, 2, ts_].unsqueeze(2).to_broadcast([128, 32, 128]), ALU.mult),
                           reads=[L0.b, sl_.b], writes=[L.b])
                        op("dve", TT(Rt.t[:], iob, sl_.t[:, 1, ts_].unsqueeze(2).to_broadcast([128, 32, 128]), ALU.is_equal),
                           reads=[iota_bf.b, sl_.b], writes=[Rt.b])
                        for q4 in range(8):
                            pg = psG.next()
                            for i in range(4):
                                t = q4 * 4 + i
                                op("pe", MM(pg.f[:, i * 128:(i + 1) * 128], L.t[:, t, :], Rt.t[:, t, :]), reads=[L.b, Rt.b], pwrites=[pg.b])
                            tt = sb * 32 + q4 * 4
                            op("act", ACTF(G.t[:, tt:tt + 4, :], pg.f[:, :].rearrange("p (a b) -> p a b", b=128), AF.Copy), reads=[pg.b], pwrites=[G.b])
                    chunks = {}

                    def load_sc(sc_):
                        uT = uTr.next()
                        vs = vr.next()
                        dma("sp", uT.t[:], uo[:, :, sc_ * SC * 128:(sc_ + 1) * SC * 128], reads=[scr["uTb"].b], writes=[uT.b])
                        dma("sp", vs.t[:], vo[:, sc_ * SC:(sc_ + 1) * SC, :], reads=[scr["vb"].b], writes=[vs.b])
                        for jj in range(SC):
                            chunks[sc_ * SC + jj] = (uT, vs, jj)

                    def stage1(j):
                        if j % SC == 0:
                            load_sc(j // SC)
                        uT, vs, jj = chunks[j]
                        pa = psAT.next()
                        for kc in range(KC):
                            op("pe", MM(pa.f[:, 0:TE], uT.t[:, kc, jj * 128:(jj + 1) * 128], h2.t[:, kc, :], start=(kc == 0), stop=(kc == KC - 1)),
                               reads=[uT.b, h2.b], pwrites=[pa.b])
                        W = Wr.next()
                        op("act", ACTF(W.t[:], pa.f[:, 0:TE], AF.Gelu), reads=[pa.b], writes=[W.b])
                        W2 = W2r.next()
                        op("dve", TT(W2.t[:], W.t[:], G.t[:, :, j], ALU.mult), reads=[W.b, G.b], writes=[W2.b])
                        return W2

                    def stage2(j, W2):
                        uT, vs, jj = chunks.pop(j)
                        for tb in range(2):
                            for dh in range(2):
                                po = psOut[tb][dh]
                                op("pe", MM(po.f[:, :], W2.t[:, tb * 128:(tb + 1) * 128], vs.t[:, jj, dh * 512:(dh + 1) * 512],
                                            start=(j == 0), stop=(j == 127)), reads=[W2.b, vs.b], pwrites=[po.b])

                    pend = stage1(0)
                    for j in range(128):
                        nxt = stage1(j + 1) if j + 1 < 128 else None
                        stage2(j, pend)
                        pend = nxt
                    for tb in range(2):
                        for dh in range(2):
                            po = psOut[tb][dh]
                            t_ = tz.next()
                            op("dve", TT(t_.t[:], po.f[:, :], gb2.t[:, dh * 512:(dh + 1) * 512], ALU.mult), reads=[po.b, gb2.b], writes=[t_.b])
                            op("dve", TT(yst.t[:, tb, dh * 512:(dh + 1) * 512], t_.t[:], x1t.t[:, tb, dh * 512:(dh + 1) * 512], ALU.add),
                               reads=[t_.b, x1t.b], pwrites=[yst.b])
                    dma("sp", y[t0:t0 + TE, :].rearrange("(j p) d -> p j d", p=128), yst.t[:], reads=[yst.b])
                ctx.barrier()

        ctx.finish()
    return nc, ctx


def host_consts():
    c = np.zeros((128, 5, 128), np.float32)
    c[:, 0, :] = np.eye(128, dtype=np.float32)
    qi = np.arange(128)[:, None]
    ki = np.arange(128)[None, :]
    c[:, 1, :] = np.where(ki <= qi, 0.0, NEG).astype(np.float32)
    c[:, 2, :] = ((qi // 64) == (ki // 64)).astype(np.float32)
    c[:, 3, :] = np.broadcast_to(np.arange(128, dtype=np.float32)[None, :], (128, 128))
    c[:, 4, :] = 1.0
    return c


def host_shared(inp):
    f = lambda a: np.ascontiguousarray(np.asarray(a, dtype=np.float32))
    sh = {}
    sh["w_ada"] = f(inp["w_ada"][0])
    sh["b_ada"] = f(inp["b_ada"][0])
    sh["b_adaT"] = f(np.asarray(inp["b_ada"][0]).reshape(48, 128).T)
    sh["n1T"] = f(np.asarray(inp["norm1_w"][0]).reshape(KC, 128).T)
    sh["n2T"] = f(np.asarray(inp["norm2_w"][0]).reshape(KC, 128).T)
    sh["w_in"] = f(inp["w_in"][0])
    sh["convT"] = f(np.asarray(inp["conv_w"][0]).reshape(3, 4, 128).transpose(2, 1, 0))
    sh["w_conv_out"] = f(inp["w_conv_out"][0])
    sh["qk_w"] = f(np.stack([np.tile(np.asarray(inp["q_norm_w"][0]), 2), np.tile(np.asarray(inp["k_norm_w"][0]), 2)], axis=1))
    sh["w_attn_out"] = f(inp["w_attn_out"][0])
    sh["w_o"] = f(inp["w_o"][0])
    sh["w_peer_q"] = f(inp["w_peer_q"][0])
    sk = np.asarray(inp["peer_sub_keys"][0])
    sh["skT"] = f(sk.transpose(1, 3, 0, 2).reshape(128, 8, 128))
    u = np.asarray(inp["peer_u"][0]).reshape(128, 128, D).transpose(1, 0, 2).reshape(NEXP, D)
    sh["uT"] = f(u.T)
    sh["vp"] = f(np.asarray(inp["peer_v"][0]).reshape(128, 128, D).transpose(1, 0, 2).reshape(NEXP, D))
    sh["consts"] = host_consts()
    return sh


def kernel(**inp):
    x = np.asarray(inp["x"], dtype=np.float32)
    c = np.asarray(inp["c"], dtype=np.float32)
    B, S, _ = x.shape
    ncores = 8
    NSEQ = B // ncores
    nc, _ = build(NSEQ, S)
    sh = host_shared(inp)
    in_maps = []
    for i in range(ncores):
        m = dict(sh)
        m["x"] = np.ascontiguousarray(x[i * NSEQ:(i + 1) * NSEQ].reshape(NSEQ * S, D))
        m["cT"] = np.ascontiguousarray(c[i * NSEQ:(i + 1) * NSEQ].reshape(NSEQ, KC, 128).transpose(2, 1, 0))
        in_maps.append(m)
    res = run_bass_kernel_spmd(nc, in_maps, core_ids=list(range(ncores)))
    out = np.concatenate([np.asarray(r["y"]).reshape(NSEQ, S, D) for r in res.results], axis=0)
    return out.astype(np.float32)
```

```python
import math
from contextlib import ExitStack

import numpy as np
import concourse.bass as bass
import concourse.mybir as mybir
from concourse.bass_utils import run_bass_kernel_spmd

F32 = mybir.dt.float32
BF16 = mybir.dt.bfloat16
U32 = mybir.dt.uint32
AF = mybir.ActivationFunctionType
ALU = mybir.AluOpType
AX = mybir.AxisListType

D = 1024
KC = 8
NIN = 5704
O_CB, O_CC, O_CX, O_Q, O_K, O_V, O_QI, O_KI, O_WI, O_GC, O_GA = 0, 512, 1024, 1536, 2048, 2560, 3072, 3584, 3648, 3656, 4680
NEXP = 16384
EPS = 1e-6
NEG = -1.0e30
NIT = 16
DCUT = 99
SELF_SYNC = True


class Buf:
    __slots__ = ("w", "r")

    def __init__(self):
        self.w = {}
        self.r = {}


class Tl:
    __slots__ = ("t", "b")

    def __init__(self, t):
        self.t = t
        self.b = Buf()


class Ring:
    def __init__(self, items):
        self.items = items
        self.i = 0

    def next(self):
        it = self.items[self.i % len(self.items)]
        self.i += 1
        return it


class Ctx:
    COMPUTE = ("pe", "act", "dve", "pool")
    ALL = ("pe", "act", "dve", "pool", "sp")
    NL = 8

    def __init__(self, nc):
        self.nc = nc
        self.prog = {n: [] for n in self.ALL}
        self.sems = {}
        self.cnt = {}
        self.known = {n: {} for n in self.ALL}
        for n in self.COMPUTE:
            self.sems[n] = nc.alloc_semaphore(name="s_" + n)
            self.cnt[n] = 0
        self.lane_rr = {}
        for q in ("sp", "act", "pool"):
            self.lane_rr[q] = 0
            for l in range(self.NL):
                k = (q, l)
                self.sems[k] = nc.alloc_semaphore(name="d_%s%d" % (q, l))
                self.cnt[k] = 0
        self.ninstr = 0

    def _wait(self, eng, key, val):
        if val <= 0:
            return
        if key == eng and (eng == "pe" or not SELF_SYNC):
            return
        if self.known[eng].get(key, 0) < val:
            self.prog[eng].append(("w", key, val))
            self.known[eng][key] = val

    def _deps(self, eng, reads, writes, pwrites):
        deps = {}
        for b in reads:
            for k, v in b.w.items():
                if deps.get(k, 0) < v:
                    deps[k] = v
        for b in writes:
            for dct in (b.w, b.r):
                for k, v in dct.items():
                    if deps.get(k, 0) < v:
                        deps[k] = v
        for b in pwrites:
            for k, v in b.r.items():
                if deps.get(k, 0) < v:
                    deps[k] = v
            for k, v in b.w.items():
                if k != eng and deps.get(k, 0) < v:
                    deps[k] = v
        for k, v in deps.items():
            self._wait(eng, k, v)

    def _mark(self, key, n, reads, writes, pwrites):
        for b in reads:
            b.r[key] = n
        for b in writes:
            b.w = {key: n}
            b.r = {}
        for b in pwrites:
            b.w[key] = n

    def op(self, eng, fn, reads=(), writes=(), pwrites=()):
        self._deps(eng, reads, writes, pwrites)
        self.cnt[eng] += 1
        self.prog[eng].append(("o", fn))
        self._mark(eng, self.cnt[eng], reads, writes, pwrites)
        self.ninstr += 1

    def dma(self, q, out, in_, reads=(), writes=(), pwrites=(), slow=False):
        l = self.lane_rr[q] % self.NL
        self.lane_rr[q] += 1
        key = (q, l)
        self._wait(q, key, self.cnt[key])
        self._deps(q, reads, writes, pwrites)
        self.cnt[key] += 16
        self.prog[q].append(("d", out, in_, key, slow))
        self._mark(key, self.cnt[key], reads, writes, pwrites)
        self.ninstr += 1

    def barrier(self):
        for e in self.ALL:
            for k in self.sems:
                self._wait(e, k, self.cnt[k])

    def finish(self):
        for k in self.sems:
            self._wait("sp", k, self.cnt[k])
        nc = self.nc

        def mk(name):
            def body(e):
                for it in self.prog[name]:
                    if it[0] == "w":
                        e.wait_ge(self.sems[it[1]], it[2])
                    elif it[0] == "o":
                        it[1](e).then_inc(self.sems[name], 1)
                    else:
                        if it[4]:
                            e.dma_start(out=it[1], in_=it[2], allow_slow_non_contiguous=True).then_inc(self.sems[it[3]], 16)
                        else:
                            e.dma_start(out=it[1], in_=it[2]).then_inc(self.sems[it[3]], 16)
            return body

        with nc.Block() as block:
            block.tensor(mk("pe"))
            block.scalar(mk("act"))
            block.vector(mk("dve"))
            block.gpsimd(mk("pool"))
            block.sync(mk("sp"))


def ACTF(out, in_, func, **kw):
    return lambda e: e.activation(out=out, in_=in_, func=func, **kw)


def MM(out, lhsT, rhs, start=True, stop=True):
    return lambda e: e.matmul(out, lhsT, rhs, start=start, stop=stop)


def TR(out, in_, ident):
    return lambda e: e.transpose(out, in_, ident)


def TS(out, in0, s1, s2, op0, op1=None, accum_out=None):
    if op1 is None:
        return lambda e: e.tensor_scalar(out=out, in0=in0, scalar1=s1, scalar2=s2, op0=op0, accum_out=accum_out)
    return lambda e: e.tensor_scalar(out=out, in0=in0, scalar1=s1, scalar2=s2, op0=op0, op1=op1, accum_out=accum_out)


def TT(out, in0, in1, op):
    return lambda e: e.tensor_tensor(out=out, in0=in0, in1=in1, op=op)


def STT(out, in0, scalar, in1, op0, op1):
    return lambda e: e.scalar_tensor_tensor(out=out, in0=in0, scalar=scalar, in1=in1, op0=op0, op1=op1)


def CP(out, in_):
    return lambda e: e.tensor_copy(out=out, in_=in_)


def RED(out, in_, op):
    return lambda e: e.tensor_reduce(out=out, in_=in_, axis=AX.X, op=op)


def RCP(out, in_):
    return lambda e: e.reciprocal(out=out, in_=in_)


def MSET(ap, c):
    return lambda e: e.memset(ap, c)


def build(NSEQ, S, GB=256, dbg=False, phases="ABCDE"):
    T = NSEQ * S
    NT = T // 128
    KSEL = min(256, S // 4)
    JB = GB // 128
    nc = bass.Bass("TRN2", target_bir_lowering=False)
    ctx = Ctx(nc)
    op, dma = ctx.op, ctx.dma

    def din(name, shape, dt=F32):
        return nc.dram_tensor(name, list(shape), dt, kind="ExternalInput").ap()

    io = {
        "x": din("x", [T, D]), "cT": din("cT", [128, KC, NSEQ]), "w_ada": din("w_ada", [D, 6 * D]),
        "b_ada": din("b_ada", [6 * D]), "b_adaT": din("b_adaT", [128, 48]),
        "n1T": din("n1T", [128, KC]), "n2T": din("n2T", [128, KC]), "w_in": din("w_in", [D, NIN]),
        "convT": din("convT", [128, 4, 3]), "w_conv_out": din("w_conv_out", [512, D]),
        "qk_w": din("qk_w", [128, 2]), "w_attn_out": din("w_attn_out", [512, D]), "w_o": din("w_o", [D, D]),
        "w_peer_q": din("w_peer_q", [D, D]), "skT": din("skT", [128, 8, 128]),
        "uT": din("uT", [D, NEXP]), "vp": din("vp", [NEXP, D]), "consts": din("consts", [128, 5, 128]),
    }
    y = nc.dram_tensor("y", [T, D], F32, kind="ExternalOutput").ap()
    skind = "ExternalOutput" if dbg else "Internal"

    def dscr(name, shape, dt):
        t = nc.dram_tensor(name, list(shape), dt, kind=skind).ap()
        return Tl(t)

    scr = {
        "mixA": dscr("s_mixA", [D, T], BF16), "sga": dscr("s_sga", [D, T], BF16),
        "qT": dscr("s_qT", [512, T], BF16), "kT": dscr("s_kT", [512, T], BF16),
        "qiT": dscr("s_qiT", [512, T], BF16), "kiT": dscr("s_kiT", [64, T], BF16),
        "V": dscr("s_V", [T, 520], BF16), "attnT": dscr("s_attnT", [512, T], BF16),
        "gbc": dscr("s_gbc", [NSEQ, 2, 128, D], F32), "x1": dscr("s_x1", [T, D], F32),
        "h2T": dscr("s_h2T", [D, T], BF16), "sel": dscr("s_sel", [3, 128, T], BF16),
        "uTb": dscr("s_uTb", [D, NEXP], BF16), "vb": dscr("s_vb", [NEXP, D], BF16),
    }

    with ExitStack() as gs:
        def tile(es, name, shape, dt):
            return Tl(es.enter_context(nc.sbuf_tensor("sb_" + name, list(shape), dt)))

        psum = gs.enter_context(nc.psum_tensor("psum", [128, 8, 512], F32))
        pb = [Buf() for _ in range(8)]

        class PB:
            def __init__(self, i):
                self.i = i
                self.b = pb[i]
                self.f = psum[:, i, :]
                self.h = psum[:, i, :].bitcast(BF16)

        PBs = [PB(i) for i in range(8)]

        cst = tile(gs, "cst", [128, 5, 128], F32)
        ident_bf = tile(gs, "ident_bf", [128, 128], BF16)
        bones_bf = tile(gs, "bones_bf", [128, 128], BF16)
        iota_bf = tile(gs, "iota_bf", [128, 128], BF16)
        epst = tile(gs, "epst", [128, 1], F32)
        modTb = tile(gs, "modTb", [128, 48, NSEQ], F32)
        A1 = tile(gs, "A1", [128, KC, NSEQ], F32)
        A2 = tile(gs, "A2", [128, KC, NSEQ], F32)
        widx = tile(gs, "widx", [128, NT, 8], F32)
        qkw = tile(gs, "qkw", [128, 2], F32)
        qws = tile(gs, "qws", [128, 1], F32)
        ident_f = cst.t[:, 0, :]
        causal_f = cst.t[:, 1, :]
        ones_f = cst.t[:, 4, :]

        dma("sp", cst.t[:], io["consts"], writes=[cst.b])
        dma("sp", qkw.t[:], io["qk_w"], writes=[qkw.b])
        op("dve", CP(ident_bf.t[:], cst.t[:, 0, :]), reads=[cst.b], writes=[ident_bf.b])
        op("dve", CP(bones_bf.t[:], cst.t[:, 2, :]), reads=[cst.b], writes=[bones_bf.b])
        op("dve", CP(iota_bf.t[:], cst.t[:, 3, :]), reads=[cst.b], writes=[iota_bf.b])
        op("dve", MSET(epst.t[:], EPS), writes=[epst.b])
        op("dve", TS(qws.t[:], qkw.t[:, 0:1], 0.125, None, ALU.mult), reads=[qkw.b], writes=[qws.b])

        if "A" in phases:
            with ExitStack() as es:
                cT = tile(es, "cT", [128, KC, NSEQ], F32)
                sc = tile(es, "sc", [128, KC, NSEQ], F32)
                scb = tile(es, "scb", [128, KC * NSEQ, 128], F32)
                badaT = tile(es, "badaT", [128, 48], F32)
                bbc = tile(es, "bbc", [128, 2, D], F32)
                n1T = tile(es, "n1T", [128, KC], F32)
                n2T = tile(es, "n2T", [128, KC], F32)
                war = Ring([tile(es, "wa%d" % i, [128, KC, 512], F32) for i in range(2)])
                gst = Ring([tile(es, "gst%d" % i, [128, 512], F32) for i in range(2)])
                dma("sp", cT.t[:], io["cT"], writes=[cT.b])
                dma("sp", badaT.t[:], io["b_adaT"], writes=[badaT.b])
                dma("sp", n1T.t[:], io["n1T"], writes=[n1T.b])
                dma("sp", n2T.t[:], io["n2T"], writes=[n2T.b])
                dma("sp", bbc.t[:, 0, :], io["b_ada"][2 * D:3 * D].partition_broadcast(128), pwrites=[bbc.b])
                dma("sp", bbc.t[:, 1, :], io["b_ada"][5 * D:6 * D].partition_broadcast(128), pwrites=[bbc.b])
                op("act", ACTF(sc.t[:], cT.t[:], AF.Silu), reads=[cT.b], writes=[sc.b])
                op("dve", CP(scb.t[:], sc.t[:].rearrange("p k s -> p (k s)").unsqueeze(2).to_broadcast([128, KC * NSEQ, 128])),
                   reads=[sc.b], writes=[scb.b])
                pM = PBs[0]
                pG = Ring([PBs[1], PBs[2]])
                wav = io["w_ada"].rearrange("(kc p) f -> p kc f", p=128)
                for blk in range(12):
                    wa = war.next()
                    dma("sp", wa.t[:], wav[:, :, blk * 512:(blk + 1) * 512], writes=[wa.b])
                    for fl in range(4):
                        fc = blk * 4 + fl
                        for kc in range(KC):
                            op("pe", MM(pM.f[:, fc * NSEQ:(fc + 1) * NSEQ], wa.t[:, kc, fl * 128:(fl + 1) * 128], sc.t[:, kc, :],
                                        start=(kc == 0), stop=(kc == KC - 1)), reads=[wa.b, sc.b], pwrites=[pM.b])
                    if blk in (4, 5, 10, 11):
                        which = 0 if blk < 6 else 1
                        half = blk % 2
                        for s in range(NSEQ):
                            p = pG.next()
                            for kc in range(KC):
                                op("pe", MM(p.f[:, :], scb.t[:, kc * NSEQ + s, :], wa.t[:, kc, :], start=(kc == 0), stop=(kc == KC - 1)),
                                   reads=[wa.b, scb.b], pwrites=[p.b])
                            g = gst.next()
                            op("dve", TT(g.t[:], p.f[:, :], bbc.t[:, which, half * 512:(half + 1) * 512], ALU.add),
                               reads=[p.b, bbc.b], writes=[g.b])
                            dma("sp", scr["gbc"].t[s, which, :, half * 512:(half + 1) * 512], g.t[:], reads=[g.b], pwrites=[scr["gbc"].b])
                op("dve", TT(modTb.t[:], pM.f[:, 0:48 * NSEQ].rearrange("p (f s) -> p f s", s=NSEQ),
                             badaT.t[:].unsqueeze(2).to_broadcast([128, 48, NSEQ]), ALU.add),
                   reads=[pM.b, badaT.b], writes=[modTb.b])
                op("dve", STT(A1.t[:], modTb.t[:, 8:16, :], 1.0, n1T.t[:].unsqueeze(2).to_broadcast([128, KC, NSEQ]), ALU.add, ALU.mult),
                   reads=[modTb.b, n1T.b], writes=[A1.b])
                op("dve", STT(A2.t[:], modTb.t[:, 32:40, :], 1.0, n2T.t[:].unsqueeze(2).to_broadcast([128, KC, NSEQ]), ALU.add, ALU.mult),
                   reads=[modTb.b, n2T.b], writes=[A2.b])
                ctx.barrier()

        if "B" in phases:
            with ExitStack() as es:
                win = tile(es, "win", [128, KC, NIN], BF16)
                wco = tile(es, "wco", [128, 4, D], BF16)
                convT = tile(es, "convT", [128, 4, 3], F32)
                dma("sp", convT.t[:], io["convT"], writes=[convT.b])
                with ExitStack() as es2:
                    stg = Ring([tile(es2, "stg%d" % i, [128, KC, 512], F32) for i in range(2)])
                    wiv = io["w_in"].rearrange("(kc p) f -> p kc f", p=128)
                    col = 0
                    i = 0
                    while col < NIN:
                        w = min(512, NIN - col)
                        st = stg.next()
                        dma("sp", st.t[:, :, 0:w], wiv[:, :, col:col + w], writes=[st.b])
                        eng = ("dve", "act", "pool")[i % 3]
                        if eng == "act":
                            op("act", ACTF(win.t[:, :, col:col + w], st.t[:, :, 0:w], AF.Copy), reads=[st.b], pwrites=[win.b])
                        else:
                            op(eng, CP(win.t[:, :, col:col + w], st.t[:, :, 0:w]), reads=[st.b], pwrites=[win.b])
                        col += w
                        i += 1
                    st = stg.next()
                    stv = st.t[:].rearrange("p k f -> p (k f)").rearrange("p (c d) -> p c d", c=4)
                    dma("sp", stv, io["w_conv_out"].rearrange("(c p) d -> p c d", p=128), writes=[st.b])
                    op("dve", CP(wco.t[:], stv), reads=[st.b], writes=[wco.b])
                    ctx.barrier()
                xgr = Ring([tile(es, "xg%d" % i, [128, JB, D], F32) for i in range(2)])
                junk = tile(es, "junkB", [128, D], BF16)
                ssq = tile(es, "ssq", [128, JB], F32)
                rt = tile(es, "rt", [128, JB], F32)
                rstd = tile(es, "rstd", [128, JB], F32)
                xs = tile(es, "xs", [128, JB, D], BF16)
                hTr = Ring([tile(es, "hT%d" % i, [128, KC, GB], BF16) for i in range(2)])
                ubuf = tile(es, "ubuf", [128, 4, GB + 2], F32)
                tmpf = Ring([tile(es, "tmpf%d" % i, [128, GB], F32) for i in range(3)])
                c1 = tile(es, "c1", [128, GB], F32)
                c2 = tile(es, "c2", [128, GB], F32)
                c3 = tile(es, "c3", [128, GB], F32)
                yA = tile(es, "yA", [128, 4, GB], BF16)
                mixA_st = tile(es, "mixA_st", [128, 8, GB], BF16)
                sga_st = tile(es, "sga_st", [128, 8, GB], BF16)
                q_st = tile(es, "q_st", [128, 4, GB], BF16)
                k_st = tile(es, "k_st", [128, 4, GB], BF16)
                qi_st = tile(es, "qi_st", [128, 4, GB], BF16)
                ki_st = tile(es, "ki_st", [64, GB], BF16)
                v_st = tile(es, "v_st", [128, JB, 8, 65], BF16)
                sqr = Ring([tile(es, "sq%d" % i, [128, GB], BF16) for i in range(2)])
                rtr = Ring([tile(es, "rtq%d" % i, [128, GB], F32) for i in range(2)])
                rrr = Ring([tile(es, "rrq%d" % i, [128, GB], F32) for i in range(2)])
                op("pool", MSET(v_st.t[:], 1.0), writes=[v_st.b])
                ptr = Ring([PBs[0], PBs[1]])
                gen = Ring([PBs[i] for i in range(2, 8)])
                evi = [0]

                def evac(out, in_, rd, wr):
                    evi[0] += 1
                    if evi[0] % 2:
                        op("act", ACTF(out, in_, AF.Copy), reads=rd, pwrites=wr)
                    else:
                        op("dve", CP(out, in_), reads=rd, pwrites=wr)

                for g in range(T // GB):
                    t0 = g * GB
                    seq = t0 // S
                    first = (t0 % S == 0)
                    xg = xgr.next()
                    hT = hTr.next()
                    dma("sp", xg.t[:], io["x"][t0:t0 + GB, :].rearrange("(j p) d -> p j d", p=128), writes=[xg.b])
                    for j in range(JB):
                        op("act", ACTF(junk.t[:], xg.t[:, j, :], AF.Square, accum_out=ssq.t[:, j:j + 1]), reads=[xg.b], writes=[junk.b], pwrites=[ssq.b])
                    op("act", ACTF(rt.t[:], ssq.t[:], AF.Sqrt, scale=1.0 / D, bias=epst.t[:, 0:1]), reads=[ssq.b, epst.b], writes=[rt.b])
                    op("dve", RCP(rstd.t[:], rt.t[:]), reads=[rt.b], writes=[rstd.b])
                    for j in range(JB):
                        op("dve", TS(xs.t[:, j, :], xg.t[:, j, :], rstd.t[:, j:j + 1], None, ALU.mult), reads=[xg.b, rstd.b], pwrites=[xs.b])
                    for kc in range(KC):
                        pt = ptr.next()
                        for j in range(JB):
                            op("pe", TR(pt.h[:, j * 128:(j + 1) * 128], xs.t[:, j, kc * 128:(kc + 1) * 128], ident_bf.t[:]),
                               reads=[xs.b, ident_bf.b], pwrites=[pt.b])
                        op("act", ACTF(hT.t[:, kc, :], pt.h[:, 0:GB], AF.Identity, scale=A1.t[:, kc, seq:seq + 1], bias=modTb.t[:, kc, seq:seq + 1]),
                           reads=[pt.b, A1.b, modTb.b], pwrites=[hT.b])

                    def proj(c0, M=128):
                        p = gen.next()
                        for kc in range(KC):
                            op("pe", MM(p.f[0:M, 0:GB], win.t[:, kc, c0:c0 + M], hT.t[:, kc, :], start=(kc == 0), stop=(kc == KC - 1)),
                               reads=[win.b, hT.b], pwrites=[p.b])
                        return p

                    if first:
                        op("dve", MSET(ubuf.t[:, :, 0:2], 0.0), pwrites=[ubuf.b])
                    for cch in range(4):
                        pcc = proj(O_CC + cch * 128)
                        pcx = proj(O_CX + cch * 128)
                        pcb = proj(O_CB + cch * 128)
                        tf = tmpf.next()
                        op("act", ACTF(tf.t[:], pcc.f[:, 0:GB], AF.Copy), reads=[pcc.b], writes=[tf.b])
                        op("dve", TT(ubuf.t[:, cch, 2:2 + GB], pcx.f[:, 0:GB], tf.t[:], ALU.mult), reads=[pcx.b, tf.b], pwrites=[ubuf.b])
                        op("dve", TS(c1.t[:], ubuf.t[:, cch, 0:GB], convT.t[:, cch, 0:1], None, ALU.mult), reads=[ubuf.b, convT.b], writes=[c1.b])
                        op("dve", STT(c2.t[:], ubuf.t[:, cch, 1:1 + GB], convT.t[:, cch, 1:2], c1.t[:], ALU.mult, ALU.add),
                           reads=[ubuf.b, convT.b, c1.b], writes=[c2.b])
                        op("dve", STT(c3.t[:], ubuf.t[:, cch, 2:2 + GB], convT.t[:, cch, 2:3], c2.t[:], ALU.mult, ALU.add),
                           reads=[ubuf.b, convT.b, c2.b], writes=[c3.b])
                        op("dve", TT(yA.t[:, cch, :], pcb.f[:, 0:GB], c3.t[:], ALU.mult), reads=[pcb.b, c3.b], pwrites=[yA.b])
                        op("dve", CP(ubuf.t[:, cch, 0:2], ubuf.t[:, cch, GB:GB + 2]), reads=[ubuf.b], pwrites=[ubuf.b])
                    for dc in range(8):
                        pyc = gen.next()
                        for cch in range(4):
                            op("pe", MM(pyc.f[:, 0:GB], wco.t[:, cch, dc * 128:(dc + 1) * 128], yA.t[:, cch, :], start=(cch == 0), stop=(cch == 3)),
                               reads=[wco.b, yA.b], pwrites=[pyc.b])
                        pgc = proj(O_GC + dc * 128)
                        tf = tmpf.next()
                        op("act", ACTF(tf.t[:], pgc.f[:, 0:GB], AF.Sigmoid), reads=[pgc.b], writes=[tf.b])
                        op("dve", TT(mixA_st.t[:, dc, :], pyc.f[:, 0:GB], tf.t[:], ALU.mult), reads=[pyc.b, tf.b], pwrites=[mixA_st.b])
                    dma("sp", scr["mixA"].t.rearrange("(dc p) t -> p dc t", p=128)[:, :, t0:t0 + GB], mixA_st.t[:], reads=[mixA_st.b], pwrites=[scr["mixA"].b])
                    for dc in range(8):
                        pga = proj(O_GA + dc * 128)
                        op("act", ACTF(sga_st.t[:, dc, :], pga.f[:, 0:GB], AF.Sigmoid), reads=[pga.b], pwrites=[sga_st.b])
                    dma("sp", scr["sga"].t.rearrange("(dc p) t -> p dc t", p=128)[:, :, t0:t0 + GB], sga_st.t[:], reads=[sga_st.b], pwrites=[scr["sga"].b])
                    for (base, wap, wb, st, nm) in ((O_Q, qws.t[:, 0:1], qws.b, q_st, "qT"), (O_K, qkw.t[:, 1:2], qkw.b, k_st, "kT")):
                        for c in range(4):
                            pq = proj(base + c * 128)
                            sq = sqr.next()
                            op("act", ACTF(sq.t[:], pq.f[:, 0:GB], AF.Square), reads=[pq.b], writes=[sq.b])
                            ps2 = gen.next()
                            op("pe", MM(ps2.f[:, 0:GB], bones_bf.t[:], sq.t[:]), reads=[bones_bf.b, sq.b], pwrites=[ps2.b])
                            r1 = rtr.next()
                            op("act", ACTF(r1.t[:], ps2.f[:, 0:GB], AF.Sqrt, scale=1.0 / 64, bias=epst.t[:, 0:1]), reads=[ps2.b, epst.b], writes=[r1.b])
                            r2 = rrr.next()
                            op("dve", RCP(r2.t[:], r1.t[:]), reads=[r1.b], writes=[r2.b])
                            op("dve", STT(st.t[:, c, :], pq.f[:, 0:GB], wap, r2.t[:], ALU.mult, ALU.mult), reads=[pq.b, wb, r2.b], pwrites=[st.b])
                        dma("sp", scr[nm].t.rearrange("(c p) t -> p c t", p=128)[:, :, t0:t0 + GB], st.t[:], reads=[st.b], pwrites=[scr[nm].b])
                    for c in range(4):
                        pq = proj(O_QI + c * 128)
                        evac(qi_st.t[:, c, :], pq.f[:, 0:GB], [pq.b], [qi_st.b])
                    dma("sp", scr["qiT"].t.rearrange("(c p) t -> p c t", p=128)[:, :, t0:t0 + GB], qi_st.t[:], reads=[qi_st.b], pwrites=[scr["qiT"].b])
                    pq = proj(O_KI, M=64)
                    evac(ki_st.t[:, :], pq.f[0:64, 0:GB], [pq.b], [ki_st.b])
                    dma("sp", scr["kiT"].t[:, t0:t0 + GB], ki_st.t[:], reads=[ki_st.b], pwrites=[scr["kiT"].b])
                    for j in range(JB):
                        p = gen.next()
                        for kc in range(KC):
                            op("pe", MM(p.f[:, 0:512], hT.t[:, kc, j * 128:(j + 1) * 128], win.t[:, kc, O_V:O_V + 512], start=(kc == 0), stop=(kc == KC - 1)),
                               reads=[win.b, hT.b], pwrites=[p.b])
                        evac(v_st.t[:, j, :, 0:64], p.f[:, 0:512].rearrange("p (h d) -> p h d", h=8), [p.b], [v_st.b])
                        p = gen.next()
                        for kc in range(KC):
                            op("pe", MM(p.f[:, 0:8], hT.t[:, kc, j * 128:(j + 1) * 128], win.t[:, kc, O_WI:O_WI + 8], start=(kc == 0), stop=(kc == KC - 1)),
                               reads=[win.b, hT.b], pwrites=[p.b])
                        op("dve", CP(widx.t[:, t0 // 128 + j, :], p.f[:, 0:8]), reads=[p.b], pwrites=[widx.b])
                    dma("sp", scr["V"].t[t0:t0 + GB, :].rearrange("(j p) f -> p j f", p=128), v_st.t[:].rearrange("p j h d -> p j (h d)"),
                        reads=[v_st.b], pwrites=[scr["V"].b])
                ctx.barrier()

        if "C" in phases:
            with ExitStack() as es:
                NKB = S // 128
                QG = min(512, S)
                kTs = tile(es, "kTs", [128, 4, S], BF16)
                Vs = tile(es, "Vs", [128, NKB, 520], BF16)
                kiTs = tile(es, "kiTs", [128, S], BF16)
                qTg = Ring([tile(es, "qTg%d" % i, [128, 4, QG], BF16) for i in range(2)])
                qiTg = Ring([tile(es, "qiTg%d" % i, [128, 4, QG], BF16) for i in range(2)])
                Dh = tile(es, "Dh", [128, 8, 128], BF16)
                rr = Ring([tile(es, "relu%d" % i, [128, 512], BF16) for i in range(4)])
                scores = [tile(es, "score%d" % i, [128, S], F32) for i in range(2)]
                junk = tile(es, "junkC", [128, S], BF16)
                biss = [tile(es, "bis%d" % i, [128, 8], F32) for i in range(2)]
                wfs = [tile(es, "wf%d" % i, [128, NIT + 2], F32) for i in range(2)]
                pw2 = tile(es, "pw2", [128, NIT + 2], F32)
                for i_ in range(NIT + 2):
                    op("dve", MSET(pw2.t[:, i_:i_ + 1], 2.0 ** -i_), pwrites=[pw2.b])
                dthr = tile(es, "dthr", [128, 128], F32)
                thrbc = tile(es, "thrbc", [128, 128], F32)
                maskT = tile(es, "maskT", [128, NKB, 128], BF16)
                maskT2 = tile(es, "maskT2", [128, NKB, 128], BF16)
                PTr = Ring([tile(es, "PT%d" % i, [128, 512], BF16) for i in range(3)])
                PMr = Ring([tile(es, "PM%d" % i, [128, 512], BF16) for i in range(3)])
                rz = tile(es, "rz", [128, 8], F32)
                attn_tm = tile(es, "attn_tm", [128, 8, 64], BF16)
                attnT_r = Ring([tile(es, "attnT_st%d" % i, [128, 4, QG], BF16) for i in range(2)])
                psL = Ring([PBs[0], PBs[1]])
                psS = PBs[2]
                psT = PBs[3]
                psA = Ring([PBs[4], PBs[5]])
                psO = [PBs[6], PBs[7]]
                maskTs = [maskT, maskT2]
                blocks = [(s_, qb_) for s_ in range(NSEQ) for qb_ in range(NKB)]
                gtiles = {}
                NQ = QG // 128

                def group_tiles(s_, qg):
                    key = (s_, qg)
                    if key not in gtiles:
                        qT = qTg.next()
                        qiT = qiTg.next()
                        tq0 = s_ * S + qg * QG
                        dma("sp", qiT.t[:], scr["qiT"].t.rearrange("(c p) t -> p c t", p=128)[:, :, tq0:tq0 + QG], reads=[scr["qiT"].b], writes=[qiT.b])
                        dma("sp", qT.t[:], scr["qT"].t.rearrange("(c p) t -> p c t", p=128)[:, :, tq0:tq0 + QG], reads=[scr["qT"].b], writes=[qT.b])
                        gtiles[key] = (qT, qiT, attnT_r.next())
                    return gtiles[key]

                xstate = {}

                def stage_X(bi, part, nparts):
                    s_, qb = blocks[bi]
                    sl = slice(s_ * S, (s_ + 1) * S)
                    qT, qiT, _ = group_tiles(s_, qb // NQ)
                    qs = qb % NQ
                    nkb = qb + 1
                    nk = nkb * 128
                    tix = (s_ * S) // 128 + qb
                    qsl = slice(qs * 128, (qs + 1) * 128)
                    score = scores[bi % 2]
                    bis = biss[bi % 2]
                    wf = wfs[bi % 2]
                    nch = (nk + 511) // 512
                    units = [(ch, h) for ch in range(nch) for h in range(8)]
                    nu = len(units)
                    ua, ub = (part * nu) // nparts, ((part + 1) * nu) // nparts

                    def logits(u):
                        ch, h = units[u]
                        hp = h % 2
                        k0 = ch * 512
                        w = min(512, nk - k0)
                        pl = psL.next()
                        op("pe", MM(pl.f[:, 0:w], qiT.t[hp * 64:(hp + 1) * 64, h // 2, qsl], kiTs.t[hp * 64:(hp + 1) * 64, k0:k0 + w]),
                           reads=[qiT.b, kiTs.b], pwrites=[pl.b])
                        xstate[(bi, u)] = pl

                    if part == 0:
                        if qb == 0:
                            dma("sp", kiTs.t[0:64, :], scr["kiT"].t[:, sl], reads=[scr["kiT"].b], writes=[kiTs.b])
                            dma("sp", kiTs.t[64:128, :], scr["kiT"].t[:, sl], reads=[scr["kiT"].b], pwrites=[kiTs.b])
                        op("dve", TT(Dh.t[:], ident_bf.t[:].unsqueeze(1).to_broadcast([128, 8, 128]),
                                     widx.t[:, tix, :].unsqueeze(2).to_broadcast([128, 8, 128]), ALU.mult),
                           reads=[ident_bf.b, widx.b], writes=[Dh.b])
                        logits(0)
                    for u in range(ua, ub):
                        if u + 1 < nu:
                            logits(u + 1)
                        ch, h = units[u]
                        k0 = ch * 512
                        w = min(512, nk - k0)
                        pl = xstate.pop((bi, u))
                        r = rr.next()
                        if h % 2 == 0:
                            op("act", ACTF(r.t[:, 0:w], pl.f[:, 0:w], AF.Relu), reads=[pl.b], writes=[r.b])
                        else:
                            op("dve", TS(r.t[:, 0:w], pl.f[:, 0:w], 0.0, None, ALU.max), reads=[pl.b], writes=[r.b])
                        op("pe", MM(psS.f[:, 0:w], Dh.t[:, h, :], r.t[:, 0:w], start=(h == 0), stop=(h == 7)),
                           reads=[Dh.b, r.b], pwrites=[psS.b])
                        if h == 7:
                            if ch == nch - 1:
                                wd = w - 128
                                if wd > 0:
                                    op("act", ACTF(score.t[:, k0:k0 + wd], psS.f[:, 0:wd], AF.Copy), reads=[psS.b], pwrites=[score.b])
                                op("dve", TT(score.t[:, nk - 128:nk], psS.f[:, wd:w], causal_f, ALU.add), reads=[psS.b, cst.b], pwrites=[score.b])
                            else:
                                op("act", ACTF(score.t[:, k0:k0 + w], psS.f[:, 0:w], AF.Copy), reads=[psS.b], pwrites=[score.b])
                    if part != nparts - 1:
                        return
                    lo, w0, mid, cnt, gg, mx = (bis.t[:, i:i + 1] for i in range(6))
                    if nk <= KSEL:
                        op("dve", MSET(lo, -1.0e29), writes=[bis.b])
                    else:
                        op("dve", RED(mx, score.t[:, 0:nk], ALU.max), reads=[score.b], writes=[bis.b])
                        op("dve", RED(lo, score.t[:, 0:nk - 128], ALU.min), reads=[score.b], writes=[bis.b])
                        op("dve", TT(w0, mx, lo, ALU.subtract), reads=[bis.b], writes=[bis.b])
                        op("dve", TS(wf.t[:], pw2.t[:], w0, None, ALU.mult), reads=[bis.b, pw2.b], writes=[wf.b])
                        op("dve", TT(mid, lo, wf.t[:, 1:2], ALU.add), reads=[bis.b, wf.b], writes=[bis.b])

                def stage_Xbis(bi, it0, it1):
                    s_, qb = blocks[bi]
                    nk = (qb + 1) * 128
                    if nk <= KSEL:
                        return
                    score = scores[bi % 2]
                    bis = biss[bi % 2]
                    wf = wfs[bi % 2]
                    lo, w0, mid, cnt, gg, mx = (bis.t[:, i:i + 1] for i in range(6))
                    for it in range(it0, it1):
                        op("dve", TS(junk.t[:, 0:nk], score.t[:, 0:nk], mid, None, ALU.is_ge, ALU.add, accum_out=cnt),
                           reads=[score.b, bis.b], writes=[bis.b, junk.b])
                        op("dve", TS(gg, cnt, KSEL - 0.5, 0.5, ALU.is_ge, ALU.subtract), reads=[bis.b], writes=[bis.b])
                        op("dve", STT(mid, gg, wf.t[:, it + 1:it + 2], mid, ALU.mult, ALU.add), reads=[bis.b, wf.b], writes=[bis.b])

                def stage_Xpost(bi):
                    s_, qb = blocks[bi]
                    nkb = qb + 1
                    nk = nkb * 128
                    mk = maskTs[bi % 2]
                    score = scores[bi % 2]
                    bis = biss[bi % 2]
                    wf = wfs[bi % 2]
                    lo, w0, mid, cnt, gg, mx = (bis.t[:, i:i + 1] for i in range(6))
                    if nk > KSEL:
                        op("dve", TT(lo, mid, wf.t[:, NIT + 1:NIT + 2], ALU.subtract), reads=[bis.b, wf.b], writes=[bis.b])
                    op("dve", TS(dthr.t[:], ident_f, lo, None, ALU.mult), reads=[cst.b, bis.b], writes=[dthr.b])
                    op("pe", MM(psT.f[:, 0:128], ones_f, dthr.t[:]), reads=[cst.b, dthr.b], pwrites=[psT.b])
                    op("act", ACTF(thrbc.t[:], psT.f[:, 0:128], AF.Copy), reads=[psT.b], writes=[thrbc.b])
                    for c4 in range((nkb + 3) // 4):
                        n4 = min(4, nkb - c4 * 4)
                        for i in range(n4):
                            kb = c4 * 4 + i
                            op("pe", TR(psT.f[:, i * 128:(i + 1) * 128], score.t[:, kb * 128:(kb + 1) * 128], ident_f),
                               reads=[score.b, cst.b], pwrites=[psT.b])
                        op("dve", TT(mk.t[:, c4 * 4:c4 * 4 + n4, :], psT.f[:, 0:n4 * 128].rearrange("p (a b) -> p a b", b=128),
                                     thrbc.t[:].unsqueeze(1).to_broadcast([128, n4, 128]), ALU.is_ge),
                           reads=[psT.b, thrbc.b], pwrites=[mk.b])

                def stage_Y(bi, hs, fin):
                    s_, qb = blocks[bi]
                    sl = slice(s_ * S, (s_ + 1) * S)
                    if qb == 0 and 0 in hs:
                        dma("sp", kTs.t[:], scr["kT"].t.rearrange("(c p) t -> p c t", p=128)[:, :, sl], reads=[scr["kT"].b], writes=[kTs.b])
                        dma("sp", Vs.t[:], scr["V"].t[sl, :].rearrange("(kb p) f -> p kb f", p=128), reads=[scr["V"].b], writes=[Vs.b])
                    qT, qiT, ast = group_tiles(s_, qb // NQ)
                    qs = qb % NQ
                    nkb = qb + 1
                    qsl = slice(qs * 128, (qs + 1) * 128)
                    mk = maskTs[bi % 2]
                    for h in hs:
                        hp = h % 2
                        po = psO[h // 4]
                        ng = (nkb + 3) // 4
                        pas = {}

                        def qk(c4, h=h, hp=hp):
                            n4 = min(4, nkb - c4 * 4)
                            pa = psA.next()
                            for i in range(n4):
                                kb = c4 * 4 + i
                                op("pe", MM(pa.f[:, i * 128:(i + 1) * 128], kTs.t[hp * 64:(hp + 1) * 64, h // 2, kb * 128:(kb + 1) * 128],
                                            qT.t[hp * 64:(hp + 1) * 64, h // 2, qsl]), reads=[kTs.b, qT.b], pwrites=[pa.b])
                            pas[c4] = pa

                        qk(0)
                        for c4 in range(ng):
                            n4 = min(4, nkb - c4 * 4)
                            if c4 + 1 < ng:
                                qk(c4 + 1)
                            pa = pas.pop(c4)
                            pt = PTr.next()
                            op("act", ACTF(pt.t[:, 0:n4 * 128], pa.f[:, 0:n4 * 128], AF.Exp), reads=[pa.b], writes=[pt.b])
                            pm = PMr.next()
                            op("dve", TT(pm.t[:, 0:n4 * 128], pt.t[:, 0:n4 * 128], mk.t[:, c4 * 4:c4 * 4 + n4, :].rearrange("p a b -> p (a b)"), ALU.mult),
                               reads=[pt.b, mk.b], writes=[pm.b])
                            for i in range(n4):
                                kb = c4 * 4 + i
                                op("pe", MM(po.f[:, (h % 4) * 65:(h % 4) * 65 + 65], pm.t[:, i * 128:(i + 1) * 128], Vs.t[:, kb, h * 65:(h + 1) * 65],
                                            start=(kb == 0), stop=(kb == nkb - 1)), reads=[pm.b, Vs.b], pwrites=[po.b])
                    if not fin:
                        return
                    for hh in range(2):
                        pv = psO[hh].f[:, 0:260].rearrange("p (h d) -> p h d", d=65)
                        op("dve", RCP(rz.t[:, hh * 4:(hh + 1) * 4], pv[:, :, 64]), reads=[psO[hh].b], pwrites=[rz.b])
                        op("dve", TT(attn_tm.t[:, hh * 4:(hh + 1) * 4, :], pv[:, :, 0:64],
                                     rz.t[:, hh * 4:(hh + 1) * 4].unsqueeze(2).to_broadcast([128, 4, 64]), ALU.mult),
                           reads=[psO[hh].b, rz.b], pwrites=[attn_tm.b])
                    px = psA.next()
                    for c in range(4):
                        op("pe", TR(px.h[:, c * 128:(c + 1) * 128], attn_tm.t[:, 2 * c:2 * c + 2, :].rearrange("p a b -> p (a b)"), ident_bf.t[:]),
                           reads=[attn_tm.b, ident_bf.b], pwrites=[px.b])
                    op("act", ACTF(ast.t[:, :, qsl], px.h[:, 0:512].rearrange("p (c t) -> p c t", c=4), AF.Copy),
                       reads=[px.b], pwrites=[ast.b])
                    if qs == NQ - 1:
                        tq0 = s_ * S + (qb // NQ) * QG
                        dma("sp", scr["attnT"].t.rearrange("(c p) t -> p c t", p=128)[:, :, tq0:tq0 + QG], ast.t[:], reads=[ast.b], pwrites=[scr["attnT"].b])

                NB = len(blocks)
                IPH = NIT // 8
                stage_X(0, 0, 1)
                stage_Xbis(0, 0, NIT)
                stage_Xpost(0)
                if NB > 1:
                    stage_X(1, 0, 1)
                for bi in range(NB):
                    for h in range(8):
                        if bi + 1 < NB:
                            stage_Xbis(bi + 1, h * IPH, (h + 1) * IPH if h < 7 else NIT)
                        if bi + 2 < NB:
                            stage_X(bi + 2, h, 8)
                        stage_Y(bi, [h], h == 7)
                    if bi + 1 < NB:
                        stage_Xpost(bi + 1)
                ctx.barrier()

        if "D" in phases:
            with ExitStack() as es:
                wao = tile(es, "wao", [128, 4, D], BF16)
                wo = tile(es, "wo", [128, KC, D], BF16)
                wpq = tile(es, "wpq", [128, KC, D], BF16)
                skTb = tile(es, "skTb", [128, 8, 128], BF16)
                with ExitStack() as es2:
                    stg = Ring([tile(es2, "stgD%d" % i, [128, KC, 512], F32) for i in range(2)])
                    ci_ = 0
                    for (src, dst) in ((io["w_o"], wo), (io["w_peer_q"], wpq)):
                        sv = src.rearrange("(kc p) f -> p kc f", p=128)
                        for hh in range(2):
                            st = stg.next()
                            dma("sp", st.t[:], sv[:, :, hh * 512:(hh + 1) * 512], writes=[st.b])
                            if ci_ % 2:
                                op("act", ACTF(dst.t[:, :, hh * 512:(hh + 1) * 512], st.t[:], AF.Copy), reads=[st.b], pwrites=[dst.b])
                            else:
                                op("dve", CP(dst.t[:, :, hh * 512:(hh + 1) * 512], st.t[:]), reads=[st.b], pwrites=[dst.b])
                            ci_ += 1
                    st = stg.next()
                    stv = st.t[:].rearrange("p k f -> p (k f)").rearrange("p (c d) -> p c d", c=4)
                    dma("sp", stv, io["w_attn_out"].rearrange("(c p) d -> p c d", p=128), writes=[st.b])
                    op("dve", CP(wao.t[:], stv), reads=[st.b], writes=[wao.b])
                    st = stg.next()
                    stv2 = st.t[:].rearrange("p k f -> p (k f)")[:, 0:1024].rearrange("p (h n) -> p h n", h=8)
                    dma("sp", stv2, io["skT"], writes=[st.b])
                    op("dve", CP(skTb.t[:], stv2), reads=[st.b], writes=[skTb.b])
                    ctx.barrier()
                gb1 = tile(es, "gb1", [128, D], F32)
                attg = Ring([tile(es, "attg%d" % i, [128, 4, GB], BF16) for i in range(2)])
                sgag = Ring([tile(es, "sgag%d" % i, [128, 8, GB], BF16) for i in range(2)])
                mxag = Ring([tile(es, "mxag%d" % i, [128, 8, GB], BF16) for i in range(2)])
                xgr = Ring([tile(es, "xgD%d" % i, [128, JB, D], F32) for i in range(2)])
                tmq = Ring([tile(es, "tmq%d" % i, [128, GB], F32) for i in range(2)])
                mixT = tile(es, "mixT", [128, 8, GB], BF16)
                tz = Ring([tile(es, "tz%d" % i, [128, 512], F32) for i in range(2)])
                x1 = tile(es, "x1", [128, JB, D], F32)
                junk = tile(es, "junkD", [128, D], BF16)
                ssq = tile(es, "ssqD", [128, JB], F32)
                rt = tile(es, "rtD", [128, JB], F32)
                rstd = tile(es, "rstdD", [128, JB], F32)
                xs = tile(es, "xsD", [128, JB, D], BF16)
                h2T = tile(es, "h2T", [128, KC, GB], BF16)
                qpT = tile(es, "qpT", [128, 8, GB], BF16)
                s_sb = tile(es, "s_sb", [128, 16, 128], F32)
                s_tmp = tile(es, "s_tmp", [128, 16, 128], F32)
                v_all = tile(es, "v_all", [128, 16, 16], F32)
                idx_all = tile(es, "idx_all", [128, 16, 16], U32)
                idx_bf = tile(es, "idx_bf", [128, 16, 16], BF16)
                cand = tile(es, "cand", [128, 8, 256], F32)
                cand2 = tile(es, "cand2", [128, 8, 256], F32)
                scv = tile(es, "scv", [128, 8, 16], F32)
                civ = tile(es, "civ", [128, 8, 16], U32)
                ca_u = tile(es, "ca_u", [128, 8, 16], U32)
                cb_u = tile(es, "cb_u", [128, 8, 16], U32)
                ca_bf = tile(es, "ca_bf", [128, 8, 16], BF16)
                cb_bf = tile(es, "cb_bf", [128, 8, 16], BF16)
                eqa = tile(es, "eqa", [128, 8, 16, 16], BF16)
                prd = tile(es, "prd", [128, 8, 16, 16], BF16)
                sm = tile(es, "sm", [128, 8, 16], F32)
                zz = tile(es, "zz", [128, 8], F32)
                sel_tm = tile(es, "sel_tm", [128, 3, 128], F32)
                selT = tile(es, "selT", [128, 3, 128], BF16)
                ptr = Ring([PBs[0], PBs[1]])
                gen = Ring([PBs[i] for i in range(2, 8)])
                evi = [0]

                def evacD(out, in_, rd, wr):
                    evi[0] += 1
                    if evi[0] % 2:
                        op("act", ACTF(out, in_, AF.Copy), reads=rd, pwrites=wr)
                    else:
                        op("dve", CP(out, in_), reads=rd, pwrites=wr)

                for g in range(T // GB):
                    t0 = g * GB
                    seq = t0 // S
                    if t0 % S == 0:
                        dma("sp", gb1.t[:], scr["gbc"].t[seq, 0, :, :], reads=[scr["gbc"].b], writes=[gb1.b])
                    at = attg.next()
                    sg = sgag.next()
                    ma = mxag.next()
                    xg = xgr.next()
                    dma("sp", at.t[:], scr["attnT"].t.rearrange("(c p) t -> p c t", p=128)[:, :, t0:t0 + GB], reads=[scr["attnT"].b], writes=[at.b])
                    dma("sp", sg.t[:], scr["sga"].t.rearrange("(c p) t -> p c t", p=128)[:, :, t0:t0 + GB], reads=[scr["sga"].b], writes=[sg.b])
                    dma("sp", ma.t[:], scr["mixA"].t.rearrange("(c p) t -> p c t", p=128)[:, :, t0:t0 + GB], reads=[scr["mixA"].b], writes=[ma.b])
                    dma("sp", xg.t[:], io["x"][t0:t0 + GB, :].rearrange("(j p) d -> p j d", p=128), writes=[xg.b])
                    for dc in range(8):
                        p = gen.next()
                        for c in range(4):
                            op("pe", MM(p.f[:, 0:GB], wao.t[:, c, dc * 128:(dc + 1) * 128], at.t[:, c, :], start=(c == 0), stop=(c == 3)),
                               reads=[wao.b, at.b], pwrites=[p.b])
                        tq = tmq.next()
                        op("dve", TT(tq.t[:], p.f[:, 0:GB], sg.t[:, dc, :], ALU.mult), reads=[p.b, sg.b], writes=[tq.b])
                        op("dve", TT(mixT.t[:, dc, :], tq.t[:], ma.t[:, dc, :], ALU.add), reads=[tq.b, ma.b], pwrites=[mixT.b])
                    for j in range(JB):
                        for dh in range(2):
                            p = gen.next()
                            for kc in range(KC):
                                op("pe", MM(p.f[:, 0:512], mixT.t[:, kc, j * 128:(j + 1) * 128], wo.t[:, kc, dh * 512:(dh + 1) * 512],
                                            start=(kc == 0), stop=(kc == KC - 1)), reads=[mixT.b, wo.b], pwrites=[p.b])
                            t_ = tz.next()
                            op("dve", TT(t_.t[:], p.f[:, 0:512], gb1.t[:, dh * 512:(dh + 1) * 512], ALU.mult), reads=[p.b, gb1.b], writes=[t_.b])
                            op("dve", TT(x1.t[:, j, dh * 512:(dh + 1) * 512], t_.t[:], xg.t[:, j, dh * 512:(dh + 1) * 512], ALU.add),
                               reads=[t_.b, xg.b], pwrites=[x1.b])
                    dma("sp", scr["x1"].t[t0:t0 + GB, :].rearrange("(j p) d -> p j d", p=128), x1.t[:], reads=[x1.b], pwrites=[scr["x1"].b])
                    for j in range(JB):
                        op("act", ACTF(junk.t[:], x1.t[:, j, :], AF.Square, accum_out=ssq.t[:, j:j + 1]), reads=[x1.b], writes=[junk.b], pwrites=[ssq.b])
                    op("act", ACTF(rt.t[:], ssq.t[:], AF.Sqrt, scale=1.0 / D, bias=epst.t[:, 0:1]), reads=[ssq.b, epst.b], writes=[rt.b])
                    op("dve", RCP(rstd.t[:], rt.t[:]), reads=[rt.b], writes=[rstd.b])
                    for j in range(JB):
                        op("dve", TS(xs.t[:, j, :], x1.t[:, j, :], rstd.t[:, j:j + 1], None, ALU.mult), reads=[x1.b, rstd.b], pwrites=[xs.b])
                    for kc in range(KC):
                        pt = ptr.next()
                        for j in range(JB):
                            op("pe", TR(pt.h[:, j * 128:(j + 1) * 128], xs.t[:, j, kc * 128:(kc + 1) * 128], ident_bf.t[:]),
                               reads=[xs.b, ident_bf.b], pwrites=[pt.b])
                        op("act", ACTF(h2T.t[:, kc, :], pt.h[:, 0:GB], AF.Identity, scale=A2.t[:, kc, seq:seq + 1], bias=modTb.t[:, 24 + kc, seq:seq + 1]),
                           reads=[pt.b, A2.b, modTb.b], pwrites=[h2T.b])
                    dma("sp", scr["h2T"].t.rearrange("(c p) t -> p c t", p=128)[:, :, t0:t0 + GB], h2T.t[:], reads=[h2T.b], pwrites=[scr["h2T"].b])
                    for h in range(8):
                        p = gen.next()
                        for kc in range(KC):
                            op("pe", MM(p.f[:, 0:GB], wpq.t[:, kc, h * 128:(h + 1) * 128], h2T.t[:, kc, :], start=(kc == 0), stop=(kc == KC - 1)),
                               reads=[wpq.b, h2T.b], pwrites=[p.b])
                        evacD(qpT.t[:, h, :], p.f[:, 0:GB], [p.b], [qpT.b])
                    for j in range(JB):
                        tt0 = t0 + j * 128
                        s4 = s_sb.t[:].rearrange("p (h s) n -> p h s n", s=2)
                        for b4 in range(4):
                            p = gen.next()
                            side, hb = b4 % 2, (b4 // 2) * 4
                            for i in range(4):
                                h = hb + i
                                op("pe", MM(p.f[:, i * 128:(i + 1) * 128], qpT.t[side * 64:(side + 1) * 64, h, j * 128:(j + 1) * 128],
                                            skTb.t[side * 64:(side + 1) * 64, h, :]), reads=[qpT.b, skTb.b], pwrites=[p.b])
                            op("act", ACTF(s4[:, hb:hb + 4, side, :], p.f[:, :].rearrange("p (a b) -> p a b", b=128), AF.Copy),
                               reads=[p.b], pwrites=[s_sb.b])
                        if DCUT < 2:
                            continue
                        for r in range(16):
                            op("dve", lambda e, r=r: e.max(out=v_all.t[:, r, 0:8], in_=s_sb.t[:, r, :]), reads=[s_sb.b], pwrites=[v_all.b])
                        for r in range(16):
                            op("dve", lambda e, r=r: e.max_index(out=idx_all.t[:, r, 0:8], in_max=v_all.t[:, r, 0:8], in_values=s_sb.t[:, r, :]),
                               reads=[s_sb.b, v_all.b], pwrites=[idx_all.b])
                        for r in range(16):
                            op("dve", lambda e, r=r: e.match_replace(out=s_tmp.t[:, r, :], in_to_replace=v_all.t[:, r, 0:8], in_values=s_sb.t[:, r, :], imm_value=NEG),
                               reads=[s_sb.b, v_all.b], pwrites=[s_tmp.b])
                        for r in range(16):
                            op("dve", lambda e, r=r: e.max(out=v_all.t[:, r, 8:16], in_=s_tmp.t[:, r, :]), reads=[s_tmp.b], pwrites=[v_all.b])
                        for r in range(16):
                            op("dve", lambda e, r=r: e.max_index(out=idx_all.t[:, r, 8:16], in_max=v_all.t[:, r, 8:16], in_values=s_tmp.t[:, r, :]),
                               reads=[s_tmp.b, v_all.b], pwrites=[idx_all.b])
                        op("dve", CP(idx_bf.t[:], idx_all.t[:]), reads=[idx_all.b], writes=[idx_bf.b])
                        v4 = v_all.t[:].rearrange("p (h s) k -> p h s k", s=2)
                        op("dve", TT(cand.t[:].rearrange("p h (a b) -> p h a b", b=16), v4[:, :, 0, :].unsqueeze(3).to_broadcast([128, 8, 16, 16]),
                                     v4[:, :, 1, :].unsqueeze(2).to_broadcast([128, 8, 16, 16]), ALU.add), reads=[v_all.b], writes=[cand.b])
                        for h in range(8):
                            op("dve", lambda e, h=h: e.max(out=scv.t[:, h, 0:8], in_=cand.t[:, h, :]), reads=[cand.b], pwrites=[scv.b])
                        for h in range(8):
                            op("dve", lambda e, h=h: e.max_index(out=civ.t[:, h, 0:8], in_max=scv.t[:, h, 0:8], in_values=cand.t[:, h, :]),
                               reads=[cand.b, scv.b], pwrites=[civ.b])
                        for h in range(8):
                            op("dve", lambda e, h=h: e.match_replace(out=cand2.t[:, h, :], in_to_replace=scv.t[:, h, 0:8], in_values=cand.t[:, h, :], imm_value=NEG),
                               reads=[cand.b, scv.b], pwrites=[cand2.b])
                        for h in range(8):
                            op("dve", lambda e, h=h: e.max(out=scv.t[:, h, 8:16], in_=cand2.t[:, h, :]), reads=[cand2.b], pwrites=[scv.b])
                        for h in range(8):
                            op("dve", lambda e, h=h: e.max_index(out=civ.t[:, h, 8:16], in_max=scv.t[:, h, 8:16], in_values=cand2.t[:, h, :]),
                               reads=[cand2.b, scv.b], pwrites=[civ.b])
                        op("dve", TT(sm.t[:], scv.t[:], scv.t[:, :, 0:1].to_broadcast([128, 8, 16]), ALU.subtract), reads=[scv.b], writes=[sm.b])
                        op("act", ACTF(sm.t[:], sm.t[:], AF.Exp), reads=[sm.b], writes=[sm.b])
                        op("dve", RED(zz.t[:], sm.t[:], ALU.add), reads=[sm.b], writes=[zz.b])
                        op("dve", RCP(zz.t[:], zz.t[:]), reads=[zz.b], writes=[zz.b])
                        op("dve", TT(sel_tm.t[:, 2, :].rearrange("p (h k) -> p h k", k=16), sm.t[:], zz.t[:].unsqueeze(2).to_broadcast([128, 8, 16]), ALU.mult),
                           reads=[sm.b, zz.b], pwrites=[sel_tm.b])
                        if DCUT < 5:
                            continue
                        op("dve", TS(ca_u.t[:], civ.t[:], 4, None, ALU.logical_shift_right), reads=[civ.b], writes=[ca_u.b])
                        op("dve", TS(cb_u.t[:], civ.t[:], 15, None, ALU.bitwise_and), reads=[civ.b], writes=[cb_u.b])
                        op("dve", CP(ca_bf.t[:], ca_u.t[:]), reads=[ca_u.b], writes=[ca_bf.b])
                        op("dve", CP(cb_bf.t[:], cb_u.t[:]), reads=[cb_u.b], writes=[cb_bf.b])
                        if DCUT < 6:
                            continue
                        i4 = idx_bf.t[:].rearrange("p (h s) k -> p h s k", s=2)
                        for side, cbf in ((0, ca_bf), (1, cb_bf)):
                            op("dve", TT(eqa.t[:], cbf.t[:].unsqueeze(3).to_broadcast([128, 8, 16, 16]),
                                         iota_bf.t[:, 0:16].unsqueeze(1).unsqueeze(1).to_broadcast([128, 8, 16, 16]), ALU.is_equal),
                               reads=[cbf.b, iota_bf.b], writes=[eqa.b])
                            op("dve", TT(prd.t[:], eqa.t[:], i4[:, :, side, :].unsqueeze(2).to_broadcast([128, 8, 16, 16]), ALU.mult),
                               reads=[eqa.b, idx_bf.b], writes=[prd.b])
                            op("dve", RED(sel_tm.t[:, side, :], prd.t[:].rearrange("p h k a -> p (h k) a"), ALU.add), reads=[prd.b], pwrites=[sel_tm.b])
                        if DCUT < 7:
                            continue
                        pt = ptr.next()
                        for c in range(3):
                            op("pe", TR(pt.f[:, c * 128:(c + 1) * 128], sel_tm.t[:, c, :], ident_f), reads=[sel_tm.b, cst.b], pwrites=[pt.b])
                        op("act", ACTF(selT.t[:], pt.f[:, 0:384].rearrange("p (c t) -> p c t", c=3), AF.Copy), reads=[pt.b], writes=[selT.b])
                        dma("sp", scr["sel"].t.rearrange("c p t -> p c t")[:, :, tt0:tt0 + 128], selT.t[:], reads=[selT.b], pwrites=[scr["sel"].b])
                ctx.barrier()

        if "E" in phases:
            with ExitStack() as es:
                sf = Ring([tile(es, "pf%d" % i, [128, 4096], F32) for i in range(3)])
                sbf = Ring([tile(es, "pb%d" % i, [128, 4096], BF16) for i in range(3)])
                uv = io["uT"].rearrange("(kc p) e -> p kc e", p=128)
                uo = scr["uTb"].t.rearrange("(kc p) e -> p kc e", p=128)
                vv = io["vp"].rearrange("(jj p) d -> p jj d", p=128)
                vo = scr["vb"].t.rearrange("(jj p) d -> p jj d", p=128)
                n = 0
                for i in range(NEXP // 512):
                    for (src, dst, key, v3) in ((uv[:, :, i * 512:(i + 1) * 512], uo[:, :, i * 512:(i + 1) * 512], "uTb", "p (a b) -> p a b"),
                                                (vv[:, i * 4:(i + 1) * 4, :], vo[:, i * 4:(i + 1) * 4, :], "vb", "p (a b) -> p a b")):
                        a_ = 8 if key == "uTb" else 4
                        f_ = sf.next()
                        b_ = sbf.next()
                        dma("sp", f_.t[:].rearrange(v3, a=a_), src, writes=[f_.b])
                        eng = ("dve", "act", "pool")[n % 3]
                        n += 1
                        if eng == "act":
                            op("act", ACTF(b_.t[:], f_.t[:], AF.Copy), reads=[f_.b], writes=[b_.b])
                        else:
                            op(eng, CP(b_.t[:], f_.t[:]), reads=[f_.b], writes=[b_.b])
                        dma("sp", dst, b_.t[:].rearrange(v3, a=a_), reads=[b_.b], pwrites=[scr[key].b])
                ctx.barrier()

        if "E" in phases:
            with ExitStack() as es:
                TE = 256
                SC = 2
                gb2 = tile(es, "gb2", [128, D], F32)
                x1r = Ring([tile(es, "x1E%d" % i, [128, 2, D], F32) for i in range(2)])
                h2r = Ring([tile(es, "h2E%d" % i, [128, KC, TE], BF16) for i in range(2)])
                selr = Ring([tile(es, "selE%d" % i, [128, 3, TE], BF16) for i in range(2)])
                Lr = Ring([tile(es, "L%d" % i, [128, 32, 128], BF16) for i in range(2)])
                L0r = Ring([tile(es, "L0%d" % i, [128, 32, 128], BF16) for i in range(1)])
                Rr = Ring([tile(es, "R%d" % i, [128, 32, 128], BF16) for i in range(2)])
                G = tile(es, "G", [128, TE, 128], BF16)
                uTr = Ring([tile(es, "uTs%d" % i, [128, KC, SC * 128], BF16) for i in range(3)])
                vr = Ring([tile(es, "vs%d" % i, [128, SC, D], BF16) for i in range(3)])
                Wr = Ring([tile(es, "W%d" % i, [128, TE], BF16) for i in range(3)])
                W2r = Ring([tile(es, "W2%d" % i, [128, TE], BF16) for i in range(3)])
                tz = Ring([tile(es, "tzE%d" % i, [128, 512], F32) for i in range(2)])
                yst = tile(es, "yst", [128, 2, D], F32)
                psOut = [[PBs[0], PBs[1]], [PBs[2], PBs[3]]]
                psAT = Ring([PBs[4], PBs[5]])
                psG = Ring([PBs[6], PBs[7]])
                uo = scr["uTb"].t.rearrange("(kc p) e -> p kc e", p=128)
                vo = scr["vb"].t.rearrange("(jj p) d -> p jj d", p=128)
                for tl in range(T // TE):
                    t0 = tl * TE
                    seq = t0 // S
                    if t0 % S == 0:
                        dma("sp", gb2.t[:], scr["gbc"].t[seq, 1, :, :], reads=[scr["gbc"].b], writes=[gb2.b])
                    x1t = x1r.next()
                    h2 = h2r.next()
                    sl_ = selr.next()
                    dma("sp", x1t.t[:], scr["x1"].t[t0:t0 + TE, :].rearrange("(j p) d -> p j d", p=128), reads=[scr["x1"].b], writes=[x1t.b])
                    dma("sp", h2.t[:], scr["h2T"].t.rearrange("(c p) t -> p c t", p=128)[:, :, t0:t0 + TE], reads=[scr["h2T"].b], writes=[h2.b])
                    dma("sp", sl_.t[:], scr["sel"].t.rearrange("c p t -> p c t")[:, :, t0:t0 + TE], reads=[scr["sel"].b], writes=[sl_.b])
                    for sb in range(TE // 32):
                        ts_ = slice(sb * 32, (sb + 1) * 32)
                        L0 = L0r.next()
                        L = Lr.next()
                        Rt = Rr.next()
                        iob = iota_bf.t[:].unsqueeze(1).to_broadcast([128, 32, 128])
                        op("dve", TT(L0.t[:], iob, sl_.t[:, 0, ts_].unsqueeze(2).to_broadcast([128, 32, 128]), ALU.is_equal),
                           reads=[iota_bf.b, sl_.b], writes=[L0.b])
                        op("dve", TT(L.t[:], L0.t[:], sl_.t[:, 2, ts_].unsqueeze(2).to_broadcast([128, 32, 128]), ALU.mult),
                           reads=[L0.b, sl_.b], writes=[L.b])
                        op("dve", TT(Rt.t[:], iob, sl_.t[:, 1, ts_].unsqueeze(2).to_broadcast([128, 32, 128]), ALU.is_equal),
                           reads=[iota_bf.b, sl_.b], writes=[Rt.b])
                        for q4 in range(8):
                            pg = psG.next()
                            for i in range(4):
                                t = q4 * 4 + i
                                op("pe", MM(pg.f[:, i * 128:(i + 1) * 128], L.t[:, t, :], Rt.t[:, t, :]), reads=[L.b, Rt.b], pwrites=[pg.b])
                            tt = sb * 32 + q4 * 4
                            op("act", ACTF(G.t[:, tt:tt + 4, :], pg.f[:, :].rearrange("p (a b) -> p a b", b=128), AF.Copy), reads=[pg.b], pwrites=[G.b])
                    chunks = {}

                    def load_sc(sc_):
                        uT = uTr.next()
                        vs = vr.next()
                        dma("sp", uT.t[:], uo[:, :, sc_ * SC * 128:(sc_ + 1) * SC * 128], reads=[scr["uTb"].b], writes=[uT.b])
                        dma("sp", vs.t[:], vo[:, sc_ * SC:(sc_ + 1) * SC, :], reads=[scr["vb"].b], writes=[vs.b])
                        for jj in range(SC):
                            chunks[sc_ * SC + jj] = (uT, vs, jj)

                    def stage1(j):
                        if j % SC == 0:
                            load_sc(j // SC)
                        uT, vs, jj = chunks[j]
                        pa = psAT.next()
                        for kc in range(KC):
                            op("pe", MM(pa.f[:, 0:TE], uT.t[:, kc, jj * 128:(jj + 1) * 128], h2.t[:, kc, :], start=(kc == 0), stop=(kc == KC - 1)),
                               reads=[uT.b, h2.b], pwrites=[pa.b])
                        W = Wr.next()
                        op("act", ACTF(W.t[:], pa.f[:, 0:TE], AF.Gelu), reads=[pa.b], writes=[W.b])
                        W2 = W2r.next()
                        op("dve", TT(W2.t[:], W.t[:], G.t[:, :, j], ALU.mult), reads=[W.b, G.b], writes=[W2.b])
                        return W2

                    def stage2(j, W2):
                        uT, vs, jj = chunks.pop(j)
                        for tb in range(2):
                            for dh in range(2):
                                po = psOut[tb][dh]
                                op("pe", MM(po.f[:, :], W2.t[:, tb * 128:(tb + 1) * 128], vs.t[:, jj, dh * 512:(dh + 1) * 512],
                                            start=(j == 0), stop=(j == 127)), reads=[W2.b, vs.b], pwrites=[po.b])

                    pend = stage1(0)
                    for j in range(128):
                        nxt = stage1(j + 1) if j + 1 < 128 else None
                        stage2(j, pend)
                        pend = nxt
                    for tb in range(2):
                        for dh in range(2):
                            po = psOut[tb][dh]
                            t_ = tz.next()
                            op("dve", TT(t_.t[:], po.f[:, :], gb2.t[:, dh * 512:(dh + 1) * 512], ALU.mult), reads=[po.b, gb2.b], writes=[t_.b])
                            op("dve", TT(yst.t[:, tb, dh * 512:(dh + 1) * 512], t_.t[:], x1t.t[:, tb, dh * 512:(dh + 1) * 512], ALU.add),
                               reads=[t_.b, x1t.b], pwrites=[yst.b])
                    dma("sp", y[t0:t0 + TE, :].rearrange("(j p) d -> p j d", p=128), yst.t[:], reads=[yst.b])
                ctx.barrier()

        ctx.finish()
    return nc, ctx


def host_consts():
    c = np.zeros((128, 5, 128), np.float32)
    c[:, 0, :] = np.eye(128, dtype=np.float32)
    qi = np.arange(128)[:, None]
    ki = np.arange(128)[None, :]
    c[:, 1, :] = np.where(ki <= qi, 0.0, NEG).astype(np.float32)
    c[:, 2, :] = ((qi // 64) == (ki // 64)).astype(np.float32)
    c[:, 3, :] = np.broadcast_to(np.arange(128, dtype=np.float32)[None, :], (128, 128))
    c[:, 4, :] = 1.0
    return c


def host_shared(inp):
    f = lambda a: np.ascontiguousarray(np.asarray(a, dtype=np.float32))
    sh = {}
    sh["w_ada"] = f(inp["w_ada"][0])
    sh["b_ada"] = f(inp["b_ada"][0])
    sh["b_adaT"] = f(np.asarray(inp["b_ada"][0]).reshape(48, 128).T)
    sh["n1T"] = f(np.asarray(inp["norm1_w"][0]).reshape(KC, 128).T)
    sh["n2T"] = f(np.asarray(inp["norm2_w"][0]).reshape(KC, 128).T)
    sh["w_in"] = f(inp["w_in"][0])
    sh["convT"] = f(np.asarray(inp["conv_w"][0]).reshape(3, 4, 128).transpose(2, 1, 0))
    sh["w_conv_out"] = f(inp["w_conv_out"][0])
    sh["qk_w"] = f(np.stack([np.tile(np.asarray(inp["q_norm_w"][0]), 2), np.tile(np.asarray(inp["k_norm_w"][0]), 2)], axis=1))
    sh["w_attn_out"] = f(inp["w_attn_out"][0])
    sh["w_o"] = f(inp["w_o"][0])
    sh["w_peer_q"] = f(inp["w_peer_q"][0])
    sk = np.asarray(inp["peer_sub_keys"][0])
    sh["skT"] = f(sk.transpose(1, 3, 0, 2).reshape(128, 8, 128))
    u = np.asarray(inp["peer_u"][0]).reshape(128, 128, D).transpose(1, 0, 2).reshape(NEXP, D)
    sh["uT"] = f(u.T)
    sh["vp"] = f(np.asarray(inp["peer_v"][0]).reshape(128, 128, D).transpose(1, 0, 2).reshape(NEXP, D))
    sh["consts"] = host_consts()
    return sh


def kernel(**inp):
    x = np.asarray(inp["x"], dtype=np.float32)
    c = np.asarray(inp["c"], dtype=np.float32)
    B, S, _ = x.shape
    ncores = 8
    NSEQ = B // ncores
    nc, _ = build(NSEQ, S)
    sh = host_shared(inp)
    in_maps = []
    for i in range(ncores):
        m = dict(sh)
        m["x"] = np.ascontiguousarray(x[i * NSEQ:(i + 1) * NSEQ].reshape(NSEQ * S, D))
        m["cT"] = np.ascontiguousarray(c[i * NSEQ:(i + 1) * NSEQ].reshape(NSEQ, KC, 128).transpose(2, 1, 0))
        in_maps.append(m)
    res = run_bass_kernel_spmd(nc, in_maps, core_ids=list(range(ncores)))
    out = np.concatenate([np.asarray(r["y"]).reshape(NSEQ, S, D) for r in res.results], axis=0)
    return out.astype(np.float32)
```

```python
import math
from contextlib import ExitStack

import numpy as np
import concourse.bass as bass
import concourse.mybir as mybir
from concourse.bass_utils import run_bass_kernel_spmd

F32 = mybir.dt.float32
BF16 = mybir.dt.bfloat16
U32 = mybir.dt.uint32
AF = mybir.ActivationFunctionType
ALU = mybir.AluOpType
AX = mybir.AxisListType

D = 1024
KC = 8
NIN = 5704
O_CB, O_CC, O_CX, O_Q, O_K, O_V, O_QI, O_KI, O_WI, O_GC, O_GA = 0, 512, 1024, 1536, 2048, 2560, 3072, 3584, 3648, 3656, 4680
NEXP = 16384
EPS = 1e-6
NEG = -1.0e30
NIT = 16
DCUT = 99
SELF_SYNC = True


class Buf:
    __slots__ = ("w", "r")

    def __init__(self):
        self.w = {}
        self.r = {}


class Tl:
    __slots__ = ("t", "b")

    def __init__(self, t):
        self.t = t
        self.b = Buf()


class Ring:
    def __init__(self, items):
        self.items = items
        self.i = 0

    def next(self):
        it = self.items[self.i % len(self.items)]
        self.i += 1
        return it


class Ctx:
    COMPUTE = ("pe", "act", "dve", "pool")
    ALL = ("pe", "act", "dve", "pool", "sp")
    NL = 8

    def __init__(self, nc):
        self.nc = nc
        self.prog = {n: [] for n in self.ALL}
        self.sems = {}
        self.cnt = {}
        self.known = {n: {} for n in self.ALL}
        for n in self.COMPUTE:
            self.sems[n] = nc.alloc_semaphore(name="s_" + n)
            self.cnt[n] = 0
        self.lane_rr = {}
        for q in ("sp", "act", "pool"):
            self.lane_rr[q] = 0
            for l in range(self.NL):
                k = (q, l)
                self.sems[k] = nc.alloc_semaphore(name="d_%s%d" % (q, l))
                self.cnt[k] = 0
        self.ninstr = 0

    def _wait(self, eng, key, val):
        if val <= 0:
            return
        if key == eng and (eng == "pe" or not SELF_SYNC):
            return
        if self.known[eng].get(key, 0) < val:
            self.prog[eng].append(("w", key, val))
            self.known[eng][key] = val

    def _deps(self, eng, reads, writes, pwrites):
        deps = {}
        for b in reads:
            for k, v in b.w.items():
                if deps.get(k, 0) < v:
                    deps[k] = v
        for b in writes:
            for dct in (b.w, b.r):
                for k, v in dct.items():
                    if deps.get(k, 0) < v:
                        deps[k] = v
        for b in pwrites:
            for k, v in b.r.items():
                if deps.get(k, 0) < v:
                    deps[k] = v
            for k, v in b.w.items():
                if k != eng and deps.get(k, 0) < v:
                    deps[k] = v
        for k, v in deps.items():
            self._wait(eng, k, v)

    def _mark(self, key, n, reads, writes, pwrites):
        for b in reads:
            b.r[key] = n
        for b in writes:
            b.w = {key: n}
            b.r = {}
        for b in pwrites:
            b.w[key] = n

    def op(self, eng, fn, reads=(), writes=(), pwrites=()):
        self._deps(eng, reads, writes, pwrites)
        self.cnt[eng] += 1
        self.prog[eng].append(("o", fn))
        self._mark(eng, self.cnt[eng], reads, writes, pwrites)
        self.ninstr += 1

    def dma(self, q, out, in_, reads=(), writes=(), pwrites=(), slow=False):
        l = self.lane_rr[q] % self.NL
        self.lane_rr[q] += 1
        key = (q, l)
        self._wait(q, key, self.cnt[key])
        self._deps(q, reads, writes, pwrites)
        self.cnt[key] += 16
        self.prog[q].append(("d", out, in_, key, slow))
        self._mark(key, self.cnt[key], reads, writes, pwrites)
        self.ninstr += 1

    def barrier(self):
        for e in self.ALL:
            for k in self.sems:
                self._wait(e, k, self.cnt[k])

    def finish(self):
        for k in self.sems:
            self._wait("sp", k, self.cnt[k])
        nc = self.nc

        def mk(name):
            def body(e):
                for it in self.prog[name]:
                    if it[0] == "w":
                        e.wait_ge(self.sems[it[1]], it[2])
                    elif it[0] == "o":
                        it[1](e).then_inc(self.sems[name], 1)
                    else:
                        if it[4]:
                            e.dma_start(out=it[1], in_=it[2], allow_slow_non_contiguous=True).then_inc(self.sems[it[3]], 16)
                        else:
                            e.dma_start(out=it[1], in_=it[2]).then_inc(self.sems[it[3]], 16)
            return body

        with nc.Block() as block:
            block.tensor(mk("pe"))
            block.scalar(mk("act"))
            block.vector(mk("dve"))
            block.gpsimd(mk("pool"))
            block.sync(mk("sp"))


def ACTF(out, in_, func, **kw):
    return lambda e: e.activation(out=out, in_=in_, func=func, **kw)


def MM(out, lhsT, rhs, start=True, stop=True):
    return lambda e: e.matmul(out, lhsT, rhs, start=start, stop=stop)


def TR(out, in_, ident):
    return lambda e: e.transpose(out, in_, ident)


def TS(out, in0, s1, s2, op0, op1=None, accum_out=None):
    if op1 is None:
        return lambda e: e.tensor_scalar(out=out, in0=in0, scalar1=s1, scalar2=s2, op0=op0, accum_out=accum_out)
    return lambda e: e.tensor_scalar(out=out, in0=in0, scalar1=s1, scalar2=s2, op0=op0, op1=op1, accum_out=accum_out)


def TT(out, in0, in1, op):
    return lambda e: e.tensor_tensor(out=out, in0=in0, in1=in1, op=op)


def STT(out, in0, scalar, in1, op0, op1):
    return lambda e: e.scalar_tensor_tensor(out=out, in0=in0, scalar=scalar, in1=in1, op0=op0, op1=op1)


def CP(out, in_):
    return lambda e: e.tensor_copy(out=out, in_=in_)


def RED(out, in_, op):
    return lambda e: e.tensor_reduce(out=out, in_=in_, axis=AX.X, op=op)


def RCP(out, in_):
    return lambda e: e.reciprocal(out=out, in_=in_)


def MSET(ap, c):
    return lambda e: e.memset(ap, c)


def build(NSEQ, S, GB=256, dbg=False, phases="ABCDE"):
    T = NSEQ * S
    NT = T // 128
    KSEL = min(256, S // 4)
    JB = GB // 128
    nc = bass.Bass("TRN2", target_bir_lowering=False)
    ctx = Ctx(nc)
    op, dma = ctx.op, ctx.dma

    def din(name, shape, dt=F32):
        return nc.dram_tensor(name, list(shape), dt, kind="ExternalInput").ap()

    io = {
        "x": din("x", [T, D]), "cT": din("cT", [128, KC, NSEQ]), "w_ada": din("w_ada", [D, 6 * D]),
        "b_ada": din("b_ada", [6 * D]), "b_adaT": din("b_adaT", [128, 48]),
        "n1T": din("n1T", [128, KC]), "n2T": din("n2T", [128, KC]), "w_in": din("w_in", [D, NIN]),
        "convT": din("convT", [128, 4, 3]), "w_conv_out": din("w_conv_out", [512, D]),
        "qk_w": din("qk_w", [128, 2]), "w_attn_out": din("w_attn_out", [512, D]), "w_o": din("w_o", [D, D]),
        "w_peer_q": din("w_peer_q", [D, D]), "skT": din("skT", [128, 8, 128]),
        "uT": din("uT", [D, NEXP]), "vp": din("vp", [NEXP, D]), "consts": din("consts", [128, 5, 128]),
    }
    y = nc.dram_tensor("y", [T, D], F32, kind="ExternalOutput").ap()
    skind = "ExternalOutput" if dbg else "Internal"

    def dscr(name, shape, dt):
        t = nc.dram_tensor(name, list(shape), dt, kind=skind).ap()
        return Tl(t)

    scr = {
        "mixA": dscr("s_mixA", [D, T], BF16), "sga": dscr("s_sga", [D, T], BF16),
        "qT": dscr("s_qT", [512, T], BF16), "kT": dscr("s_kT", [512, T], BF16),
        "qiT": dscr("s_qiT", [512, T], BF16), "kiT": dscr("s_kiT", [64, T], BF16),
        "V": dscr("s_V", [T, 520], BF16), "attnT": dscr("s_attnT", [512, T], BF16),
        "gbc": dscr("s_gbc", [NSEQ, 2, 128, D], F32), "x1": dscr("s_x1", [T, D], F32),
        "h2T": dscr("s_h2T", [D, T], BF16), "sel": dscr("s_sel", [3, 128, T], BF16),
        "uTb": dscr("s_uTb", [D, NEXP], BF16), "vb": dscr("s_vb", [NEXP, D], BF16),
    }

    with ExitStack() as gs:
        def tile(es, name, shape, dt):
            return Tl(es.enter_context(nc.sbuf_tensor("sb_" + name, list(shape), dt)))

        psum = gs.enter_context(nc.psum_tensor("psum", [128, 8, 512], F32))
        pb = [Buf() for _ in range(8)]

        class PB:
            def __init__(self, i):
                self.i = i
                self.b = pb[i]
                self.f = psum[:, i, :]
                self.h = psum[:, i, :].bitcast(BF16)

        PBs = [PB(i) for i in range(8)]

        cst = tile(gs, "cst", [128, 5, 128], F32)
        ident_bf = tile(gs, "ident_bf", [128, 128], BF16)
        bones_bf = tile(gs, "bones_bf", [128, 128], BF16)
        iota_bf = tile(gs, "iota_bf", [128, 128], BF16)
        epst = tile(gs, "epst", [128, 1], F32)
        modTb = tile(gs, "modTb", [128, 48, NSEQ], F32)
        A1 = tile(gs, "A1", [128, KC, NSEQ], F32)
        A2 = tile(gs, "A2", [128, KC, NSEQ], F32)
        widx = tile(gs, "widx", [128, NT, 8], F32)
        qkw = tile(gs, "qkw", [128, 2], F32)
        qws = tile(gs, "qws", [128, 1], F32)
        ident_f = cst.t[:, 0, :]
        causal_f = cst.t[:, 1, :]
        ones_f = cst.t[:, 4, :]

        dma("sp", cst.t[:], io["consts"], writes=[cst.b])
        dma("sp", qkw.t[:], io["qk_w"], writes=[qkw.b])
        op("dve", CP(ident_bf.t[:], cst.t[:, 0, :]), reads=[cst.b], writes=[ident_bf.b])
        op("dve", CP(bones_bf.t[:], cst.t[:, 2, :]), reads=[cst.b], writes=[bones_bf.b])
        op("dve", CP(iota_bf.t[:], cst.t[:, 3, :]), reads=[cst.b], writes=[iota_bf.b])
        op("dve", MSET(epst.t[:], EPS), writes=[epst.b])
        op("dve", TS(qws.t[:], qkw.t[:, 0:1], 0.125, None, ALU.mult), reads=[qkw.b], writes=[qws.b])

        if "A" in phases:
            with ExitStack() as es:
                cT = tile(es, "cT", [128, KC, NSEQ], F32)
                sc = tile(es, "sc", [128, KC, NSEQ], F32)
                scb = tile(es, "scb", [128, KC * NSEQ, 128], F32)
                badaT = tile(es, "badaT", [128, 48], F32)
                bbc = tile(es, "bbc", [128, 2, D], F32)
                n1T = tile(es, "n1T", [128, KC], F32)
                n2T = tile(es, "n2T", [128, KC], F32)
                war = Ring([tile(es, "wa%d" % i, [128, KC, 512], F32) for i in range(2)])
                gst = Ring([tile(es, "gst%d" % i, [128, 512], F32) for i in range(2)])
                dma("sp", cT.t[:], io["cT"], writes=[cT.b])
                dma("sp", badaT.t[:], io["b_adaT"], writes=[badaT.b])
                dma("sp", n1T.t[:], io["n1T"], writes=[n1T.b])
                dma("sp", n2T.t[:], io["n2T"], writes=[n2T.b])
                dma("sp", bbc.t[:, 0, :], io["b_ada"][2 * D:3 * D].partition_broadcast(128), pwrites=[bbc.b])
                dma("sp", bbc.t[:, 1, :], io["b_ada"][5 * D:6 * D].partition_broadcast(128), pwrites=[bbc.b])
                op("act", ACTF(sc.t[:], cT.t[:], AF.Silu), reads=[cT.b], writes=[sc.b])
                op("dve", CP(scb.t[:], sc.t[:].rearrange("p k s -> p (k s)").unsqueeze(2).to_broadcast([128, KC * NSEQ, 128])),
                   reads=[sc.b], writes=[scb.b])
                pM = PBs[0]
                pG = Ring([PBs[1], PBs[2]])
                wav = io["w_ada"].rearrange("(kc p) f -> p kc f", p=128)
                for blk in range(12):
                    wa = war.next()
                    dma("sp", wa.t[:], wav[:, :, blk * 512:(blk + 1) * 512], writes=[wa.b])
                    for fl in range(4):
                        fc = blk * 4 + fl
                        for kc in range(KC):
                            op("pe", MM(pM.f[:, fc * NSEQ:(fc + 1) * NSEQ], wa.t[:, kc, fl * 128:(fl + 1) * 128], sc.t[:, kc, :],
                                        start=(kc == 0), stop=(kc == KC - 1)), reads=[wa.b, sc.b], pwrites=[pM.b])
                    if blk in (4, 5, 10, 11):
                        which = 0 if blk < 6 else 1
                        half = blk % 2
                        for s in range(NSEQ):
                            p = pG.next()
                            for kc in range(KC):
                                op("pe", MM(p.f[:, :], scb.t[:, kc * NSEQ + s, :], wa.t[:, kc, :], start=(kc == 0), stop=(kc == KC - 1)),
                                   reads=[wa.b, scb.b], pwrites=[p.b])
                            g = gst.next()
                            op("dve", TT(g.t[:], p.f[:, :], bbc.t[:, which, half * 512:(half + 1) * 512], ALU.add),
                               reads=[p.b, bbc.b], writes=[g.b])
                            dma("sp", scr["gbc"].t[s, which, :, half * 512:(half + 1) * 512], g.t[:], reads=[g.b], pwrites=[scr["gbc"].b])
                op("dve", TT(modTb.t[:], pM.f[:, 0:48 * NSEQ].rearrange("p (f s) -> p f s", s=NSEQ),
                             badaT.t[:].unsqueeze(2).to_broadcast([128, 48, NSEQ]), ALU.add),
                   reads=[pM.b, badaT.b], writes=[modTb.b])
                op("dve", STT(A1.t[:], modTb.t[:, 8:16, :], 1.0, n1T.t[:].unsqueeze(2).to_broadcast([128, KC, NSEQ]), ALU.add, ALU.mult),
                   reads=[modTb.b, n1T.b], writes=[A1.b])
                op("dve", STT(A2.t[:], modTb.t[:, 32:40, :], 1.0, n2T.t[:].unsqueeze(2).to_broadcast([128, KC, NSEQ]), ALU.add, ALU.mult),
                   reads=[modTb.b, n2T.b], writes=[A2.b])
                ctx.barrier()

        if "B" in phases:
            with ExitStack() as es:
                win = tile(es, "win", [128, KC, NIN], BF16)
                wco = tile(es, "wco", [128, 4, D], BF16)
                convT = tile(es, "convT", [128, 4, 3], F32)
                dma("sp", convT.t[:], io["convT"], writes=[convT.b])
                with ExitStack() as es2:
                    stg = Ring([tile(es2, "stg%d" % i, [128, KC, 512], F32) for i in range(2)])
                    wiv = io["w_in"].rearrange("(kc p) f -> p kc f", p=128)
                    col = 0
                    i = 0
                    while col < NIN:
                        w = min(512, NIN - col)
                        st = stg.next()
                        dma("sp", st.t[:, :, 0:w], wiv[:, :, col:col + w], writes=[st.b])
                        eng = ("dve", "act", "pool")[i % 3]
                        if eng == "act":
                            op("act", ACTF(win.t[:, :, col:col + w], st.t[:, :, 0:w], AF.Copy), reads=[st.b], pwrites=[win.b])
                        else:
                            op(eng, CP(win.t[:, :, col:col + w], st.t[:, :, 0:w]), reads=[st.b], pwrites=[win.b])
                        col += w
                        i += 1
                    st = stg.next()
                    stv = st.t[:].rearrange("p k f -> p (k f)").rearrange("p (c d) -> p c d", c=4)
                    dma("sp", stv, io["w_conv_out"].rearrange("(c p) d -> p c d", p=128), writes=[st.b])
                    op("dve", CP(wco.t[:], stv), reads=[st.b], writes=[wco.b])
                    ctx.barrier()
                xgr = Ring([tile(es, "xg%d" % i, [128, JB, D], F32) for i in range(2)])
                junk = tile(es, "junkB", [128, D], BF16)
                ssq = tile(es, "ssq", [128, JB], F32)
                rt = tile(es, "rt", [128, JB], F32)
                rstd = tile(es, "rstd", [128, JB], F32)
                xs = tile(es, "xs", [128, JB, D], BF16)
                hTr = Ring([tile(es, "hT%d" % i, [128, KC, GB], BF16) for i in range(2)])
                ubuf = tile(es, "ubuf", [128, 4, GB + 2], F32)
                tmpf = Ring([tile(es, "tmpf%d" % i, [128, GB], F32) for i in range(3)])
                c1 = tile(es, "c1", [128, GB], F32)
                c2 = tile(es, "c2", [128, GB], F32)
                c3 = tile(es, "c3", [128, GB], F32)
                yA = tile(es, "yA", [128, 4, GB], BF16)
                mixA_st = tile(es, "mixA_st", [128, 8, GB], BF16)
                sga_st = tile(es, "sga_st", [128, 8, GB], BF16)
                q_st = tile(es, "q_st", [128, 4, GB], BF16)
                k_st = tile(es, "k_st", [128, 4, GB], BF16)
                qi_st = tile(es, "qi_st", [128, 4, GB], BF16)
                ki_st = tile(es, "ki_st", [64, GB], BF16)
                v_st = tile(es, "v_st", [128, JB, 8, 65], BF16)
                sqr = Ring([tile(es, "sq%d" % i, [128, GB], BF16) for i in range(2)])
                rtr = Ring([tile(es, "rtq%d" % i, [128, GB], F32) for i in range(2)])
                rrr = Ring([tile(es, "rrq%d" % i, [128, GB], F32) for i in range(2)])
                op("pool", MSET(v_st.t[:], 1.0), writes=[v_st.b])
                ptr = Ring([PBs[0], PBs[1]])
                gen = Ring([PBs[i] for i in range(2, 8)])
                evi = [0]

                def evac(out, in_, rd, wr):
                    evi[0] += 1
                    if evi[0] % 2:
                        op("act", ACTF(out, in_, AF.Copy), reads=rd, pwrites=wr)
                    else:
                        op("dve", CP(out, in_), reads=rd, pwrites=wr)

                for g in range(T // GB):
                    t0 = g * GB
                    seq = t0 // S
                    first = (t0 % S == 0)
                    xg = xgr.next()
                    hT = hTr.next()
                    dma("sp", xg.t[:], io["x"][t0:t0 + GB, :].rearrange("(j p) d -> p j d", p=128), writes=[xg.b])
                    for j in range(JB):
                        op("act", ACTF(junk.t[:], xg.t[:, j, :], AF.Square, accum_out=ssq.t[:, j:j + 1]), reads=[xg.b], writes=[junk.b], pwrites=[ssq.b])
                    op("act", ACTF(rt.t[:], ssq.t[:], AF.Sqrt, scale=1.0 / D, bias=epst.t[:, 0:1]), reads=[ssq.b, epst.b], writes=[rt.b])
                    op("dve", RCP(rstd.t[:], rt.t[:]), reads=[rt.b], writes=[rstd.b])
                    for j in range(JB):
                        op("dve", TS(xs.t[:, j, :], xg.t[:, j, :], rstd.t[:, j:j + 1], None, ALU.mult), reads=[xg.b, rstd.b], pwrites=[xs.b])
                    for kc in range(KC):
                        pt = ptr.next()
                        for j in range(JB):
                            op("pe", TR(pt.h[:, j * 128:(j + 1) * 128], xs.t[:, j, kc * 128:(kc + 1) * 128], ident_bf.t[:]),
                               reads=[xs.b, ident_bf.b], pwrites=[pt.b])
                        op("act", ACTF(hT.t[:, kc, :], pt.h[:, 0:GB], AF.Identity, scale=A1.t[:, kc, seq:seq + 1], bias=modTb.t[:, kc, seq:seq + 1]),
                           reads=[pt.b, A1.b, modTb.b], pwrites=[hT.b])

                    def proj(c0, M=128):
                        p = gen.next()
                        for kc in range(KC):
                            op("pe", MM(p.f[0:M, 0:GB], win.t[:, kc, c0:c0 + M], hT.t[:, kc, :], start=(kc == 0), stop=(kc == KC - 1)),
                               reads=[win.b, hT.b], pwrites=[p.b])
                        return p

                    if first:
                        op("dve", MSET(ubuf.t[:, :, 0:2], 0.0), pwrites=[ubuf.b])
                    for cch in range(4):
                        pcc = proj(O_CC + cch * 128)
                        pcx = proj(O_CX + cch * 128)
                        pcb = proj(O_CB + cch * 128)
                        tf = tmpf.next()
                        op("act", ACTF(tf.t[:], pcc.f[:, 0:GB], AF.Copy), reads=[pcc.b], writes=[tf.b])
                        op("dve", TT(ubuf.t[:, cch, 2:2 + GB], pcx.f[:, 0:GB], tf.t[:], ALU.mult), reads=[pcx.b, tf.b], pwrites=[ubuf.b])
                        op("dve", TS(c1.t[:], ubuf.t[:, cch, 0:GB], convT.t[:, cch, 0:1], None, ALU.mult), reads=[ubuf.b, convT.b], writes=[c1.b])
                        op("dve", STT(c2.t[:], ubuf.t[:, cch, 1:1 + GB], convT.t[:, cch, 1:2], c1.t[:], ALU.mult, ALU.add),
                           reads=[ubuf.b, convT.b, c1.b], writes=[c2.b])
                        op("dve", STT(c3.t[:], ubuf.t[:, cch, 2:2 + GB], convT.t[:, cch, 2:3], c2.t[:], ALU.mult, ALU.add),
                           reads=[ubuf.b, convT.b, c2.b], writes=[c3.b])
                        op("dve", TT(yA.t[:, cch, :], pcb.f[:, 0:GB], c3.t[:], ALU.mult), reads=[pcb.b, c3.b], pwrites=[yA.b])
                        op("dve", CP(ubuf.t[:, cch, 0:2], ubuf.t[:, cch, GB:GB + 2]), reads=[ubuf.b], pwrites=[ubuf.b])
                    for dc in range(8):
                        pyc = gen.next()
                        for cch in range(4):
                            op("pe", MM(pyc.f[:, 0:GB], wco.t[:, cch, dc * 128:(dc + 1) * 128], yA.t[:, cch, :], start=(cch == 0), stop=(cch == 3)),
                               reads=[wco.b, yA.b], pwrites=[pyc.b])
                        pgc = proj(O_GC + dc * 128)
                        tf = tmpf.next()
                        op("act", ACTF(tf.t[:], pgc.f[:, 0:GB], AF.Sigmoid), reads=[pgc.b], writes=[tf.b])
                        op("dve", TT(mixA_st.t[:, dc, :], pyc.f[:, 0:GB], tf.t[:], ALU.mult), reads=[pyc.b, tf.b], pwrites=[mixA_st.b])
                    dma("sp", scr["mixA"].t.rearrange("(dc p) t -> p dc t", p=128)[:, :, t0:t0 + GB], mixA_st.t[:], reads=[mixA_st.b], pwrites=[scr["mixA"].b])
                    for dc in range(8):
                        pga = proj(O_GA + dc * 128)
                        op("act", ACTF(sga_st.t[:, dc, :], pga.f[:, 0:GB], AF.Sigmoid), reads=[pga.b], pwrites=[sga_st.b])
                    dma("sp", scr["sga"].t.rearrange("(dc p) t -> p dc t", p=128)[:, :, t0:t0 + GB], sga_st.t[:], reads=[sga_st.b], pwrites=[scr["sga"].b])
                    for (base, wap, wb, st, nm) in ((O_Q, qws.t[:, 0:1], qws.b, q_st, "qT"), (O_K, qkw.t[:, 1:2], qkw.b, k_st, "kT")):
                        for c in range(4):
                            pq = proj(base + c * 128)
                            sq = sqr.next()
                            op("act", ACTF(sq.t[:], pq.f[:, 0:GB], AF.Square), reads=[pq.b], writes=[sq.b])
                            ps2 = gen.next()
                            op("pe", MM(ps2.f[:, 0:GB], bones_bf.t[:], sq.t[:]), reads=[bones_bf.b, sq.b], pwrites=[ps2.b])
                            r1 = rtr.next()
                            op("act", ACTF(r1.t[:], ps2.f[:, 0:GB], AF.Sqrt, scale=1.0 / 64, bias=epst.t[:, 0:1]), reads=[ps2.b, epst.b], writes=[r1.b])
                            r2 = rrr.next()
                            op("dve", RCP(r2.t[:], r1.t[:]), reads=[r1.b], writes=[r2.b])
                            op("dve", STT(st.t[:, c, :], pq.f[:, 0:GB], wap, r2.t[:], ALU.mult, ALU.mult), reads=[pq.b, wb, r2.b], pwrites=[st.b])
                        dma("sp", scr[nm].t.rearrange("(c p) t -> p c t", p=128)[:, :, t0:t0 + GB], st.t[:], reads=[st.b], pwrites=[scr[nm].b])
                    for c in range(4):
                        pq = proj(O_QI + c * 128)
                        evac(qi_st.t[:, c, :], pq.f[:, 0:GB], [pq.b], [qi_st.b])
                    dma("sp", scr["qiT"].t.rearrange("(c p) t -> p c t", p=128)[:, :, t0:t0 + GB], qi_st.t[:], reads=[qi_st.b], pwrites=[scr["qiT"].b])
                    pq = proj(O_KI, M=64)
                    evac(ki_st.t[:, :], pq.f[0:64, 0:GB], [pq.b], [ki_st.b])
                    dma("sp", scr["kiT"].t[:, t0:t0 + GB], ki_st.t[:], reads=[ki_st.b], pwrites=[scr["kiT"].b])
                    for j in range(JB):
                        p = gen.next()
                        for kc in range(KC):
                            op("pe", MM(p.f[:, 0:512], hT.t[:, kc, j * 128:(j + 1) * 128], win.t[:, kc, O_V:O_V + 512], start=(kc == 0), stop=(kc == KC - 1)),
                               reads=[win.b, hT.b], pwrites=[p.b])
                        evac(v_st.t[:, j, :, 0:64], p.f[:, 0:512].rearrange("p (h d) -> p h d", h=8), [p.b], [v_st.b])
                        p = gen.next()
                        for kc in range(KC):
                            op("pe", MM(p.f[:, 0:8], hT.t[:, kc, j * 128:(j + 1) * 128], win.t[:, kc, O_WI:O_WI + 8], start=(kc == 0), stop=(kc == KC - 1)),
                               reads=[win.b, hT.b], pwrites=[p.b])
                        op("dve", CP(widx.t[:, t0 // 128 + j, :], p.f[:, 0:8]), reads=[p.b], pwrites=[widx.b])
                    dma("sp", scr["V"].t[t0:t0 + GB, :].rearrange("(j p) f -> p j f", p=128), v_st.t[:].rearrange("p j h d -> p j (h d)"),
                        reads=[v_st.b], pwrites=[scr["V"].b])
                ctx.barrier()

        if "C" in phases:
            with ExitStack() as es:
                NKB = S // 128
                QG = min(512, S)
                kTs = tile(es, "kTs", [128, 4, S], BF16)
                Vs = tile(es, "Vs", [128, NKB, 520], BF16)
                kiTs = tile(es, "kiTs", [128, S], BF16)
                qTg = Ring([tile(es, "qTg%d" % i, [128, 4, QG], BF16) for i in range(2)])
                qiTg = Ring([tile(es, "qiTg%d" % i, [128, 4, QG], BF16) for i in range(2)])
                Dh = tile(es, "Dh", [128, 8, 128], BF16)
                rr = Ring([tile(es, "relu%d" % i, [128, 512], BF16) for i in range(4)])
                scores = [tile(es, "score%d" % i, [128, S], F32) for i in range(2)]
                junk = tile(es, "junkC", [128, S], BF16)
                biss = [tile(es, "bis%d" % i, [128, 8], F32) for i in range(2)]
                wfs = [tile(es, "wf%d" % i, [128, NIT + 2], F32) for i in range(2)]
                pw2 = tile(es, "pw2", [128, NIT + 2], F32)
                for i_ in range(NIT + 2):
                    op("dve", MSET(pw2.t[:, i_:i_ + 1], 2.0 ** -i_), pwrites=[pw2.b])
                dthr = tile(es, "dthr", [128, 128], F32)
                thrbc = tile(es, "thrbc", [128, 128], F32)
                maskT = tile(es, "maskT", [128, NKB, 128], BF16)
                maskT2 = tile(es, "maskT2", [128, NKB, 128], BF16)
                PTr = Ring([tile(es, "PT%d" % i, [128, 512], BF16) for i in range(3)])
                PMr = Ring([tile(es, "PM%d" % i, [128, 512], BF16) for i in range(3)])
                rz = tile(es, "rz", [128, 8], F32)
                attn_tm = tile(es, "attn_tm", [128, 8, 64], BF16)
                attnT_r = Ring([tile(es, "attnT_st%d" % i, [128, 4, QG], BF16) for i in range(2)])
                psL = Ring([PBs[0], PBs[1]])
                psS = PBs[2]
                psT = PBs[3]
                psA = Ring([PBs[4], PBs[5]])
                psO = [PBs[6], PBs[7]]
                maskTs = [maskT, maskT2]
                blocks = [(s_, qb_) for s_ in range(NSEQ) for qb_ in range(NKB)]
                gtiles = {}
                NQ = QG // 128

                def group_tiles(s_, qg):
                    key = (s_, qg)
                    if key not in gtiles:
                        qT = qTg.next()
                        qiT = qiTg.next()
                        tq0 = s_ * S + qg * QG
                        dma("sp", qiT.t[:], scr["qiT"].t.rearrange("(c p) t -> p c t", p=128)[:, :, tq0:tq0 + QG], reads=[scr["qiT"].b], writes=[qiT.b])
                        dma("sp", qT.t[:], scr["qT"].t.rearrange("(c p) t -> p c t", p=128)[:, :, tq0:tq0 + QG], reads=[scr["qT"].b], writes=[qT.b])
                        gtiles[key] = (qT, qiT, attnT_r.next())
                    return gtiles[key]

                xstate = {}

                def stage_X(bi, part, nparts):
                    s_, qb = blocks[bi]
                    sl = slice(s_ * S, (s_ + 1) * S)
                    qT, qiT, _ = group_tiles(s_, qb // NQ)
                    qs = qb % NQ
                    nkb = qb + 1
                    nk = nkb * 128
                    tix = (s_ * S) // 128 + qb
                    qsl = slice(qs * 128, (qs + 1) * 128)
                    score = scores[bi % 2]
                    bis = biss[bi % 2]
                    wf = wfs[bi % 2]
                    nch = (nk + 511) // 512
                    units = [(ch, h) for ch in range(nch) for h in range(8)]
                    nu = len(units)
                    ua, ub = (part * nu) // nparts, ((part + 1) * nu) // nparts

                    def logits(u):
                        ch, h = units[u]
                        hp = h % 2
                        k0 = ch * 512
                        w = min(512, nk - k0)
                        pl = psL.next()
                        op("pe", MM(pl.f[:, 0:w], qiT.t[hp * 64:(hp + 1) * 64, h // 2, qsl], kiTs.t[hp * 64:(hp + 1) * 64, k0:k0 + w]),
                           reads=[qiT.b, kiTs.b], pwrites=[pl.b])
                        xstate[(bi, u)] = pl

                    if part == 0:
                        if qb == 0:
                            dma("sp", kiTs.t[0:64, :], scr["kiT"].t[:, sl], reads=[scr["kiT"].b], writes=[kiTs.b])
                            dma("sp", kiTs.t[64:128, :], scr["kiT"].t[:, sl], reads=[scr["kiT"].b], pwrites=[kiTs.b])
                        op("dve", TT(Dh.t[:], ident_bf.t[:].unsqueeze(1).to_broadcast([128, 8, 128]),
                                     widx.t[:, tix, :].unsqueeze(2).to_broadcast([128, 8, 128]), ALU.mult),
                           reads=[ident_bf.b, widx.b], writes=[Dh.b])
                        logits(0)
                    for u in range(ua, ub):
                        if u + 1 < nu:
                            logits(u + 1)
                        ch, h = units[u]
                        k0 = ch * 512
                        w = min(512, nk - k0)
                        pl = xstate.pop((bi, u))
                        r = rr.next()
                        op("act", ACTF(r.t[:, 0:w], pl.f[:, 0:w], AF.Relu), reads=[pl.b], writes=[r.b])
                        op("pe", MM(psS.f[:, 0:w], Dh.t[:, h, :], r.t[:, 0:w], start=(h == 0), stop=(h == 7)),
                           reads=[Dh.b, r.b], pwrites=[psS.b])
                        if h == 7:
                            if ch == nch - 1:
                                wd = w - 128
                                if wd > 0:
                                    op("act", ACTF(score.t[:, k0:k0 + wd], psS.f[:, 0:wd], AF.Copy), reads=[psS.b], pwrites=[score.b])
                                op("dve", TT(score.t[:, nk - 128:nk], psS.f[:, wd:w], causal_f, ALU.add), reads=[psS.b, cst.b], pwrites=[score.b])
                            else:
                                op("act", ACTF(score.t[:, k0:k0 + w], psS.f[:, 0:w], AF.Copy), reads=[psS.b], pwrites=[score.b])
                    if part != nparts - 1:
                        return
                    lo, w0, mid, cnt, gg, mx = (bis.t[:, i:i + 1] for i in range(6))
                    if nk <= KSEL:
                        op("dve", MSET(lo, -1.0e29), writes=[bis.b])
                    else:
                        op("dve", RED(mx, score.t[:, 0:nk], ALU.max), reads=[score.b], writes=[bis.b])
                        op("dve", RED(lo, score.t[:, 0:nk - 128], ALU.min), reads=[score.b], writes=[bis.b])
                        op("dve", TT(w0, mx, lo, ALU.subtract), reads=[bis.b], writes=[bis.b])
                        op("dve", TS(wf.t[:], pw2.t[:], w0, None, ALU.mult), reads=[bis.b, pw2.b], writes=[wf.b])
                        op("dve", TT(mid, lo, wf.t[:, 1:2], ALU.add), reads=[bis.b, wf.b], writes=[bis.b])

                def stage_Xbis(bi, it0, it1):
                    s_, qb = blocks[bi]
                    nk = (qb + 1) * 128
                    if nk <= KSEL:
                        return
                    score = scores[bi % 2]
                    bis = biss[bi % 2]
                    wf = wfs[bi % 2]
                    lo, w0, mid, cnt, gg, mx = (bis.t[:, i:i + 1] for i in range(6))
                    for it in range(it0, it1):
                        op("dve", TS(junk.t[:, 0:nk], score.t[:, 0:nk], mid, None, ALU.is_ge, ALU.add, accum_out=cnt),
                           reads=[score.b, bis.b], writes=[bis.b, junk.b])
                        op("dve", TS(gg, cnt, KSEL - 0.5, 0.5, ALU.is_ge, ALU.subtract), reads=[bis.b], writes=[bis.b])
                        op("dve", STT(mid, gg, wf.t[:, it + 1:it + 2], mid, ALU.mult, ALU.add), reads=[bis.b, wf.b], writes=[bis.b])

                def stage_Xpost(bi):
                    s_, qb = blocks[bi]
                    nkb = qb + 1
                    nk = nkb * 128
                    mk = maskTs[bi % 2]
                    score = scores[bi % 2]
                    bis = biss[bi % 2]
                    wf = wfs[bi % 2]
                    lo, w0, mid, cnt, gg, mx = (bis.t[:, i:i + 1] for i in range(6))
                    if nk > KSEL:
                        op("dve", TT(lo, mid, wf.t[:, NIT + 1:NIT + 2], ALU.subtract), reads=[bis.b, wf.b], writes=[bis.b])
                    op("dve", TS(dthr.t[:], ident_f, lo, None, ALU.mult), reads=[cst.b, bis.b], writes=[dthr.b])
                    op("pe", MM(psT.f[:, 0:128], ones_f, dthr.t[:]), reads=[cst.b, dthr.b], pwrites=[psT.b])
                    op("act", ACTF(thrbc.t[:], psT.f[:, 0:128], AF.Copy), reads=[psT.b], writes=[thrbc.b])
                    for c4 in range((nkb + 3) // 4):
                        n4 = min(4, nkb - c4 * 4)
                        for i in range(n4):
                            kb = c4 * 4 + i
                            op("pe", TR(psT.f[:, i * 128:(i + 1) * 128], score.t[:, kb * 128:(kb + 1) * 128], ident_f),
                               reads=[score.b, cst.b], pwrites=[psT.b])
                        op("dve", TT(mk.t[:, c4 * 4:c4 * 4 + n4, :], psT.f[:, 0:n4 * 128].rearrange("p (a b) -> p a b", b=128),
                                     thrbc.t[:].unsqueeze(1).to_broadcast([128, n4, 128]), ALU.is_ge),
                           reads=[psT.b, thrbc.b], pwrites=[mk.b])

                def stage_Y(bi, hs, fin):
                    s_, qb = blocks[bi]
                    sl = slice(s_ * S, (s_ + 1) * S)
                    if qb == 0 and 0 in hs:
                        dma("sp", kTs.t[:], scr["kT"].t.rearrange("(c p) t -> p c t", p=128)[:, :, sl], reads=[scr["kT"].b], writes=[kTs.b])
                        dma("sp", Vs.t[:], scr["V"].t[sl, :].rearrange("(kb p) f -> p kb f", p=128), reads=[scr["V"].b], writes=[Vs.b])
                    qT, qiT, ast = group_tiles(s_, qb // NQ)
                    qs = qb % NQ
                    nkb = qb + 1
                    qsl = slice(qs * 128, (qs + 1) * 128)
                    mk = maskTs[bi % 2]
                    for h in hs:
                        hp = h % 2
                        po = psO[h // 4]
                        ng = (nkb + 3) // 4
                        pas = {}

                        def qk(c4, h=h, hp=hp):
                            n4 = min(4, nkb - c4 * 4)
                            pa = psA.next()
                            for i in range(n4):
                                kb = c4 * 4 + i
                                op("pe", MM(pa.f[:, i * 128:(i + 1) * 128], kTs.t[hp * 64:(hp + 1) * 64, h // 2, kb * 128:(kb + 1) * 128],
                                            qT.t[hp * 64:(hp + 1) * 64, h // 2, qsl]), reads=[kTs.b, qT.b], pwrites=[pa.b])
                            pas[c4] = pa

                        qk(0)
                        for c4 in range(ng):
                            n4 = min(4, nkb - c4 * 4)
                            if c4 + 1 < ng:
                                qk(c4 + 1)
                            pa = pas.pop(c4)
                            pt = PTr.next()
                            op("act", ACTF(pt.t[:, 0:n4 * 128], pa.f[:, 0:n4 * 128], AF.Exp), reads=[pa.b], writes=[pt.b])
                            pm = PMr.next()
                            op("dve", TT(pm.t[:, 0:n4 * 128], pt.t[:, 0:n4 * 128], mk.t[:, c4 * 4:c4 * 4 + n4, :].rearrange("p a b -> p (a b)"), ALU.mult),
                               reads=[pt.b, mk.b], writes=[pm.b])
                            for i in range(n4):
                                kb = c4 * 4 + i
                                op("pe", MM(po.f[:, (h % 4) * 65:(h % 4) * 65 + 65], pm.t[:, i * 128:(i + 1) * 128], Vs.t[:, kb, h * 65:(h + 1) * 65],
                                            start=(kb == 0), stop=(kb == nkb - 1)), reads=[pm.b, Vs.b], pwrites=[po.b])
                    if not fin:
                        return
                    for hh in range(2):
                        pv = psO[hh].f[:, 0:260].rearrange("p (h d) -> p h d", d=65)
                        op("dve", RCP(rz.t[:, hh * 4:(hh + 1) * 4], pv[:, :, 64]), reads=[psO[hh].b], pwrites=[rz.b])
                        op("dve", TT(attn_tm.t[:, hh * 4:(hh + 1) * 4, :], pv[:, :, 0:64],
                                     rz.t[:, hh * 4:(hh + 1) * 4].unsqueeze(2).to_broadcast([128, 4, 64]), ALU.mult),
                           reads=[psO[hh].b, rz.b], pwrites=[attn_tm.b])
                    px = psA.next()
                    for c in range(4):
                        op("pe", TR(px.h[:, c * 128:(c + 1) * 128], attn_tm.t[:, 2 * c:2 * c + 2, :].rearrange("p a b -> p (a b)"), ident_bf.t[:]),
                           reads=[attn_tm.b, ident_bf.b], pwrites=[px.b])
                    op("act", ACTF(ast.t[:, :, qsl], px.h[:, 0:512].rearrange("p (c t) -> p c t", c=4), AF.Copy),
                       reads=[px.b], pwrites=[ast.b])
                    if qs == NQ - 1:
                        tq0 = s_ * S + (qb // NQ) * QG
                        dma("sp", scr["attnT"].t.rearrange("(c p) t -> p c t", p=128)[:, :, tq0:tq0 + QG], ast.t[:], reads=[ast.b], pwrites=[scr["attnT"].b])

                NB = len(blocks)
                IPH = NIT // 8
                stage_X(0, 0, 1)
                stage_Xbis(0, 0, NIT)
                stage_Xpost(0)
                if NB > 1:
                    stage_X(1, 0, 1)
                for bi in range(NB):
                    for h in range(8):
                        if bi + 1 < NB:
                            stage_Xbis(bi + 1, h * IPH, (h + 1) * IPH if h < 7 else NIT)
                        if bi + 2 < NB:
                            stage_X(bi + 2, h, 8)
                        stage_Y(bi, [h], h == 7)
                    if bi + 1 < NB:
                        stage_Xpost(bi + 1)
                ctx.barrier()

        if "D" in phases:
            with ExitStack() as es:
                wao = tile(es, "wao", [128, 4, D], BF16)
                wo = tile(es, "wo", [128, KC, D], BF16)
                wpq = tile(es, "wpq", [128, KC, D], BF16)
                skTb = tile(es, "skTb", [128, 8, 128], BF16)
                with ExitStack() as es2:
                    stg = Ring([tile(es2, "stgD%d" % i, [128, KC, 512], F32) for i in range(2)])
                    ci_ = 0
                    for (src, dst) in ((io["w_o"], wo), (io["w_peer_q"], wpq)):
                        sv = src.rearrange("(kc p) f -> p kc f", p=128)
                        for hh in range(2):
                            st = stg.next()
                            dma("sp", st.t[:], sv[:, :, hh * 512:(hh + 1) * 512], writes=[st.b])
                            if ci_ % 2:
                                op("act", ACTF(dst.t[:, :, hh * 512:(hh + 1) * 512], st.t[:], AF.Copy), reads=[st.b], pwrites=[dst.b])
                            else:
                                op("dve", CP(dst.t[:, :, hh * 512:(hh + 1) * 512], st.t[:]), reads=[st.b], pwrites=[dst.b])
                            ci_ += 1
                    st = stg.next()
                    stv = st.t[:].rearrange("p k f -> p (k f)").rearrange("p (c d) -> p c d", c=4)
                    dma("sp", stv, io["w_attn_out"].rearrange("(c p) d -> p c d", p=128), writes=[st.b])
                    op("dve", CP(wao.t[:], stv), reads=[st.b], writes=[wao.b])
                    st = stg.next()
                    stv2 = st.t[:].rearrange("p k f -> p (k f)")[:, 0:1024].rearrange("p (h n) -> p h n", h=8)
                    dma("sp", stv2, io["skT"], writes=[st.b])
                    op("dve", CP(skTb.t[:], stv2), reads=[st.b], writes=[skTb.b])
                    ctx.barrier()
                gb1 = tile(es, "gb1", [128, D], F32)
                attg = Ring([tile(es, "attg%d" % i, [128, 4, GB], BF16) for i in range(2)])
                sgag = Ring([tile(es, "sgag%d" % i, [128, 8, GB], BF16) for i in range(2)])
                mxag = Ring([tile(es, "mxag%d" % i, [128, 8, GB], BF16) for i in range(2)])
                xgr = Ring([tile(es, "xgD%d" % i, [128, JB, D], F32) for i in range(2)])
                tmq = Ring([tile(es, "tmq%d" % i, [128, GB], F32) for i in range(2)])
                mixT = tile(es, "mixT", [128, 8, GB], BF16)
                tz = Ring([tile(es, "tz%d" % i, [128, 512], F32) for i in range(2)])
                x1 = tile(es, "x1", [128, JB, D], F32)
                junk = tile(es, "junkD", [128, D], BF16)
                ssq = tile(es, "ssqD", [128, JB], F32)
                rt = tile(es, "rtD", [128, JB], F32)
                rstd = tile(es, "rstdD", [128, JB], F32)
                xs = tile(es, "xsD", [128, JB, D], BF16)
                h2T = tile(es, "h2T", [128, KC, GB], BF16)
                qpT = tile(es, "qpT", [128, 8, GB], BF16)
                s_sb = tile(es, "s_sb", [128, 16, 128], F32)
                s_tmp = tile(es, "s_tmp", [128, 16, 128], F32)
                v_all = tile(es, "v_all", [128, 16, 16], F32)
                idx_all = tile(es, "idx_all", [128, 16, 16], U32)
                idx_bf = tile(es, "idx_bf", [128, 16, 16], BF16)
                cand = tile(es, "cand", [128, 8, 256], F32)
                cand2 = tile(es, "cand2", [128, 8, 256], F32)
                scv = tile(es, "scv", [128, 8, 16], F32)
                civ = tile(es, "civ", [128, 8, 16], U32)
                ca_u = tile(es, "ca_u", [128, 8, 16], U32)
                cb_u = tile(es, "cb_u", [128, 8, 16], U32)
                ca_bf = tile(es, "ca_bf", [128, 8, 16], BF16)
                cb_bf = tile(es, "cb_bf", [128, 8, 16], BF16)
                eqa = tile(es, "eqa", [128, 8, 16, 16], BF16)
                prd = tile(es, "prd", [128, 8, 16, 16], BF16)
                sm = tile(es, "sm", [128, 8, 16], F32)
                zz = tile(es, "zz", [128, 8], F32)
                sel_tm = tile(es, "sel_tm", [128, 3, 128], F32)
                selT = tile(es, "selT", [128, 3, 128], BF16)
                ptr = Ring([PBs[0], PBs[1]])
                gen = Ring([PBs[i] for i in range(2, 8)])
                evi = [0]

                def evacD(out, in_, rd, wr):
                    evi[0] += 1
                    if evi[0] % 2:
                        op("act", ACTF(out, in_, AF.Copy), reads=rd, pwrites=wr)
                    else:
                        op("dve", CP(out, in_), reads=rd, pwrites=wr)

                for g in range(T // GB):
                    t0 = g * GB
                    seq = t0 // S
                    if t0 % S == 0:
                        dma("sp", gb1.t[:], scr["gbc"].t[seq, 0, :, :], reads=[scr["gbc"].b], writes=[gb1.b])
                    at = attg.next()
                    sg = sgag.next()
                    ma = mxag.next()
                    xg = xgr.next()
                    dma("sp", at.t[:], scr["attnT"].t.rearrange("(c p) t -> p c t", p=128)[:, :, t0:t0 + GB], reads=[scr["attnT"].b], writes=[at.b])
                    dma("sp", sg.t[:], scr["sga"].t.rearrange("(c p) t -> p c t", p=128)[:, :, t0:t0 + GB], reads=[scr["sga"].b], writes=[sg.b])
                    dma("sp", ma.t[:], scr["mixA"].t.rearrange("(c p) t -> p c t", p=128)[:, :, t0:t0 + GB], reads=[scr["mixA"].b], writes=[ma.b])
                    dma("sp", xg.t[:], io["x"][t0:t0 + GB, :].rearrange("(j p) d -> p j d", p=128), writes=[xg.b])
                    for dc in range(8):
                        p = gen.next()
                        for c in range(4):
                            op("pe", MM(p.f[:, 0:GB], wao.t[:, c, dc * 128:(dc + 1) * 128], at.t[:, c, :], start=(c == 0), stop=(c == 3)),
                               reads=[wao.b, at.b], pwrites=[p.b])
                        tq = tmq.next()
                        op("dve", TT(tq.t[:], p.f[:, 0:GB], sg.t[:, dc, :], ALU.mult), reads=[p.b, sg.b], writes=[tq.b])
                        op("dve", TT(mixT.t[:, dc, :], tq.t[:], ma.t[:, dc, :], ALU.add), reads=[tq.b, ma.b], pwrites=[mixT.b])
                    for j in range(JB):
                        for dh in range(2):
                            p = gen.next()
                            for kc in range(KC):
                                op("pe", MM(p.f[:, 0:512], mixT.t[:, kc, j * 128:(j + 1) * 128], wo.t[:, kc, dh * 512:(dh + 1) * 512],
                                            start=(kc == 0), stop=(kc == KC - 1)), reads=[mixT.b, wo.b], pwrites=[p.b])
                            t_ = tz.next()
                            op("dve", TT(t_.t[:], p.f[:, 0:512], gb1.t[:, dh * 512:(dh + 1) * 512], ALU.mult), reads=[p.b, gb1.b], writes=[t_.b])
                            op("dve", TT(x1.t[:, j, dh * 512:(dh + 1) * 512], t_.t[:], xg.t[:, j, dh * 512:(dh + 1) * 512], ALU.add),
                               reads=[t_.b, xg.b], pwrites=[x1.b])
                    dma("sp", scr["x1"].t[t0:t0 + GB, :].rearrange("(j p) d -> p j d", p=128), x1.t[:], reads=[x1.b], pwrites=[scr["x1"].b])
                    for j in range(JB):
                        op("act", ACTF(junk.t[:], x1.t[:, j, :], AF.Square, accum_out=ssq.t[:, j:j + 1]), reads=[x1.b], writes=[junk.b], pwrites=[ssq.b])
                    op("act", ACTF(rt.t[:], ssq.t[:], AF.Sqrt, scale=1.0 / D, bias=epst.t[:, 0:1]), reads=[ssq.b, epst.b], writes=[rt.b])
                    op("dve", RCP(rstd.t[:], rt.t[:]), reads=[rt.b], writes=[rstd.b])
                    for j in range(JB):
                        op("dve", TS(xs.t[:, j, :], x1.t[:, j, :], rstd.t[:, j:j + 1], None, ALU.mult), reads=[x1.b, rstd.b], pwrites=[xs.b])
                    for kc in range(KC):
                        pt = ptr.next()
                        for j in range(JB):
                            op("pe", TR(pt.h[:, j * 128:(j + 1) * 128], xs.t[:, j, kc * 128:(kc + 1) * 128], ident_bf.t[:]),
                               reads=[xs.b, ident_bf.b], pwrites=[pt.b])
                        op("act", ACTF(h2T.t[:, kc, :], pt.h[:, 0:GB], AF.Identity, scale=A2.t[:, kc, seq:seq + 1], bias=modTb.t[:, 24 + kc, seq:seq + 1]),
                           reads=[pt.b, A2.b, modTb.b], pwrites=[h2T.b])
                    dma("sp", scr["h2T"].t.rearrange("(c p) t -> p c t", p=128)[:, :, t0:t0 + GB], h2T.t[:], reads=[h2T.b], pwrites=[scr["h2T"].b])
                    for h in range(8):
                        p = gen.next()
                        for kc in range(KC):
                            op("pe", MM(p.f[:, 0:GB], wpq.t[:, kc, h * 128:(h + 1) * 128], h2T.t[:, kc, :], start=(kc == 0), stop=(kc == KC - 1)),
                               reads=[wpq.b, h2T.b], pwrites=[p.b])
                        evacD(qpT.t[:, h, :], p.f[:, 0:GB], [p.b], [qpT.b])
                    for j in range(JB):
                        tt0 = t0 + j * 128
                        s4 = s_sb.t[:].rearrange("p (h s) n -> p h s n", s=2)
                        for b4 in range(4):
                            p = gen.next()
                            side, hb = b4 % 2, (b4 // 2) * 4
                            for i in range(4):
                                h = hb + i
                                op("pe", MM(p.f[:, i * 128:(i + 1) * 128], qpT.t[side * 64:(side + 1) * 64, h, j * 128:(j + 1) * 128],
                                            skTb.t[side * 64:(side + 1) * 64, h, :]), reads=[qpT.b, skTb.b], pwrites=[p.b])
                            op("act", ACTF(s4[:, hb:hb + 4, side, :], p.f[:, :].rearrange("p (a b) -> p a b", b=128), AF.Copy),
                               reads=[p.b], pwrites=[s_sb.b])
                        if DCUT < 2:
                            continue
                        for r in range(16):
                            op("dve", lambda e, r=r: e.max(out=v_all.t[:, r, 0:8], in_=s_sb.t[:, r, :]), reads=[s_sb.b], pwrites=[v_all.b])
                        for r in range(16):
                            op("dve", lambda e, r=r: e.max_index(out=idx_all.t[:, r, 0:8], in_max=v_all.t[:, r, 0:8], in_values=s_sb.t[:, r, :]),
                               reads=[s_sb.b, v_all.b], pwrites=[idx_all.b])
                        for r in range(16):
                            op("dve", lambda e, r=r: e.match_replace(out=s_tmp.t[:, r, :], in_to_replace=v_all.t[:, r, 0:8], in_values=s_sb.t[:, r, :], imm_value=NEG),
                               reads=[s_sb.b, v_all.b], pwrites=[s_tmp.b])
                        for r in range(16):
                            op("dve", lambda e, r=r: e.max(out=v_all.t[:, r, 8:16], in_=s_tmp.t[:, r, :]), reads=[s_tmp.b], pwrites=[v_all.b])
                        for r in range(16):
                            op("dve", lambda e, r=r: e.max_index(out=idx_all.t[:, r, 8:16], in_max=v_all.t[:, r, 8:16], in_values=s_tmp.t[:, r, :]),
                               reads=[s_tmp.b, v_all.b], pwrites=[idx_all.b])
                        op("dve", CP(idx_bf.t[:], idx_all.t[:]), reads=[idx_all.b], writes=[idx_bf.b])
                        v4 = v_all.t[:].rearrange("p (h s) k -> p h s k", s=2)
                        op("dve", TT(cand.t[:].rearrange("p h (a b) -> p h a b", b=16), v4[:, :, 0, :].unsqueeze(3).to_broadcast([128, 8, 16, 16]),
                                     v4[:, :, 1, :].unsqueeze(2).to_broadcast([128, 8, 16, 16]), ALU.add), reads=[v_all.b], writes=[cand.b])
                        for h in range(8):
                            op("dve", lambda e, h=h: e.max(out=scv.t[:, h, 0:8], in_=cand.t[:, h, :]), reads=[cand.b], pwrites=[scv.b])
                        for h in range(8):
                            op("dve", lambda e, h=h: e.max_index(out=civ.t[:, h, 0:8], in_max=scv.t[:, h, 0:8], in_values=cand.t[:, h, :]),
                               reads=[cand.b, scv.b], pwrites=[civ.b])
                        for h in range(8):
                            op("dve", lambda e, h=h: e.match_replace(out=cand2.t[:, h, :], in_to_replace=scv.t[:, h, 0:8], in_values=cand.t[:, h, :], imm_value=NEG),
                               reads=[cand.b, scv.b], pwrites=[cand2.b])
                        for h in range(8):
                            op("dve", lambda e, h=h: e.max(out=scv.t[:, h, 8:16], in_=cand2.t[:, h, :]), reads=[cand2.b], pwrites=[scv.b])
                        for h in range(8):
                            op("dve", lambda e, h=h: e.max_index(out=civ.t[:, h, 8:16], in_max=scv.t[:, h, 8:16], in_values=cand2.t[:, h, :]),
                               reads=[cand2.b, scv.b], pwrites=[civ.b])
                        op("dve", TT(sm.t[:], scv.t[:], scv.t[:, :, 0:1].to_broadcast([128, 8, 16]), ALU.subtract), reads=[scv.b], writes=[sm.b])
                        op("act", ACTF(sm.t[:], sm.t[:], AF.Exp), reads=[sm.b], writes=[sm.b])
                        op("dve", RED(zz.t[:], sm.t[:], ALU.add), reads=[sm.b], writes=[zz.b])
                        op("dve", RCP(zz.t[:], zz.t[:]), reads=[zz.b], writes=[zz.b])
                        op("dve", TT(sel_tm.t[:, 2, :].rearrange("p (h k) -> p h k", k=16), sm.t[:], zz.t[:].unsqueeze(2).to_broadcast([128, 8, 16]), ALU.mult),
                           reads=[sm.b, zz.b], pwrites=[sel_tm.b])
                        if DCUT < 5:
                            continue
                        op("dve", TS(ca_u.t[:], civ.t[:], 4, None, ALU.logical_shift_right), reads=[civ.b], writes=[ca_u.b])
                        op("dve", TS(cb_u.t[:], civ.t[:], 15, None, ALU.bitwise_and), reads=[civ.b], writes=[cb_u.b])
                        op("dve", CP(ca_bf.t[:], ca_u.t[:]), reads=[ca_u.b], writes=[ca_bf.b])
                        op("dve", CP(cb_bf.t[:], cb_u.t[:]), reads=[cb_u.b], writes=[cb_bf.b])
                        if DCUT < 6:
                            continue
                        i4 = idx_bf.t[:].rearrange("p (h s) k -> p h s k", s=2)
                        for side, cbf in ((0, ca_bf), (1, cb_bf)):
                            op("dve", TT(eqa.t[:], cbf.t[:].unsqueeze(3).to_broadcast([128, 8, 16, 16]),
                                         iota_bf.t[:, 0:16].unsqueeze(1).unsqueeze(1).to_broadcast([128, 8, 16, 16]), ALU.is_equal),
                               reads=[cbf.b, iota_bf.b], writes=[eqa.b])
                            op("dve", TT(prd.t[:], eqa.t[:], i4[:, :, side, :].unsqueeze(2).to_broadcast([128, 8, 16, 16]), ALU.mult),
                               reads=[eqa.b, idx_bf.b], writes=[prd.b])
                            op("dve", RED(sel_tm.t[:, side, :], prd.t[:].rearrange("p h k a -> p (h k) a"), ALU.add), reads=[prd.b], pwrites=[sel_tm.b])
                        if DCUT < 7:
                            continue
                        pt = ptr.next()
                        for c in range(3):
                            op("pe", TR(pt.f[:, c * 128:(c + 1) * 128], sel_tm.t[:, c, :], ident_f), reads=[sel_tm.b, cst.b], pwrites=[pt.b])
                        op("act", ACTF(selT.t[:], pt.f[:, 0:384].rearrange("p (c t) -> p c t", c=3), AF.Copy), reads=[pt.b], writes=[selT.b])
                        dma("sp", scr["sel"].t.rearrange("c p t -> p c t")[:, :, tt0:tt0 + 128], selT.t[:], reads=[selT.b], pwrites=[scr["sel"].b])
                ctx.barrier()

        if "E" in phases:
            with ExitStack() as es:
                sf = Ring([tile(es, "pf%d" % i, [128, 4096], F32) for i in range(3)])
                sbf = Ring([tile(es, "pb%d" % i, [128, 4096], BF16) for i in range(3)])
                uv = io["uT"].rearrange("(kc p) e -> p kc e", p=128)
                uo = scr["uTb"].t.rearrange("(kc p) e -> p kc e", p=128)
                vv = io["vp"].rearrange("(jj p) d -> p jj d", p=128)
                vo = scr["vb"].t.rearrange("(jj p) d -> p jj d", p=128)
                n = 0
                for i in range(NEXP // 512):
                    for (src, dst, key, v3) in ((uv[:, :, i * 512:(i + 1) * 512], uo[:, :, i * 512:(i + 1) * 512], "uTb", "p (a b) -> p a b"),
                                                (vv[:, i * 4:(i + 1) * 4, :], vo[:, i * 4:(i + 1) * 4, :], "vb", "p (a b) -> p a b")):
                        a_ = 8 if key == "uTb" else 4
                        f_ = sf.next()
                        b_ = sbf.next()
                        dma("sp", f_.t[:].rearrange(v3, a=a_), src, writes=[f_.b])
                        eng = ("dve", "act", "pool")[n % 3]
                        n += 1
                        if eng == "act":
                            op("act", ACTF(b_.t[:], f_.t[:], AF.Copy), reads=[f_.b], writes=[b_.b])
                        else:
                            op(eng, CP(b_.t[:], f_.t[:]), reads=[f_.b], writes=[b_.b])
                        dma("sp", dst, b_.t[:].rearrange(v3, a=a_), reads=[b_.b], pwrites=[scr[key].b])
                ctx.barrier()

        if "E" in phases:
            with ExitStack() as es:
                TE = 256
                SC = 2
                gb2 = tile(es, "gb2", [128, D], F32)
                x1r = Ring([tile(es, "x1E%d" % i, [128, 2, D], F32) for i in range(2)])
                h2r = Ring([tile(es, "h2E%d" % i, [128, KC, TE], BF16) for i in range(2)])
                selr = Ring([tile(es, "selE%d" % i, [128, 3, TE], BF16) for i in range(2)])
                Lr = Ring([tile(es, "L%d" % i, [128, 32, 128], BF16) for i in range(2)])
                L0r = Ring([tile(es, "L0%d" % i, [128, 32, 128], BF16) for i in range(1)])
                Rr = Ring([tile(es, "R%d" % i, [128, 32, 128], BF16) for i in range(2)])
                G = tile(es, "G", [128, TE, 128], BF16)
                uTr = Ring([tile(es, "uTs%d" % i, [128, KC, SC * 128], BF16) for i in range(3)])
                vr = Ring([tile(es, "vs%d" % i, [128, SC, D], BF16) for i in range(3)])
                Wr = Ring([tile(es, "W%d" % i, [128, TE], BF16) for i in range(3)])
                W2r = Ring([tile(es, "W2%d" % i, [128, TE], BF16) for i in range(3)])
                tz = Ring([tile(es, "tzE%d" % i, [128, 512], F32) for i in range(2)])
                yst = tile(es, "yst", [128, 2, D], F32)
                psOut = [[PBs[0], PBs[1]], [PBs[2], PBs[3]]]
                psAT = Ring([PBs[4], PBs[5]])
                psG = Ring([PBs[6], PBs[7]])
                uo = scr["uTb"].t.rearrange("(kc p) e -> p kc e", p=128)
                vo = scr["vb"].t.rearrange("(jj p) d -> p jj d", p=128)
                for tl in range(T // TE):
                    t0 = tl * TE
                    seq = t0 // S
                    if t0 % S == 0:
                        dma("sp", gb2.t[:], scr["gbc"].t[seq, 1, :, :], reads=[scr["gbc"].b], writes=[gb2.b])
                    x1t = x1r.next()
                    h2 = h2r.next()
                    sl_ = selr.next()
                    dma("sp", x1t.t[:], scr["x1"].t[t0:t0 + TE, :].rearrange("(j p) d -> p j d", p=128), reads=[scr["x1"].b], writes=[x1t.b])
                    dma("sp", h2.t[:], scr["h2T"].t.rearrange("(c p) t -> p c t", p=128)[:, :, t0:t0 + TE], reads=[scr["h2T"].b], writes=[h2.b])
                    dma("sp", sl_.t[:], scr["sel"].t.rearrange("c p t -> p c t")[:, :, t0:t0 + TE], reads=[scr["sel"].b], writes=[sl_.b])
                    for sb in range(TE // 32):
                        ts_ = slice(sb * 32, (sb + 1) * 32)
                        L0 = L0r.next()
                        L = Lr.next()
                        Rt = Rr.next()
                        iob = iota_bf.t[:].unsqueeze(1).to_broadcast([128, 32, 128])
                        op("dve", TT(L0.t[:], iob, sl_.t[:, 0, ts_].unsqueeze(2).to_broadcast([128, 32, 128]), ALU.is_equal),
                           reads=[iota_bf.b, sl_.b], writes=[L0.b])
                        op("dve", TT(L.t[:], L0.t[:], sl_.t[:, 2, ts_].unsqueeze(2).to_broadcast([128, 32, 128]), ALU.mult),
                           reads=[L0.b, sl_.b], writes=[L.b])
                        op("dve", TT(Rt.t[:], iob, sl_.t[:, 1, ts_].unsqueeze(2).to_broadcast([128, 32, 128]), ALU.is_equal),
                           reads=[iota_bf.b, sl_.b], writes=[Rt.b])
                        for q4 in range(8):
                            pg = psG.next()
                            for i in range(4):
                                t = q4 * 4 + i
                                op("pe", MM(pg.f[:, i * 128:(i + 1) * 128], L.t[:, t, :], Rt.t[:, t, :]), reads=[L.b, Rt.b], pwrites=[pg.b])
                            tt = sb * 32 + q4 * 4
                            op("act", ACTF(G.t[:, tt:tt + 4, :], pg.f[:, :].rearrange("p (a b) -> p a b", b=128), AF.Copy), reads=[pg.b], pwrites=[G.b])
                    chunks = {}

                    def load_sc(sc_):
                        uT = uTr.next()
                        vs = vr.next()
                        dma("sp", uT.t[:], uo[:, :, sc_ * SC * 128:(sc_ + 1) * SC * 128], reads=[scr["uTb"].b], writes=[uT.b])
                        dma("sp", vs.t[:], vo[:, sc_ * SC:(sc_ + 1) * SC, :], reads=[scr["vb"].b], writes=[vs.b])
                        for jj in range(SC):
                            chunks[sc_ * SC + jj] = (uT, vs, jj)

                    def stage1(j):
                        if j % SC == 0:
                            load_sc(j // SC)
                        uT, vs, jj = chunks[j]
                        pa = psAT.next()
                        for kc in range(KC):
                            op("pe", MM(pa.f[:, 0:TE], uT.t[:, kc, jj * 128:(jj + 1) * 128], h2.t[:, kc, :], start=(kc == 0), stop=(kc == KC - 1)),
                               reads=[uT.b, h2.b], pwrites=[pa.b])
                        W = Wr.next()
                        op("act", ACTF(W.t[:], pa.f[:, 0:TE], AF.Gelu), reads=[pa.b], writes=[W.b])
                        W2 = W2r.next()
                        op("dve", TT(W2.t[:], W.t[:], G.t[:, :, j], ALU.mult), reads=[W.b, G.b], writes=[W2.b])
                        return W2

                    def stage2(j, W2):
                        uT, vs, jj = chunks.pop(j)
                        for tb in range(2):
                            for dh in range(2):
                                po = psOut[tb][dh]
                                op("pe", MM(po.f[:, :], W2.t[:, tb * 128:(tb + 1) * 128], vs.t[:, jj, dh * 512:(dh + 1) * 512],
                                            start=(j == 0), stop=(j == 127)), reads=[W2.b, vs.b], pwrites=[po.b])

                    pend = stage1(0)
                    for j in range(128):
                        nxt = stage1(j + 1) if j + 1 < 128 else None
                        stage2(j, pend)
                        pend = nxt
                    for tb in range(2):
                        for dh in range(2):
                            po = psOut[tb][dh]
                            t_ = tz.next()
                            op("dve", TT(t_.t[:], po.f[:, :], gb2.t[:, dh * 512:(dh + 1) * 512], ALU.mult), reads=[po.b, gb2.b], writes=[t_.b])
                            op("dve", TT(yst.t[:, tb, dh * 512:(dh + 1) * 512], t_.t[:], x1t.t[:, tb, dh * 512:(dh + 1) * 512], ALU.add),
                               reads=[t_.b, x1t.b], pwrites=[yst.b])
                    dma("sp", y[t0:t0 + TE, :].rearrange("(j p) d -> p j d", p=128), yst.t[:], reads=[yst.b])
                ctx.barrier()

        ctx.finish()
    return nc, ctx


def host_consts():
    c = np.zeros((128, 5, 128), np.float32)
    c[:, 0, :] = np.eye(128, dtype=np.float32)
    qi = np.arange(128)[:, None]
    ki = np.arange(128)[None, :]
    c[:, 1, :] = np.where(ki <= qi, 0.0, NEG).astype(np.float32)
    c[:, 2, :] = ((qi // 64) == (ki // 64)).astype(np.float32)
    c[:, 3, :] = np.broadcast_to(np.arange(128, dtype=np.float32)[None, :], (128, 128))
    c[:, 4, :] = 1.0
    return c


def host_shared(inp):
    f = lambda a: np.ascontiguousarray(np.asarray(a, dtype=np.float32))
    sh = {}
    sh["w_ada"] = f(inp["w_ada"][0])
    sh["b_ada"] = f(inp["b_ada"][0])
    sh["b_adaT"] = f(np.asarray(inp["b_ada"][0]).reshape(48, 128).T)
    sh["n1T"] = f(np.asarray(inp["norm1_w"][0]).reshape(KC, 128).T)
    sh["n2T"] = f(np.asarray(inp["norm2_w"][0]).reshape(KC, 128).T)
    sh["w_in"] = f(inp["w_in"][0])
    sh["convT"] = f(np.asarray(inp["conv_w"][0]).reshape(3, 4, 128).transpose(2, 1, 0))
    sh["w_conv_out"] = f(inp["w_conv_out"][0])
    sh["qk_w"] = f(np.stack([np.tile(np.asarray(inp["q_norm_w"][0]), 2), np.tile(np.asarray(inp["k_norm_w"][0]), 2)], axis=1))
    sh["w_attn_out"] = f(inp["w_attn_out"][0])
    sh["w_o"] = f(inp["w_o"][0])
    sh["w_peer_q"] = f(inp["w_peer_q"][0])
    sk = np.asarray(inp["peer_sub_keys"][0])
    sh["skT"] = f(sk.transpose(1, 3, 0, 2).reshape(128, 8, 128))
    u = np.asarray(inp["peer_u"][0]).reshape(128, 128, D).transpose(1, 0, 2).reshape(NEXP, D)
    sh["uT"] = f(u.T)
    sh["vp"] = f(np.asarray(inp["peer_v"][0]).reshape(128, 128, D).transpose(1, 0, 2).reshape(NEXP, D))
    sh["consts"] = host_consts()
    return sh


def kernel(**inp):
    x = np.asarray(inp["x"], dtype=np.float32)
    c = np.asarray(inp["c"], dtype=np.float32)
    B, S, _ = x.shape
    ncores = 8
    NSEQ = B // ncores
    nc, _ = build(NSEQ, S)
    sh = host_shared(inp)
    in_maps = []
    for i in range(ncores):
        m = dict(sh)
        m["x"] = np.ascontiguousarray(x[i * NSEQ:(i + 1) * NSEQ].reshape(NSEQ * S, D))
        m["cT"] = np.ascontiguousarray(c[i * NSEQ:(i + 1) * NSEQ].reshape(NSEQ, KC, 128).transpose(2, 1, 0))
        in_maps.append(m)
    res = run_bass_kernel_spmd(nc, in_maps, core_ids=list(range(ncores)))
    out = np.concatenate([np.asarray(r["y"]).reshape(NSEQ, S, D) for r in res.results], axis=0)
    return out.astype(np.float32)
```
